# Optimizing a Trainium2 kernel written in Bass

```python
import math
import jax, jax.numpy as jnp
from jax import lax
import numpy as np

D_MODEL = 1024
BATCH = 8
SEQ = 4096
DEPTH = 1

MIX_WIDTH = D_MODEL
ATT_WIDTH = MIX_WIDTH // 2
RWKV_WIDTH = MIX_WIDTH - ATT_WIDTH
HD_ATT = 64
H_ATT = ATT_WIDTH // HD_ATT
HD_RWKV = 64
H_RWKV = RWKV_WIDTH // HD_RWKV
Q_LORA = 256
KV_LATENT = 128
IDX_HEADS = 8
IDX_DIM = 64
TOPK_MAX = 256
Q_BLOCK = 128
NUM_BUCKETS = 32
MAX_DISTANCE = 128
DECAY_LORA = 64
AAA_LORA = 64
GATE_LORA = 160
GN_EPS = 64e-5
ATT_COLS = Q_LORA + KV_LATENT + IDX_DIM + IDX_HEADS
RWKV_COLS = 3 * RWKV_WIDTH + DECAY_LORA + AAA_LORA + GATE_LORA
IN_COLS = ATT_COLS + RWKV_COLS
D_FF = -(-8 * D_MODEL // 768) * 256
NORM_EPS = 1e-6

kernel_name = "hybrid_dsa_rwkv7_sandwich_adaln"


def rms_norm(x, gain):
    xf = x.astype(jnp.float32)
    y = xf * lax.rsqrt(jnp.mean(xf * xf, axis=-1, keepdims=True) + NORM_EPS)
    return (y * gain.astype(jnp.float32)).astype(x.dtype)


def t5_bucket(rel):
    max_exact = NUM_BUCKETS // 2
    nf = jnp.maximum(rel, 1).astype(jnp.float32)
    large = max_exact + (jnp.log(nf / max_exact) / math.log(MAX_DISTANCE / max_exact)
                         * (NUM_BUCKETS - max_exact)).astype(jnp.int32)
    large = jnp.minimum(large, NUM_BUCKETS - 1)
    return jnp.where(rel < max_exact, rel, large)


def dsa_attention(c_q, c_kv, idx_k_raw, idx_w_raw, q_norm, w_uq, w_idx_q, kv_norm, idx_k_norm, w_uk, w_uv, rel_bias):
    B, T, _ = c_q.shape
    topk = min(TOPK_MAX, T // 4)
    cq = rms_norm(c_q, q_norm)
    q = jnp.einsum('btr,rhd->bthd', cq, w_uq)
    q_abs = jnp.einsum('bthd,hcd->bthc', q, w_uk) * (HD_ATT ** -0.5)
    ckv = rms_norm(c_kv, kv_norm)
    iq = jnp.einsum('btr,rhd->bthd', cq, w_idx_q).astype(jnp.float32)
    ik = rms_norm(idx_k_raw, idx_k_norm).astype(jnp.float32)
    iw = idx_w_raw.astype(jnp.float32) * (IDX_HEADS ** -0.5 * IDX_DIM ** -0.5)
    s_pos = jnp.arange(T)

    def block(i):
        t0 = i * Q_BLOCK
        qi = lax.dynamic_slice_in_dim(iq, t0, Q_BLOCK, axis=1)
        wi = lax.dynamic_slice_in_dim(iw, t0, Q_BLOCK, axis=1)
        qa = lax.dynamic_slice_in_dim(q_abs, t0, Q_BLOCK, axis=1)
        t_pos = t0 + jnp.arange(Q_BLOCK)
        score = jnp.einsum('bth,bths->bts', wi, jax.nn.relu(jnp.einsum('bthd,bsd->bths', qi, ik)))
        causal = s_pos[None, :] <= t_pos[:, None]
        score = jnp.where(causal[None], score, -jnp.inf)
        _, sel = lax.top_k(score, topk)
        kv_sel = jax.vmap(lambda m, j: m[j])(ckv, sel)
        rel = t_pos[None, :, None] - sel
        bias = rel_bias[t5_bucket(jnp.maximum(rel, 0))]
        logits = (jnp.einsum('bthc,btkc->bthk', qa, kv_sel).astype(jnp.float32)
                  + jnp.moveaxis(bias, -1, 2).astype(jnp.float32))
        logits = jnp.where((rel >= 0)[:, :, None, :], logits, -jnp.inf)
        p = jax.nn.softmax(logits, axis=-1).astype(kv_sel.dtype)
        return jnp.einsum('bthk,btkc->bthc', p, kv_sel)

    o = lax.map(block, jnp.arange(T // Q_BLOCK))
    o = jnp.moveaxis(o, 0, 1).reshape(B, T, H_ATT, KV_LATENT)
    return jnp.einsum('bthc,hcd->bthd', o, w_uv).reshape(B, T, ATT_WIDTH)


def rwkv7_scan(r, w, k, v, a_vec, b_vec):
    B, T, H, N = r.shape

    def step(S, inp):
        r_t, w_t, k_t, v_t, a_t, b_t = inp
        sa = jnp.einsum('bhij,bhj->bhi', S, a_t)
        S = S * w_t[:, :, None, :] + sa[..., None] * b_t[:, :, None, :] + v_t[..., None] * k_t[:, :, None, :]
        return S, jnp.einsum('bhij,bhj->bhi', S, r_t)

    xs = tuple(jnp.moveaxis(z, 1, 0) for z in (r, w, k, v, a_vec, b_vec))
    _, y = lax.scan(step, jnp.zeros((B, H, N, N), jnp.float32), xs)
    return jnp.moveaxis(y, 0, 1)


def rwkv7_group(p, mu_shift, w0, w_decay_up, a0, w_aaa_up, w_gate_up, k_k, k_a, r_k, ln_x_gain, ln_x_bias):
    B, T, _ = p.shape
    dt = p.dtype
    p_prev = jnp.pad(p, ((0, 0), (1, 0), (0, 0)))[:, :T]
    p = p + mu_shift * (p_prev - p)
    splits = [RWKV_WIDTH, 2 * RWKV_WIDTH, 3 * RWKV_WIDTH, 3 * RWKV_WIDTH + DECAY_LORA, 3 * RWKV_WIDTH + DECAY_LORA + AAA_LORA]
    r, k, v, wd, ad, gd = jnp.split(p, splits, axis=-1)
    w_log = -jax.nn.softplus(-(w0 + jnp.tanh(wd) @ w_decay_up)) - 0.5
    decay = jnp.exp(-jnp.exp(w_log.astype(jnp.float32)))
    a = jax.nn.sigmoid(a0 + ad @ w_aaa_up)
    g = jax.nn.sigmoid(gd) @ w_gate_up
    hs = (B, T, H_RWKV, HD_RWKV)
    kk = (k * k_k).astype(jnp.float32).reshape(hs)
    kk = kk / jnp.maximum(jnp.linalg.norm(kk, axis=-1, keepdims=True), 1e-12)
    k = k * (1 + (a - 1) * k_a)
    rf = r.astype(jnp.float32).reshape(hs)
    kf = k.astype(jnp.float32).reshape(hs)
    vf = v.astype(jnp.float32).reshape(hs)
    af = a.astype(jnp.float32).reshape(hs)
    y = rwkv7_scan(rf, decay.reshape(hs), kf, vf, -kk, kk * af)
    mean = jnp.mean(y, axis=-1, keepdims=True)
    var = jnp.mean(jnp.square(y - mean), axis=-1, keepdims=True)
    y = ((y - mean) * lax.rsqrt(var + GN_EPS)).reshape(B, T, RWKV_WIDTH)
    y = y * ln_x_gain.astype(jnp.float32) + ln_x_bias.astype(jnp.float32)
    bonus = jnp.sum(rf * kf * r_k.astype(jnp.float32), axis=-1, keepdims=True) * vf
    y = (y + bonus.reshape(B, T, RWKV_WIDTH)) * g.astype(jnp.float32)
    return y.astype(dt)


def hybrid_layer(x, c, rel_bias, ada_w, ada_b, mix_pre_norm, mix_post_norm, ffn_pre_norm, ffn_post_norm,
                 w_in, q_norm, w_uq, w_idx_q, kv_norm, idx_k_norm, w_uk, w_uv,
                 mu_shift, w0, w_decay_up, a0, w_aaa_up, w_gate_up, k_k, k_a, r_k, ln_x_gain, ln_x_bias,
                 w_out, w_ffn_gate, w_ffn_up, w_ffn_down):
    mod = jax.nn.silu(c) @ ada_w + ada_b
    sh_m, sc_m, g_m, sh_f, sc_f, g_f = [m[:, None, :] for m in jnp.split(mod, 6, axis=-1)]
    h = rms_norm(x, mix_pre_norm) * (1 + sc_m) + sh_m
    proj = h @ w_in
    att_p, rwkv_p = proj[..., :ATT_COLS], proj[..., ATT_COLS:]
    c_q, c_kv, idx_k_raw, idx_w_raw = jnp.split(att_p, [Q_LORA, Q_LORA + KV_LATENT, Q_LORA + KV_LATENT + IDX_DIM], axis=-1)
    y_att = dsa_attention(c_q, c_kv, idx_k_raw, idx_w_raw, q_norm, w_uq, w_idx_q, kv_norm, idx_k_norm, w_uk, w_uv, rel_bias)
    y_rwkv = rwkv7_group(rwkv_p, mu_shift, w0, w_decay_up, a0, w_aaa_up, w_gate_up, k_k, k_a, r_k, ln_x_gain, ln_x_bias)
    mix = jnp.concatenate([y_att, y_rwkv], axis=-1) @ w_out
    x = x + g_m * rms_norm(mix, mix_post_norm)
    hf = rms_norm(x, ffn_pre_norm) * (1 + sc_f) + sh_f
    f = (jax.nn.silu(hf @ w_ffn_gate) * (hf @ w_ffn_up)) @ w_ffn_down
    return x + g_f * rms_norm(f, ffn_post_norm)


def setup_inputs(seed: int = 0) -> dict:
    key = jax.random.key(seed)
    ks = jax.random.split(key, 40)
    f32 = jnp.float32
    L = DEPTH

    def nrm(k, shape, scale):
        return jax.random.normal(k, shape, f32) * scale

    def gain(k, n):
        return 1.0 + 0.05 * jax.random.normal(k, (L, n), f32)

    return {
        "x": jax.random.normal(ks[0], (BATCH, SEQ, D_MODEL), f32),
        "c": jax.random.normal(ks[1], (BATCH, D_MODEL), f32),
        "rel_bias": nrm(ks[2], (NUM_BUCKETS, H_ATT), 0.5),
        "ada_w": nrm(ks[3], (L, D_MODEL, 6 * D_MODEL), 0.5 * D_MODEL ** -0.5),
        "ada_b": nrm(ks[4], (L, 6 * D_MODEL), 0.02),
        "mix_pre_norm": gain(ks[5], D_MODEL),
        "mix_post_norm": gain(ks[6], D_MODEL),
        "ffn_pre_norm": gain(ks[7], D_MODEL),
        "ffn_post_norm": gain(ks[8], D_MODEL),
        "w_in": nrm(ks[9], (L, D_MODEL, IN_COLS), D_MODEL ** -0.5),
        "q_norm": gain(ks[10], Q_LORA),
        "w_uq": nrm(ks[11], (L, Q_LORA, H_ATT, HD_ATT), Q_LORA ** -0.5),
        "w_idx_q": nrm(ks[12], (L, Q_LORA, IDX_HEADS, IDX_DIM), Q_LORA ** -0.5),
        "kv_norm": gain(ks[13], KV_LATENT),
        "idx_k_norm": gain(ks[14], IDX_DIM),
        "w_uk": nrm(ks[15], (L, H_ATT, KV_LATENT, HD_ATT), KV_LATENT ** -0.5),
        "w_uv": nrm(ks[16], (L, H_ATT, KV_LATENT, HD_ATT), KV_LATENT ** -0.5),
        "mu_shift": jax.random.uniform(ks[17], (L, RWKV_COLS), f32),
        "w0": nrm(ks[18], (L, RWKV_WIDTH), 0.5),
        "w_decay_up": nrm(ks[19], (L, DECAY_LORA, RWKV_WIDTH), 0.1 * DECAY_LORA ** -0.5),
        "a0": nrm(ks[20], (L, RWKV_WIDTH), 0.1),
        "w_aaa_up": nrm(ks[21], (L, AAA_LORA, RWKV_WIDTH), 0.5 * AAA_LORA ** -0.5),
        "w_gate_up": nrm(ks[22], (L, GATE_LORA, RWKV_WIDTH), GATE_LORA ** -0.5),
        "k_k": 0.85 + 0.05 * jax.random.normal(ks[23], (L, RWKV_WIDTH), f32),
        "k_a": 1.0 + 0.05 * jax.random.normal(ks[24], (L, RWKV_WIDTH), f32),
        "r_k": nrm(ks[25], (L, H_RWKV, HD_RWKV), 0.1),
        "ln_x_gain": gain(ks[26], RWKV_WIDTH),
        "ln_x_bias": nrm(ks[27], (L, RWKV_WIDTH), 0.02),
        "w_out": nrm(ks[28], (L, MIX_WIDTH, D_MODEL), MIX_WIDTH ** -0.5),
        "w_ffn_gate": nrm(ks[29], (L, D_MODEL, D_FF), D_MODEL ** -0.5),
        "w_ffn_up": nrm(ks[30], (L, D_MODEL, D_FF), D_MODEL ** -0.5),
        "w_ffn_down": nrm(ks[31], (L, D_FF, D_MODEL), D_FF ** -0.5),
    }


def reference(x, c, rel_bias, ada_w, ada_b, mix_pre_norm, mix_post_norm, ffn_pre_norm, ffn_post_norm,
              w_in, q_norm, w_uq, w_idx_q, kv_norm, idx_k_norm, w_uk, w_uv,
              mu_shift, w0, w_decay_up, a0, w_aaa_up, w_gate_up, k_k, k_a, r_k, ln_x_gain, ln_x_bias,
              w_out, w_ffn_gate, w_ffn_up, w_ffn_down):
    layer_params = (ada_w, ada_b, mix_pre_norm, mix_post_norm, ffn_pre_norm, ffn_post_norm,
                    w_in, q_norm, w_uq, w_idx_q, kv_norm, idx_k_norm, w_uk, w_uv,
                    mu_shift, w0, w_decay_up, a0, w_aaa_up, w_gate_up, k_k, k_a, r_k, ln_x_gain, ln_x_bias,
                    w_out, w_ffn_gate, w_ffn_up, w_ffn_down)
    for l in range(DEPTH):
        x = hybrid_layer(x, c, rel_bias, *[p[l] for p in layer_params])
    return x
```

```python
import math
from contextlib import ExitStack
import numpy as np
import concourse.bass as bass
import concourse.mybir as mybir
from concourse.bass_utils import run_bass_kernel_spmd

F32 = mybir.dt.float32
BF16 = mybir.dt.bfloat16
AF = mybir.ActivationFunctionType
ALU = mybir.AluOpType
AX = mybir.AxisListType

D = 1024
DFF = 2816
NB_IT = 20
BIS_R = 16.0
LWC = math.exp(-0.5)

C_ID, C_ONE, C_BLK, C_MSU, C_MUI, C_MSL, C_NEG, C_J, C_SEL, C_OH, C_SCAN, C_HIND, C_END = (
    0, 128, 256, 384, 512, 640, 768, 896, 1024, 1152, 1408, 1664, 1668)


class Tl:
    def __init__(self, P, h, name):
        self.h = h
        self.name = name
        self.lw = None
        self.rd = dict(P.fence_tokens)

    def __getitem__(self, i):
        return self.h[i]


class TlG:
    def __init__(self, P, h, name, n):
        self.h = h
        self.name = name
        self.p = [Tl(P, h, "%s_%d" % (name, i)) for i in range(n)]

    def __getitem__(self, i):
        return self.h[i]


class Bank:
    def __init__(self, tl, h, lo):
        self.tl = tl
        self.h = h
        self.lo = lo

    def ap(self, a, b):
        return self.h[:, self.lo + a:self.lo + b]


def _flat(ts):
    out = []
    for t in ts:
        if isinstance(t, TlG):
            out.extend(t.p)
        else:
            out.append(t)
    return out


class Rec:
    def __init__(self):
        self.call = None

    def __getattr__(self, name):
        def f(*a, **k):
            self.call = (name, a, k)
            return self
        return f


class Stream:
    def __init__(self, key):
        self.key = key
        self.count = 0
        self.mark = -1


class Prog:
    ENGS = ("pe", "act", "dve", "pool", "sp")

    def __init__(self, nc, es):
        self.nc = nc
        self.es = es
        self.q = {e: [] for e in self.ENGS}
        self.cnt = {e: 0 for e in self.ENGS}
        self.waited = {e: {} for e in self.ENGS}
        self.semh = {}
        self.streams = {}
        self.fence_tokens = {}
        self.enabled = True
        self.nops = 0
        import os as _os
        self.printops = bool(_os.environ.get("PRINTOPS"))
        self.maxops = int(_os.environ.get("MAXOPS", "100000000"))
        for e in self.ENGS:
            self.semh[e] = es.enter_context(nc.semaphore("s_" + e))

    def stream(self, key):
        if key not in self.streams:
            self.streams[key] = Stream(key)
            self.semh[key] = self.es.enter_context(self.nc.semaphore("d_" + key))
        return self.streams[key]

    def tile(self, shape, dt, name):
        h = self.es_cur.enter_context(self.nc.sbuf_tensor("sb_" + name, list(shape), dt))
        return Tl(self, h, name)

    def fence(self):
        ft = {}
        for e in self.ENGS:
            if self.cnt[e] > 0:
                ft[e] = (e, self.cnt[e], e, False)
        for k, st in self.streams.items():
            if st.count > 0:
                ft[k] = (k, st.count, None, True)
        self.fence_tokens = ft

    def emit(self, eng, fn, r=(), w=(), stream=None):
        if not self.enabled:
            return None
        self.nops += 1
        if self.nops > self.maxops:
            return None
        r = _flat(r)
        w = _flat(w)
        rec = Rec()
        fn(rec)
        fn = rec.call
        assert fn is not None
        if self.printops:
            print("OP", self.nops, eng, fn[0], [t.name for t in r], "->", [t.name for t in w])
        need = {}

        def add(tok, kind):
            key, val, teng, isdma = tok
            if isdma:
                val = self.streams[key].count
                self.streams[key].mark = val
            elif stream is None and teng == eng:
                if eng == "pe":
                    return
            if need.get(key, 0) < val:
                need[key] = val

        for t in r:
            if t.lw is not None:
                add(t.lw, "raw")
        for t in w:
            if t.lw is not None:
                add(t.lw, "waw")
            for tok in t.rd.values():
                add(tok, "war")
        if stream is not None:
            st0 = self.stream(stream)
            if st0.mark == st0.count and st0.count > 0:
                need[st0.key] = st0.count
        for key, val in need.items():
            if self.waited[eng].get(key, 0) < val:
                self.waited[eng][key] = val
                self.q[eng].append(("w", key, val))
        if stream is None:
            self.cnt[eng] += 1
            tok = (eng, self.cnt[eng], eng, False)
            self.q[eng].append(("op", fn, eng, 1))
        else:
            st = self.stream(stream)
            st.count += 16
            tok = (st.key, st.count, None, True)
            self.q[eng].append(("op", fn, st.key, 16))
        for t in w:
            t.lw = tok
            t.rd = {}
        for t in r:
            if t not in w:
                t.rd[tok[0]] = tok
        return tok

    def wait_all(self, eng, keys):
        for key in keys:
            if key in self.streams:
                val = self.streams[key].count
            elif key in self.cnt:
                val = self.cnt[key]
            else:
                continue
            if val > 0 and self.waited[eng].get(key, 0) < val:
                self.waited[eng][key] = val
                self.q[eng].append(("w", key, val))

    def replay(self, eng, e):
        for ent in self.q[eng]:
            if ent[0] == "w":
                e.wait_ge(self.semh[ent[1]], ent[2])
            else:
                name, a, k = ent[1]
                ins = getattr(e, name)(*a, **k)
                ins.then_inc(self.semh[ent[2]], ent[3])

    def pe(self, fn, r=(), w=()):
        return self.emit("pe", fn, r, w)

    def act(self, fn, r=(), w=()):
        return self.emit("act", fn, r, w)

    def dve(self, fn, r=(), w=()):
        return self.emit("dve", fn, r, w)

    def pool(self, fn, r=(), w=()):
        return self.emit("pool", fn, r, w)

    def dma(self, out, in_, stream, r=(), w=(), q="sp"):
        return self.emit(q, lambda e: e.dma_start(out=out, in_=in_), r, w, stream=stream)


def build(T, dbg=False, upto=3):
    NT = T // 128
    NS = T // 256
    KTOP = min(256, T // 4)
    nc = bass.Bass("TRN2", target_bir_lowering=False)

    def din(name, shape):
        return nc.dram_tensor(name, list(shape), F32, kind="ExternalInput").ap()

    x_d = din("x", [T, D])
    ccol_d = din("ccol", [128, 8])
    adaw_d = din("ada_w", [D, 6 * D])
    vec_d = din("vecs", [128, 128])
    cst_d = din("cst", [128, C_END])
    watt_d = din("w_att", [D, 512])
    wiw_d = din("w_iw", [D, 8])
    wrw_d = din("w_rw", [D, 1824])
    wiq_d = din("w_iq", [256, 1024])
    wuq_d = din("w_uq", [256, 512])
    wuk_d = din("w_ukT", [128, 1024])
    wuv_d = din("w_uv", [128, 1024])
    relb_d = din("rel_bias", [32, 8])
    wlora_d = din("w_lora", [128, 1024])
    wgate_d = din("w_gate", [160, 512])
    lnrow_d = din("lnrow", [2, 512])
    wout_d = din("w_out", [D, D])
    wfg_d = din("w_fg", [D, DFF])
    wfu_d = din("w_fu", [D, DFF])
    wfd_d = din("w_fd", [DFF, D])
    out_d = nc.dram_tensor("out", [T, D], F32, kind="ExternalOutput").ap()
    dscr = nc.dram_tensor("dscr", [8, 384], F32, kind="Internal").ap()
    dbg_d = {}

    def ddbg(name, shape):
        if dbg:
            dbg_d[name] = nc.dram_tensor("dbg_" + name, list(shape), F32, kind="ExternalOutput").ap()

    ddbg("modc", [128, 48])
    ddbg("bias", [128, 3 * 1024])
    ddbg("yatt", [128, 4 * T])
    ddbg("thr", [128, NT])
    ddbg("yrw", [T, 512])
    ddbg("x1", [T, D])

    with ExitStack() as es:
        P = Prog(nc, es)
        P.es_cur = es
        def pst(name, shape, dt):
            return Tl(P, es.enter_context(nc.psum_tensor("ps_" + name, list(shape), dt)), name)
        def pstg(name, shape, dt, n):
            return TlG(P, es.enter_context(nc.psum_tensor("ps_" + name, list(shape), dt)), name, n)
        PA = pstg("PA", [128, 1024], F32, 2)
        PB = pstg("PB", [128, 1024], F32, 2)
        PC = pstg("PC", [128, 1024], F32, 2)
        PD = pst("PD", [128, 512], F32)
        PT = pstg("PT", [128, 1024], BF16, 8)

        cst = P.tile([128, C_END], F32, "cst")
        vec = P.tile([128, 128], F32, "vec")
        modc = P.tile([128, 48], F32, "modc")
        gsh = P.tile([128, 48], F32, "gsh")
        GMrow = P.tile([128, D], F32, "GMrow")
        GFrow = P.tile([128, D], F32, "GFrow")
        identb = P.tile([128, 128], BF16, "identb")
        onesb = P.tile([128, 128], BF16, "onesb")
        epsc = P.tile([128, 4], F32, "epsc")
        yscr = nc.dram_tensor("yscr", [NT * 128, 512], F32, kind="ExternalOutput").ap()
        yscr_t = Tl(P, None, "yscr")

        ident = lambda: cst[:, C_ID:C_ID + 128]
        ones32 = lambda: cst[:, C_ONE:C_ONE + 128]

        P.dma(cst[:, :], cst_d[:, :], "cw", w=[cst])
        P.dma(vec[:, :], vec_d[:, :], "cw", w=[vec])
        P.dve(lambda e: e.tensor_copy(out=identb[:, :], in_=cst[:, C_ID:C_ID + 128]), r=[cst], w=[identb])
        P.dve(lambda e: e.tensor_copy(out=onesb[:, :], in_=cst[:, C_ONE:C_ONE + 128]), r=[cst], w=[onesb])
        P.dve(lambda e: e.memset(epsc[:, 0:1], 1e-6), w=[epsc])
        P.dve(lambda e: e.memset(epsc[:, 1:2], 64e-5), w=[epsc])
        P.dve(lambda e: e.memset(epsc[:, 2:3], 0.0), w=[epsc])
        V_MPRE, V_MPOST, V_FPRE, V_FPOST, V_ADAB, V_QN, V_KVN, V_IKN, V_W0, V_A0, V_KK, V_KA, V_RK = (
            0, 8, 16, 24, 32, 80, 82, 83, 84, 88, 92, 96, 100)

        def dump(name, src_ap, tl, dst_ap=None):
            if dbg:
                P.dma(dbg_d[name][:, :] if dst_ap is None else dst_ap, src_ap, "dbg", r=[tl])

        with ExitStack() as es0:
            P.es_cur = es0
            ccol = P.tile([128, 8], F32, "ccol")
            scol = P.tile([128, 8], F32, "scol")
            stg = [P.tile([128, 8, 512], F32, "adastg%d" % i) for i in range(4)]
            dg = P.tile([128, 8, 128], F32, "dg")
            P.dma(ccol[:, :], ccol_d[:, :], "cw", w=[ccol])
            P.act(lambda e: e.activation(out=scol[:, :], in_=ccol[:, :], func=AF.Silu), r=[ccol], w=[scol])
            for s in range(12):
                st = stg[s % 4]
                P.dma(st[:, :, :], adaw_d[:, s * 512:(s + 1) * 512].rearrange("(k p) n -> p k n", p=128),
                      "ada%d" % (s % 4), w=[st])
                for mm in range(4):
                    m = s * 4 + mm
                    for k in range(8):
                        P.pe(lambda e, st=st, mm=mm, m=m, k=k: e.matmul(
                            PD[:, m:m + 1], lhsT=st[:, k, mm * 128:(mm + 1) * 128], rhs=scol[:, k:k + 1],
                            start=(k == 0), stop=(k == 7)), r=[st, scol], w=[PD])
            P.dve(lambda e: e.tensor_tensor(out=modc[:, :], in0=PD[:, 0:48], in1=vec[:, V_ADAB:V_ADAB + 48],
                                            op=ALU.add), r=[PD, vec], w=[modc])
            dump("modc", modc[:, :], modc)
            P.dve(lambda e: e.tensor_scalar(out=gsh[:, 32:40], in0=modc[:, 8:16], scalar1=1.0, scalar2=None,
                                            op0=ALU.add), r=[modc], w=[gsh])
            P.dve(lambda e: e.tensor_scalar(out=gsh[:, 40:48], in0=modc[:, 32:40], scalar1=1.0, scalar2=None,
                                            op0=ALU.add), r=[modc, gsh], w=[gsh])
            P.dve(lambda e: e.tensor_tensor(out=gsh[:, 0:8], in0=gsh[:, 32:40], in1=vec[:, V_MPRE:V_MPRE + 8],
                                            op=ALU.mult), r=[gsh, vec], w=[gsh])
            P.dve(lambda e: e.tensor_tensor(out=gsh[:, 8:16], in0=gsh[:, 40:48], in1=vec[:, V_FPRE:V_FPRE + 8],
                                            op=ALU.mult), r=[gsh, vec], w=[gsh])
            P.dve(lambda e: e.tensor_tensor(out=gsh[:, 16:24], in0=modc[:, 16:24], in1=vec[:, V_MPOST:V_MPOST + 8],
                                            op=ALU.mult), r=[gsh, modc, vec], w=[gsh])
            P.dve(lambda e: e.tensor_tensor(out=gsh[:, 24:32], in0=modc[:, 40:48], in1=vec[:, V_FPOST:V_FPOST + 8],
                                            op=ALU.mult), r=[gsh, modc, vec], w=[gsh])
            for (c0, row) in ((16, GMrow), (24, GFrow)):
                for k in range(8):
                    P.dve(lambda e, c0=c0, k=k: e.tensor_scalar(
                        out=dg[:, k, :], in0=cst[:, C_ID:C_ID + 128], scalar1=gsh[:, c0 + k:c0 + k + 1],
                        scalar2=None, op0=ALU.mult), r=[cst, gsh], w=[dg])
                for k in range(8):
                    P.pe(lambda e, k=k: e.matmul(PA[:, k * 128:(k + 1) * 128], lhsT=cst[:, C_ONE:C_ONE + 128],
                                                 rhs=dg[:, k, :], start=True, stop=True), r=[cst, dg], w=[PA])
                P.act(lambda e, row=row: e.activation(out=row[:, :], in_=PA[:, :], func=AF.Copy), r=[PA], w=[row])

        print("ops after P0", P.nops)
        P.fence()
        if upto < 1:
            P.enabled = False
        with ExitStack() as es1:
            P.es_cur = es1
            w_att = P.tile([128, 8, 512], F32, "w_att")
            w_iw = P.tile([128, 8, 8], F32, "w_iw")
            w_iq = P.tile([128, 2, 1024], F32, "w_iq")
            w_uqb = P.tile([128, 2, 512], BF16, "w_uqb")
            w_ukb = P.tile([128, 8, 128], BF16, "w_ukb")
            w_uvb = P.tile([128, 8, 128], BF16, "w_uvb")
            relb = P.tile([32, 8], F32, "relb")
            biasT = P.tile([128, 3, 1024], F32, "biasT")
            ckvT = P.tile([128, T], BF16, "ckvT")
            ckvtm = P.tile([128, NT, 128], BF16, "ckvtm")
            ikA = P.tile([128, T], BF16, "ikA")
            ikB = P.tile([128, T], BF16, "ikB")
            P.dve(lambda e: e.memset(ikB[:, :], 0.0), w=[ikB])
            es1b = ExitStack()
            P.es_cur = es1b
            wst = P.tile([128, 1024], F32, "wst1")
            P.dma(w_att[:, :, :], watt_d.rearrange("(k p) n -> p k n", p=128), "cw", w=[w_att])
            P.dma(w_iw[:, :, :], wiw_d.rearrange("(k p) n -> p k n", p=128), "cw", w=[w_iw])
            P.dma(w_iq[:, :, :], wiq_d.rearrange("(k p) n -> p k n", p=128), "cw", w=[w_iq])
            P.dma(relb[:, :], relb_d[:, :], "cw", w=[relb])
            P.dma(wst[:, :].rearrange("p (k n) -> p k n", k=2), wuq_d.rearrange("(k p) n -> p k n", p=128), "wst", w=[wst])
            P.dve(lambda e: e.tensor_copy(out=w_uqb[:, :, :], in_=wst[:, :].rearrange("p (k n) -> p k n", k=2)),
                  r=[wst], w=[w_uqb])
            P.dma(wst[:, :], wuk_d[:, :], "wst", w=[wst])
            P.dve(lambda e: e.tensor_copy(out=w_ukb[:, :, :], in_=wst[:, :].rearrange("p (k n) -> p k n", k=8)),
                  r=[wst], w=[w_ukb])
            P.dma(wst[:, :], wuv_d[:, :], "wst", w=[wst])
            P.dve(lambda e: e.tensor_copy(out=w_uvb[:, :, :], in_=wst[:, :].rearrange("p (k n) -> p k n", k=8)),
                  r=[wst], w=[w_uvb])
            brel = P.tile([8, 384], F32, "brel")
            relx = P.tile([32, 8, 128], F32, "relx")
            qtl = P.tile([128, 2, 1024], F32, "qtl")
            P.pe(lambda e: e.matmul(PD[0:8, 0:256], lhsT=relb[:, :], rhs=cst[0:32, C_OH:C_OH + 256],
                                    start=True, stop=True), r=[relb, cst], w=[PD])
            P.dve(lambda e: e.memset(brel[:, :], 0.0), w=[brel])
            P.dve(lambda e: e.tensor_copy(out=brel[:, 127:383], in_=PD[0:8, 0:256]), r=[PD, brel], w=[brel])
            dsc = Tl(P, None, "dscr")
            P.dma(dscr[:, :], brel[:, :], "cw", r=[brel], w=[dsc])
            for dl in range(2):
                src = bass.AP(tensor=dscr.tensor, offset=128 * dl, ap=[[1, 128], [384, 8], [1, 128]])
                P.dma(qtl[:, dl, :].rearrange("p (h t) -> p h t", h=8), src, "cw", r=[dsc], w=[qtl])
            for dl in range(2):
                for c in range(2):
                    P.pe(lambda e, dl=dl, c=c: e.matmul(PA[:, c * 512:(c + 1) * 512], lhsT=cst[:, C_J:C_J + 128],
                                                        rhs=qtl[:, dl, c * 512:(c + 1) * 512], start=True, stop=True),
                         r=[cst, qtl], w=[PA])
                P.act(lambda e, dl=dl: e.activation(out=biasT[:, dl, :], in_=PA[:, :], func=AF.Copy), r=[PA], w=[biasT])
            for h in range(8):
                P.dve(lambda e, h=h: e.tensor_scalar(out=relx[:, h, :], in0=cst[0:32, C_ONE:C_ONE + 128],
                                                     scalar1=relb[:, h:h + 1], scalar2=None, op0=ALU.mult),
                      r=[cst, relb], w=[relx])
            for c in range(2):
                P.pe(lambda e, c=c: e.matmul(PA[:, c * 512:(c + 1) * 512], lhsT=cst[0:32, C_SEL:C_SEL + 128],
                                             rhs=relx[:, c * 4:(c + 1) * 4, :], start=True, stop=True),
                     r=[cst, relx], w=[PA])
            P.act(lambda e: e.activation(out=biasT[:, 2, :], in_=PA[:, :], func=AF.Copy), r=[PA], w=[biasT])
            dump("bias", biasT[:, :, :].rearrange("p a b -> p (a b)"), biasT)
            es1b.close()
            P.es_cur = es1
            P.fence()

            xt = P.tile([128, 2, D], F32, "xt")
            sqj = P.tile([128, D], BF16, "sqj")
            st4 = P.tile([128, 8], F32, "st4")
            dgt = P.tile([128, 2, 128], F32, "dgt")
            hT = P.tile([128, 8, 256], F32, "hT")
            cqraw = P.tile([128, 4, 256], F32, "cqraw")
            sq = P.tile([128, 4, 256], F32, "sq")
            rq = P.tile([128, 3, 256], F32, "rq")
            cqT = P.tile([128, 2, 256], F32, "cqT")
            cqTb = P.tile([128, 2, 256], BF16, "cqTb")
            iqT = P.tile([128, 8, 256], BF16, "iqP")
            iqtmp = P.tile([128, 4, 256], BF16, "iqtmp")
            ikf = P.tile([128, 256], F32, "ikf")
            iw = P.tile([128, 2, 8], F32, "iw")
            qTb = P.tile([128, 4, 256], BF16, "qTb")
            qaT2 = [P.tile([128, 8, 256], BF16, "qaT%d" % i) for i in range(2)]
            sc = TlG(P, es1.enter_context(nc.sbuf_tensor("sb_sc", [128, T], F32)), "sc", max(T // 512, 1))
            mk = [P.tile([128, T], BF16, "mk%d" % i) for i in range(2)]
            rl = [P.tile([128, 512], F32, "rl%d" % i) for i in range(3)]
            bsn = P.tile([128, 1], F32, "bsn")
            bss = P.tile([128, 1], F32, "bss")
            bsu = P.tile([128, 1], F32, "bsu")
            lg = [P.tile([128, 512], F32, "lg%d" % i) for i in range(3)]
            Ee = [P.tile([128, 512], BF16, "Ee%d" % i) for i in range(3)]
            Em = [P.tile([128, 512], BF16, "Em%d" % i) for i in range(3)]
            rD = P.tile([128, 1024], F32, "rD")
            oTb = P.tile([128, 8, 128], BF16, "oTb")
            thrs = P.tile([128, NT], F32, "thrs")
            ytl = [P.tile([128, 4, 128], F32, "ytl%d" % i) for i in range(2)]
            ydb = P.tile([128, 4, 128], F32, "ydb") if dbg else None
            rot = [Bank(PA.p[0], PA.h, 0), Bank(PA.p[1], PA.h, 512), Bank(PD, PD.h, 0)]
            pending = [None, 0]

            def emit_S(qi, j):
                S = (qi + 1) * 128
                ncc = (S + 511) // 512
                idx = 0
                for h in range(8):
                    for cc in range(ncc):
                        wd = min(512, S - cc * 512)
                        bk = rot[idx % 3]
                        rb = rl[idx % 3]
                        idx += 1
                        scp = sc.p[cc]
                        P.pe(lambda e: e.matmul(bk.ap(0, wd), lhsT=iqT[:, h, j * 128:(j + 1) * 128],
                                                rhs=ikA[:, cc * 512:cc * 512 + wd], start=True, stop=False),
                             r=[iqT, ikA], w=[bk.tl])
                        P.pe(lambda e: e.matmul(bk.ap(0, wd), lhsT=iqT[:, h, j * 128:(j + 1) * 128],
                                                rhs=ikB[:, cc * 512:cc * 512 + wd], start=False, stop=True),
                             r=[iqT, ikB], w=[bk.tl])
                        P.act(lambda e: e.activation(out=rb[:, 0:wd], in_=bk.ap(0, wd), func=AF.Relu), r=[bk.tl], w=[rb])
                        if h == 0:
                            P.dve(lambda e: e.tensor_scalar(out=sc[:, cc * 512:cc * 512 + wd], in0=rb[:, 0:wd],
                                                            scalar1=iw[:, j, 0:1], scalar2=None, op0=ALU.mult),
                                  r=[rb, iw], w=[scp])
                        else:
                            P.dve(lambda e: e.scalar_tensor_tensor(out=sc[:, cc * 512:cc * 512 + wd], in0=rb[:, 0:wd],
                                                                   scalar=iw[:, j, h:h + 1],
                                                                   in1=sc[:, cc * 512:cc * 512 + wd], op0=ALU.mult, op1=ALU.add),
                                  r=[rb, iw, scp], w=[scp])
                scd = sc.p[(qi * 128) // 512]
                P.dve(lambda e: e.tensor_tensor(out=sc[:, qi * 128:(qi + 1) * 128], in0=sc[:, qi * 128:(qi + 1) * 128],
                                                in1=cst[:, C_NEG:C_NEG + 128], op=ALU.add), r=[scd, cst], w=[scd])

            def gen_B(qi):
                S = (qi + 1) * 128
                ncc = (S + 511) // 512
                scr = sc.p[0:ncc]
                mkb = mk[qi % 2]
                thr_c = float(2 * KTOP - S) - 0.5
                P.pool(lambda e: e.memset(bsn[:, :], 0.0), w=[bsn])
                for it in range(NB_IT):
                    ck = BIS_R / (2 ** it)
                    cn = ck / 2 if it < NB_IT - 1 else ck
                    P.act(lambda e: e.activation(out=mkb[:, 0:S], in_=sc[:, 0:S], func=AF.Sign, bias=bsn[:, 0:1],
                                                 accum_out=bss[:, 0:1]), r=scr + [bsn], w=[mkb, bss])
                    P.pool(lambda e: e.tensor_scalar(out=bsu[:, :], in0=bss[:, :], scalar1=thr_c, scalar2=-ck,
                                                     op0=ALU.is_ge, op1=ALU.mult), r=[bss], w=[bsu])
                    P.pool(lambda e: e.tensor_scalar(out=bsn[:, :], in0=bsu[:, :], scalar1=bsn[:, 0:1], scalar2=cn,
                                                     op0=ALU.add, op1=ALU.add), r=[bsu, bsn], w=[bsn])
                    yield
                P.act(lambda e: e.activation(out=mkb[:, 0:S], in_=sc[:, 0:S], func=AF.Sign, bias=bsn[:, 0:1]),
                      r=scr + [bsn], w=[mkb])
                if dbg:
                    P.dve(lambda e: e.tensor_scalar(out=thrs[:, qi:qi + 1], in0=bsn[:, 0:1], scalar1=-1.0, scalar2=None,
                                                    op0=ALU.mult), r=[bsn], w=[thrs])
                yield

            def gen_P(qi, j, qb):
                mkb = mk[qi % 2]
                qa = qaT2[qb]
                steps = [(kj, c) for kj in range(qi + 1) for c in range(2)]
                n = len(steps)

                def slot_of(kj):
                    k2 = kj % 2
                    return PT, PT[:, k2 * 512:k2 * 512 + 128]

                for i in range(n + 3):
                    if i < n:
                        kj, c = steps[i]
                        dl = min(qi - kj, 2)
                        bk = rot[i % 3]
                        if c == 0:
                            pts, ptap = slot_of(kj)
                            P.pe(lambda e: e.transpose(ptap, mkb[:, kj * 128:(kj + 1) * 128], identb[:, :]),
                                 r=[mkb, identb], w=[pts])
                        P.pe(lambda e: e.matmul(bk.ap(0, 512), lhsT=ckvT[:, kj * 128:(kj + 1) * 128],
                                                rhs=qa[:, c * 4:(c + 1) * 4, j * 128:(j + 1) * 128], start=True, stop=True),
                             r=[ckvT, qa], w=[bk.tl])
                    if 0 <= i - 3 < n:
                        kj, c = steps[i - 3]
                        emt = Em[(i - 3) % 3]
                        P.pe(lambda e: e.matmul(PB[:, c * 512:(c + 1) * 512], lhsT=ckvtm[:, kj, :], rhs=emt[:, :],
                                                start=(kj == 0), stop=(kj == qi)), r=[ckvtm, emt], w=[PB.p[c]])
                        P.pe(lambda e: e.matmul(PC[:, c * 512:(c + 1) * 512], lhsT=onesb[:, :], rhs=emt[:, :],
                                                start=(kj == 0), stop=(kj == qi)), r=[onesb, emt], w=[PC.p[c]])
                    if 0 <= i - 2 < n:
                        kj, c = steps[i - 2]
                        pts, ptap = slot_of(kj)
                        eet, emt = Ee[(i - 2) % 3], Em[(i - 2) % 3]
                        P.dve(lambda e: e.scalar_tensor_tensor(
                            out=emt[:, :].rearrange("p (h t) -> p h t", h=4),
                            in0=ptap.unsqueeze(1).to_broadcast([128, 4, 128]), scalar=1.0,
                            in1=eet[:, :].rearrange("p (h t) -> p h t", h=4), op0=ALU.add, op1=ALU.mult),
                            r=[eet, pts], w=[emt])
                    if i < n:
                        kj, c = steps[i]
                        dl = min(qi - kj, 2)
                        bk = rot[i % 3]
                        lgt = lg[i % 3]
                        P.dve(lambda e: e.tensor_tensor(out=lgt[:, :], in0=bk.ap(0, 512),
                                                        in1=biasT[:, dl, c * 512:(c + 1) * 512], op=ALU.add),
                              r=[bk.tl, biasT], w=[lgt])
                    if 0 <= i - 1 < n:
                        lgt, eet = lg[(i - 1) % 3], Ee[(i - 1) % 3]
                        P.act(lambda e: e.activation(out=eet[:, :], in_=lgt[:, :], func=AF.Exp), r=[lgt], w=[eet])
                    yield
                hs = n
                P.dve(lambda e: e.reciprocal(out=rD[:, :], in_=PC[:, :]), r=[PC], w=[rD])
                P.dve(lambda e: e.tensor_tensor(out=oTb[:, :, :].rearrange("p h t -> p (h t)"), in0=PB[:, :],
                                                in1=rD[:, :], op=ALU.mult), r=[PB, rD], w=[oTb])
                bk = rot[hs % 3]
                for m in range(4):
                    for hh in range(2):
                        h = 2 * m + hh
                        P.pe(lambda e: e.matmul(bk.ap(m * 128, (m + 1) * 128), lhsT=w_uvb[:, h, :], rhs=oTb[:, h, :],
                                                start=(hh == 0), stop=(hh == 1)), r=[w_uvb, oTb], w=[bk.tl])
                yt_ = ytl[qi % 2]
                P.act(lambda e: e.activation(out=yt_[:, :, :], in_=bk.ap(0, 512).rearrange("p (m t) -> p m t", m=4),
                                             func=AF.Copy), r=[bk.tl], w=[yt_])
                P.dma(yscr[qi * 128:(qi + 1) * 128, :], yt_[:, :, :].rearrange("p m t -> p (m t)"), "yst",
                      r=[yt_], w=[yscr_t], q="sp")
                if dbg:
                    P.dve(lambda e: e.tensor_copy(out=ydb[:, :, :], in_=bk.ap(0, 512).rearrange("p (m t) -> p m t", m=4)),
                          r=[bk.tl], w=[ydb])
                    dump("yatt", ydb[:, :, :], ydb,
                         dbg_d["yatt"][:, :].rearrange("p (m t) -> p m t", m=4)[:, :, qi * 128:(qi + 1) * 128])
                yield

            def interleave(ga, gb, na, nb):
                ia = ib = 0
                da = db = False
                while not (da and db):
                    if not da and (db or ia * nb <= ib * na):
                        try:
                            next(ga)
                            ia += 1
                        except StopIteration:
                            da = True
                    else:
                        try:
                            next(gb)
                            ib += 1
                        except StopIteration:
                            db = True

            for s in range(NS):
                for j in range(2):
                    r0 = s * 256 + j * 128
                    P.dma(xt[:, j, :], x_d[r0:r0 + 128, :], "xin", w=[xt])
                for j in range(2):
                    P.act(lambda e, j=j: e.activation(out=sqj[:, :], in_=xt[:, j, :], func=AF.Square,
                                                      accum_out=st4[:, j:j + 1]), r=[xt], w=[sqj, st4])
                P.act(lambda e: e.activation(out=st4[:, 2:4], in_=st4[:, 0:2], func=AF.Sqrt, scale=1.0 / D,
                                             bias=epsc[:, 0:1]), r=[st4, epsc], w=[st4])
                P.dve(lambda e: e.reciprocal(out=st4[:, 4:6], in_=st4[:, 2:4]), r=[st4], w=[st4])
                for j in range(2):
                    P.dve(lambda e, j=j: e.tensor_scalar(out=dgt[:, j, :], in0=cst[:, C_ID:C_ID + 128],
                                                         scalar1=st4[:, 4 + j:5 + j], scalar2=None, op0=ALU.mult),
                          r=[cst, st4], w=[dgt])
                for kk in range(2):
                    for k4 in range(4):
                        k = kk * 4 + k4
                        for j in range(2):
                            P.pe(lambda e, k=k, k4=k4, j=j: e.matmul(
                                PA[:, k4 * 256 + j * 128:k4 * 256 + (j + 1) * 128],
                                lhsT=xt[:, j, k * 128:(k + 1) * 128], rhs=dgt[:, j, :], start=True, stop=True),
                                r=[xt, dgt], w=[PA])
                    for k4 in range(4):
                        k = kk * 4 + k4
                        P.dve(lambda e, k=k, k4=k4: e.tensor_scalar(
                            out=hT[:, k, :], in0=PA[:, k4 * 256:(k4 + 1) * 256], scalar1=gsh[:, k:k + 1],
                            scalar2=modc[:, k:k + 1], op0=ALU.mult, op1=ALU.add), r=[PA, gsh, modc], w=[hT])
                for c in range(4):
                    for k in range(8):
                        P.pe(lambda e, c=c, k=k: e.matmul(PB[:, c * 256:(c + 1) * 256],
                                                          lhsT=w_att[:, k, c * 128:(c + 1) * 128], rhs=hT[:, k, :],
                                                          start=(k == 0), stop=(k == 7)), r=[w_att, hT], w=[PB])
                P.act(lambda e: e.activation(out=cqraw[:, :, :], in_=PB[:, :].rearrange("p (c t) -> p c t", c=4),
                                             func=AF.Copy), r=[PB], w=[cqraw])
                P.act(lambda e: e.activation(out=sq[:, :, :], in_=PB[:, :].rearrange("p (c t) -> p c t", c=4),
                                             func=AF.Square), r=[PB], w=[sq])
                for j in range(2):
                    for k in range(8):
                        P.pe(lambda e, j=j, k=k: e.matmul(PD[:, j * 8:(j + 1) * 8], lhsT=hT[:, k, j * 128:(j + 1) * 128],
                                                          rhs=w_iw[:, k, :], start=(k == 0), stop=(k == 7)),
                             r=[hT, w_iw], w=[PD])
                P.act(lambda e: e.activation(out=iw[:, :, :], in_=PD[:, 0:16].rearrange("p (j h) -> p j h", j=2),
                                             func=AF.Copy, scale=float(8 ** -0.5 * 64 ** -0.5)), r=[PD], w=[iw])
                for c in range(2):
                    P.pe(lambda e, c=c: e.matmul(PC[:, 0:256], lhsT=cst[:, C_ONE:C_ONE + 128], rhs=sq[:, c, :],
                                                 start=(c == 0), stop=(c == 1)), r=[cst, sq], w=[PC])
                P.pe(lambda e: e.matmul(PC[:, 256:512], lhsT=cst[:, C_ONE:C_ONE + 128], rhs=sq[:, 2, :],
                                        start=True, stop=True), r=[cst, sq], w=[PC])
                P.pe(lambda e: e.matmul(PC[:, 512:768], lhsT=cst[:, C_BLK:C_BLK + 128], rhs=sq[:, 3, :],
                                        start=True, stop=True), r=[cst, sq], w=[PC])
                for i, dv in enumerate((256.0, 128.0, 64.0)):
                    P.act(lambda e, i=i, dv=dv: e.activation(out=rq[:, i, :], in_=PC[:, i * 256:(i + 1) * 256],
                                                             func=AF.Sqrt, scale=1.0 / dv, bias=epsc[:, 0:1]),
                          r=[PC, epsc], w=[rq])
                P.dve(lambda e: e.reciprocal(out=rq[:, :, :], in_=rq[:, :, :]), r=[rq], w=[rq])
                for c in range(2):
                    P.dve(lambda e, c=c: e.scalar_tensor_tensor(out=cqT[:, c, :], in0=cqraw[:, c, :],
                                                                scalar=vec[:, V_QN + c:V_QN + c + 1], in1=rq[:, 0, :],
                                                                op0=ALU.mult, op1=ALU.mult), r=[cqraw, vec, rq], w=[cqT])
                P.act(lambda e: e.activation(out=cqTb[:, :, :], in_=cqT[:, :, :], func=AF.Copy), r=[cqT], w=[cqTb])
                P.dve(lambda e, s=s: e.scalar_tensor_tensor(out=ckvT[:, s * 256:(s + 1) * 256], in0=cqraw[:, 2, :],
                                                            scalar=vec[:, V_KVN:V_KVN + 1], in1=rq[:, 1, :],
                                                            op0=ALU.mult, op1=ALU.mult), r=[cqraw, vec, rq], w=[ckvT])
                P.dve(lambda e: e.scalar_tensor_tensor(out=ikf[:, :], in0=cqraw[:, 3, :],
                                                       scalar=vec[:, V_IKN:V_IKN + 1], in1=rq[:, 2, :],
                                                       op0=ALU.mult, op1=ALU.mult), r=[cqraw, vec, rq], w=[ikf])
                P.act(lambda e, s=s: e.activation(out=ikA[:, s * 256:(s + 1) * 256], in_=ikf[:, :], func=AF.Copy),
                      r=[ikf], w=[ikA])
                P.dve(lambda e, s=s: e.tensor_tensor(out=ikB[0:64, s * 256:(s + 1) * 256], in0=ikf[0:64, :],
                                                     in1=ikA[0:64, s * 256:(s + 1) * 256], op=ALU.subtract),
                      r=[ikf, ikA], w=[ikB])
                for j in range(2):
                    tix = s * 2 + j
                    P.pe(lambda e, j=j, tix=tix: e.transpose(PT[:, j * 128:(j + 1) * 128],
                                                             ckvT[:, tix * 128:(tix + 1) * 128], identb[:, :]),
                         r=[ckvT, identb], w=[PT])
                P.act(lambda e, s=s: e.activation(out=ckvtm[:, 2 * s:2 * s + 2, :],
                                                  in_=PT[:, 0:256].rearrange("p (j c) -> p j c", j=2), func=AF.Copy),
                      r=[PT], w=[ckvtm])
                for hf in range(2):
                    for h4 in range(4):
                        h = hf * 4 + h4
                        for c in range(2):
                            P.pe(lambda e, h=h, h4=h4, c=c: e.matmul(PB[:, h4 * 256:(h4 + 1) * 256],
                                                                     lhsT=w_iq[:, c, h * 128:(h + 1) * 128], rhs=cqT[:, c, :],
                                                                     start=(c == 0), stop=(c == 1)), r=[w_iq, cqT], w=[PB])
                    hsl = slice(hf * 4, hf * 4 + 4)
                    pb3 = lambda rows: PB[rows, :].rearrange("p (m t) -> p m t", m=4)
                    P.act(lambda e, hsl=hsl: e.activation(out=iqT[0:64, hsl, :], in_=pb3(slice(0, 64)), func=AF.Copy),
                          r=[PB], w=[iqT])
                    P.act(lambda e: e.activation(out=iqtmp[64:128, :, :], in_=pb3(slice(64, 128)), func=AF.Copy),
                          r=[PB], w=[iqtmp])
                    P.dve(lambda e, hsl=hsl: e.tensor_tensor(out=iqT[64:128, hsl, :], in0=pb3(slice(64, 128)),
                                                             in1=iqtmp[64:128, :, :], op=ALU.subtract),
                          r=[PB, iqtmp], w=[iqT])
                for m in range(4):
                    for c in range(2):
                        P.pe(lambda e, m=m, c=c: e.matmul(PC[:, m * 256:(m + 1) * 256],
                                                          lhsT=w_uqb[:, c, m * 128:(m + 1) * 128], rhs=cqTb[:, c, :],
                                                          start=(c == 0), stop=(c == 1)), r=[w_uqb, cqTb], w=[PC])
                P.dve(lambda e: e.tensor_copy(out=qTb[:, :, :], in_=PC[:, :].rearrange("p (m t) -> p m t", m=4)),
                      r=[PC], w=[qTb])
                for hf in range(2):
                    for h4 in range(4):
                        h = hf * 4 + h4
                        pr = slice((h % 2) * 64, (h % 2) * 64 + 64)
                        P.pe(lambda e, h=h, h4=h4, pr=pr: e.matmul(PB[:, h4 * 256:(h4 + 1) * 256],
                                                                   lhsT=w_ukb[:, h, :], rhs=qTb[:, h // 2, :],
                                                                   start=True, stop=True), r=[w_ukb, qTb], w=[PB])
                    P.act(lambda e, hf=hf, s=s: e.activation(out=qaT2[s % 2][:, hf * 4:(hf + 1) * 4, :],
                                                             in_=PB[:, :].rearrange("p (h t) -> p h t", h=4), func=AF.Copy,
                                                             scale=0.125), r=[PB], w=[qaT2[s % 2]])
                for j in range(2):
                    qi = s * 2 + j
                    emit_S(qi, j)
                    gB = gen_B(qi)
                    if pending[0] is not None:
                        interleave(gB, pending[0], NB_IT + 1, pending[1])
                    else:
                        for _ in gB:
                            pass
                    pending[0] = gen_P(qi, j, s % 2)
                    pending[1] = 2 * (qi + 1) + 4
            for _ in pending[0]:
                pass
            if dbg:
                dump("thr", thrs[:, :], thrs)

        print("ops after A1", P.nops)
        P.fence()
        if upto < 2:
            P.enabled = False
        x1t = Tl(P, None, "x1_hbm")
        with ExitStack() as es2:
            P.es_cur = es2
            w_rwb = P.tile([128, 8, 1824], BF16, "w_rwb")
            w_lorab = P.tile([128, 1024], BF16, "w_lorab")
            w_gateb = P.tile([128, 2, 512], BF16, "w_gateb")
            w_outb = P.tile([128, 8, D], BF16, "w_outb")
            mucol = P.tile([128, 32], F32, "mucol")
            lnrow = P.tile([128, 2, 512], F32, "lnrow")
            wst = P.tile([128, 2048], F32, "wst2")
            P.dve(lambda e: e.tensor_copy(out=mucol[:, 0:15], in_=vec[:, 108:123]), r=[vec], w=[mucol])
            P.dve(lambda e: e.tensor_scalar(out=mucol[:, 16:31], in0=vec[:, 108:123], scalar1=-1.0, scalar2=1.0,
                                            op0=ALU.mult, op1=ALU.add), r=[vec, mucol], w=[mucol])
            for k in range(8):
                P.dma(wst[:, 0:1824], wrw_d[k * 128:(k + 1) * 128, :], "wst", w=[wst])
                P.act(lambda e, k=k: e.activation(out=w_rwb[:, k, :], in_=wst[:, 0:1824], func=AF.Copy), r=[wst], w=[w_rwb])
            for k in range(8):
                P.dma(wst[:, 0:D], wout_d[k * 128:(k + 1) * 128, :], "wst", w=[wst])
                P.act(lambda e, k=k: e.activation(out=w_outb[:, k, :], in_=wst[:, 0:D], func=AF.Copy), r=[wst], w=[w_outb])
            P.dma(wst[:, 0:1024], wlora_d[:, :], "wst", w=[wst])
            P.act(lambda e: e.activation(out=w_lorab[:, :], in_=wst[:, 0:1024], func=AF.Copy), r=[wst], w=[w_lorab])
            P.dve(lambda e: e.memset(w_gateb[:, :, :], 0.0), w=[w_gateb])
            P.dma(wst[:, 0:512], wgate_d[0:128, :], "wst", w=[wst])
            P.act(lambda e: e.activation(out=w_gateb[:, 0, :], in_=wst[:, 0:512], func=AF.Copy), r=[wst], w=[w_gateb])
            P.dma(wst[96:128, 0:512], wgate_d[128:160, :], "wst", w=[wst])
            P.act(lambda e: e.activation(out=w_gateb[96:128, 1, :], in_=wst[96:128, 0:512], func=AF.Copy), r=[wst], w=[w_gateb])
            for i in range(2):
                P.dma(lnrow[:, i, :], lnrow_d[i, :].partition_broadcast(128), "cw", w=[lnrow])

            xt = P.tile([128, 2, D], F32, "xt2")
            sqj = P.tile([128, D], BF16, "sqj2")
            st4 = P.tile([128, 8], F32, "st42")
            dgt = P.tile([128, 2, 128], F32, "dgt2")
            hTb = P.tile([128, 8, 256], BF16, "hTb")
            lastc = TlG(P, es2.enter_context(nc.sbuf_tensor("sb_lastc", [128, 16], F32)), "lastc", 16)
            ptmp = [P.tile([128, 256], F32, "ptmp%d" % i) for i in range(3)]
            pch = [P.tile([128, 256], F32, "pch%d" % i) for i in range(3)]
            rT = P.tile([128, 4, 256], F32, "rT")
            kT = P.tile([128, 4, 256], F32, "kT")
            vTb = P.tile([128, 4, 256], BF16, "vTb")
            twd = P.tile([128, 256], BF16, "twd")
            sgd = P.tile([128, 2, 256], BF16, "sgd")
            ART = P.tile([128, 4, 2, 256], BF16, "ART")
            btT = P.tile([128, 8, 256], BF16, "btT")
            ktT = P.tile([128, 8, 256], BF16, "ktT")
            Hbz = P.tile([128, 8, 64], BF16, "Hbz")
            P.dve(lambda e: e.memset(btT[:, :, :], 0.0), w=[btT])
            P.dve(lambda e: e.memset(ktT[:, :, :], 0.0), w=[ktT])
            P.dve(lambda e: e.memset(Hbz[:, :, :], 0.0), w=[Hbz])
            bhT = P.tile([128, 4, 256], BF16, "bhT")
            khT = P.tile([128, 4, 256], BF16, "khT")
            rkT = P.tile([128, 4, 256], BF16, "rkT")
            WC = P.tile([128, 4, 2], F32, "WC")
            f = [P.tile([128, 256], F32, "f%d" % i) for i in range(10)]
            Vtm = P.tile([128, 512], BF16, "Vtm")
            Bhtm = P.tile([128, 512], BF16, "Bhtm")
            Khtm = P.tile([128, 512], BF16, "Khtm")
            ATab = P.tile([128, 8, 256], BF16, "ATab")
            ATak = P.tile([128, 8, 256], BF16, "ATak")
            Nk = [P.tile([128, 8, 128], BF16, "Nk%d" % i) for i in range(2)]
            Mk = [P.tile([128, 8, 128], BF16, "Mk%d" % i) for i in range(2)]
            Pk = [P.tile([128, 8, 128], BF16, "Pk%d" % i) for i in range(2)]
            Hs = P.tile([128, 4, 64], F32, "Hs")
            Hb = P.tile([128, 4, 64], BF16, "Hb")
            Hd = P.tile([128, 4, 64], F32, "Hd")
            WCf = P.tile([128, 4, 64], F32, "WCf")
            Gb = P.tile([128, 512], BF16, "Gb")
            Ub = P.tile([128, 512], BF16, "Ub")
            Ysb = P.tile([128, 512], F32, "Ysb")
            Ysq = P.tile([128, 512], F32, "Ysq")
            gst = P.tile([128, 48], F32, "gst")
            vtok = P.tile([128, 512], F32, "vtok")
            yrw = P.tile([128, 512], F32, "yrw")
            yrwb = P.tile([128, 512], BF16, "yrwb")
            ycat = P.tile([128, 4, 128], BF16, "ycat")
            mix = P.tile([128, D], F32, "mix")
            x1s = [P.tile([128, D], F32, "x1s%d" % i) for i in range(1)]
            ya = P.tile([128, 4, 128], BF16, "ya")
            yaf = P.tile([128, 512], F32, "yaf")
            oka = P.tile([128, 4], F32, "oka")
            hindb = P.tile([128, 2], BF16, "hindb")
            P.dve(lambda e: e.tensor_scalar(out=oka[:, :], in0=vec[:, V_KA:V_KA + 4], scalar1=-1.0, scalar2=1.0,
                                            op0=ALU.mult, op1=ALU.add), r=[vec], w=[oka])
            P.dve(lambda e: e.tensor_copy(out=hindb[:, :], in_=cst[:, C_HIND:C_HIND + 2]), r=[cst], w=[hindb])
            P.dve(lambda e: e.memset(lastc[:, :], 0.0), w=[lastc])
            P.dve(lambda e: e.memset(Hs[:, :, :], 0.0), w=[Hs])
            P.dve(lambda e: e.memset(sgd[:, :, :], 0.0), w=[sgd])

            iprot = [Bank(PD, PD.h, 0), Bank(PC.p[0], PC.h, 0), Bank(PC.p[1], PC.h, 512), Bank(PB.p[1], PB.h, 512)]

            def inproj(n, width=128):
                c0 = n * 128 if n < 14 else 1824 - 128
                bk = iprot[n % 4]
                for k in range(8):
                    P.pe(lambda e, k=k: e.matmul(bk.ap(0, 256), lhsT=w_rwb[:, k, c0:c0 + 128], rhs=hTb[:, k, :],
                                                 start=(k == 0), stop=(k == 7)), r=[w_rwb, hTb], w=[bk.tl])
                pt = ptmp[n % 3]
                pc = pch[n % 3]
                lc = lastc.p[n]
                P.act(lambda e: e.activation(out=pt[:, :], in_=bk.ap(0, 256), func=AF.Copy,
                                             scale=mucol[:, 16 + n:17 + n]), r=[bk.tl, mucol], w=[pt])
                P.dve(lambda e: e.scalar_tensor_tensor(out=pc[:, 1:256], in0=bk.ap(0, 255),
                                                       scalar=mucol[:, n:n + 1], in1=pt[:, 1:256],
                                                       op0=ALU.mult, op1=ALU.add), r=[bk.tl, mucol, pt], w=[pc])
                P.dve(lambda e: e.scalar_tensor_tensor(out=pc[:, 0:1], in0=lastc[:, n:n + 1],
                                                       scalar=mucol[:, n:n + 1], in1=pt[:, 0:1],
                                                       op0=ALU.mult, op1=ALU.add), r=[lc, mucol, pt, pc], w=[pc])
                P.dve(lambda e: e.tensor_copy(out=lastc[:, n:n + 1], in_=bk.ap(255, 256)), r=[bk.tl, lc], w=[lc])
                return pc

            for s in range(NS):
                for j in range(2):
                    r0 = s * 256 + j * 128
                    P.dma(xt[:, j, :], x_d[r0:r0 + 128, :], "xin", w=[xt])
                for j in range(2):
                    P.act(lambda e, j=j: e.activation(out=sqj[:, :], in_=xt[:, j, :], func=AF.Square,
                                                      accum_out=st4[:, j:j + 1]), r=[xt], w=[sqj, st4])
                P.act(lambda e: e.activation(out=st4[:, 2:4], in_=st4[:, 0:2], func=AF.Sqrt, scale=1.0 / D,
                                             bias=epsc[:, 0:1]), r=[st4, epsc], w=[st4])
                P.dve(lambda e: e.reciprocal(out=st4[:, 4:6], in_=st4[:, 2:4]), r=[st4], w=[st4])
                for j in range(2):
                    P.dve(lambda e, j=j: e.tensor_scalar(out=dgt[:, j, :], in0=cst[:, C_ID:C_ID + 128],
                                                         scalar1=st4[:, 4 + j:5 + j], scalar2=None, op0=ALU.mult),
                          r=[cst, st4], w=[dgt])
                for kk in range(2):
                    for k4 in range(4):
                        k = kk * 4 + k4
                        for j in range(2):
                            P.pe(lambda e, k=k, k4=k4, j=j: e.matmul(
                                PA[:, k4 * 256 + j * 128:k4 * 256 + (j + 1) * 128],
                                lhsT=xt[:, j, k * 128:(k + 1) * 128], rhs=dgt[:, j, :], start=True, stop=True),
                                r=[xt, dgt], w=[PA])
                    for k4 in range(4):
                        k = kk * 4 + k4
                        P.dve(lambda e, k=k, k4=k4: e.tensor_scalar(
                            out=hTb[:, k, :], in0=PA[:, k4 * 256:(k4 + 1) * 256], scalar1=gsh[:, k:k + 1],
                            scalar2=modc[:, k:k + 1], op0=ALU.mult, op1=ALU.add), r=[PA, gsh, modc], w=[hTb])
                for m in range(4):
                    pc = inproj(m)
                    P.act(lambda e, m=m, pc=pc: e.activation(out=rT[:, m, :], in_=pc[:, :], func=AF.Copy), r=[pc], w=[rT])
                for m in range(4):
                    pc = inproj(4 + m)
                    P.act(lambda e, m=m, pc=pc: e.activation(out=kT[:, m, :], in_=pc[:, :], func=AF.Copy), r=[pc], w=[kT])
                for m in range(4):
                    pc = inproj(8 + m)
                    P.act(lambda e, m=m, pc=pc: e.activation(out=vTb[:, m, :], in_=pc[:, :], func=AF.Copy), r=[pc], w=[vTb])
                pc = inproj(12)
                P.act(lambda e, pc=pc: e.activation(out=twd[0:64, :], in_=pc[0:64, :], func=AF.Tanh), r=[pc], w=[twd])
                P.act(lambda e, pc=pc: e.activation(out=twd[64:128, :], in_=pc[64:128, :], func=AF.Copy), r=[pc], w=[twd])
                pc = inproj(13)
                P.act(lambda e, pc=pc: e.activation(out=sgd[:, 0, :], in_=pc[:, :], func=AF.Sigmoid), r=[pc], w=[sgd])
                pc = inproj(14)
                P.act(lambda e, pc=pc: e.activation(out=sgd[:, 1, :], in_=pc[:, :], func=AF.Sigmoid), r=[pc], w=[sgd])
                for m in range(4):
                    ms = slice(m * 128, (m + 1) * 128)
                    P.pe(lambda e, m=m: e.matmul(PB[:, 0:256], lhsT=w_lorab[:, m * 128:(m + 1) * 128], rhs=twd[:, :],
                                                 start=True, stop=True), r=[w_lorab, twd], w=[PB])
                    P.pe(lambda e, m=m: e.matmul(PB[:, 256:512], lhsT=w_lorab[:, 512 + m * 128:512 + (m + 1) * 128], rhs=twd[:, :],
                                                 start=True, stop=True), r=[w_lorab, twd], w=[PB])
                    sgw, cs, wt, winv, wprev, kk0, asig, kp, t1, t2 = f
                    P.act(lambda e, m=m: e.activation(out=sgw[:, :], in_=PB[:, 0:256], func=AF.Sigmoid,
                                                      bias=vec[:, V_W0 + m:V_W0 + m + 1]), r=[PB, vec], w=[sgw])
                    P.act(lambda e, m=m: e.activation(out=asig[:, :], in_=PB[:, 256:512], func=AF.Sigmoid,
                                                      bias=vec[:, V_A0 + m:V_A0 + m + 1]), r=[PB, vec], w=[asig])
                    P.dve(lambda e: e.tensor_tensor_scan(out=cs[:, :], data0=cst[:, C_SCAN:C_SCAN + 256], data1=sgw[:, :],
                                                         initial=0.0, op0=ALU.mult, op1=ALU.add), r=[cst, sgw], w=[cs])
                    P.act(lambda e: e.activation(out=wt[:, :], in_=cs[:, :], func=AF.Exp, scale=-LWC), r=[cs], w=[wt])
                    P.act(lambda e: e.activation(out=winv[:, :], in_=cs[:, :], func=AF.Exp, scale=LWC), r=[cs], w=[winv])
                    P.dve(lambda e: e.tensor_tensor(out=t1[:, :], in0=cs[:, :], in1=sgw[:, :], op=ALU.subtract),
                          r=[cs, sgw], w=[t1])
                    P.act(lambda e: e.activation(out=wprev[:, :], in_=t1[:, :], func=AF.Exp, scale=-LWC), r=[t1], w=[wprev])
                    for j in range(2):
                        cj = slice(j * 128, (j + 1) * 128)
                        P.dve(lambda e, m=m, j=j: e.tensor_scalar(out=WC[:, m, j:j + 1], in0=cs[:, j * 128 + 127:j * 128 + 128],
                                                                  scalar1=-LWC, scalar2=None, op0=ALU.mult),
                              r=[cs], w=[WC])
                        P.act(lambda e, m=m, j=j, cj=cj: e.activation(out=t2[:, cj], in_=cs[:, cj], func=AF.Exp, scale=LWC,
                                                                      bias=WC[:, m, j:j + 1]), r=[cs, WC], w=[t2])
                    P.act(lambda e, m=m: e.activation(out=WC[:, m, :], in_=WC[:, m, :], func=AF.Exp), r=[WC], w=[WC])
                    P.dve(lambda e, m=m: e.tensor_scalar(out=kk0[:, :], in0=kT[:, m, :], scalar1=vec[:, V_KK + m:V_KK + m + 1],
                                                         scalar2=None, op0=ALU.mult), r=[kT, vec], w=[kk0])
                    P.act(lambda e: e.activation(out=t1[:, :], in_=kk0[:, :], func=AF.Square), r=[kk0], w=[t1])
                    P.pe(lambda e: e.matmul(PB[:, 512:768], lhsT=cst[:, C_BLK:C_BLK + 128], rhs=t1[:, :],
                                            start=True, stop=True), r=[cst, t1], w=[PB])
                    P.act(lambda e: e.activation(out=t1[:, :], in_=PB[:, 512:768], func=AF.Sqrt), r=[PB], w=[t1])
                    P.dve(lambda e: e.tensor_scalar(out=t1[:, :], in0=t1[:, :], scalar1=1e-12, scalar2=None, op0=ALU.max),
                          r=[t1], w=[t1])
                    P.dve(lambda e: e.reciprocal(out=t1[:, :], in_=t1[:, :]), r=[t1], w=[t1])
                    P.dve(lambda e: e.tensor_tensor(out=kk0[:, :], in0=kk0[:, :], in1=t1[:, :], op=ALU.mult),
                          r=[kk0, t1], w=[kk0])
                    P.dve(lambda e, m=m: e.tensor_scalar(out=t1[:, :], in0=asig[:, :], scalar1=vec[:, V_KA + m:V_KA + m + 1],
                                                         scalar2=oka[:, m:m + 1], op0=ALU.mult, op1=ALU.add),
                          r=[asig, vec, oka], w=[t1])
                    P.dve(lambda e, m=m: e.tensor_tensor(out=kp[:, :], in0=kT[:, m, :], in1=t1[:, :], op=ALU.mult),
                          r=[kT, t1], w=[kp])
                    P.dve(lambda e, m=m: e.scalar_tensor_tensor(out=rkT[:, m, :], in0=rT[:, m, :],
                                                                scalar=vec[:, V_RK + m:V_RK + m + 1], in1=kp[:, :],
                                                                op0=ALU.mult, op1=ALU.mult), r=[rT, vec, kp], w=[rkT])
                    P.dve(lambda e, m=m: e.tensor_tensor(out=ART[:, m, 1, :], in0=rT[:, m, :], in1=wt[:, :], op=ALU.mult),
                          r=[rT, wt], w=[ART])
                    P.dve(lambda e, m=m: e.scalar_tensor_tensor(out=ART[:, m, 0, :], in0=kk0[:, :], scalar=-1.0,
                                                                in1=wprev[:, :], op0=ALU.mult, op1=ALU.mult),
                          r=[kk0, wprev], w=[ART])
                    P.dve(lambda e: e.tensor_tensor(out=t1[:, :], in0=kk0[:, :], in1=asig[:, :], op=ALU.mult),
                          r=[kk0, asig], w=[t1])
                    for hh in range(2):
                        prr = slice(hh * 64, hh * 64 + 64)
                        P.dve(lambda e, m=m, hh=hh, prr=prr: e.tensor_tensor(out=btT[prr, 2 * m + hh, :], in0=t1[prr, :],
                                                                             in1=winv[prr, :], op=ALU.mult),
                              r=[t1, winv], w=[btT])
                    P.dve(lambda e, m=m: e.tensor_tensor(out=bhT[:, m, :], in0=t1[:, :], in1=t2[:, :], op=ALU.mult),
                          r=[t1, t2], w=[bhT])
                    for hh in range(2):
                        prr = slice(hh * 64, hh * 64 + 64)
                        P.dve(lambda e, m=m, hh=hh, prr=prr: e.tensor_tensor(out=ktT[prr, 2 * m + hh, :], in0=kp[prr, :],
                                                                             in1=winv[prr, :], op=ALU.mult),
                              r=[kp, winv], w=[ktT])
                    P.dve(lambda e, m=m: e.tensor_tensor(out=khT[:, m, :], in0=kp[:, :], in1=t2[:, :], op=ALU.mult),
                          r=[kp, t2], w=[khT])
                for j in range(2):
                    ti = s * 2 + j
                    tb = slice(j * 128, (j + 1) * 128)
                    for (src, dst) in ((vTb, Vtm), (bhT, Bhtm), (khT, Khtm)):
                        for m in range(4):
                            P.pe(lambda e, src=src, m=m: e.transpose(PT[:, m * 128:(m + 1) * 128], src[:, m, tb], identb[:, :]),
                                 r=[src, identb], w=[PT])
                        P.act(lambda e, dst=dst: e.activation(out=dst[:, :], in_=PT[:, 0:512], func=AF.Copy), r=[PT], w=[dst])
                    P.dve(lambda e: e.tensor_copy(out=vtok[:, :], in_=Vtm[:, :]), r=[Vtm], w=[vtok])
                    for hf in range(2):
                        for h4 in range(4):
                            h = hf * 4 + h4
                            m = h // 2
                            pr = slice((h % 2) * 64, (h % 2) * 64 + 64)
                            P.pe(lambda e, h4=h4, m=m, h=h: e.matmul(PA[:, h4 * 256:(h4 + 1) * 256], lhsT=btT[:, h, tb],
                                                                     rhs=ART[:, m, :, tb], start=True, stop=True),
                                 r=[btT, ART], w=[PA])
                            P.pe(lambda e, h4=h4, m=m, h=h: e.matmul(PB[:, h4 * 256:(h4 + 1) * 256], lhsT=ktT[:, h, tb],
                                                                     rhs=ART[:, m, :, tb], start=True, stop=True),
                                 r=[ktT, ART], w=[PB])
                        mur = cst[:, C_MSU:C_MSU + 256].unsqueeze(1).to_broadcast([128, 4, 256])
                        msl = cst[:, C_MSL:C_MSL + 128].unsqueeze(1).to_broadcast([128, 4, 128])
                        msu = cst[:, C_MSU:C_MSU + 128].unsqueeze(1).to_broadcast([128, 4, 128])
                        hs4 = slice(hf * 4, hf * 4 + 4)
                        P.dve(lambda e, hs4=hs4, mur=mur: e.tensor_tensor(
                            out=ATab[:, hs4, :], in0=PA[:, :].rearrange("p (h t) -> p h t", h=4), in1=mur, op=ALU.mult),
                            r=[PA, cst], w=[ATab])
                        P.dve(lambda e, hs4=hs4, mur=mur: e.tensor_tensor(
                            out=ATak[:, hs4, :], in0=PB[:, :].rearrange("p (h t) -> p h t", h=4), in1=mur, op=ALU.mult),
                            r=[PB, cst], w=[ATak])
                    P.act(lambda e: e.activation(out=Mk[0][:, :, :], in_=ATab[:, :, 0:128], func=AF.Copy), r=[ATab], w=[Mk[0]])
                    for h in range(8):
                        P.pe(lambda e, h=h: e.transpose(PT[:, h * 128:(h + 1) * 128], Mk[0][:, h, :], identb[:, :]),
                             r=[Mk[0], identb], w=[PT])
                    P.act(lambda e: e.activation(out=Nk[0][:, :, :].rearrange("p h t -> p (h t)"), in_=PT[:, :], func=AF.Copy),
                          r=[PT], w=[Nk[0]])
                    P.dve(lambda e: e.tensor_tensor(out=Pk[0][:, :, :], in0=ATab[:, :, 0:128],
                                                    in1=identb[:, :].unsqueeze(1).to_broadcast([128, 8, 128]), op=ALU.add),
                          r=[ATab, identb], w=[Pk[0]])
                    for lv in range(6):
                        a, b = lv % 2, (lv + 1) % 2
                        for h in range(8):
                            P.pe(lambda e, h=h, a=a: e.matmul(PA[:, h * 128:(h + 1) * 128], lhsT=Mk[a][:, h, :], rhs=Nk[a][:, h, :],
                                                              start=True, stop=True), r=[Mk[a], Nk[a]], w=[PA])
                        P.act(lambda e, b=b: e.activation(out=Nk[b][:, :, :].rearrange("p h t -> p (h t)"), in_=PA[:, :],
                                                          func=AF.Copy), r=[PA], w=[Nk[b]])
                        if lv < 5:
                            for h in range(8):
                                P.pe(lambda e, h=h, a=a: e.matmul(PB[:, h * 128:(h + 1) * 128], lhsT=Nk[a][:, h, :],
                                                                  rhs=Mk[a][:, h, :], start=True, stop=True),
                                     r=[Mk[a], Nk[a]], w=[PB])
                            P.act(lambda e, b=b: e.activation(out=Mk[b][:, :, :].rearrange("p h t -> p (h t)"), in_=PB[:, :],
                                                              func=AF.Copy), r=[PB], w=[Mk[b]])
                        for h in range(8):
                            P.pe(lambda e, h=h, a=a, b=b: e.matmul(PC[:, h * 128:(h + 1) * 128], lhsT=Nk[b][:, h, :],
                                                                   rhs=Pk[a][:, h, :], start=True, stop=True),
                                 r=[Nk[b], Pk[a]], w=[PC])
                        P.dve(lambda e, a=a, b=b: e.tensor_tensor(out=Pk[b][:, :, :].rearrange("p h t -> p (h t)"), in0=PC[:, :],
                                                                  in1=Pk[a][:, :, :].rearrange("p h t -> p (h t)"), op=ALU.add),
                              r=[PC, Pk[a]], w=[Pk[b]])
                    XT = Pk[0]
                    for m in range(4):
                        P.dve(lambda e, m=m: e.tensor_scalar(out=WCf[:, m, :], in0=cst[:, C_ONE:C_ONE + 64],
                                                             scalar1=WC[:, m, j:j + 1], scalar2=None, op0=ALU.mult),
                              r=[cst, WC], w=[WCf])
                    P.dve(lambda e: e.tensor_tensor(out=Hd[:, :, :], in0=Hs[:, :, :], in1=WCf[:, :, :], op=ALU.mult),
                          r=[Hs, WCf], w=[Hd])
                    for h in range(8):
                        m = h // 2
                        pr = slice((h % 2) * 64, (h % 2) * 64 + 64)
                        hc = slice(h * 64, (h + 1) * 64)
                        P.pe(lambda e, m=m, h=h, hc=hc: e.matmul(PD[:, hc], lhsT=ART[:, m, 0, tb], rhs=Hbz[:, h, :],
                                                                 start=True, stop=False), r=[ART, Hbz], w=[PD])
                        P.pe(lambda e, h=h, hc=hc: e.matmul(PD[:, hc], lhsT=ATak[:, h, 0:128], rhs=Vtm[:, hc],
                                                            start=False, stop=True), r=[ATak, Vtm], w=[PD])
                    P.act(lambda e: e.activation(out=Gb[:, :], in_=PD[:, :], func=AF.Copy), r=[PD], w=[Gb])
                    for h in range(8):
                        hc = slice(h * 64, (h + 1) * 64)
                        P.pe(lambda e, h=h, hc=hc: e.matmul(PA[:, hc], lhsT=XT[:, h, :], rhs=Gb[:, hc], start=True, stop=True),
                             r=[XT, Gb], w=[PA])
                    P.act(lambda e: e.activation(out=Ub[:, :], in_=PA[:, 0:512], func=AF.Copy), r=[PA], w=[Ub])
                    for h in range(8):
                        m = h // 2
                        pr = slice((h % 2) * 64, (h % 2) * 64 + 64)
                        hc = slice(h * 64, (h + 1) * 64)
                        P.pe(lambda e, m=m, h=h, hc=hc: e.matmul(PB[:, hc], lhsT=ART[:, m, 1, tb], rhs=Hbz[:, h, :],
                                                                 start=True, stop=False), r=[ART, Hbz], w=[PB])
                        P.pe(lambda e, h=h, hc=hc: e.matmul(PB[:, hc], lhsT=ATab[:, h, 128:256], rhs=Ub[:, hc],
                                                            start=False, stop=False), r=[ATab, Ub], w=[PB])
                        P.pe(lambda e, h=h, hc=hc: e.matmul(PB[:, hc], lhsT=ATak[:, h, 128:256], rhs=Vtm[:, hc],
                                                            start=False, stop=True), r=[ATak, Vtm], w=[PB])
                    for m in range(4):
                        ms = slice(m * 128, (m + 1) * 128)
                        P.pe(lambda e, ms=ms: e.matmul(PC[:, ms], lhsT=Bhtm[:, ms], rhs=Ub[:, ms], start=True, stop=False),
                             r=[Bhtm, Ub], w=[PC])
                        P.pe(lambda e, ms=ms: e.matmul(PC[:, ms], lhsT=Khtm[:, ms], rhs=Vtm[:, ms], start=False, stop=True),
                             r=[Khtm, Vtm], w=[PC])
                    for hh in range(2):
                        pr = slice(hh * 64, hh * 64 + 64)
                        src = PC[pr, 0:512].rearrange("p (m c) -> p m c", m=4)[:, :, hh * 64:hh * 64 + 64]
                        P.dve(lambda e, pr=pr, src=src: e.tensor_tensor(out=Hs[pr, :, :], in0=Hd[pr, :, :], in1=src, op=ALU.add),
                              r=[Hd, PC], w=[Hs])
                    for hh in range(2):
                        prr = slice(hh * 64, hh * 64 + 64)
                        P.act(lambda e, hh=hh, prr=prr: e.activation(out=Hbz[prr, hh:8:2, :], in_=Hs[prr, :, :], func=AF.Copy),
                              r=[Hs], w=[Hbz])
                    P.act(lambda e: e.activation(out=Ysb[:, :], in_=PB[:, 0:512], func=AF.Copy), r=[PB], w=[Ysb])
                    P.act(lambda e: e.activation(out=Ysq[:, :], in_=PB[:, 0:512], func=AF.Square), r=[PB], w=[Ysq])
                    P.dve(lambda e: e.tensor_reduce(out=gst[:, 0:8], in_=Ysb[:, :].rearrange("p (h i) -> p h i", h=8),
                                                    axis=AX.X, op=ALU.add), r=[Ysb], w=[gst])
                    P.dve(lambda e: e.tensor_reduce(out=gst[:, 8:16], in_=Ysq[:, :].rearrange("p (h i) -> p h i", h=8),
                                                    axis=AX.X, op=ALU.add), r=[Ysq, gst], w=[gst])
                    P.dve(lambda e: e.tensor_scalar(out=gst[:, 16:24], in0=gst[:, 0:8], scalar1=1.0 / 64, scalar2=None,
                                                    op0=ALU.mult), r=[gst], w=[gst])
                    P.dve(lambda e: e.tensor_tensor(out=gst[:, 24:32], in0=gst[:, 16:24], in1=gst[:, 16:24], op=ALU.mult),
                          r=[gst], w=[gst])
                    P.dve(lambda e: e.scalar_tensor_tensor(out=gst[:, 32:40], in0=gst[:, 8:16], scalar=1.0 / 64,
                                                           in1=gst[:, 24:32], op0=ALU.mult, op1=ALU.subtract),
                          r=[gst], w=[gst])
                    P.act(lambda e: e.activation(out=gst[:, 40:48], in_=gst[:, 32:40], func=AF.Sqrt, bias=epsc[:, 1:2]),
                          r=[gst, epsc], w=[gst])
                    P.dve(lambda e: e.reciprocal(out=gst[:, 40:48], in_=gst[:, 40:48]), r=[gst], w=[gst])
                    y3 = lambda t: t[:, :].rearrange("p (h i) -> p h i", h=8)
                    P.dve(lambda e: e.tensor_tensor(out=y3(Ysb), in0=y3(Ysb),
                                                    in1=gst[:, 16:24].unsqueeze(2).to_broadcast([128, 8, 64]), op=ALU.subtract),
                          r=[Ysb, gst], w=[Ysb])
                    P.dve(lambda e: e.tensor_tensor(out=y3(Ysb), in0=y3(Ysb),
                                                    in1=gst[:, 40:48].unsqueeze(2).to_broadcast([128, 8, 64]), op=ALU.mult),
                          r=[Ysb, gst], w=[Ysb])
                    P.dve(lambda e: e.tensor_tensor(out=Ysb[:, :], in0=Ysb[:, :], in1=lnrow[:, 0, :], op=ALU.mult),
                          r=[Ysb, lnrow], w=[Ysb])
                    P.dve(lambda e: e.tensor_tensor(out=Ysb[:, :], in0=Ysb[:, :], in1=lnrow[:, 1, :], op=ALU.add),
                          r=[Ysb, lnrow], w=[Ysb])
                    for m in range(4):
                        P.pe(lambda e, m=m: e.matmul(PD[:, 2 * m:2 * m + 2], lhsT=rkT[:, m, tb], rhs=hindb[:, :],
                                                     start=True, stop=True), r=[rkT, hindb], w=[PD])
                    P.act(lambda e: e.activation(out=gst[:, 0:8], in_=PD[:, 0:8], func=AF.Copy), r=[PD, gst], w=[gst])
                    P.dve(lambda e: e.tensor_tensor(out=y3(Ysq), in0=y3(vtok),
                                                    in1=gst[:, 0:8].unsqueeze(2).to_broadcast([128, 8, 64]), op=ALU.mult),
                          r=[vtok, gst], w=[Ysq])
                    P.dve(lambda e: e.tensor_tensor(out=Ysb[:, :], in0=Ysb[:, :], in1=Ysq[:, :], op=ALU.add),
                          r=[Ysb, Ysq], w=[Ysb])
                    P.pe(lambda e: e.matmul(PA[:, 512:1024], lhsT=sgd[:, 0, tb], rhs=w_gateb[:, 0, :], start=True, stop=False),
                         r=[sgd, w_gateb], w=[PA])
                    P.pe(lambda e: e.matmul(PA[:, 512:1024], lhsT=sgd[:, 1, tb], rhs=w_gateb[:, 1, :], start=False, stop=True),
                         r=[sgd, w_gateb], w=[PA])
                    P.dve(lambda e: e.tensor_tensor(out=yrw[:, :], in0=Ysb[:, :], in1=PA[:, 512:1024], op=ALU.mult),
                          r=[Ysb, PA], w=[yrw])
                    dump("yrw", yrw[:, :], yrw, dbg_d["yrw"][ti * 128:(ti + 1) * 128, :] if dbg else None)
                    P.act(lambda e: e.activation(out=yrwb[:, :], in_=yrw[:, :], func=AF.Copy), r=[yrw], w=[yrwb])
                    for m in range(4):
                        P.pe(lambda e, m=m: e.transpose(PT[:, m * 128:(m + 1) * 128], yrwb[:, m * 128:(m + 1) * 128], identb[:, :]),
                             r=[yrwb, identb], w=[PT])
                    P.act(lambda e: e.activation(out=ycat[:, :, :].rearrange("p m t -> p (m t)"), in_=PT[:, 0:512], func=AF.Copy),
                          r=[PT], w=[ycat])
                    P.dma(yaf[:, :], yscr[ti * 128:(ti + 1) * 128, :], "yld", r=[yscr_t], w=[yaf])
                    P.act(lambda e: e.activation(out=ya[:, :, :].rearrange("p m t -> p (m t)"), in_=yaf[:, :], func=AF.Copy),
                          r=[yaf], w=[ya])
                    for c in range(2):
                        cs_ = slice(c * 512, (c + 1) * 512)
                        for k in range(8):
                            lhs = (lambda k=k: ya[:, k, :]) if k < 4 else (lambda k=k: ycat[:, k - 4, :])
                            P.pe(lambda e, k=k, cs_=cs_, lhs=lhs: e.matmul(PC[:, cs_], lhsT=lhs(), rhs=w_outb[:, k, cs_],
                                                                           start=(k == 0), stop=(k == 7)),
                                 r=[ya, ycat, w_outb], w=[PC])
                    P.act(lambda e: e.activation(out=mix[:, :], in_=PC[:, :], func=AF.Copy), r=[PC], w=[mix])
                    P.act(lambda e: e.activation(out=sqj[:, :], in_=PC[:, :], func=AF.Square, accum_out=st4[:, 6:7]),
                          r=[PC], w=[sqj, st4])
                    P.act(lambda e: e.activation(out=st4[:, 7:8], in_=st4[:, 6:7], func=AF.Sqrt, scale=1.0 / D,
                                                 bias=epsc[:, 0:1]), r=[st4, epsc], w=[st4])
                    P.dve(lambda e: e.reciprocal(out=st4[:, 7:8], in_=st4[:, 7:8]), r=[st4], w=[st4])
                    xo = x1s[0]
                    P.dve(lambda e: e.scalar_tensor_tensor(out=mix[:, :], in0=mix[:, :], scalar=st4[:, 7:8], in1=GMrow[:, :],
                                                           op0=ALU.mult, op1=ALU.mult), r=[mix, st4, GMrow], w=[mix])
                    P.dve(lambda e, xo=xo: e.tensor_tensor(out=xo[:, :], in0=mix[:, :], in1=xt[:, j, :], op=ALU.add),
                          r=[mix, xt], w=[xo])
                    P.dma(out_d[ti * 128:(ti + 1) * 128, :], xo[:, :], "x1st", r=[xo], w=[x1t], q="sp")
                    dump("x1", xo[:, :], xo, dbg_d["x1"][ti * 128:(ti + 1) * 128, :] if dbg else None)

        print("ops after A2", P.nops)
        P.fence()
        if upto < 3:
            P.enabled = False
        outt = Tl(P, None, "out_hbm")
        with ExitStack() as es3:
            P.es_cur = es3
            w_fgb = P.tile([128, 8, DFF], BF16, "w_fgb")
            w_fub = P.tile([128, 8, DFF], BF16, "w_fub")
            w_fdb = P.tile([128, 22, D], BF16, "w_fdb")
            wst = [P.tile([128, 1024], F32, "wst3%d" % i) for i in range(2)]
            i = 0
            jobs = []
            for (wd_, wb) in ((wfg_d, w_fgb), (wfu_d, w_fub)):
                for k in range(8):
                    for (c0, c1) in ((0, 1024), (1024, 2048), (2048, DFF)):
                        jobs.append((wd_, wb, k, c0, c1))
            for k in range(22):
                jobs.append((wfd_d, w_fdb, k, 0, D))
            for (wd_, wb, k, c0, c1) in jobs:
                ws = wst[i % 2]
                P.dma(ws[:, 0:c1 - c0], wd_[k * 128:(k + 1) * 128, c0:c1], "wst3%d" % (i % 2), w=[ws])
                if i % 2 == 0:
                    P.act(lambda e, ws=ws, wb=wb, k=k, c0=c0, c1=c1: e.activation(out=wb[:, k, c0:c1], in_=ws[:, 0:c1 - c0],
                                                                                 func=AF.Copy), r=[ws], w=[wb])
                else:
                    P.dve(lambda e, ws=ws, wb=wb, k=k, c0=c0, c1=c1: e.tensor_copy(out=wb[:, k, c0:c1], in_=ws[:, 0:c1 - c0]),
                          r=[ws], w=[wb])
                i += 1
            xt = P.tile([128, 2, D], F32, "xt3")
            sqj = P.tile([128, D], BF16, "sqj3")
            st4 = P.tile([128, 8], F32, "st43")
            dgt = P.tile([128, 2, 128], F32, "dgt3")
            hfT = P.tile([128, 8, 256], BF16, "hfT")
            sg = [P.tile([128, 256], F32, "sg%d" % i) for i in range(2)]
            aT = P.tile([128, 22, 256], BF16, "aT")
            fo = P.tile([128, D], F32, "fo")
            ost = [P.tile([128, D], F32, "ost%d" % i) for i in range(1)]
            for s in range(NS):
                for j in range(2):
                    r0 = s * 256 + j * 128
                    P.dma(xt[:, j, :], out_d[r0:r0 + 128, :], "xin3", r=[x1t], w=[xt])
                for j in range(2):
                    P.act(lambda e, j=j: e.activation(out=sqj[:, :], in_=xt[:, j, :], func=AF.Square,
                                                      accum_out=st4[:, j:j + 1]), r=[xt], w=[sqj, st4])
                P.act(lambda e: e.activation(out=st4[:, 2:4], in_=st4[:, 0:2], func=AF.Sqrt, scale=1.0 / D,
                                             bias=epsc[:, 0:1]), r=[st4, epsc], w=[st4])
                P.dve(lambda e: e.reciprocal(out=st4[:, 4:6], in_=st4[:, 2:4]), r=[st4], w=[st4])
                for j in range(2):
                    P.dve(lambda e, j=j: e.tensor_scalar(out=dgt[:, j, :], in0=cst[:, C_ID:C_ID + 128],
                                                         scalar1=st4[:, 4 + j:5 + j], scalar2=None, op0=ALU.mult),
                          r=[cst, st4], w=[dgt])
                for kk in range(2):
                    for k4 in range(4):
                        k = kk * 4 + k4
                        for j in range(2):
                            P.pe(lambda e, k=k, k4=k4, j=j: e.matmul(
                                PA[:, k4 * 256 + j * 128:k4 * 256 + (j + 1) * 128],
                                lhsT=xt[:, j, k * 128:(k + 1) * 128], rhs=dgt[:, j, :], start=True, stop=True),
                                r=[xt, dgt], w=[PA])
                    for k4 in range(4):
                        k = kk * 4 + k4
                        P.dve(lambda e, k=k, k4=k4: e.tensor_scalar(
                            out=hfT[:, k, :], in0=PA[:, k4 * 256:(k4 + 1) * 256], scalar1=gsh[:, 8 + k:9 + k],
                            scalar2=modc[:, 24 + k:25 + k], op0=ALU.mult, op1=ALU.add), r=[PA, gsh, modc], w=[hfT])
                for n in range(22):
                    ns = slice(n * 128, (n + 1) * 128)
                    pg = PB if n % 2 == 0 else PC
                    for k in range(8):
                        P.pe(lambda e, k=k, ns=ns, pg=pg: e.matmul(pg[:, 0:256], lhsT=w_fgb[:, k, ns], rhs=hfT[:, k, :],
                                                                   start=(k == 0), stop=(k == 7)), r=[w_fgb, hfT], w=[pg])
                    for k in range(8):
                        P.pe(lambda e, k=k, ns=ns, pg=pg: e.matmul(pg[:, 256:512], lhsT=w_fub[:, k, ns], rhs=hfT[:, k, :],
                                                                   start=(k == 0), stop=(k == 7)), r=[w_fub, hfT], w=[pg])
                    sgt = sg[n % 2]
                    P.act(lambda e, pg=pg, sgt=sgt: e.activation(out=sgt[:, :], in_=pg[:, 0:256], func=AF.Silu), r=[pg], w=[sgt])
                    P.dve(lambda e, pg=pg, sgt=sgt, n=n: e.tensor_tensor(out=aT[:, n, :], in0=sgt[:, :], in1=pg[:, 256:512],
                                                                         op=ALU.mult), r=[sgt, pg], w=[aT])
                for j in range(2):
                    ti = s * 2 + j
                    for c in range(2):
                        cs_ = slice(c * 512, (c + 1) * 512)
                        for n in range(22):
                            P.pe(lambda e, n=n, cs_=cs_, j=j: e.matmul(PA[:, cs_], lhsT=aT[:, n, j * 128:(j + 1) * 128],
                                                                       rhs=w_fdb[:, n, cs_], start=(n == 0), stop=(n == 21)),
                                 r=[aT, w_fdb], w=[PA])
                    P.act(lambda e: e.activation(out=fo[:, :], in_=PA[:, :], func=AF.Copy), r=[PA], w=[fo])
                    P.act(lambda e: e.activation(out=sqj[:, :], in_=PA[:, :], func=AF.Square, accum_out=st4[:, 6:7]),
                          r=[PA], w=[sqj, st4])
                    P.act(lambda e: e.activation(out=st4[:, 7:8], in_=st4[:, 6:7], func=AF.Sqrt, scale=1.0 / D,
                                                 bias=epsc[:, 0:1]), r=[st4, epsc], w=[st4])
                    P.dve(lambda e: e.reciprocal(out=st4[:, 7:8], in_=st4[:, 7:8]), r=[st4], w=[st4])
                    oo = ost[0]
                    P.dve(lambda e: e.scalar_tensor_tensor(out=fo[:, :], in0=fo[:, :], scalar=st4[:, 7:8], in1=GFrow[:, :],
                                                           op0=ALU.mult, op1=ALU.mult), r=[fo, st4, GFrow], w=[fo])
                    P.dve(lambda e, oo=oo, j=j: e.tensor_tensor(out=oo[:, :], in0=fo[:, :], in1=xt[:, j, :], op=ALU.add),
                          r=[fo, xt], w=[oo])
                    P.dma(out_d[ti * 128:(ti + 1) * 128, :], oo[:, :], "ost", r=[oo], w=[outt], q="sp")
            P.enabled = True
            P.wait_all("sp", ["ost", "x1st", "dbg"] + [k for k in P.streams if k not in ("ost", "x1st", "dbg")])
            P.wait_all("pool", ["ost"])

            with nc.Block() as block:
                @block.sync
                def _(e):
                    P.replay("sp", e)

                @block.tensor
                def _(e):
                    P.replay("pe", e)

                @block.scalar
                def _(e):
                    P.replay("act", e)

                @block.vector
                def _(e):
                    P.replay("dve", e)

                @block.gpsimd
                def _(e):
                    P.replay("pool", e)
    return nc, list(dbg_d.keys())


def t5_bucket_np(rel):
    rel = np.asarray(rel)
    max_exact = 16
    nf = np.maximum(rel, 1).astype(np.float32)
    large = max_exact + (np.log(nf / np.float32(max_exact)) / np.float32(math.log(128 / max_exact))
                         * np.float32(32 - max_exact)).astype(np.int32)
    large = np.minimum(large, 31)
    return np.where(rel < max_exact, rel, large)


def make_consts():
    c = np.zeros((128, C_END), np.float32)
    p = np.arange(128)[:, None]
    f = np.arange(128)[None, :]
    c[:, C_ID:C_ID + 128] = (p == f)
    c[:, C_ONE:C_ONE + 128] = 1.0
    c[:, C_BLK:C_BLK + 128] = ((p // 64) == (f // 64))
    c[:, C_MSU:C_MSU + 128] = (f > p)
    c[:, C_MUI:C_MUI + 128] = (f >= p)
    c[:, C_MSL:C_MSL + 128] = (f < p)
    c[:, C_NEG:C_NEG + 128] = np.where(f > p, -1e30, 0.0)
    c[:, C_J:C_J + 128] = (p + f == 127)
    c[31, C_SEL:C_SEL + 128] = 1.0
    bk = t5_bucket_np(np.arange(256))
    c[0:32, C_OH:C_OH + 256] = (np.arange(32)[:, None] == bk[None, :])
    sm = np.ones((128, 256), np.float32)
    sm[:, 0] = 0.0
    sm[:, 128] = 0.0
    c[:, C_SCAN:C_SCAN + 256] = sm
    c[:, C_HIND] = (np.arange(128) < 64)
    c[:, C_HIND + 1] = (np.arange(128) >= 64)
    return c


def col8(v):
    return np.ascontiguousarray(v.reshape(-1, 128).T)


def prep_shared(inp):
    f32 = np.float32
    g = lambda k: np.asarray(inp[k], f32)
    w_in = g("w_in")[0]
    sh = {}
    sh["ada_w"] = np.ascontiguousarray(g("ada_w")[0])
    sh["cst"] = make_consts()
    sh["w_att"] = np.ascontiguousarray(np.concatenate([w_in[:, 0:384], w_in[:, 384:448], w_in[:, 384:448]], axis=1))
    sh["w_iw"] = np.ascontiguousarray(w_in[:, 448:456])
    sh["w_rw"] = np.ascontiguousarray(w_in[:, 456:])
    wiq = g("w_idx_q")[0]
    sh["w_iq"] = np.ascontiguousarray(np.concatenate([wiq, wiq], axis=2).reshape(256, 1024))
    sh["w_uq"] = np.ascontiguousarray(g("w_uq")[0].reshape(256, 512))
    wuk = g("w_uk")[0]
    t = np.zeros((128, 8, 128), f32)
    for h in range(8):
        t[(h % 2) * 64:(h % 2) * 64 + 64, h, :] = wuk[h].T
    sh["w_ukT"] = t.reshape(128, 1024)
    wuv = g("w_uv")[0]
    t = np.zeros((128, 8, 128), f32)
    for h in range(8):
        t[:, h, (h % 2) * 64:(h % 2) * 64 + 64] = wuv[h]
    sh["w_uv"] = t.reshape(128, 1024)
    sh["rel_bias"] = np.ascontiguousarray(g("rel_bias"))
    t = np.zeros((128, 1024), f32)
    t[0:64, 0:512] = g("w_decay_up")[0]
    t[64:128, 512:1024] = g("w_aaa_up")[0]
    sh["w_lora"] = t
    sh["w_gate"] = np.ascontiguousarray(g("w_gate_up")[0])
    sh["lnrow"] = np.ascontiguousarray(np.stack([g("ln_x_gain")[0], g("ln_x_bias")[0]], axis=0))
    sh["w_out"] = np.ascontiguousarray(g("w_out")[0])
    sh["w_fg"] = np.ascontiguousarray(g("w_ffn_gate")[0])
    sh["w_fu"] = np.ascontiguousarray(g("w_ffn_up")[0])
    sh["w_fd"] = np.ascontiguousarray(g("w_ffn_down")[0])
    vec = np.zeros((128, 128), f32)
    vec[:, 0:8] = col8(g("mix_pre_norm")[0])
    vec[:, 8:16] = col8(g("mix_post_norm")[0])
    vec[:, 16:24] = col8(g("ffn_pre_norm")[0])
    vec[:, 24:32] = col8(g("ffn_post_norm")[0])
    vec[:, 32:80] = g("ada_b")[0].reshape(48, 128).T
    vec[:, 80:82] = g("q_norm")[0].reshape(2, 128).T
    vec[:, 82] = g("kv_norm")[0]
    vec[:, 83] = np.concatenate([g("idx_k_norm")[0], g("idx_k_norm")[0]])
    vec[:, 84:88] = g("w0")[0].reshape(4, 128).T
    vec[:, 88:92] = g("a0")[0].reshape(4, 128).T
    vec[:, 92:96] = g("k_k")[0].reshape(4, 128).T
    vec[:, 96:100] = g("k_a")[0].reshape(4, 128).T
    vec[:, 100:104] = g("r_k")[0].reshape(4, 128).T
    mus = g("mu_shift")[0]
    vec[:, 108:122] = mus[:14 * 128].reshape(14, 128).T
    vec[:, 122] = mus[1824 - 128:1824]
    sh["vecs"] = vec
    return sh


def kernel(**inputs):
    x = np.asarray(inputs["x"], np.float32)
    c = np.asarray(inputs["c"], np.float32)
    B, T, _ = x.shape
    sh = prep_shared(inputs)
    nc, _ = build(T)
    in_maps = []
    for b in range(B):
        m = dict(sh)
        m["x"] = np.ascontiguousarray(x[b])
        m["ccol"] = col8(c[b])
        in_maps.append(m)
    res = run_bass_kernel_spmd(nc, in_maps, core_ids=list(range(B)))
    return np.stack([np.asarray(r["out"], np.float32) for r in res.results], axis=0)
```

```python
import math
from contextlib import ExitStack
import numpy as np
import concourse.bass as bass
import concourse.mybir as mybir
from concourse.bass_utils import run_bass_kernel_spmd

F32 = mybir.dt.float32
BF16 = mybir.dt.bfloat16
AF = mybir.ActivationFunctionType
ALU = mybir.AluOpType
AX = mybir.AxisListType

D = 1024
DFF = 2816
NB_IT = 20
BIS_R = 16.0
LWC = math.exp(-0.5)

C_ID, C_ONE, C_BLK, C_MSU, C_MUI, C_MSL, C_NEG, C_J, C_SEL, C_OH, C_SCAN, C_HIND, C_END = (
    0, 128, 256, 384, 512, 640, 768, 896, 1024, 1152, 1408, 1664, 1668)


class Tl:
    def __init__(self, P, h, name):
        self.h = h
        self.name = name
        self.lw = None
        self.rd = dict(P.fence_tokens)

    def __getitem__(self, i):
        return self.h[i]


class TlG:
    def __init__(self, P, h, name, n):
        self.h = h
        self.name = name
        self.p = [Tl(P, h, "%s_%d" % (name, i)) for i in range(n)]

    def __getitem__(self, i):
        return self.h[i]


class Bank:
    def __init__(self, tl, h, lo):
        self.tl = tl
        self.h = h
        self.lo = lo

    def ap(self, a, b):
        return self.h[:, self.lo + a:self.lo + b]


def _flat(ts):
    out = []
    for t in ts:
        if isinstance(t, TlG):
            out.extend(t.p)
        else:
            out.append(t)
    return out


class Rec:
    def __init__(self):
        self.call = None

    def __getattr__(self, name):
        def f(*a, **k):
            self.call = (name, a, k)
            return self
        return f


class Stream:
    def __init__(self, key):
        self.key = key
        self.count = 0
        self.mark = -1


class Prog:
    ENGS = ("pe", "act", "dve", "pool", "sp")

    def __init__(self, nc, es):
        self.nc = nc
        self.es = es
        self.q = {e: [] for e in self.ENGS}
        self.cnt = {e: 0 for e in self.ENGS}
        self.waited = {e: {} for e in self.ENGS}
        self.semh = {}
        self.streams = {}
        self.fence_tokens = {}
        self.enabled = True
        self.nops = 0
        import os as _os
        self.printops = bool(_os.environ.get("PRINTOPS"))
        self.maxops = int(_os.environ.get("MAXOPS", "100000000"))
        for e in self.ENGS:
            self.semh[e] = es.enter_context(nc.semaphore("s_" + e))

    def stream(self, key):
        if key not in self.streams:
            self.streams[key] = Stream(key)
            self.semh[key] = self.es.enter_context(self.nc.semaphore("d_" + key))
        return self.streams[key]

    def tile(self, shape, dt, name):
        h = self.es_cur.enter_context(self.nc.sbuf_tensor("sb_" + name, list(shape), dt))
        return Tl(self, h, name)

    def fence(self):
        ft = {}
        for e in self.ENGS:
            if self.cnt[e] > 0:
                ft[e] = (e, self.cnt[e], e, False)
        for k, st in self.streams.items():
            if st.count > 0:
                ft[k] = (k, st.count, None, True)
        self.fence_tokens = ft

    def emit(self, eng, fn, r=(), w=(), stream=None):
        if not self.enabled:
            return None
        self.nops += 1
        if self.nops > self.maxops:
            return None
        r = _flat(r)
        w = _flat(w)
        rec = Rec()
        fn(rec)
        fn = rec.call
        assert fn is not None
        if self.printops:
            print("OP", self.nops, eng, fn[0], [t.name for t in r], "->", [t.name for t in w])
        need = {}

        def add(tok, kind):
            key, val, teng, isdma = tok
            if isdma:
                val = self.streams[key].count
                self.streams[key].mark = val
            elif stream is None and teng == eng:
                if eng == "pe":
                    return
            if need.get(key, 0) < val:
                need[key] = val

        for t in r:
            if t.lw is not None:
                add(t.lw, "raw")
        for t in w:
            if t.lw is not None:
                add(t.lw, "waw")
            for tok in t.rd.values():
                add(tok, "war")
        if stream is not None:
            st0 = self.stream(stream)
            if st0.mark == st0.count and st0.count > 0:
                need[st0.key] = st0.count
        for key, val in need.items():
            if self.waited[eng].get(key, 0) < val:
                self.waited[eng][key] = val
                self.q[eng].append(("w", key, val))
        if stream is None:
            self.cnt[eng] += 1
            tok = (eng, self.cnt[eng], eng, False)
            self.q[eng].append(("op", fn, eng, 1))
        else:
            st = self.stream(stream)
            st.count += 16
            tok = (st.key, st.count, None, True)
            self.q[eng].append(("op", fn, st.key, 16))
        for t in w:
            t.lw = tok
            t.rd = {}
        for t in r:
            if t not in w:
                t.rd[tok[0]] = tok
        return tok

    def wait_all(self, eng, keys):
        for key in keys:
            if key in self.streams:
                val = self.streams[key].count
            elif key in self.cnt:
                val = self.cnt[key]
            else:
                continue
            if val > 0 and self.waited[eng].get(key, 0) < val:
                self.waited[eng][key] = val
                self.q[eng].append(("w", key, val))

    def replay(self, eng, e):
        for ent in self.q[eng]:
            if ent[0] == "w":
                e.wait_ge(self.semh[ent[1]], ent[2])
            else:
                name, a, k = ent[1]
                ins = getattr(e, name)(*a, **k)
                ins.then_inc(self.semh[ent[2]], ent[3])

    def pe(self, fn, r=(), w=()):
        return self.emit("pe", fn, r, w)

    def act(self, fn, r=(), w=()):
        return self.emit("act", fn, r, w)

    def dve(self, fn, r=(), w=()):
        return self.emit("dve", fn, r, w)

    def pool(self, fn, r=(), w=()):
        return self.emit("pool", fn, r, w)

    def dma(self, out, in_, stream, r=(), w=(), q="sp"):
        return self.emit(q, lambda e: e.dma_start(out=out, in_=in_), r, w, stream=stream)


def build(T, dbg=False, upto=3):
    NT = T // 128
    NS = T // 256
    KTOP = min(256, T // 4)
    nc = bass.Bass("TRN2", target_bir_lowering=False)

    def din(name, shape):
        return nc.dram_tensor(name, list(shape), F32, kind="ExternalInput").ap()

    x_d = din("x", [T, D])
    ccol_d = din("ccol", [128, 8])
    adaw_d = din("ada_w", [D, 6 * D])
    vec_d = din("vecs", [128, 128])
    cst_d = din("cst", [128, C_END])
    watt_d = din("w_att", [D, 512])
    wiw_d = din("w_iw", [D, 8])
    wrw_d = din("w_rw", [D, 1824])
    wiq_d = din("w_iq", [256, 1024])
    wuq_d = din("w_uq", [256, 512])
    wuk_d = din("w_ukT", [128, 1024])
    wuv_d = din("w_uv", [128, 1024])
    relb_d = din("rel_bias", [32, 8])
    wlora_d = din("w_lora", [128, 1024])
    wgate_d = din("w_gate", [160, 512])
    lnrow_d = din("lnrow", [2, 512])
    wout_d = din("w_out", [D, D])
    wfg_d = din("w_fg", [D, DFF])
    wfu_d = din("w_fu", [D, DFF])
    wfd_d = din("w_fd", [DFF, D])
    out_d = nc.dram_tensor("out", [T, D], F32, kind="ExternalOutput").ap()
    dscr = nc.dram_tensor("dscr", [8, 384], F32, kind="Internal").ap()
    dbg_d = {}

    def ddbg(name, shape):
        if dbg:
            dbg_d[name] = nc.dram_tensor("dbg_" + name, list(shape), F32, kind="ExternalOutput").ap()

    ddbg("modc", [128, 48])
    ddbg("bias", [128, 3 * 1024])
    ddbg("yatt", [128, 4 * T])
    ddbg("thr", [128, NT])
    ddbg("yrw", [T, 512])
    ddbg("x1", [T, D])

    with ExitStack() as es:
        P = Prog(nc, es)
        P.es_cur = es
        def pst(name, shape, dt):
            return Tl(P, es.enter_context(nc.psum_tensor("ps_" + name, list(shape), dt)), name)
        def pstg(name, shape, dt, n):
            return TlG(P, es.enter_context(nc.psum_tensor("ps_" + name, list(shape), dt)), name, n)
        PA = pstg("PA", [128, 1024], F32, 2)
        PB = pstg("PB", [128, 1024], F32, 2)
        PC = pstg("PC", [128, 1024], F32, 2)
        PD = pst("PD", [128, 512], F32)
        PT = pstg("PT", [128, 1024], BF16, 8)

        cst = P.tile([128, C_END], F32, "cst")
        vec = P.tile([128, 128], F32, "vec")
        modc = P.tile([128, 48], F32, "modc")
        gsh = P.tile([128, 48], F32, "gsh")
        GMrow = P.tile([128, D], F32, "GMrow")
        GFrow = P.tile([128, D], F32, "GFrow")
        identb = P.tile([128, 128], BF16, "identb")
        onesb = P.tile([128, 128], BF16, "onesb")
        epsc = P.tile([128, 4], F32, "epsc")
        yscr = nc.dram_tensor("yscr", [NT * 128, 512], F32, kind="ExternalOutput").ap()
        yscr_t = Tl(P, None, "yscr")

        ident = lambda: cst[:, C_ID:C_ID + 128]
        ones32 = lambda: cst[:, C_ONE:C_ONE + 128]

        P.dma(cst[:, :], cst_d[:, :], "cw", w=[cst])
        P.dma(vec[:, :], vec_d[:, :], "cw", w=[vec])
        P.dve(lambda e: e.tensor_copy(out=identb[:, :], in_=cst[:, C_ID:C_ID + 128]), r=[cst], w=[identb])
        P.dve(lambda e: e.tensor_copy(out=onesb[:, :], in_=cst[:, C_ONE:C_ONE + 128]), r=[cst], w=[onesb])
        P.dve(lambda e: e.memset(epsc[:, 0:1], 1e-6), w=[epsc])
        P.dve(lambda e: e.memset(epsc[:, 1:2], 64e-5), w=[epsc])
        P.dve(lambda e: e.memset(epsc[:, 2:3], 0.0), w=[epsc])
        V_MPRE, V_MPOST, V_FPRE, V_FPOST, V_ADAB, V_QN, V_KVN, V_IKN, V_W0, V_A0, V_KK, V_KA, V_RK = (
            0, 8, 16, 24, 32, 80, 82, 83, 84, 88, 92, 96, 100)

        def dump(name, src_ap, tl, dst_ap=None):
            if dbg:
                P.dma(dbg_d[name][:, :] if dst_ap is None else dst_ap, src_ap, "dbg", r=[tl])

        with ExitStack() as es0:
            P.es_cur = es0
            ccol = P.tile([128, 8], F32, "ccol")
            scol = P.tile([128, 8], F32, "scol")
            stg = [P.tile([128, 8, 512], F32, "adastg%d" % i) for i in range(2)]
            dg = P.tile([128, 8, 128], F32, "dg")
            P.dma(ccol[:, :], ccol_d[:, :], "cw", w=[ccol])
            P.act(lambda e: e.activation(out=scol[:, :], in_=ccol[:, :], func=AF.Silu), r=[ccol], w=[scol])
            for s in range(12):
                st = stg[s % 2]
                P.dma(st[:, :, :], adaw_d[:, s * 512:(s + 1) * 512].rearrange("(k p) n -> p k n", p=128),
                      "ada%d" % (s % 2), w=[st])
                for mm in range(4):
                    m = s * 4 + mm
                    for k in range(8):
                        P.pe(lambda e, st=st, mm=mm, m=m, k=k: e.matmul(
                            PD[:, m:m + 1], lhsT=st[:, k, mm * 128:(mm + 1) * 128], rhs=scol[:, k:k + 1],
                            start=(k == 0), stop=(k == 7)), r=[st, scol], w=[PD])
            P.dve(lambda e: e.tensor_tensor(out=modc[:, :], in0=PD[:, 0:48], in1=vec[:, V_ADAB:V_ADAB + 48],
                                            op=ALU.add), r=[PD, vec], w=[modc])
            dump("modc", modc[:, :], modc)
            P.dve(lambda e: e.tensor_scalar(out=gsh[:, 32:40], in0=modc[:, 8:16], scalar1=1.0, scalar2=None,
                                            op0=ALU.add), r=[modc], w=[gsh])
            P.dve(lambda e: e.tensor_scalar(out=gsh[:, 40:48], in0=modc[:, 32:40], scalar1=1.0, scalar2=None,
                                            op0=ALU.add), r=[modc, gsh], w=[gsh])
            P.dve(lambda e: e.tensor_tensor(out=gsh[:, 0:8], in0=gsh[:, 32:40], in1=vec[:, V_MPRE:V_MPRE + 8],
                                            op=ALU.mult), r=[gsh, vec], w=[gsh])
            P.dve(lambda e: e.tensor_tensor(out=gsh[:, 8:16], in0=gsh[:, 40:48], in1=vec[:, V_FPRE:V_FPRE + 8],
                                            op=ALU.mult), r=[gsh, vec], w=[gsh])
            P.dve(lambda e: e.tensor_tensor(out=gsh[:, 16:24], in0=modc[:, 16:24], in1=vec[:, V_MPOST:V_MPOST + 8],
                                            op=ALU.mult), r=[gsh, modc, vec], w=[gsh])
            P.dve(lambda e: e.tensor_tensor(out=gsh[:, 24:32], in0=modc[:, 40:48], in1=vec[:, V_FPOST:V_FPOST + 8],
                                            op=ALU.mult), r=[gsh, modc, vec], w=[gsh])
            for (c0, row) in ((16, GMrow), (24, GFrow)):
                for k in range(8):
                    P.dve(lambda e, c0=c0, k=k: e.tensor_scalar(
                        out=dg[:, k, :], in0=cst[:, C_ID:C_ID + 128], scalar1=gsh[:, c0 + k:c0 + k + 1],
                        scalar2=None, op0=ALU.mult), r=[cst, gsh], w=[dg])
                for k in range(8):
                    P.pe(lambda e, k=k: e.matmul(PA[:, k * 128:(k + 1) * 128], lhsT=cst[:, C_ONE:C_ONE + 128],
                                                 rhs=dg[:, k, :], start=True, stop=True), r=[cst, dg], w=[PA])
                P.act(lambda e, row=row: e.activation(out=row[:, :], in_=PA[:, :], func=AF.Copy), r=[PA], w=[row])

        print("ops after P0", P.nops)
        P.fence()
        if upto < 1:
            P.enabled = False
        with ExitStack() as es1:
            P.es_cur = es1
            w_att = P.tile([128, 8, 512], F32, "w_att")
            w_iw = P.tile([128, 8, 8], F32, "w_iw")
            w_iq = P.tile([128, 2, 1024], F32, "w_iq")
            w_uqb = P.tile([128, 2, 512], BF16, "w_uqb")
            w_ukb = P.tile([128, 8, 128], BF16, "w_ukb")
            w_uvb = P.tile([128, 8, 128], BF16, "w_uvb")
            relb = P.tile([32, 8], F32, "relb")
            biasT = P.tile([128, 3, 1024], F32, "biasT")
            ckvT = P.tile([128, T], BF16, "ckvT")
            ckvtm = P.tile([128, NT, 128], BF16, "ckvtm")
            ikA = P.tile([128, T], BF16, "ikA")
            ikB = P.tile([128, T], BF16, "ikB")
            P.dve(lambda e: e.memset(ikB[:, :], 0.0), w=[ikB])
            es1b = ExitStack()
            P.es_cur = es1b
            wst = P.tile([128, 1024], F32, "wst1")
            P.dma(w_att[:, :, :], watt_d.rearrange("(k p) n -> p k n", p=128), "cw", w=[w_att])
            P.dma(w_iw[:, :, :], wiw_d.rearrange("(k p) n -> p k n", p=128), "cw", w=[w_iw])
            P.dma(w_iq[:, :, :], wiq_d.rearrange("(k p) n -> p k n", p=128), "cw", w=[w_iq])
            P.dma(relb[:, :], relb_d[:, :], "cw", w=[relb])
            P.dma(wst[:, :].rearrange("p (k n) -> p k n", k=2), wuq_d.rearrange("(k p) n -> p k n", p=128), "wst", w=[wst])
            P.dve(lambda e: e.tensor_copy(out=w_uqb[:, :, :], in_=wst[:, :].rearrange("p (k n) -> p k n", k=2)),
                  r=[wst], w=[w_uqb])
            P.dma(wst[:, :], wuk_d[:, :], "wst", w=[wst])
            P.dve(lambda e: e.tensor_copy(out=w_ukb[:, :, :], in_=wst[:, :].rearrange("p (k n) -> p k n", k=8)),
                  r=[wst], w=[w_ukb])
            P.dma(wst[:, :], wuv_d[:, :], "wst", w=[wst])
            P.dve(lambda e: e.tensor_copy(out=w_uvb[:, :, :], in_=wst[:, :].rearrange("p (k n) -> p k n", k=8)),
                  r=[wst], w=[w_uvb])
            brel = P.tile([8, 384], F32, "brel")
            relx = P.tile([32, 8, 128], F32, "relx")
            qtl = P.tile([128, 2, 1024], F32, "qtl")
            P.pe(lambda e: e.matmul(PD[0:8, 0:256], lhsT=relb[:, :], rhs=cst[0:32, C_OH:C_OH + 256],
                                    start=True, stop=True), r=[relb, cst], w=[PD])
            P.dve(lambda e: e.memset(brel[:, :], 0.0), w=[brel])
            P.dve(lambda e: e.tensor_copy(out=brel[:, 127:383], in_=PD[0:8, 0:256]), r=[PD, brel], w=[brel])
            dsc = Tl(P, None, "dscr")
            P.dma(dscr[:, :], brel[:, :], "cw", r=[brel], w=[dsc])
            for dl in range(2):
                src = bass.AP(tensor=dscr.tensor, offset=128 * dl, ap=[[1, 128], [384, 8], [1, 128]])
                P.dma(qtl[:, dl, :].rearrange("p (h t) -> p h t", h=8), src, "cw", r=[dsc], w=[qtl])
            for dl in range(2):
                for c in range(2):
                    P.pe(lambda e, dl=dl, c=c: e.matmul(PA[:, c * 512:(c + 1) * 512], lhsT=cst[:, C_J:C_J + 128],
                                                        rhs=qtl[:, dl, c * 512:(c + 1) * 512], start=True, stop=True),
                         r=[cst, qtl], w=[PA])
                P.act(lambda e, dl=dl: e.activation(out=biasT[:, dl, :], in_=PA[:, :], func=AF.Copy), r=[PA], w=[biasT])
            for h in range(8):
                P.dve(lambda e, h=h: e.tensor_scalar(out=relx[:, h, :], in0=cst[0:32, C_ONE:C_ONE + 128],
                                                     scalar1=relb[:, h:h + 1], scalar2=None, op0=ALU.mult),
                      r=[cst, relb], w=[relx])
            for c in range(2):
                P.pe(lambda e, c=c: e.matmul(PA[:, c * 512:(c + 1) * 512], lhsT=cst[0:32, C_SEL:C_SEL + 128],
                                             rhs=relx[:, c * 4:(c + 1) * 4, :], start=True, stop=True),
                     r=[cst, relx], w=[PA])
            P.act(lambda e: e.activation(out=biasT[:, 2, :], in_=PA[:, :], func=AF.Copy), r=[PA], w=[biasT])
            dump("bias", biasT[:, :, :].rearrange("p a b -> p (a b)"), biasT)
            es1b.close()
            P.es_cur = es1
            P.fence()

            xt = P.tile([128, 2, D], F32, "xt")
            sqj = P.tile([128, D], BF16, "sqj")
            st4 = P.tile([128, 8], F32, "st4")
            dgt = P.tile([128, 2, 128], F32, "dgt")
            hT = P.tile([128, 8, 256], F32, "hT")
            cqraw = P.tile([128, 4, 256], F32, "cqraw")
            sq = P.tile([128, 4, 256], F32, "sq")
            rq = P.tile([128, 3, 256], F32, "rq")
            cqT = P.tile([128, 2, 256], F32, "cqT")
            cqTb = P.tile([128, 2, 256], BF16, "cqTb")
            iqT = P.tile([128, 8, 256], BF16, "iqP")
            iqtmp = P.tile([128, 4, 256], BF16, "iqtmp")
            ikf = P.tile([128, 256], F32, "ikf")
            iw = P.tile([128, 2, 8], F32, "iw")
            qTb = P.tile([128, 4, 256], BF16, "qTb")
            qaT2 = [P.tile([128, 8, 256], BF16, "qaT%d" % i) for i in range(2)]
            sc = TlG(P, es1.enter_context(nc.sbuf_tensor("sb_sc", [128, T], F32)), "sc", max(T // 512, 1))
            mk = [P.tile([128, T], BF16, "mk%d" % i) for i in range(2)]
            rl = [P.tile([128, 512], F32, "rl%d" % i) for i in range(3)]
            bsn = P.tile([128, 1], F32, "bsn")
            bss = P.tile([128, 1], F32, "bss")
            bsu = P.tile([128, 1], F32, "bsu")
            lg = [P.tile([128, 512], F32, "lg%d" % i) for i in range(3)]
            Ee = [P.tile([128, 512], BF16, "Ee%d" % i) for i in range(3)]
            Em = [P.tile([128, 512], BF16, "Em%d" % i) for i in range(3)]
            rD = P.tile([128, 1024], F32, "rD")
            oTb = P.tile([128, 8, 128], BF16, "oTb")
            thrs = P.tile([128, NT], F32, "thrs")
            ytl = [P.tile([128, 4, 128], F32, "ytl%d" % i) for i in range(2)]
            ydb = P.tile([128, 4, 128], F32, "ydb") if dbg else None
            print("SBUFREM A1", nc.sbuf_bytes_remaining)
            rot = [Bank(PA.p[0], PA.h, 0), Bank(PA.p[1], PA.h, 512), Bank(PD, PD.h, 0)]
            pending = [None, 0]

            def emit_S(qi, j):
                S = (qi + 1) * 128
                ncc = (S + 511) // 512
                idx = 0
                for h in range(8):
                    for cc in range(ncc):
                        wd = min(512, S - cc * 512)
                        bk = rot[idx % 3]
                        rb = rl[idx % 3]
                        idx += 1
                        scp = sc.p[cc]
                        P.pe(lambda e: e.matmul(bk.ap(0, wd), lhsT=iqT[:, h, j * 128:(j + 1) * 128],
                                                rhs=ikA[:, cc * 512:cc * 512 + wd], start=True, stop=False),
                             r=[iqT, ikA], w=[bk.tl])
                        P.pe(lambda e: e.matmul(bk.ap(0, wd), lhsT=iqT[:, h, j * 128:(j + 1) * 128],
                                                rhs=ikB[:, cc * 512:cc * 512 + wd], start=False, stop=True),
                             r=[iqT, ikB], w=[bk.tl])
                        P.act(lambda e: e.activation(out=rb[:, 0:wd], in_=bk.ap(0, wd), func=AF.Relu), r=[bk.tl], w=[rb])
                        if h == 0:
                            P.dve(lambda e: e.tensor_scalar(out=sc[:, cc * 512:cc * 512 + wd], in0=rb[:, 0:wd],
                                                            scalar1=iw[:, j, 0:1], scalar2=None, op0=ALU.mult),
                                  r=[rb, iw], w=[scp])
                        else:
                            P.dve(lambda e: e.scalar_tensor_tensor(out=sc[:, cc * 512:cc * 512 + wd], in0=rb[:, 0:wd],
                                                                   scalar=iw[:, j, h:h + 1],
                                                                   in1=sc[:, cc * 512:cc * 512 + wd], op0=ALU.mult, op1=ALU.add),
                                  r=[rb, iw, scp], w=[scp])
                scd = sc.p[(qi * 128) // 512]
                P.dve(lambda e: e.tensor_tensor(out=sc[:, qi * 128:(qi + 1) * 128], in0=sc[:, qi * 128:(qi + 1) * 128],
                                                in1=cst[:, C_NEG:C_NEG + 128], op=ALU.add), r=[scd, cst], w=[scd])

            def gen_B(qi):
                S = (qi + 1) * 128
                ncc = (S + 511) // 512
                scr = sc.p[0:ncc]
                mkb = mk[qi % 2]
                thr_c = float(2 * KTOP - S) - 0.5
                P.pool(lambda e: e.memset(bsn[:, :], 0.0), w=[bsn])
                for it in range(NB_IT):
                    ck = BIS_R / (2 ** it)
                    cn = ck / 2 if it < NB_IT - 1 else ck
                    P.act(lambda e: e.activation(out=mkb[:, 0:S], in_=sc[:, 0:S], func=AF.Sign, bias=bsn[:, 0:1],
                                                 accum_out=bss[:, 0:1]), r=scr + [bsn], w=[mkb, bss])
                    P.pool(lambda e: e.tensor_scalar(out=bsu[:, :], in0=bss[:, :], scalar1=thr_c, scalar2=-ck,
                                                     op0=ALU.is_ge, op1=ALU.mult), r=[bss], w=[bsu])
                    P.pool(lambda e: e.tensor_scalar(out=bsn[:, :], in0=bsu[:, :], scalar1=bsn[:, 0:1], scalar2=cn,
                                                     op0=ALU.add, op1=ALU.add), r=[bsu, bsn], w=[bsn])
                    yield
                P.act(lambda e: e.activation(out=mkb[:, 0:S], in_=sc[:, 0:S], func=AF.Sign, bias=bsn[:, 0:1]),
                      r=scr + [bsn], w=[mkb])
                if dbg:
                    P.dve(lambda e: e.tensor_scalar(out=thrs[:, qi:qi + 1], in0=bsn[:, 0:1], scalar1=-1.0, scalar2=None,
                                                    op0=ALU.mult), r=[bsn], w=[thrs])
                yield

            def gen_P(qi, j, qb):
                mkb = mk[qi % 2]
                qa = qaT2[qb]
                steps = [(kj, c) for kj in range(qi + 1) for c in range(2)]
                n = len(steps)

                def slot_of(kj):
                    k2 = kj % 2
                    return PT, PT[:, k2 * 512:k2 * 512 + 128]

                for i in range(n + 3):
                    if i < n:
                        kj, c = steps[i]
                        dl = min(qi - kj, 2)
                        bk = rot[i % 3]
                        if c == 0:
                            pts, ptap = slot_of(kj)
                            P.pe(lambda e: e.transpose(ptap, mkb[:, kj * 128:(kj + 1) * 128], identb[:, :]),
                                 r=[mkb, identb], w=[pts])
                        P.pe(lambda e: e.matmul(bk.ap(0, 512), lhsT=ckvT[:, kj * 128:(kj + 1) * 128],
                                                rhs=qa[:, c * 4:(c + 1) * 4, j * 128:(j + 1) * 128], start=True, stop=True),
                             r=[ckvT, qa], w=[bk.tl])
                    if 0 <= i - 3 < n:
                        kj, c = steps[i - 3]
                        emt = Em[(i - 3) % 3]
                        P.pe(lambda e: e.matmul(PB[:, c * 512:(c + 1) * 512], lhsT=ckvtm[:, kj, :], rhs=emt[:, :],
                                                start=(kj == 0), stop=(kj == qi)), r=[ckvtm, emt], w=[PB.p[c]])
                        P.pe(lambda e: e.matmul(PC[:, c * 512:(c + 1) * 512], lhsT=onesb[:, :], rhs=emt[:, :],
                                                start=(kj == 0), stop=(kj == qi)), r=[onesb, emt], w=[PC.p[c]])
                    if 0 <= i - 2 < n:
                        kj, c = steps[i - 2]
                        pts, ptap = slot_of(kj)
                        eet, emt = Ee[(i - 2) % 3], Em[(i - 2) % 3]
                        P.dve(lambda e: e.scalar_tensor_tensor(
                            out=emt[:, :].rearrange("p (h t) -> p h t", h=4),
                            in0=ptap.unsqueeze(1).to_broadcast([128, 4, 128]), scalar=1.0,
                            in1=eet[:, :].rearrange("p (h t) -> p h t", h=4), op0=ALU.add, op1=ALU.mult),
                            r=[eet, pts], w=[emt])
                    if i < n:
                        kj, c = steps[i]
                        dl = min(qi - kj, 2)
                        bk = rot[i % 3]
                        lgt = lg[i % 3]
                        P.dve(lambda e: e.tensor_tensor(out=lgt[:, :], in0=bk.ap(0, 512),
                                                        in1=biasT[:, dl, c * 512:(c + 1) * 512], op=ALU.add),
                              r=[bk.tl, biasT], w=[lgt])
                    if 0 <= i - 1 < n:
                        lgt, eet = lg[(i - 1) % 3], Ee[(i - 1) % 3]
                        P.act(lambda e: e.activation(out=eet[:, :], in_=lgt[:, :], func=AF.Exp), r=[lgt], w=[eet])
                    yield
                hs = n
                P.dve(lambda e: e.reciprocal(out=rD[:, :], in_=PC[:, :]), r=[PC], w=[rD])
                P.dve(lambda e: e.tensor_tensor(out=oTb[:, :, :].rearrange("p h t -> p (h t)"), in0=PB[:, :],
                                                in1=rD[:, :], op=ALU.mult), r=[PB, rD], w=[oTb])
                bk = rot[hs % 3]
                for m in range(4):
                    for hh in range(2):
                        h = 2 * m + hh
                        P.pe(lambda e: e.matmul(bk.ap(m * 128, (m + 1) * 128), lhsT=w_uvb[:, h, :], rhs=oTb[:, h, :],
                                                start=(hh == 0), stop=(hh == 1)), r=[w_uvb, oTb], w=[bk.tl])
                yt_ = ytl[qi % 2]
                P.act(lambda e: e.activation(out=yt_[:, :, :], in_=bk.ap(0, 512).rearrange("p (m t) -> p m t", m=4),
                                             func=AF.Copy), r=[bk.tl], w=[yt_])
                P.dma(yscr[qi * 128:(qi + 1) * 128, :], yt_[:, :, :].rearrange("p m t -> p (m t)"), "yst",
                      r=[yt_], w=[yscr_t], q="sp")
                if dbg:
                    P.dve(lambda e: e.tensor_copy(out=ydb[:, :, :], in_=bk.ap(0, 512).rearrange("p (m t) -> p m t", m=4)),
                          r=[bk.tl], w=[ydb])
                    dump("yatt", ydb[:, :, :], ydb,
                         dbg_d["yatt"][:, :].rearrange("p (m t) -> p m t", m=4)[:, :, qi * 128:(qi + 1) * 128])
                yield

            def interleave(ga, gb, na, nb):
                ia = ib = 0
                da = db = False
                while not (da and db):
                    if not da and (db or ia * nb <= ib * na):
                        try:
                            next(ga)
                            ia += 1
                        except StopIteration:
                            da = True
                    else:
                        try:
                            next(gb)
                            ib += 1
                        except StopIteration:
                            db = True

            for s in range(NS):
                for j in range(2):
                    r0 = s * 256 + j * 128
                    P.dma(xt[:, j, :], x_d[r0:r0 + 128, :], "xin", w=[xt])
                for j in range(2):
                    P.act(lambda e, j=j: e.activation(out=sqj[:, :], in_=xt[:, j, :], func=AF.Square,
                                                      accum_out=st4[:, j:j + 1]), r=[xt], w=[sqj, st4])
                P.act(lambda e: e.activation(out=st4[:, 2:4], in_=st4[:, 0:2], func=AF.Sqrt, scale=1.0 / D,
                                             bias=epsc[:, 0:1]), r=[st4, epsc], w=[st4])
                P.dve(lambda e: e.reciprocal(out=st4[:, 4:6], in_=st4[:, 2:4]), r=[st4], w=[st4])
                for j in range(2):
                    P.dve(lambda e, j=j: e.tensor_scalar(out=dgt[:, j, :], in0=cst[:, C_ID:C_ID + 128],
                                                         scalar1=st4[:, 4 + j:5 + j], scalar2=None, op0=ALU.mult),
                          r=[cst, st4], w=[dgt])
                for kk in range(2):
                    for k4 in range(4):
                        k = kk * 4 + k4
                        for j in range(2):
                            P.pe(lambda e, k=k, k4=k4, j=j: e.matmul(
                                PA[:, k4 * 256 + j * 128:k4 * 256 + (j + 1) * 128],
                                lhsT=xt[:, j, k * 128:(k + 1) * 128], rhs=dgt[:, j, :], start=True, stop=True),
                                r=[xt, dgt], w=[PA])
                    for k4 in range(4):
                        k = kk * 4 + k4
                        P.dve(lambda e, k=k, k4=k4: e.tensor_scalar(
                            out=hT[:, k, :], in0=PA[:, k4 * 256:(k4 + 1) * 256], scalar1=gsh[:, k:k + 1],
                            scalar2=modc[:, k:k + 1], op0=ALU.mult, op1=ALU.add), r=[PA, gsh, modc], w=[hT])
                for c in range(4):
                    for k in range(8):
                        P.pe(lambda e, c=c, k=k: e.matmul(PB[:, c * 256:(c + 1) * 256],
                                                          lhsT=w_att[:, k, c * 128:(c + 1) * 128], rhs=hT[:, k, :],
                                                          start=(k == 0), stop=(k == 7)), r=[w_att, hT], w=[PB])
                P.act(lambda e: e.activation(out=cqraw[:, :, :], in_=PB[:, :].rearrange("p (c t) -> p c t", c=4),
                                             func=AF.Copy), r=[PB], w=[cqraw])
                P.act(lambda e: e.activation(out=sq[:, :, :], in_=PB[:, :].rearrange("p (c t) -> p c t", c=4),
                                             func=AF.Square), r=[PB], w=[sq])
                for j in range(2):
                    for k in range(8):
                        P.pe(lambda e, j=j, k=k: e.matmul(PD[:, j * 8:(j + 1) * 8], lhsT=hT[:, k, j * 128:(j + 1) * 128],
                                                          rhs=w_iw[:, k, :], start=(k == 0), stop=(k == 7)),
                             r=[hT, w_iw], w=[PD])
                P.act(lambda e: e.activation(out=iw[:, :, :], in_=PD[:, 0:16].rearrange("p (j h) -> p j h", j=2),
                                             func=AF.Copy, scale=float(8 ** -0.5 * 64 ** -0.5)), r=[PD], w=[iw])
                for c in range(2):
                    P.pe(lambda e, c=c: e.matmul(PC[:, 0:256], lhsT=cst[:, C_ONE:C_ONE + 128], rhs=sq[:, c, :],
                                                 start=(c == 0), stop=(c == 1)), r=[cst, sq], w=[PC])
                P.pe(lambda e: e.matmul(PC[:, 256:512], lhsT=cst[:, C_ONE:C_ONE + 128], rhs=sq[:, 2, :],
                                        start=True, stop=True), r=[cst, sq], w=[PC])
                P.pe(lambda e: e.matmul(PC[:, 512:768], lhsT=cst[:, C_BLK:C_BLK + 128], rhs=sq[:, 3, :],
                                        start=True, stop=True), r=[cst, sq], w=[PC])
                for i, dv in enumerate((256.0, 128.0, 64.0)):
                    P.act(lambda e, i=i, dv=dv: e.activation(out=rq[:, i, :], in_=PC[:, i * 256:(i + 1) * 256],
                                                             func=AF.Sqrt, scale=1.0 / dv, bias=epsc[:, 0:1]),
                          r=[PC, epsc], w=[rq])
                P.dve(lambda e: e.reciprocal(out=rq[:, :, :], in_=rq[:, :, :]), r=[rq], w=[rq])
                for c in range(2):
                    P.dve(lambda e, c=c: e.scalar_tensor_tensor(out=cqT[:, c, :], in0=cqraw[:, c, :],
                                                                scalar=vec[:, V_QN + c:V_QN + c + 1], in1=rq[:, 0, :],
                                                                op0=ALU.mult, op1=ALU.mult), r=[cqraw, vec, rq], w=[cqT])
                P.act(lambda e: e.activation(out=cqTb[:, :, :], in_=cqT[:, :, :], func=AF.Copy), r=[cqT], w=[cqTb])
                P.dve(lambda e, s=s: e.scalar_tensor_tensor(out=ckvT[:, s * 256:(s + 1) * 256], in0=cqraw[:, 2, :],
                                                            scalar=vec[:, V_KVN:V_KVN + 1], in1=rq[:, 1, :],
                                                            op0=ALU.mult, op1=ALU.mult), r=[cqraw, vec, rq], w=[ckvT])
                P.dve(lambda e: e.scalar_tensor_tensor(out=ikf[:, :], in0=cqraw[:, 3, :],
                                                       scalar=vec[:, V_IKN:V_IKN + 1], in1=rq[:, 2, :],
                                                       op0=ALU.mult, op1=ALU.mult), r=[cqraw, vec, rq], w=[ikf])
                P.act(lambda e, s=s: e.activation(out=ikA[:, s * 256:(s + 1) * 256], in_=ikf[:, :], func=AF.Copy),
                      r=[ikf], w=[ikA])
                P.dve(lambda e, s=s: e.tensor_tensor(out=ikB[0:64, s * 256:(s + 1) * 256], in0=ikf[0:64, :],
                                                     in1=ikA[0:64, s * 256:(s + 1) * 256], op=ALU.subtract),
                      r=[ikf, ikA], w=[ikB])
                for j in range(2):
                    tix = s * 2 + j
                    P.pe(lambda e, j=j, tix=tix: e.transpose(PT[:, j * 128:(j + 1) * 128],
                                                             ckvT[:, tix * 128:(tix + 1) * 128], identb[:, :]),
                         r=[ckvT, identb], w=[PT])
                P.act(lambda e, s=s: e.activation(out=ckvtm[:, 2 * s:2 * s + 2, :],
                                                  in_=PT[:, 0:256].rearrange("p (j c) -> p j c", j=2), func=AF.Copy),
                      r=[PT], w=[ckvtm])
                for hf in range(2):
                    for h4 in range(4):
                        h = hf * 4 + h4
                        for c in range(2):
                            P.pe(lambda e, h=h, h4=h4, c=c: e.matmul(PB[:, h4 * 256:(h4 + 1) * 256],
                                                                     lhsT=w_iq[:, c, h * 128:(h + 1) * 128], rhs=cqT[:, c, :],
                                                                     start=(c == 0), stop=(c == 1)), r=[w_iq, cqT], w=[PB])
                    hsl = slice(hf * 4, hf * 4 + 4)
                    pb3 = lambda rows: PB[rows, :].rearrange("p (m t) -> p m t", m=4)
                    P.act(lambda e, hsl=hsl: e.activation(out=iqT[0:64, hsl, :], in_=pb3(slice(0, 64)), func=AF.Copy),
                          r=[PB], w=[iqT])
                    P.act(lambda e: e.activation(out=iqtmp[64:128, :, :], in_=pb3(slice(64, 128)), func=AF.Copy),
                          r=[PB], w=[iqtmp])
                    P.dve(lambda e, hsl=hsl: e.tensor_tensor(out=iqT[64:128, hsl, :], in0=pb3(slice(64, 128)),
                                                             in1=iqtmp[64:128, :, :], op=ALU.subtract),
                          r=[PB, iqtmp], w=[iqT])
                for m in range(4):
                    for c in range(2):
                        P.pe(lambda e, m=m, c=c: e.matmul(PC[:, m * 256:(m + 1) * 256],
                                                          lhsT=w_uqb[:, c, m * 128:(m + 1) * 128], rhs=cqTb[:, c, :],
                                                          start=(c == 0), stop=(c == 1)), r=[w_uqb, cqTb], w=[PC])
                P.dve(lambda e: e.tensor_copy(out=qTb[:, :, :], in_=PC[:, :].rearrange("p (m t) -> p m t", m=4)),
                      r=[PC], w=[qTb])
                for hf in range(2):
                    for h4 in range(4):
                        h = hf * 4 + h4
                        pr = slice((h % 2) * 64, (h % 2) * 64 + 64)
                        P.pe(lambda e, h=h, h4=h4, pr=pr: e.matmul(PB[:, h4 * 256:(h4 + 1) * 256],
                                                                   lhsT=w_ukb[:, h, :], rhs=qTb[:, h // 2, :],
                                                                   start=True, stop=True), r=[w_ukb, qTb], w=[PB])
                    P.act(lambda e, hf=hf, s=s: e.activation(out=qaT2[s % 2][:, hf * 4:(hf + 1) * 4, :],
                                                             in_=PB[:, :].rearrange("p (h t) -> p h t", h=4), func=AF.Copy,
                                                             scale=0.125), r=[PB], w=[qaT2[s % 2]])
                for j in range(2):
                    qi = s * 2 + j
                    emit_S(qi, j)
                    gB = gen_B(qi)
                    if pending[0] is not None:
                        interleave(gB, pending[0], NB_IT + 1, pending[1])
                    else:
                        for _ in gB:
                            pass
                    pending[0] = gen_P(qi, j, s % 2)
                    pending[1] = 2 * (qi + 1) + 4
            for _ in pending[0]:
                pass
            if dbg:
                dump("thr", thrs[:, :], thrs)

        print("ops after A1", P.nops)
        P.fence()
        if upto < 2:
            P.enabled = False
        x1t = Tl(P, None, "x1_hbm")
        with ExitStack() as es2:
            P.es_cur = es2
            w_rwb = P.tile([128, 8, 1824], BF16, "w_rwb")
            w_lorab = P.tile([128, 1024], BF16, "w_lorab")
            w_gateb = P.tile([128, 2, 512], BF16, "w_gateb")
            w_outb = P.tile([128, 8, D], BF16, "w_outb")
            mucol = P.tile([128, 32], F32, "mucol")
            lnrow = P.tile([128, 2, 512], F32, "lnrow")
            wst = P.tile([128, 2048], F32, "wst2")
            P.dve(lambda e: e.tensor_copy(out=mucol[:, 0:15], in_=vec[:, 108:123]), r=[vec], w=[mucol])
            P.dve(lambda e: e.tensor_scalar(out=mucol[:, 16:31], in0=vec[:, 108:123], scalar1=-1.0, scalar2=1.0,
                                            op0=ALU.mult, op1=ALU.add), r=[vec, mucol], w=[mucol])
            for k in range(8):
                P.dma(wst[:, 0:1824], wrw_d[k * 128:(k + 1) * 128, :], "wst", w=[wst])
                P.act(lambda e, k=k: e.activation(out=w_rwb[:, k, :], in_=wst[:, 0:1824], func=AF.Copy), r=[wst], w=[w_rwb])
            for k in range(8):
                P.dma(wst[:, 0:D], wout_d[k * 128:(k + 1) * 128, :], "wst", w=[wst])
                P.act(lambda e, k=k: e.activation(out=w_outb[:, k, :], in_=wst[:, 0:D], func=AF.Copy), r=[wst], w=[w_outb])
            P.dma(wst[:, 0:1024], wlora_d[:, :], "wst", w=[wst])
            P.act(lambda e: e.activation(out=w_lorab[:, :], in_=wst[:, 0:1024], func=AF.Copy), r=[wst], w=[w_lorab])
            P.dve(lambda e: e.memset(w_gateb[:, :, :], 0.0), w=[w_gateb])
            P.dma(wst[:, 0:512], wgate_d[0:128, :], "wst", w=[wst])
            P.act(lambda e: e.activation(out=w_gateb[:, 0, :], in_=wst[:, 0:512], func=AF.Copy), r=[wst], w=[w_gateb])
            P.dma(wst[96:128, 0:512], wgate_d[128:160, :], "wst", w=[wst])
            P.act(lambda e: e.activation(out=w_gateb[96:128, 1, :], in_=wst[96:128, 0:512], func=AF.Copy), r=[wst], w=[w_gateb])
            for i in range(2):
                P.dma(lnrow[:, i, :], lnrow_d[i, :].partition_broadcast(128), "cw", w=[lnrow])

            xt = P.tile([128, 2, D], F32, "xt2")
            sqj = P.tile([128, D], BF16, "sqj2")
            st4 = P.tile([128, 8], F32, "st42")
            dgt = P.tile([128, 2, 128], F32, "dgt2")
            hTb = P.tile([128, 8, 256], BF16, "hTb")
            lastc = TlG(P, es2.enter_context(nc.sbuf_tensor("sb_lastc", [128, 16], F32)), "lastc", 16)
            ptmp = [P.tile([128, 256], F32, "ptmp%d" % i) for i in range(3)]
            pch = [P.tile([128, 256], F32, "pch%d" % i) for i in range(3)]
            rT = P.tile([128, 4, 256], F32, "rT")
            kT = P.tile([128, 4, 256], F32, "kT")
            vTb = P.tile([128, 4, 256], BF16, "vTb")
            twd = P.tile([128, 256], BF16, "twd")
            sgd = P.tile([128, 2, 256], BF16, "sgd")
            ART = P.tile([128, 4, 2, 256], BF16, "ART")
            btT = P.tile([128, 8, 256], BF16, "btT")
            ktT = P.tile([128, 8, 256], BF16, "ktT")
            Hbz = P.tile([128, 8, 64], BF16, "Hbz")
            P.dve(lambda e: e.memset(btT[:, :, :], 0.0), w=[btT])
            P.dve(lambda e: e.memset(ktT[:, :, :], 0.0), w=[ktT])
            P.dve(lambda e: e.memset(Hbz[:, :, :], 0.0), w=[Hbz])
            bhT = P.tile([128, 4, 256], BF16, "bhT")
            khT = P.tile([128, 4, 256], BF16, "khT")
            rkT = P.tile([128, 4, 256], BF16, "rkT")
            WC = P.tile([128, 4, 2], F32, "WC")
            fsets = [[P.tile([128, 256], F32, "f%d_%d" % (q, i)) for i in range(9)] for q in range(2)]
            kk4 = P.tile([128, 4, 256], F32, "kk4")
            sq4 = P.tile([128, 4, 256], F32, "sq4")
            gtmp = P.tile([128, 256], F32, "gtmp")
            hw = P.tile([128, 8], F32, "hw")
            P.dve(lambda e: e.tensor_scalar(out=hw[:, :], in0=vec[:, V_W0:V_W0 + 8], scalar1=0.5, scalar2=None, op0=ALU.mult),
                  r=[vec], w=[hw])
            Vtm = P.tile([128, 512], BF16, "Vtm")
            Bhtm = P.tile([128, 512], BF16, "Bhtm")
            Khtm = P.tile([128, 512], BF16, "Khtm")
            ATab = P.tile([128, 8, 256], BF16, "ATab")
            ATak = P.tile([128, 8, 256], BF16, "ATak")
            def tlg2(name):
                return TlG(P, es2.enter_context(nc.sbuf_tensor("sb_" + name, [128, 8, 128], BF16)), name, 2)
            Nk = [tlg2("Nk%d" % i) for i in range(2)]
            Mk = [tlg2("Mk%d" % i) for i in range(2)]
            Pk = [tlg2("Pk%d" % i) for i in range(2)]
            Hs = P.tile([128, 4, 64], F32, "Hs")
            Hb = P.tile([128, 4, 64], BF16, "Hb")
            Hd = P.tile([128, 4, 64], F32, "Hd")
            WCf = P.tile([128, 4, 64], F32, "WCf")
            Gb = P.tile([128, 512], BF16, "Gb")
            Ub = P.tile([128, 512], BF16, "Ub")
            Ysb = P.tile([128, 512], F32, "Ysb")
            Ysq = P.tile([128, 512], F32, "Ysq")
            gst = P.tile([128, 48], F32, "gst")
            vtok = P.tile([128, 512], F32, "vtok")
            yrw = P.tile([128, 512], F32, "yrw")
            yrwb = P.tile([128, 512], BF16, "yrwb")
            ycat = P.tile([128, 4, 128], BF16, "ycat")
            mix = P.tile([128, D], F32, "mix")
            x1s = [P.tile([128, D], F32, "x1s%d" % i) for i in range(1)]
            ya = P.tile([128, 4, 128], BF16, "ya")
            yaf = P.tile([128, 512], F32, "yaf")
            oka = P.tile([128, 4], F32, "oka")
            hindb = P.tile([128, 2], BF16, "hindb")
            P.dve(lambda e: e.tensor_scalar(out=oka[:, :], in0=vec[:, V_KA:V_KA + 4], scalar1=-1.0, scalar2=1.0,
                                            op0=ALU.mult, op1=ALU.add), r=[vec], w=[oka])
            P.dve(lambda e: e.tensor_copy(out=hindb[:, :], in_=cst[:, C_HIND:C_HIND + 2]), r=[cst], w=[hindb])
            P.dve(lambda e: e.memset(lastc[:, :], 0.0), w=[lastc])
            print("SBUFREM A2", nc.sbuf_bytes_remaining)
            P.dve(lambda e: e.memset(Hs[:, :, :], 0.0), w=[Hs])
            P.dve(lambda e: e.memset(sgd[:, :, :], 0.0), w=[sgd])

            iprot = [Bank(PD, PD.h, 0), Bank(PC.p[0], PC.h, 0), Bank(PC.p[1], PC.h, 512), Bank(PB.p[1], PB.h, 512)]

            def inproj(n, width=128):
                c0 = n * 128 if n < 14 else 1824 - 128
                bk = iprot[n % 4]
                for k in range(8):
                    P.pe(lambda e, k=k: e.matmul(bk.ap(0, 256), lhsT=w_rwb[:, k, c0:c0 + 128], rhs=hTb[:, k, :],
                                                 start=(k == 0), stop=(k == 7)), r=[w_rwb, hTb], w=[bk.tl])
                pt = ptmp[n % 3]
                pc = pch[n % 3]
                lc = lastc.p[n]
                P.act(lambda e: e.activation(out=pt[:, :], in_=bk.ap(0, 256), func=AF.Copy,
                                             scale=mucol[:, 16 + n:17 + n]), r=[bk.tl, mucol], w=[pt])
                P.dve(lambda e: e.scalar_tensor_tensor(out=pc[:, 1:256], in0=bk.ap(0, 255),
                                                       scalar=mucol[:, n:n + 1], in1=pt[:, 1:256],
                                                       op0=ALU.mult, op1=ALU.add), r=[bk.tl, mucol, pt], w=[pc])
                P.dve(lambda e: e.scalar_tensor_tensor(out=pc[:, 0:1], in0=lastc[:, n:n + 1],
                                                       scalar=mucol[:, n:n + 1], in1=pt[:, 0:1],
                                                       op0=ALU.mult, op1=ALU.add), r=[lc, mucol, pt, pc], w=[pc])
                P.dve(lambda e: e.tensor_copy(out=lastc[:, n:n + 1], in_=bk.ap(255, 256)), r=[bk.tl, lc], w=[lc])
                return pc

            for s in range(NS):
                for j in range(2):
                    r0 = s * 256 + j * 128
                    P.dma(xt[:, j, :], x_d[r0:r0 + 128, :], "xin", w=[xt])
                for j in range(2):
                    P.act(lambda e, j=j: e.activation(out=sqj[:, :], in_=xt[:, j, :], func=AF.Square,
                                                      accum_out=st4[:, j:j + 1]), r=[xt], w=[sqj, st4])
                P.act(lambda e: e.activation(out=st4[:, 2:4], in_=st4[:, 0:2], func=AF.Sqrt, scale=1.0 / D,
                                             bias=epsc[:, 0:1]), r=[st4, epsc], w=[st4])
                P.dve(lambda e: e.reciprocal(out=st4[:, 4:6], in_=st4[:, 2:4]), r=[st4], w=[st4])
                for j in range(2):
                    P.dve(lambda e, j=j: e.tensor_scalar(out=dgt[:, j, :], in0=cst[:, C_ID:C_ID + 128],
                                                         scalar1=st4[:, 4 + j:5 + j], scalar2=None, op0=ALU.mult),
                          r=[cst, st4], w=[dgt])
                for kk in range(2):
                    for k4 in range(4):
                        k = kk * 4 + k4
                        for j in range(2):
                            P.pe(lambda e, k=k, k4=k4, j=j: e.matmul(
                                PA[:, k4 * 256 + j * 128:k4 * 256 + (j + 1) * 128],
                                lhsT=xt[:, j, k * 128:(k + 1) * 128], rhs=dgt[:, j, :], start=True, stop=True),
                                r=[xt, dgt], w=[PA])
                    for k4 in range(4):
                        k = kk * 4 + k4
                        P.dve(lambda e, k=k, k4=k4: e.tensor_scalar(
                            out=hTb[:, k, :], in0=PA[:, k4 * 256:(k4 + 1) * 256], scalar1=gsh[:, k:k + 1],
                            scalar2=modc[:, k:k + 1], op0=ALU.mult, op1=ALU.add), r=[PA, gsh, modc], w=[hTb])
                for m in range(4):
                    pc = inproj(m)
                    P.act(lambda e, m=m, pc=pc: e.activation(out=rT[:, m, :], in_=pc[:, :], func=AF.Copy), r=[pc], w=[rT])
                for m in range(4):
                    pc = inproj(4 + m)
                    P.act(lambda e, m=m, pc=pc: e.activation(out=kT[:, m, :], in_=pc[:, :], func=AF.Copy), r=[pc], w=[kT])
                for m in range(4):
                    pc = inproj(8 + m)
                    P.act(lambda e, m=m, pc=pc: e.activation(out=vTb[:, m, :], in_=pc[:, :], func=AF.Copy), r=[pc], w=[vTb])
                pc = inproj(12)
                P.act(lambda e, pc=pc: e.activation(out=twd[0:64, :], in_=pc[0:64, :], func=AF.Tanh), r=[pc], w=[twd])
                P.act(lambda e, pc=pc: e.activation(out=twd[64:128, :], in_=pc[64:128, :], func=AF.Copy), r=[pc], w=[twd])
                pc = inproj(13)
                P.act(lambda e, pc=pc: e.activation(out=gtmp[:, :], in_=pc[:, :], func=AF.Tanh, scale=0.5), r=[pc], w=[gtmp])
                P.dve(lambda e: e.tensor_scalar(out=sgd[:, 0, :], in0=gtmp[:, :], scalar1=0.5, scalar2=0.5, op0=ALU.mult,
                                                op1=ALU.add), r=[gtmp], w=[sgd])
                pc = inproj(14)
                P.act(lambda e, pc=pc: e.activation(out=gtmp[:, :], in_=pc[:, :], func=AF.Tanh, scale=0.5), r=[pc], w=[gtmp])
                P.dve(lambda e: e.tensor_scalar(out=sgd[:, 1, :], in0=gtmp[:, :], scalar1=0.5, scalar2=0.5, op0=ALU.mult,
                                                op1=ALU.add), r=[gtmp], w=[sgd])
                for m in range(4):
                    P.dve(lambda e, m=m: e.tensor_scalar(out=kk4[:, m, :], in0=kT[:, m, :], scalar1=vec[:, V_KK + m:V_KK + m + 1],
                                                         scalar2=None, op0=ALU.mult), r=[kT, vec], w=[kk4])
                P.act(lambda e: e.activation(out=sq4[:, :, :], in_=kk4[:, :, :], func=AF.Square), r=[kk4], w=[sq4])
                for m in range(4):
                    P.pe(lambda e, m=m: e.matmul(PA[:, m * 256:(m + 1) * 256], lhsT=cst[:, C_BLK:C_BLK + 128], rhs=sq4[:, m, :],
                                                 start=True, stop=True), r=[cst, sq4], w=[PA])
                sq4f = sq4[:, :, :].rearrange("p m t -> p (m t)")
                P.act(lambda e: e.activation(out=sq4f, in_=PA[:, :], func=AF.Sqrt), r=[PA], w=[sq4])
                P.dve(lambda e: e.tensor_scalar(out=sq4f, in0=sq4f, scalar1=1e-12, scalar2=None, op0=ALU.max), r=[sq4], w=[sq4])
                P.dve(lambda e: e.reciprocal(out=sq4f, in_=sq4f), r=[sq4], w=[sq4])
                P.dve(lambda e: e.tensor_tensor(out=kk4[:, :, :], in0=kk4[:, :, :], in1=sq4[:, :, :], op=ALU.mult),
                      r=[kk4, sq4], w=[kk4])
                for m in range(4):
                    ms = slice(m * 128, (m + 1) * 128)
                    pg = PB if m % 2 == 0 else PC
                    P.pe(lambda e, m=m: e.matmul(pg[:, 0:256], lhsT=w_lorab[:, m * 128:(m + 1) * 128], rhs=twd[:, :],
                                                 start=True, stop=True), r=[w_lorab, twd], w=[pg.p[0]])
                    P.pe(lambda e, m=m: e.matmul(pg[:, 256:512], lhsT=w_lorab[:, 512 + m * 128:512 + (m + 1) * 128], rhs=twd[:, :],
                                                 start=True, stop=True), r=[w_lorab, twd], w=[pg.p[0]])
                    sgw, cs, wt, winv, wprev, asig, kp, t1, t2 = fsets[m % 2]
                    P.act(lambda e, m=m: e.activation(out=t1[:, :], in_=pg[:, 0:256], func=AF.Tanh, scale=0.5,
                                                      bias=hw[:, m:m + 1]), r=[pg.p[0], hw], w=[t1])
                    P.dve(lambda e: e.tensor_scalar(out=sgw[:, :], in0=t1[:, :], scalar1=0.5, scalar2=0.5, op0=ALU.mult,
                                                    op1=ALU.add), r=[t1], w=[sgw])
                    P.act(lambda e, m=m: e.activation(out=t2[:, :], in_=pg[:, 256:512], func=AF.Tanh, scale=0.5,
                                                      bias=hw[:, 4 + m:5 + m]), r=[pg.p[0], hw], w=[t2])
                    P.dve(lambda e: e.tensor_scalar(out=asig[:, :], in0=t2[:, :], scalar1=0.5, scalar2=0.5, op0=ALU.mult,
                                                    op1=ALU.add), r=[t2], w=[asig])
                    P.dve(lambda e: e.tensor_tensor_scan(out=cs[:, :], data0=cst[:, C_SCAN:C_SCAN + 256], data1=sgw[:, :],
                                                         initial=0.0, op0=ALU.mult, op1=ALU.add), r=[cst, sgw], w=[cs])
                    P.act(lambda e: e.activation(out=wt[:, :], in_=cs[:, :], func=AF.Exp, scale=-LWC), r=[cs], w=[wt])
                    P.act(lambda e: e.activation(out=winv[:, :], in_=cs[:, :], func=AF.Exp, scale=LWC), r=[cs], w=[winv])
                    P.dve(lambda e: e.tensor_tensor(out=t1[:, :], in0=cs[:, :], in1=sgw[:, :], op=ALU.subtract),
                          r=[cs, sgw], w=[t1])
                    P.act(lambda e: e.activation(out=wprev[:, :], in_=t1[:, :], func=AF.Exp, scale=-LWC), r=[t1], w=[wprev])
                    for j in range(2):
                        cj = slice(j * 128, (j + 1) * 128)
                        P.dve(lambda e, m=m, j=j: e.tensor_scalar(out=WC[:, m, j:j + 1], in0=cs[:, j * 128 + 127:j * 128 + 128],
                                                                  scalar1=-LWC, scalar2=None, op0=ALU.mult),
                              r=[cs], w=[WC])
                        P.act(lambda e, m=m, j=j, cj=cj: e.activation(out=t2[:, cj], in_=cs[:, cj], func=AF.Exp, scale=LWC,
                                                                      bias=WC[:, m, j:j + 1]), r=[cs, WC], w=[t2])
                    P.act(lambda e, m=m: e.activation(out=WC[:, m, :], in_=WC[:, m, :], func=AF.Exp), r=[WC], w=[WC])
                    P.dve(lambda e, m=m: e.tensor_scalar(out=t1[:, :], in0=asig[:, :], scalar1=vec[:, V_KA + m:V_KA + m + 1],
                                                         scalar2=oka[:, m:m + 1], op0=ALU.mult, op1=ALU.add),
                          r=[asig, vec, oka], w=[t1])
                    P.dve(lambda e, m=m: e.tensor_tensor(out=kp[:, :], in0=kT[:, m, :], in1=t1[:, :], op=ALU.mult),
                          r=[kT, t1], w=[kp])
                    P.dve(lambda e, m=m: e.scalar_tensor_tensor(out=rkT[:, m, :], in0=rT[:, m, :],
                                                                scalar=vec[:, V_RK + m:V_RK + m + 1], in1=kp[:, :],
                                                                op0=ALU.mult, op1=ALU.mult), r=[rT, vec, kp], w=[rkT])
                    P.dve(lambda e, m=m: e.tensor_tensor(out=ART[:, m, 1, :], in0=rT[:, m, :], in1=wt[:, :], op=ALU.mult),
                          r=[rT, wt], w=[ART])
                    P.dve(lambda e, m=m: e.scalar_tensor_tensor(out=ART[:, m, 0, :], in0=kk4[:, m, :], scalar=-1.0,
                                                                in1=wprev[:, :], op0=ALU.mult, op1=ALU.mult),
                          r=[kk4, wprev], w=[ART])
                    P.dve(lambda e, m=m: e.tensor_tensor(out=t1[:, :], in0=kk4[:, m, :], in1=asig[:, :], op=ALU.mult),
                          r=[kk4, asig], w=[t1])
                    for hh in range(2):
                        prr = slice(hh * 64, hh * 64 + 64)
                        P.dve(lambda e, m=m, hh=hh, prr=prr: e.tensor_tensor(out=btT[prr, 2 * m + hh, :], in0=t1[prr, :],
                                                                             in1=winv[prr, :], op=ALU.mult),
                              r=[t1, winv], w=[btT])
                    P.dve(lambda e, m=m: e.tensor_tensor(out=bhT[:, m, :], in0=t1[:, :], in1=t2[:, :], op=ALU.mult),
                          r=[t1, t2], w=[bhT])
                    for hh in range(2):
                        prr = slice(hh * 64, hh * 64 + 64)
                        P.dve(lambda e, m=m, hh=hh, prr=prr: e.tensor_tensor(out=ktT[prr, 2 * m + hh, :], in0=kp[prr, :],
                                                                             in1=winv[prr, :], op=ALU.mult),
                              r=[kp, winv], w=[ktT])
                    P.dve(lambda e, m=m: e.tensor_tensor(out=khT[:, m, :], in0=kp[:, :], in1=t2[:, :], op=ALU.mult),
                          r=[kp, t2], w=[khT])
                for j in range(2):
                    ti = s * 2 + j
                    tb = slice(j * 128, (j + 1) * 128)
                    for (src, dst) in ((vTb, Vtm), (bhT, Bhtm), (khT, Khtm)):
                        for m in range(4):
                            P.pe(lambda e, src=src, m=m: e.transpose(PT[:, m * 128:(m + 1) * 128], src[:, m, tb], identb[:, :]),
                                 r=[src, identb], w=[PT])
                        P.act(lambda e, dst=dst: e.activation(out=dst[:, :], in_=PT[:, 0:512], func=AF.Copy), r=[PT], w=[dst])
                    P.dve(lambda e: e.tensor_copy(out=vtok[:, :], in_=Vtm[:, :]), r=[Vtm], w=[vtok])
                    for hf in range(2):
                        for h4 in range(4):
                            h = hf * 4 + h4
                            m = h // 2
                            pr = slice((h % 2) * 64, (h % 2) * 64 + 64)
                            P.pe(lambda e, h4=h4, m=m, h=h: e.matmul(PA[:, h4 * 256:(h4 + 1) * 256], lhsT=btT[:, h, tb],
                                                                     rhs=ART[:, m, :, tb], start=True, stop=True),
                                 r=[btT, ART], w=[PA])
                            P.pe(lambda e, h4=h4, m=m, h=h: e.matmul(PB[:, h4 * 256:(h4 + 1) * 256], lhsT=ktT[:, h, tb],
                                                                     rhs=ART[:, m, :, tb], start=True, stop=True),
                                 r=[ktT, ART], w=[PB])
                        mur = cst[:, C_MSU:C_MSU + 256].unsqueeze(1).to_broadcast([128, 4, 256])
                        msl = cst[:, C_MSL:C_MSL + 128].unsqueeze(1).to_broadcast([128, 4, 128])
                        msu = cst[:, C_MSU:C_MSU + 128].unsqueeze(1).to_broadcast([128, 4, 128])
                        hs4 = slice(hf * 4, hf * 4 + 4)
                        P.dve(lambda e, hs4=hs4, mur=mur: e.tensor_tensor(
                            out=ATab[:, hs4, :], in0=PA[:, :].rearrange("p (h t) -> p h t", h=4), in1=mur, op=ALU.mult),
                            r=[PA, cst], w=[ATab])
                        P.dve(lambda e, hs4=hs4, mur=mur: e.tensor_tensor(
                            out=ATak[:, hs4, :], in0=PB[:, :].rearrange("p (h t) -> p h t", h=4), in1=mur, op=ALU.mult),
                            r=[PB, cst], w=[ATak])
                    P.act(lambda e: e.activation(out=Mk[0][:, :, :], in_=ATab[:, :, 0:128], func=AF.Copy), r=[ATab], w=[Mk[0]])
                    for h in range(8):
                        P.pe(lambda e, h=h: e.transpose(PT[:, h * 128:(h + 1) * 128], Mk[0][:, h, :], identb[:, :]),
                             r=[Mk[0], identb], w=[PT])
                    P.act(lambda e: e.activation(out=Nk[0][:, :, :].rearrange("p h t -> p (h t)"), in_=PT[:, :], func=AF.Copy),
                          r=[PT], w=[Nk[0]])
                    P.dve(lambda e: e.tensor_tensor(out=Pk[0][:, :, :], in0=ATab[:, :, 0:128],
                                                    in1=identb[:, :].unsqueeze(1).to_broadcast([128, 8, 128]), op=ALU.add),
                          r=[ATab, identb], w=[Pk[0]])
                    for lv in range(6):
                        a, b = lv % 2, (lv + 1) % 2
                        for g in range(2):
                            for h in range(4 * g, 4 * g + 4):
                                P.pe(lambda e: e.matmul(PA[:, h * 128:(h + 1) * 128], lhsT=Mk[a][:, h, :], rhs=Nk[a][:, h, :],
                                                        start=True, stop=True), r=[Mk[a].p[g], Nk[a].p[g]], w=[PA.p[g]])
                        for g in range(2):
                            P.act(lambda e: e.activation(out=Nk[b][:, 4 * g:4 * g + 4, :].rearrange("p h t -> p (h t)"),
                                                         in_=PA[:, g * 512:(g + 1) * 512], func=AF.Copy),
                                  r=[PA.p[g]], w=[Nk[b].p[g]])
                        if lv < 5:
                            for g in range(2):
                                for h in range(4 * g, 4 * g + 4):
                                    P.pe(lambda e: e.matmul(PB[:, h * 128:(h + 1) * 128], lhsT=Nk[a][:, h, :], rhs=Mk[a][:, h, :],
                                                            start=True, stop=True), r=[Mk[a].p[g], Nk[a].p[g]], w=[PB.p[g]])
                            P.act(lambda e: e.activation(out=Mk[b][:, 0:4, :].rearrange("p h t -> p (h t)"),
                                                         in_=PB[:, 0:512], func=AF.Copy), r=[PB.p[0]], w=[Mk[b].p[0]])
                            P.dve(lambda e: e.tensor_copy(out=Mk[b][:, 4:8, :].rearrange("p h t -> p (h t)"), in_=PB[:, 512:1024]),
                                  r=[PB.p[1]], w=[Mk[b].p[1]])
                        for g in range(2):
                            for h in range(4 * g, 4 * g + 4):
                                P.pe(lambda e: e.matmul(PC[:, h * 128:(h + 1) * 128], lhsT=Nk[b][:, h, :], rhs=Pk[a][:, h, :],
                                                        start=True, stop=True), r=[Nk[b].p[g], Pk[a].p[g]], w=[PC.p[g]])
                        for g in range(2):
                            P.dve(lambda e: e.tensor_tensor(out=Pk[b][:, 4 * g:4 * g + 4, :].rearrange("p h t -> p (h t)"),
                                                            in0=PC[:, g * 512:(g + 1) * 512],
                                                            in1=Pk[a][:, 4 * g:4 * g + 4, :].rearrange("p h t -> p (h t)"),
                                                            op=ALU.add), r=[PC.p[g], Pk[a].p[g]], w=[Pk[b].p[g]])
                    XT = Pk[0]
                    for m in range(4):
                        P.dve(lambda e, m=m: e.tensor_scalar(out=WCf[:, m, :], in0=cst[:, C_ONE:C_ONE + 64],
                                                             scalar1=WC[:, m, j:j + 1], scalar2=None, op0=ALU.mult),
                              r=[cst, WC], w=[WCf])
                    P.dve(lambda e: e.tensor_tensor(out=Hd[:, :, :], in0=Hs[:, :, :], in1=WCf[:, :, :], op=ALU.mult),
                          r=[Hs, WCf], w=[Hd])
                    for h in range(8):
                        m = h // 2
                        pr = slice((h % 2) * 64, (h % 2) * 64 + 64)
                        hc = slice(h * 64, (h + 1) * 64)
                        P.pe(lambda e, m=m, h=h, hc=hc: e.matmul(PD[:, hc], lhsT=ART[:, m, 0, tb], rhs=Hbz[:, h, :],
                                                                 start=True, stop=False), r=[ART, Hbz], w=[PD])
                        P.pe(lambda e, h=h, hc=hc: e.matmul(PD[:, hc], lhsT=ATak[:, h, 0:128], rhs=Vtm[:, hc],
                                                            start=False, stop=True), r=[ATak, Vtm], w=[PD])
                    P.act(lambda e: e.activation(out=Gb[:, :], in_=PD[:, :], func=AF.Copy), r=[PD], w=[Gb])
                    for h in range(8):
                        hc = slice(h * 64, (h + 1) * 64)
                        P.pe(lambda e, h=h, hc=hc: e.matmul(PA[:, hc], lhsT=XT[:, h, :], rhs=Gb[:, hc], start=True, stop=True),
                             r=[XT, Gb], w=[PA])
                    P.act(lambda e: e.activation(out=Ub[:, :], in_=PA[:, 0:512], func=AF.Copy), r=[PA], w=[Ub])
                    for h in range(8):
                        m = h // 2
                        pr = slice((h % 2) * 64, (h % 2) * 64 + 64)
                        hc = slice(h * 64, (h + 1) * 64)
                        P.pe(lambda e, m=m, h=h, hc=hc: e.matmul(PB[:, hc], lhsT=ART[:, m, 1, tb], rhs=Hbz[:, h, :],
                                                                 start=True, stop=False), r=[ART, Hbz], w=[PB])
                        P.pe(lambda e, h=h, hc=hc: e.matmul(PB[:, hc], lhsT=ATab[:, h, 128:256], rhs=Ub[:, hc],
                                                            start=False, stop=False), r=[ATab, Ub], w=[PB])
                        P.pe(lambda e, h=h, hc=hc: e.matmul(PB[:, hc], lhsT=ATak[:, h, 128:256], rhs=Vtm[:, hc],
                                                            start=False, stop=True), r=[ATak, Vtm], w=[PB])
                    for m in range(4):
                        ms = slice(m * 128, (m + 1) * 128)
                        P.pe(lambda e, ms=ms: e.matmul(PC[:, ms], lhsT=Bhtm[:, ms], rhs=Ub[:, ms], start=True, stop=False),
                             r=[Bhtm, Ub], w=[PC])
                        P.pe(lambda e, ms=ms: e.matmul(PC[:, ms], lhsT=Khtm[:, ms], rhs=Vtm[:, ms], start=False, stop=True),
                             r=[Khtm, Vtm], w=[PC])
                    for hh in range(2):
                        pr = slice(hh * 64, hh * 64 + 64)
                        src = PC[pr, 0:512].rearrange("p (m c) -> p m c", m=4)[:, :, hh * 64:hh * 64 + 64]
                        P.dve(lambda e, pr=pr, src=src: e.tensor_tensor(out=Hs[pr, :, :], in0=Hd[pr, :, :], in1=src, op=ALU.add),
                              r=[Hd, PC], w=[Hs])
                    for hh in range(2):
                        prr = slice(hh * 64, hh * 64 + 64)
                        P.act(lambda e, hh=hh, prr=prr: e.activation(out=Hbz[prr, hh:8:2, :], in_=Hs[prr, :, :], func=AF.Copy),
                              r=[Hs], w=[Hbz])
                    P.act(lambda e: e.activation(out=Ysb[:, :], in_=PB[:, 0:512], func=AF.Copy), r=[PB], w=[Ysb])
                    P.act(lambda e: e.activation(out=Ysq[:, :], in_=PB[:, 0:512], func=AF.Square), r=[PB], w=[Ysq])
                    P.dve(lambda e: e.tensor_reduce(out=gst[:, 0:8], in_=Ysb[:, :].rearrange("p (h i) -> p h i", h=8),
                                                    axis=AX.X, op=ALU.add), r=[Ysb], w=[gst])
                    P.dve(lambda e: e.tensor_reduce(out=gst[:, 8:16], in_=Ysq[:, :].rearrange("p (h i) -> p h i", h=8),
                                                    axis=AX.X, op=ALU.add), r=[Ysq, gst], w=[gst])
                    P.dve(lambda e: e.tensor_scalar(out=gst[:, 16:24], in0=gst[:, 0:8], scalar1=1.0 / 64, scalar2=None,
                                                    op0=ALU.mult), r=[gst], w=[gst])
                    P.dve(lambda e: e.tensor_tensor(out=gst[:, 24:32], in0=gst[:, 16:24], in1=gst[:, 16:24], op=ALU.mult),
                          r=[gst], w=[gst])
                    P.dve(lambda e: e.scalar_tensor_tensor(out=gst[:, 32:40], in0=gst[:, 8:16], scalar=1.0 / 64,
                                                           in1=gst[:, 24:32], op0=ALU.mult, op1=ALU.subtract),
                          r=[gst], w=[gst])
                    P.act(lambda e: e.activation(out=gst[:, 40:48], in_=gst[:, 32:40], func=AF.Sqrt, bias=epsc[:, 1:2]),
                          r=[gst, epsc], w=[gst])
                    P.dve(lambda e: e.reciprocal(out=gst[:, 40:48], in_=gst[:, 40:48]), r=[gst], w=[gst])
                    y3 = lambda t: t[:, :].rearrange("p (h i) -> p h i", h=8)
                    P.dve(lambda e: e.tensor_tensor(out=y3(Ysb), in0=y3(Ysb),
                                                    in1=gst[:, 16:24].unsqueeze(2).to_broadcast([128, 8, 64]), op=ALU.subtract),
                          r=[Ysb, gst], w=[Ysb])
                    P.dve(lambda e: e.tensor_tensor(out=y3(Ysb), in0=y3(Ysb),
                                                    in1=gst[:, 40:48].unsqueeze(2).to_broadcast([128, 8, 64]), op=ALU.mult),
                          r=[Ysb, gst], w=[Ysb])
                    P.dve(lambda e: e.tensor_tensor(out=Ysb[:, :], in0=Ysb[:, :], in1=lnrow[:, 0, :], op=ALU.mult),
                          r=[Ysb, lnrow], w=[Ysb])
                    P.dve(lambda e: e.tensor_tensor(out=Ysb[:, :], in0=Ysb[:, :], in1=lnrow[:, 1, :], op=ALU.add),
                          r=[Ysb, lnrow], w=[Ysb])
                    for m in range(4):
                        P.pe(lambda e, m=m: e.matmul(PD[:, 2 * m:2 * m + 2], lhsT=rkT[:, m, tb], rhs=hindb[:, :],
                                                     start=True, stop=True), r=[rkT, hindb], w=[PD])
                    P.act(lambda e: e.activation(out=gst[:, 0:8], in_=PD[:, 0:8], func=AF.Copy), r=[PD, gst], w=[gst])
                    P.dve(lambda e: e.tensor_tensor(out=y3(Ysq), in0=y3(vtok),
                                                    in1=gst[:, 0:8].unsqueeze(2).to_broadcast([128, 8, 64]), op=ALU.mult),
                          r=[vtok, gst], w=[Ysq])
                    P.dve(lambda e: e.tensor_tensor(out=Ysb[:, :], in0=Ysb[:, :], in1=Ysq[:, :], op=ALU.add),
                          r=[Ysb, Ysq], w=[Ysb])
                    P.pe(lambda e: e.matmul(PA[:, 512:1024], lhsT=sgd[:, 0, tb], rhs=w_gateb[:, 0, :], start=True, stop=False),
                         r=[sgd, w_gateb], w=[PA])
                    P.pe(lambda e: e.matmul(PA[:, 512:1024], lhsT=sgd[:, 1, tb], rhs=w_gateb[:, 1, :], start=False, stop=True),
                         r=[sgd, w_gateb], w=[PA])
                    P.dve(lambda e: e.tensor_tensor(out=yrw[:, :], in0=Ysb[:, :], in1=PA[:, 512:1024], op=ALU.mult),
                          r=[Ysb, PA], w=[yrw])
                    dump("yrw", yrw[:, :], yrw, dbg_d["yrw"][ti * 128:(ti + 1) * 128, :] if dbg else None)
                    P.act(lambda e: e.activation(out=yrwb[:, :], in_=yrw[:, :], func=AF.Copy), r=[yrw], w=[yrwb])
                    for m in range(4):
                        P.pe(lambda e, m=m: e.transpose(PT[:, m * 128:(m + 1) * 128], yrwb[:, m * 128:(m + 1) * 128], identb[:, :]),
                             r=[yrwb, identb], w=[PT])
                    P.act(lambda e: e.activation(out=ycat[:, :, :].rearrange("p m t -> p (m t)"), in_=PT[:, 0:512], func=AF.Copy),
                          r=[PT], w=[ycat])
                    P.dma(yaf[:, :], yscr[ti * 128:(ti + 1) * 128, :], "yld", r=[yscr_t], w=[yaf])
                    P.act(lambda e: e.activation(out=ya[:, :, :].rearrange("p m t -> p (m t)"), in_=yaf[:, :], func=AF.Copy),
                          r=[yaf], w=[ya])
                    for c in range(2):
                        cs_ = slice(c * 512, (c + 1) * 512)
                        for k in range(8):
                            lhs = (lambda k=k: ya[:, k, :]) if k < 4 else (lambda k=k: ycat[:, k - 4, :])
                            P.pe(lambda e, k=k, cs_=cs_, lhs=lhs: e.matmul(PC[:, cs_], lhsT=lhs(), rhs=w_outb[:, k, cs_],
                                                                           start=(k == 0), stop=(k == 7)),
                                 r=[ya, ycat, w_outb], w=[PC])
                    P.act(lambda e: e.activation(out=mix[:, :], in_=PC[:, :], func=AF.Copy), r=[PC], w=[mix])
                    P.act(lambda e: e.activation(out=sqj[:, :], in_=PC[:, :], func=AF.Square, accum_out=st4[:, 6:7]),
                          r=[PC], w=[sqj, st4])
                    P.act(lambda e: e.activation(out=st4[:, 7:8], in_=st4[:, 6:7], func=AF.Sqrt, scale=1.0 / D,
                                                 bias=epsc[:, 0:1]), r=[st4, epsc], w=[st4])
                    P.dve(lambda e: e.reciprocal(out=st4[:, 7:8], in_=st4[:, 7:8]), r=[st4], w=[st4])
                    xo = x1s[0]
                    P.dve(lambda e: e.scalar_tensor_tensor(out=mix[:, :], in0=mix[:, :], scalar=st4[:, 7:8], in1=GMrow[:, :],
                                                           op0=ALU.mult, op1=ALU.mult), r=[mix, st4, GMrow], w=[mix])
                    P.dve(lambda e, xo=xo: e.tensor_tensor(out=xo[:, :], in0=mix[:, :], in1=xt[:, j, :], op=ALU.add),
                          r=[mix, xt], w=[xo])
                    P.dma(out_d[ti * 128:(ti + 1) * 128, :], xo[:, :], "x1st", r=[xo], w=[x1t], q="sp")
                    dump("x1", xo[:, :], xo, dbg_d["x1"][ti * 128:(ti + 1) * 128, :] if dbg else None)

        print("ops after A2", P.nops)
        P.fence()
        if upto < 3:
            P.enabled = False
        outt = Tl(P, None, "out_hbm")
        with ExitStack() as es3:
            P.es_cur = es3
            w_fgb = P.tile([128, 8, DFF], BF16, "w_fgb")
            w_fub = P.tile([128, 8, DFF], BF16, "w_fub")
            w_fdb = P.tile([128, 22, D], BF16, "w_fdb")
            wst = [P.tile([128, 1024], F32, "wst3%d" % i) for i in range(2)]
            i = 0
            jobs = []
            for (wd_, wb) in ((wfg_d, w_fgb), (wfu_d, w_fub)):
                for k in range(8):
                    for (c0, c1) in ((0, 1024), (1024, 2048), (2048, DFF)):
                        jobs.append((wd_, wb, k, c0, c1))
            for k in range(22):
                jobs.append((wfd_d, w_fdb, k, 0, D))
            for (wd_, wb, k, c0, c1) in jobs:
                ws = wst[i % 2]
                P.dma(ws[:, 0:c1 - c0], wd_[k * 128:(k + 1) * 128, c0:c1], "wst3%d" % (i % 2), w=[ws])
                if i % 2 == 0:
                    P.act(lambda e, ws=ws, wb=wb, k=k, c0=c0, c1=c1: e.activation(out=wb[:, k, c0:c1], in_=ws[:, 0:c1 - c0],
                                                                                 func=AF.Copy), r=[ws], w=[wb])
                else:
                    P.dve(lambda e, ws=ws, wb=wb, k=k, c0=c0, c1=c1: e.tensor_copy(out=wb[:, k, c0:c1], in_=ws[:, 0:c1 - c0]),
                          r=[ws], w=[wb])
                i += 1
            xt = P.tile([128, 2, D], F32, "xt3")
            sqj = P.tile([128, D], BF16, "sqj3")
            st4 = P.tile([128, 8], F32, "st43")
            dgt = P.tile([128, 2, 128], F32, "dgt3")
            hfT = P.tile([128, 8, 256], BF16, "hfT")
            sg = [P.tile([128, 256], F32, "sg%d" % i) for i in range(2)]
            aT = P.tile([128, 22, 256], BF16, "aT")
            fo = P.tile([128, D], F32, "fo")
            ost = [P.tile([128, D], F32, "ost%d" % i) for i in range(1)]
            print("SBUFREM B", nc.sbuf_bytes_remaining)
            for s in range(NS):
                for j in range(2):
                    r0 = s * 256 + j * 128
                    P.dma(xt[:, j, :], out_d[r0:r0 + 128, :], "xin3", r=[x1t], w=[xt])
                for j in range(2):
                    P.act(lambda e, j=j: e.activation(out=sqj[:, :], in_=xt[:, j, :], func=AF.Square,
                                                      accum_out=st4[:, j:j + 1]), r=[xt], w=[sqj, st4])
                P.act(lambda e: e.activation(out=st4[:, 2:4], in_=st4[:, 0:2], func=AF.Sqrt, scale=1.0 / D,
                                             bias=epsc[:, 0:1]), r=[st4, epsc], w=[st4])
                P.dve(lambda e: e.reciprocal(out=st4[:, 4:6], in_=st4[:, 2:4]), r=[st4], w=[st4])
                for j in range(2):
                    P.dve(lambda e, j=j: e.tensor_scalar(out=dgt[:, j, :], in0=cst[:, C_ID:C_ID + 128],
                                                         scalar1=st4[:, 4 + j:5 + j], scalar2=None, op0=ALU.mult),
                          r=[cst, st4], w=[dgt])
                for kk in range(2):
                    for k4 in range(4):
                        k = kk * 4 + k4
                        for j in range(2):
                            P.pe(lambda e, k=k, k4=k4, j=j: e.matmul(
                                PA[:, k4 * 256 + j * 128:k4 * 256 + (j + 1) * 128],
                                lhsT=xt[:, j, k * 128:(k + 1) * 128], rhs=dgt[:, j, :], start=True, stop=True),
                                r=[xt, dgt], w=[PA])
                    for k4 in range(4):
                        k = kk * 4 + k4
                        P.dve(lambda e, k=k, k4=k4: e.tensor_scalar(
                            out=hfT[:, k, :], in0=PA[:, k4 * 256:(k4 + 1) * 256], scalar1=gsh[:, 8 + k:9 + k],
                            scalar2=modc[:, 24 + k:25 + k], op0=ALU.mult, op1=ALU.add), r=[PA, gsh, modc], w=[hfT])
                for n in range(22):
                    ns = slice(n * 128, (n + 1) * 128)
                    pg = PB if n % 2 == 0 else PC
                    for k in range(8):
                        P.pe(lambda e, k=k, ns=ns, pg=pg: e.matmul(pg[:, 0:256], lhsT=w_fgb[:, k, ns], rhs=hfT[:, k, :],
                                                                   start=(k == 0), stop=(k == 7)), r=[w_fgb, hfT], w=[pg])
                    for k in range(8):
                        P.pe(lambda e, k=k, ns=ns, pg=pg: e.matmul(pg[:, 256:512], lhsT=w_fub[:, k, ns], rhs=hfT[:, k, :],
                                                                   start=(k == 0), stop=(k == 7)), r=[w_fub, hfT], w=[pg])
                    sgt = sg[n % 2]
                    P.act(lambda e, pg=pg, sgt=sgt: e.activation(out=sgt[:, :], in_=pg[:, 0:256], func=AF.Silu), r=[pg], w=[sgt])
                    P.dve(lambda e, pg=pg, sgt=sgt, n=n: e.tensor_tensor(out=aT[:, n, :], in0=sgt[:, :], in1=pg[:, 256:512],
                                                                         op=ALU.mult), r=[sgt, pg], w=[aT])
                for j in range(2):
                    ti = s * 2 + j
                    for c in range(2):
                        cs_ = slice(c * 512, (c + 1) * 512)
                        for n in range(22):
                            P.pe(lambda e, n=n, cs_=cs_, j=j: e.matmul(PA[:, cs_], lhsT=aT[:, n, j * 128:(j + 1) * 128],
                                                                       rhs=w_fdb[:, n, cs_], start=(n == 0), stop=(n == 21)),
                                 r=[aT, w_fdb], w=[PA])
                    P.act(lambda e: e.activation(out=fo[:, :], in_=PA[:, :], func=AF.Copy), r=[PA], w=[fo])
                    P.act(lambda e: e.activation(out=sqj[:, :], in_=PA[:, :], func=AF.Square, accum_out=st4[:, 6:7]),
                          r=[PA], w=[sqj, st4])
                    P.act(lambda e: e.activation(out=st4[:, 7:8], in_=st4[:, 6:7], func=AF.Sqrt, scale=1.0 / D,
                                                 bias=epsc[:, 0:1]), r=[st4, epsc], w=[st4])
                    P.dve(lambda e: e.reciprocal(out=st4[:, 7:8], in_=st4[:, 7:8]), r=[st4], w=[st4])
                    oo = ost[0]
                    P.dve(lambda e: e.scalar_tensor_tensor(out=fo[:, :], in0=fo[:, :], scalar=st4[:, 7:8], in1=GFrow[:, :],
                                                           op0=ALU.mult, op1=ALU.mult), r=[fo, st4, GFrow], w=[fo])
                    P.dve(lambda e, oo=oo, j=j: e.tensor_tensor(out=oo[:, :], in0=fo[:, :], in1=xt[:, j, :], op=ALU.add),
                          r=[fo, xt], w=[oo])
                    P.dma(out_d[ti * 128:(ti + 1) * 128, :], oo[:, :], "ost", r=[oo], w=[outt], q="sp")
            P.enabled = True
            P.wait_all("sp", ["ost", "x1st", "dbg"] + [k for k in P.streams if k not in ("ost", "x1st", "dbg")])
            P.wait_all("pool", ["ost"])

            with nc.Block() as block:
                @block.sync
                def _(e):
                    P.replay("sp", e)

                @block.tensor
                def _(e):
                    P.replay("pe", e)

                @block.scalar
                def _(e):
                    P.replay("act", e)

                @block.vector
                def _(e):
                    P.replay("dve", e)

                @block.gpsimd
                def _(e):
                    P.replay("pool", e)
    return nc, list(dbg_d.keys())


def t5_bucket_np(rel):
    rel = np.asarray(rel)
    max_exact = 16
    nf = np.maximum(rel, 1).astype(np.float32)
    large = max_exact + (np.log(nf / np.float32(max_exact)) / np.float32(math.log(128 / max_exact))
                         * np.float32(32 - max_exact)).astype(np.int32)
    large = np.minimum(large, 31)
    return np.where(rel < max_exact, rel, large)


def make_consts():
    c = np.zeros((128, C_END), np.float32)
    p = np.arange(128)[:, None]
    f = np.arange(128)[None, :]
    c[:, C_ID:C_ID + 128] = (p == f)
    c[:, C_ONE:C_ONE + 128] = 1.0
    c[:, C_BLK:C_BLK + 128] = ((p // 64) == (f // 64))
    c[:, C_MSU:C_MSU + 128] = (f > p)
    c[:, C_MUI:C_MUI + 128] = (f >= p)
    c[:, C_MSL:C_MSL + 128] = (f < p)
    c[:, C_NEG:C_NEG + 128] = np.where(f > p, -1e30, 0.0)
    c[:, C_J:C_J + 128] = (p + f == 127)
    c[31, C_SEL:C_SEL + 128] = 1.0
    bk = t5_bucket_np(np.arange(256))
    c[0:32, C_OH:C_OH + 256] = (np.arange(32)[:, None] == bk[None, :])
    sm = np.ones((128, 256), np.float32)
    sm[:, 0] = 0.0
    sm[:, 128] = 0.0
    c[:, C_SCAN:C_SCAN + 256] = sm
    c[:, C_HIND] = (np.arange(128) < 64)
    c[:, C_HIND + 1] = (np.arange(128) >= 64)
    return c


def col8(v):
    return np.ascontiguousarray(v.reshape(-1, 128).T)


def prep_shared(inp):
    f32 = np.float32
    g = lambda k: np.asarray(inp[k], f32)
    w_in = g("w_in")[0]
    sh = {}
    sh["ada_w"] = np.ascontiguousarray(g("ada_w")[0])
    sh["cst"] = make_consts()
    sh["w_att"] = np.ascontiguousarray(np.concatenate([w_in[:, 0:384], w_in[:, 384:448], w_in[:, 384:448]], axis=1))
    sh["w_iw"] = np.ascontiguousarray(w_in[:, 448:456])
    sh["w_rw"] = np.ascontiguousarray(w_in[:, 456:])
    wiq = g("w_idx_q")[0]
    sh["w_iq"] = np.ascontiguousarray(np.concatenate([wiq, wiq], axis=2).reshape(256, 1024))
    sh["w_uq"] = np.ascontiguousarray(g("w_uq")[0].reshape(256, 512))
    wuk = g("w_uk")[0]
    t = np.zeros((128, 8, 128), f32)
    for h in range(8):
        t[(h % 2) * 64:(h % 2) * 64 + 64, h, :] = wuk[h].T
    sh["w_ukT"] = t.reshape(128, 1024)
    wuv = g("w_uv")[0]
    t = np.zeros((128, 8, 128), f32)
    for h in range(8):
        t[:, h, (h % 2) * 64:(h % 2) * 64 + 64] = wuv[h]
    sh["w_uv"] = t.reshape(128, 1024)
    sh["rel_bias"] = np.ascontiguousarray(g("rel_bias"))
    t = np.zeros((128, 1024), f32)
    t[0:64, 0:512] = g("w_decay_up")[0]
    t[64:128, 512:1024] = g("w_aaa_up")[0]
    sh["w_lora"] = t
    sh["w_gate"] = np.ascontiguousarray(g("w_gate_up")[0])
    sh["lnrow"] = np.ascontiguousarray(np.stack([g("ln_x_gain")[0], g("ln_x_bias")[0]], axis=0))
    sh["w_out"] = np.ascontiguousarray(g("w_out")[0])
    sh["w_fg"] = np.ascontiguousarray(g("w_ffn_gate")[0])
    sh["w_fu"] = np.ascontiguousarray(g("w_ffn_up")[0])
    sh["w_fd"] = np.ascontiguousarray(g("w_ffn_down")[0])
    vec = np.zeros((128, 128), f32)
    vec[:, 0:8] = col8(g("mix_pre_norm")[0])
    vec[:, 8:16] = col8(g("mix_post_norm")[0])
    vec[:, 16:24] = col8(g("ffn_pre_norm")[0])
    vec[:, 24:32] = col8(g("ffn_post_norm")[0])
    vec[:, 32:80] = g("ada_b")[0].reshape(48, 128).T
    vec[:, 80:82] = g("q_norm")[0].reshape(2, 128).T
    vec[:, 82] = g("kv_norm")[0]
    vec[:, 83] = np.concatenate([g("idx_k_norm")[0], g("idx_k_norm")[0]])
    vec[:, 84:88] = g("w0")[0].reshape(4, 128).T
    vec[:, 88:92] = g("a0")[0].reshape(4, 128).T
    vec[:, 92:96] = g("k_k")[0].reshape(4, 128).T
    vec[:, 96:100] = g("k_a")[0].reshape(4, 128).T
    vec[:, 100:104] = g("r_k")[0].reshape(4, 128).T
    mus = g("mu_shift")[0]
    vec[:, 108:122] = mus[:14 * 128].reshape(14, 128).T
    vec[:, 122] = mus[1824 - 128:1824]
    sh["vecs"] = vec
    return sh


def kernel(**inputs):
    x = np.asarray(inputs["x"], np.float32)
    c = np.asarray(inputs["c"], np.float32)
    B, T, _ = x.shape
    sh = prep_shared(inputs)
    nc, _ = build(T)
    in_maps = []
    for b in range(B):
        m = dict(sh)
        m["x"] = np.ascontiguousarray(x[b])
        m["ccol"] = col8(c[b])
        in_maps.append(m)
    res = run_bass_kernel_spmd(nc, in_maps, core_ids=list(range(B)))
    return np.stack([np.asarray(r["out"], np.float32) for r in res.results], axis=0)
```

```python
import math
from contextlib import ExitStack
import numpy as np
import concourse.bass as bass
import concourse.mybir as mybir
from concourse.bass_utils import run_bass_kernel_spmd

F32 = mybir.dt.float32
BF16 = mybir.dt.bfloat16
AF = mybir.ActivationFunctionType
ALU = mybir.AluOpType
AX = mybir.AxisListType

D = 1024
DFF = 2816
NB_IT = 20
BIS_R = 16.0
LWC = math.exp(-0.5)

C_ID, C_ONE, C_BLK, C_MSU, C_MUI, C_MSL, C_NEG, C_J, C_SEL, C_OH, C_SCAN, C_HIND, C_END = (
    0, 128, 256, 384, 512, 640, 768, 896, 1024, 1152, 1408, 1664, 1668)


class Tl:
    def __init__(self, P, h, name):
        self.h = h
        self.name = name
        self.lw = None
        self.rd = dict(P.fence_tokens)

    def __getitem__(self, i):
        return self.h[i]


class TlG:
    def __init__(self, P, h, name, n):
        self.h = h
        self.name = name
        self.p = [Tl(P, h, "%s_%d" % (name, i)) for i in range(n)]

    def __getitem__(self, i):
        return self.h[i]


class Bank:
    def __init__(self, tl, h, lo):
        self.tl = tl
        self.h = h
        self.lo = lo

    def ap(self, a, b):
        return self.h[:, self.lo + a:self.lo + b]


def _flat(ts):
    out = []
    for t in ts:
        if isinstance(t, TlG):
            out.extend(t.p)
        else:
            out.append(t)
    return out


class Rec:
    def __init__(self):
        self.call = None

    def __getattr__(self, name):
        def f(*a, **k):
            self.call = (name, a, k)
            return self
        return f


class Stream:
    def __init__(self, key):
        self.key = key
        self.count = 0
        self.mark = -1


class Prog:
    ENGS = ("pe", "act", "dve", "pool", "sp")

    def __init__(self, nc, es):
        self.nc = nc
        self.es = es
        self.q = {e: [] for e in self.ENGS}
        self.cnt = {e: 0 for e in self.ENGS}
        self.waited = {e: {} for e in self.ENGS}
        self.semh = {}
        self.streams = {}
        self.fence_tokens = {}
        self.enabled = True
        self.nops = 0
        import os as _os
        self.printops = bool(_os.environ.get("PRINTOPS"))
        self.maxops = int(_os.environ.get("MAXOPS", "100000000"))
        for e in self.ENGS:
            self.semh[e] = es.enter_context(nc.semaphore("s_" + e))

    def stream(self, key):
        if key not in self.streams:
            self.streams[key] = Stream(key)
            self.semh[key] = self.es.enter_context(self.nc.semaphore("d_" + key))
        return self.streams[key]

    def tile(self, shape, dt, name):
        h = self.es_cur.enter_context(self.nc.sbuf_tensor("sb_" + name, list(shape), dt))
        return Tl(self, h, name)

    def fence(self):
        ft = {}
        for e in self.ENGS:
            if self.cnt[e] > 0:
                ft[e] = (e, self.cnt[e], e, False)
        for k, st in self.streams.items():
            if st.count > 0:
                ft[k] = (k, st.count, None, True)
        self.fence_tokens = ft

    def emit(self, eng, fn, r=(), w=(), stream=None):
        if not self.enabled:
            return None
        self.nops += 1
        if self.nops > self.maxops:
            return None
        r = _flat(r)
        w = _flat(w)
        rec = Rec()
        fn(rec)
        fn = rec.call
        assert fn is not None
        if self.printops:
            print("OP", self.nops, eng, fn[0], [t.name for t in r], "->", [t.name for t in w])
        need = {}

        def add(tok, kind):
            key, val, teng, isdma = tok
            if isdma:
                val = self.streams[key].count
                self.streams[key].mark = val
            elif stream is None and teng == eng:
                if eng == "pe":
                    return
            if need.get(key, 0) < val:
                need[key] = val

        for t in r:
            if t.lw is not None:
                add(t.lw, "raw")
        for t in w:
            if t.lw is not None:
                add(t.lw, "waw")
            for tok in t.rd.values():
                add(tok, "war")
        if stream is not None:
            st0 = self.stream(stream)
            if st0.mark == st0.count and st0.count > 0:
                need[st0.key] = st0.count
        for key, val in need.items():
            if self.waited[eng].get(key, 0) < val:
                self.waited[eng][key] = val
                self.q[eng].append(("w", key, val))
        if stream is None:
            self.cnt[eng] += 1
            tok = (eng, self.cnt[eng], eng, False)
            self.q[eng].append(("op", fn, eng, 1))
        else:
            st = self.stream(stream)
            st.count += 16
            tok = (st.key, st.count, None, True)
            self.q[eng].append(("op", fn, st.key, 16))
        for t in w:
            t.lw = tok
            t.rd = {}
        for t in r:
            if t not in w:
                t.rd[tok[0]] = tok
        return tok

    def wait_all(self, eng, keys):
        for key in keys:
            if key in self.streams:
                val = self.streams[key].count
            elif key in self.cnt:
                val = self.cnt[key]
            else:
                continue
            if val > 0 and self.waited[eng].get(key, 0) < val:
                self.waited[eng][key] = val
                self.q[eng].append(("w", key, val))

    def replay(self, eng, e):
        for ent in self.q[eng]:
            if ent[0] == "w":
                e.wait_ge(self.semh[ent[1]], ent[2])
            else:
                name, a, k = ent[1]
                ins = getattr(e, name)(*a, **k)
                ins.then_inc(self.semh[ent[2]], ent[3])

    def pe(self, fn, r=(), w=()):
        return self.emit("pe", fn, r, w)

    def act(self, fn, r=(), w=()):
        return self.emit("act", fn, r, w)

    def dve(self, fn, r=(), w=()):
        return self.emit("dve", fn, r, w)

    def pool(self, fn, r=(), w=()):
        return self.emit("pool", fn, r, w)

    def dma(self, out, in_, stream, r=(), w=(), q="sp"):
        return self.emit(q, lambda e: e.dma_start(out=out, in_=in_), r, w, stream=stream)


def build(T, dbg=False, upto=3):
    NT = T // 128
    NS = T // 256
    KTOP = min(256, T // 4)
    nc = bass.Bass("TRN2", target_bir_lowering=False)

    def din(name, shape):
        return nc.dram_tensor(name, list(shape), F32, kind="ExternalInput").ap()

    x_d = din("x", [T, D])
    ccol_d = din("ccol", [128, 8])
    adaw_d = din("ada_w", [D, 6 * D])
    vec_d = din("vecs", [128, 128])
    adab_d = din("adab_row", [1, 6 * D])
    cst_d = din("cst", [128, C_END])
    watt_d = din("w_att", [D, 512])
    wiw_d = din("w_iw", [D, 8])
    wrw_d = din("w_rw", [D, 1824])
    wiq_d = din("w_iq", [256, 1024])
    wuq_d = din("w_uq", [256, 512])
    wuk_d = din("w_ukT", [128, 1024])
    wuv_d = din("w_uv", [128, 1024])
    relb_d = din("rel_bias", [32, 8])
    wlora_d = din("w_lora", [128, 1024])
    wgate_d = din("w_gate", [160, 512])
    lnrow_d = din("lnrow", [2, 512])
    wout_d = din("w_out", [D, D])
    wfg_d = din("w_fg", [D, DFF])
    wfu_d = din("w_fu", [D, DFF])
    wfd_d = din("w_fd", [DFF, D])
    out_d = nc.dram_tensor("out", [T, D], F32, kind="ExternalOutput").ap()
    dscr = nc.dram_tensor("dscr", [8, 384], F32, kind="Internal").ap()
    dbg_d = {}

    def ddbg(name, shape):
        if dbg:
            dbg_d[name] = nc.dram_tensor("dbg_" + name, list(shape), F32, kind="ExternalOutput").ap()

    ddbg("modc", [128, 48])
    ddbg("bias", [128, 3 * 1024])
    ddbg("yatt", [128, 4 * T])
    ddbg("thr", [128, NT])
    ddbg("yrw", [T, 512])
    ddbg("x1", [T, D])

    with ExitStack() as es:
        P = Prog(nc, es)
        P.es_cur = es
        def pst(name, shape, dt):
            return Tl(P, es.enter_context(nc.psum_tensor("ps_" + name, list(shape), dt)), name)
        def pstg(name, shape, dt, n):
            return TlG(P, es.enter_context(nc.psum_tensor("ps_" + name, list(shape), dt)), name, n)
        PA = pstg("PA", [128, 1024], F32, 2)
        PB = pstg("PB", [128, 1024], F32, 2)
        PC = pstg("PC", [128, 1024], F32, 2)
        PD = pst("PD", [128, 512], F32)
        PT = pstg("PT", [128, 1024], BF16, 8)

        cst = P.tile([128, C_END], F32, "cst")
        vec = P.tile([128, 128], F32, "vec")
        modc = P.tile([128, 48], F32, "modc")
        gsh = P.tile([128, 48], F32, "gsh")
        GMrow = P.tile([128, D], F32, "GMrow")
        GFrow = P.tile([128, D], F32, "GFrow")
        identb = P.tile([128, 128], BF16, "identb")
        onesb = P.tile([128, 128], BF16, "onesb")
        epsc = P.tile([128, 4], F32, "epsc")
        yscr = nc.dram_tensor("yscr", [NT * 128, 512], F32, kind="ExternalOutput").ap()
        yscr_t = Tl(P, None, "yscr")

        ident = lambda: cst[:, C_ID:C_ID + 128]
        ones32 = lambda: cst[:, C_ONE:C_ONE + 128]

        P.dma(cst[:, :], cst_d[:, :], "cw", w=[cst])
        P.dma(vec[:, :], vec_d[:, :], "cw", w=[vec])
        P.dve(lambda e: e.tensor_copy(out=identb[:, :], in_=cst[:, C_ID:C_ID + 128]), r=[cst], w=[identb])
        P.dve(lambda e: e.tensor_copy(out=onesb[:, :], in_=cst[:, C_ONE:C_ONE + 128]), r=[cst], w=[onesb])
        P.dve(lambda e: e.memset(epsc[:, 0:1], 1e-6), w=[epsc])
        P.dve(lambda e: e.memset(epsc[:, 1:2], 64e-5), w=[epsc])
        P.dve(lambda e: e.memset(epsc[:, 2:3], 0.0), w=[epsc])
        V_MPRE, V_MPOST, V_FPRE, V_FPOST, V_ADAB, V_QN, V_KVN, V_IKN, V_W0, V_A0, V_KK, V_KA, V_RK = (
            0, 8, 16, 24, 32, 80, 82, 83, 84, 88, 92, 96, 100)

        def dump(name, src_ap, tl, dst_ap=None):
            if dbg:
                P.dma(dbg_d[name][:, :] if dst_ap is None else dst_ap, src_ap, "dbg", r=[tl])

        with ExitStack() as es0:
            P.es_cur = es0
            ccol = P.tile([128, 8], F32, "ccol")
            scol = P.tile([128, 8], F32, "scol")
            stg = [P.tile([128, 8, 512], F32, "adastg%d" % i) for i in range(4)]
            dg = P.tile([128, 8, 128], F32, "dg")
            P.dma(ccol[:, :], ccol_d[:, :], "cw", w=[ccol])
            P.act(lambda e: e.activation(out=scol[:, :], in_=ccol[:, :], func=AF.Silu), r=[ccol], w=[scol])
            adabr = P.tile([1, 6 * D], F32, "adabr")
            modrow = P.tile([1, 6 * D], F32, "modrow")
            P.dma(adabr[:, :], adab_d[:, :], "cw", w=[adabr])
            p0b = [Bank(PD, PD.h, 0), Bank(PC.p[0], PC.h, 0)]
            for s in range(12):
                st = stg[s % 4]
                P.dma(st[:, :, :], adaw_d[:, s * 512:(s + 1) * 512].rearrange("(k p) n -> p k n", p=128),
                      "ada%d" % (s % 4), w=[st])
                bk = p0b[s % 2]
                for k in range(8):
                    P.pe(lambda e: e.matmul(bk.h[0:1, bk.lo:bk.lo + 512], lhsT=scol[:, k:k + 1], rhs=st[:, k, :],
                                            start=(k == 0), stop=(k == 7)), r=[st, scol], w=[bk.tl])
                P.dve(lambda e: e.tensor_tensor(out=modrow[0:1, s * 512:(s + 1) * 512], in0=bk.h[0:1, bk.lo:bk.lo + 512],
                                                in1=adabr[0:1, s * 512:(s + 1) * 512], op=ALU.add),
                      r=[bk.tl, adabr], w=[modrow])
            for m in range(48):
                P.pe(lambda e: e.matmul(PB[:, m:m + 1], lhsT=modrow[0:1, m * 128:(m + 1) * 128],
                                        rhs=cst[0:1, C_ONE:C_ONE + 1], start=True, stop=True),
                     r=[modrow, cst], w=[PB.p[0]])
            P.dve(lambda e: e.tensor_copy(out=modc[:, :], in_=PB[:, 0:48]), r=[PB.p[0]], w=[modc])
            dump("modc", modc[:, :], modc)
            P.dve(lambda e: e.tensor_scalar(out=gsh[:, 32:40], in0=modc[:, 8:16], scalar1=1.0, scalar2=None,
                                            op0=ALU.add), r=[modc], w=[gsh])
            P.dve(lambda e: e.tensor_scalar(out=gsh[:, 40:48], in0=modc[:, 32:40], scalar1=1.0, scalar2=None,
                                            op0=ALU.add), r=[modc, gsh], w=[gsh])
            P.dve(lambda e: e.tensor_tensor(out=gsh[:, 0:8], in0=gsh[:, 32:40], in1=vec[:, V_MPRE:V_MPRE + 8],
                                            op=ALU.mult), r=[gsh, vec], w=[gsh])
            P.dve(lambda e: e.tensor_tensor(out=gsh[:, 8:16], in0=gsh[:, 40:48], in1=vec[:, V_FPRE:V_FPRE + 8],
                                            op=ALU.mult), r=[gsh, vec], w=[gsh])
            P.dve(lambda e: e.tensor_tensor(out=gsh[:, 16:24], in0=modc[:, 16:24], in1=vec[:, V_MPOST:V_MPOST + 8],
                                            op=ALU.mult), r=[gsh, modc, vec], w=[gsh])
            P.dve(lambda e: e.tensor_tensor(out=gsh[:, 24:32], in0=modc[:, 40:48], in1=vec[:, V_FPOST:V_FPOST + 8],
                                            op=ALU.mult), r=[gsh, modc, vec], w=[gsh])
            for (c0, row) in ((16, GMrow), (24, GFrow)):
                for k in range(8):
                    P.dve(lambda e, c0=c0, k=k: e.tensor_scalar(
                        out=dg[:, k, :], in0=cst[:, C_ID:C_ID + 128], scalar1=gsh[:, c0 + k:c0 + k + 1],
                        scalar2=None, op0=ALU.mult), r=[cst, gsh], w=[dg])
                for k in range(8):
                    P.pe(lambda e, k=k: e.matmul(PA[:, k * 128:(k + 1) * 128], lhsT=cst[:, C_ONE:C_ONE + 128],
                                                 rhs=dg[:, k, :], start=True, stop=True), r=[cst, dg], w=[PA])
                P.act(lambda e, row=row: e.activation(out=row[:, :], in_=PA[:, :], func=AF.Copy), r=[PA], w=[row])

        print("ops after P0", P.nops)
        P.fence()
        if upto < 1:
            P.enabled = False
        with ExitStack() as es1:
            P.es_cur = es1
            w_att = P.tile([128, 8, 512], F32, "w_att")
            w_iw = P.tile([128, 8, 8], F32, "w_iw")
            w_iq = P.tile([128, 2, 1024], F32, "w_iq")
            w_uqb = P.tile([128, 2, 512], BF16, "w_uqb")
            w_ukb = P.tile([128, 8, 128], BF16, "w_ukb")
            w_uvb = P.tile([128, 8, 128], BF16, "w_uvb")
            relb = P.tile([32, 8], F32, "relb")
            biasT = P.tile([128, 3, 1024], F32, "biasT")
            ckvT = P.tile([128, T], BF16, "ckvT")
            ckvtm = P.tile([128, NT, 128], BF16, "ckvtm")
            ikA = P.tile([128, T], BF16, "ikA")
            ikB = P.tile([128, T], BF16, "ikB")
            P.dve(lambda e: e.memset(ikB[:, :], 0.0), w=[ikB])
            es1b = ExitStack()
            P.es_cur = es1b
            wst = P.tile([128, 1024], F32, "wst1")
            P.dma(w_att[:, :, :], watt_d.rearrange("(k p) n -> p k n", p=128), "cw", w=[w_att])
            P.dma(w_iw[:, :, :], wiw_d.rearrange("(k p) n -> p k n", p=128), "cw", w=[w_iw])
            P.dma(w_iq[:, :, :], wiq_d.rearrange("(k p) n -> p k n", p=128), "cw", w=[w_iq])
            P.dma(relb[:, :], relb_d[:, :], "cw", w=[relb])
            P.dma(wst[:, :].rearrange("p (k n) -> p k n", k=2), wuq_d.rearrange("(k p) n -> p k n", p=128), "wst", w=[wst])
            P.dve(lambda e: e.tensor_copy(out=w_uqb[:, :, :], in_=wst[:, :].rearrange("p (k n) -> p k n", k=2)),
                  r=[wst], w=[w_uqb])
            P.dma(wst[:, :], wuk_d[:, :], "wst", w=[wst])
            P.dve(lambda e: e.tensor_copy(out=w_ukb[:, :, :], in_=wst[:, :].rearrange("p (k n) -> p k n", k=8)),
                  r=[wst], w=[w_ukb])
            P.dma(wst[:, :], wuv_d[:, :], "wst", w=[wst])
            P.dve(lambda e: e.tensor_copy(out=w_uvb[:, :, :], in_=wst[:, :].rearrange("p (k n) -> p k n", k=8)),
                  r=[wst], w=[w_uvb])
            brel = P.tile([8, 384], F32, "brel")
            relx = P.tile([32, 8, 128], F32, "relx")
            qtl = P.tile([128, 2, 1024], F32, "qtl")
            P.pe(lambda e: e.matmul(PD[0:8, 0:256], lhsT=relb[:, :], rhs=cst[0:32, C_OH:C_OH + 256],
                                    start=True, stop=True), r=[relb, cst], w=[PD])
            P.dve(lambda e: e.memset(brel[:, :], 0.0), w=[brel])
            P.dve(lambda e: e.tensor_copy(out=brel[:, 127:383], in_=PD[0:8, 0:256]), r=[PD, brel], w=[brel])
            dsc = Tl(P, None, "dscr")
            P.dma(dscr[:, :], brel[:, :], "cw", r=[brel], w=[dsc])
            for dl in range(2):
                src = bass.AP(tensor=dscr.tensor, offset=128 * dl, ap=[[1, 128], [384, 8], [1, 128]])
                P.dma(qtl[:, dl, :].rearrange("p (h t) -> p h t", h=8), src, "cw", r=[dsc], w=[qtl])
            for dl in range(2):
                for c in range(2):
                    P.pe(lambda e, dl=dl, c=c: e.matmul(PA[:, c * 512:(c + 1) * 512], lhsT=cst[:, C_J:C_J + 128],
                                                        rhs=qtl[:, dl, c * 512:(c + 1) * 512], start=True, stop=True),
                         r=[cst, qtl], w=[PA])
                P.act(lambda e, dl=dl: e.activation(out=biasT[:, dl, :], in_=PA[:, :], func=AF.Copy), r=[PA], w=[biasT])
            for h in range(8):
                P.dve(lambda e, h=h: e.tensor_scalar(out=relx[:, h, :], in0=cst[0:32, C_ONE:C_ONE + 128],
                                                     scalar1=relb[:, h:h + 1], scalar2=None, op0=ALU.mult),
                      r=[cst, relb], w=[relx])
            for c in range(2):
                P.pe(lambda e, c=c: e.matmul(PA[:, c * 512:(c + 1) * 512], lhsT=cst[0:32, C_SEL:C_SEL + 128],
                                             rhs=relx[:, c * 4:(c + 1) * 4, :], start=True, stop=True),
                     r=[cst, relx], w=[PA])
            P.act(lambda e: e.activation(out=biasT[:, 2, :], in_=PA[:, :], func=AF.Copy), r=[PA], w=[biasT])
            dump("bias", biasT[:, :, :].rearrange("p a b -> p (a b)"), biasT)
            es1b.close()
            P.es_cur = es1
            P.fence()

            xt = P.tile([128, 2, D], F32, "xt")
            sqj = P.tile([128, D], BF16, "sqj")
            st4 = P.tile([128, 8], F32, "st4")
            dgt = P.tile([128, 2, 128], F32, "dgt")
            hT = P.tile([128, 8, 256], F32, "hT")
            cqraw = P.tile([128, 4, 256], F32, "cqraw")
            sq = P.tile([128, 4, 256], F32, "sq")
            rq = P.tile([128, 3, 256], F32, "rq")
            cqT = P.tile([128, 2, 256], F32, "cqT")
            cqTb = P.tile([128, 2, 256], BF16, "cqTb")
            iqT = P.tile([128, 8, 256], BF16, "iqP")
            iqtmp = P.tile([128, 4, 256], BF16, "iqtmp")
            ikf = P.tile([128, 256], F32, "ikf")
            iw = P.tile([128, 2, 8], F32, "iw")
            qTb = P.tile([128, 4, 256], BF16, "qTb")
            qaT2 = [P.tile([128, 8, 256], BF16, "qaT%d" % i) for i in range(2)]
            sc = TlG(P, es1.enter_context(nc.sbuf_tensor("sb_sc", [128, T], F32)), "sc", max(T // 512, 1))
            mk = [P.tile([128, T], BF16, "mk%d" % i) for i in range(2)]
            rl = [P.tile([128, 512], F32, "rl%d" % i) for i in range(3)]
            bsn = P.tile([128, 1], F32, "bsn")
            bss = P.tile([128, 1], F32, "bss")
            bsu = P.tile([128, 1], F32, "bsu")
            lg = [P.tile([128, 512], F32, "lg%d" % i) for i in range(3)]
            Ee = [P.tile([128, 512], BF16, "Ee%d" % i) for i in range(3)]
            Em = [P.tile([128, 512], BF16, "Em%d" % i) for i in range(3)]
            rD = P.tile([128, 1024], F32, "rD")
            oTb = P.tile([128, 8, 128], BF16, "oTb")
            thrs = P.tile([128, NT], F32, "thrs")
            ytl = [P.tile([128, 4, 128], F32, "ytl%d" % i) for i in range(2)]
            ydb = P.tile([128, 4, 128], F32, "ydb") if dbg else None
            print("SBUFREM A1", nc.sbuf_bytes_remaining)
            rot = [Bank(PA.p[0], PA.h, 0), Bank(PA.p[1], PA.h, 512), Bank(PD, PD.h, 0)]
            pending = [None, 0]

            def emit_S(qi, j):
                S = (qi + 1) * 128
                ncc = (S + 511) // 512
                idx = 0
                for h in range(8):
                    for cc in range(ncc):
                        wd = min(512, S - cc * 512)
                        bk = rot[idx % 3]
                        rb = rl[idx % 3]
                        idx += 1
                        scp = sc.p[cc]
                        P.pe(lambda e: e.matmul(bk.ap(0, wd), lhsT=iqT[:, h, j * 128:(j + 1) * 128],
                                                rhs=ikA[:, cc * 512:cc * 512 + wd], start=True, stop=False),
                             r=[iqT, ikA], w=[bk.tl])
                        P.pe(lambda e: e.matmul(bk.ap(0, wd), lhsT=iqT[:, h, j * 128:(j + 1) * 128],
                                                rhs=ikB[:, cc * 512:cc * 512 + wd], start=False, stop=True),
                             r=[iqT, ikB], w=[bk.tl])
                        P.act(lambda e: e.activation(out=rb[:, 0:wd], in_=bk.ap(0, wd), func=AF.Relu), r=[bk.tl], w=[rb])
                        if h == 0:
                            P.dve(lambda e: e.tensor_scalar(out=sc[:, cc * 512:cc * 512 + wd], in0=rb[:, 0:wd],
                                                            scalar1=iw[:, j, 0:1], scalar2=None, op0=ALU.mult),
                                  r=[rb, iw], w=[scp])
                        else:
                            P.dve(lambda e: e.scalar_tensor_tensor(out=sc[:, cc * 512:cc * 512 + wd], in0=rb[:, 0:wd],
                                                                   scalar=iw[:, j, h:h + 1],
                                                                   in1=sc[:, cc * 512:cc * 512 + wd], op0=ALU.mult, op1=ALU.add),
                                  r=[rb, iw, scp], w=[scp])
                scd = sc.p[(qi * 128) // 512]
                P.dve(lambda e: e.tensor_tensor(out=sc[:, qi * 128:(qi + 1) * 128], in0=sc[:, qi * 128:(qi + 1) * 128],
                                                in1=cst[:, C_NEG:C_NEG + 128], op=ALU.add), r=[scd, cst], w=[scd])

            def gen_B(qi):
                S = (qi + 1) * 128
                ncc = (S + 511) // 512
                scr = sc.p[0:ncc]
                mkb = mk[qi % 2]
                thr_c = float(2 * KTOP - S) - 0.5
                P.pool(lambda e: e.memset(bsn[:, :], 0.0), w=[bsn])
                for it in range(NB_IT):
                    ck = BIS_R / (2 ** it)
                    cn = ck / 2 if it < NB_IT - 1 else ck
                    P.act(lambda e: e.activation(out=mkb[:, 0:S], in_=sc[:, 0:S], func=AF.Sign, bias=bsn[:, 0:1],
                                                 accum_out=bss[:, 0:1]), r=scr + [bsn], w=[mkb, bss])
                    P.pool(lambda e: e.tensor_scalar(out=bsu[:, :], in0=bss[:, :], scalar1=thr_c, scalar2=-ck,
                                                     op0=ALU.is_ge, op1=ALU.mult), r=[bss], w=[bsu])
                    P.pool(lambda e: e.tensor_scalar(out=bsn[:, :], in0=bsu[:, :], scalar1=bsn[:, 0:1], scalar2=cn,
                                                     op0=ALU.add, op1=ALU.add), r=[bsu, bsn], w=[bsn])
                    yield
                P.act(lambda e: e.activation(out=mkb[:, 0:S], in_=sc[:, 0:S], func=AF.Sign, bias=bsn[:, 0:1]),
                      r=scr + [bsn], w=[mkb])
                if dbg:
                    P.dve(lambda e: e.tensor_scalar(out=thrs[:, qi:qi + 1], in0=bsn[:, 0:1], scalar1=-1.0, scalar2=None,
                                                    op0=ALU.mult), r=[bsn], w=[thrs])
                yield

            def gen_P(qi, j, qb):
                mkb = mk[qi % 2]
                qa = qaT2[qb]
                steps = [(kj, c) for kj in range(qi + 1) for c in range(2)]
                n = len(steps)

                def slot_of(kj):
                    k2 = kj % 2
                    return PT, PT[:, k2 * 512:k2 * 512 + 128]

                for i in range(n + 3):
                    if i < n:
                        kj, c = steps[i]
                        dl = min(qi - kj, 2)
                        bk = rot[i % 3]
                        if c == 0:
                            pts, ptap = slot_of(kj)
                            P.pe(lambda e: e.transpose(ptap, mkb[:, kj * 128:(kj + 1) * 128], identb[:, :]),
                                 r=[mkb, identb], w=[pts])
                        P.pe(lambda e: e.matmul(bk.ap(0, 512), lhsT=ckvT[:, kj * 128:(kj + 1) * 128],
                                                rhs=qa[:, c * 4:(c + 1) * 4, j * 128:(j + 1) * 128], start=True, stop=True),
                             r=[ckvT, qa], w=[bk.tl])
                    if 0 <= i - 3 < n:
                        kj, c = steps[i - 3]
                        emt = Em[(i - 3) % 3]
                        P.pe(lambda e: e.matmul(PB[:, c * 512:(c + 1) * 512], lhsT=ckvtm[:, kj, :], rhs=emt[:, :],
                                                start=(kj == 0), stop=(kj == qi)), r=[ckvtm, emt], w=[PB.p[c]])
                        P.pe(lambda e: e.matmul(PC[:, c * 512:(c + 1) * 512], lhsT=onesb[:, :], rhs=emt[:, :],
                                                start=(kj == 0), stop=(kj == qi)), r=[onesb, emt], w=[PC.p[c]])
                    if 0 <= i - 2 < n:
                        kj, c = steps[i - 2]
                        pts, ptap = slot_of(kj)
                        eet, emt = Ee[(i - 2) % 3], Em[(i - 2) % 3]
                        P.dve(lambda e: e.scalar_tensor_tensor(
                            out=emt[:, :].rearrange("p (h t) -> p h t", h=4),
                            in0=ptap.unsqueeze(1).to_broadcast([128, 4, 128]), scalar=1.0,
                            in1=eet[:, :].rearrange("p (h t) -> p h t", h=4), op0=ALU.add, op1=ALU.mult),
                            r=[eet, pts], w=[emt])
                    if i < n:
                        kj, c = steps[i]
                        dl = min(qi - kj, 2)
                        bk = rot[i % 3]
                        lgt = lg[i % 3]
                        P.dve(lambda e: e.tensor_tensor(out=lgt[:, :], in0=bk.ap(0, 512),
                                                        in1=biasT[:, dl, c * 512:(c + 1) * 512], op=ALU.add),
                              r=[bk.tl, biasT], w=[lgt])
                    if 0 <= i - 1 < n:
                        lgt, eet = lg[(i - 1) % 3], Ee[(i - 1) % 3]
                        P.act(lambda e: e.activation(out=eet[:, :], in_=lgt[:, :], func=AF.Exp), r=[lgt], w=[eet])
                    yield
                hs = n
                P.dve(lambda e: e.reciprocal(out=rD[:, :], in_=PC[:, :]), r=[PC], w=[rD])
                P.dve(lambda e: e.tensor_tensor(out=oTb[:, :, :].rearrange("p h t -> p (h t)"), in0=PB[:, :],
                                                in1=rD[:, :], op=ALU.mult), r=[PB, rD], w=[oTb])
                bk = rot[hs % 3]
                for m in range(4):
                    for hh in range(2):
                        h = 2 * m + hh
                        P.pe(lambda e: e.matmul(bk.ap(m * 128, (m + 1) * 128), lhsT=w_uvb[:, h, :], rhs=oTb[:, h, :],
                                                start=(hh == 0), stop=(hh == 1)), r=[w_uvb, oTb], w=[bk.tl])
                yt_ = ytl[qi % 2]
                P.act(lambda e: e.activation(out=yt_[:, :, :], in_=bk.ap(0, 512).rearrange("p (m t) -> p m t", m=4),
                                             func=AF.Copy), r=[bk.tl], w=[yt_])
                P.dma(yscr[qi * 128:(qi + 1) * 128, :], yt_[:, :, :].rearrange("p m t -> p (m t)"), "yst",
                      r=[yt_], w=[yscr_t], q="sp")
                if dbg:
                    P.dve(lambda e: e.tensor_copy(out=ydb[:, :, :], in_=bk.ap(0, 512).rearrange("p (m t) -> p m t", m=4)),
                          r=[bk.tl], w=[ydb])
                    dump("yatt", ydb[:, :, :], ydb,
                         dbg_d["yatt"][:, :].rearrange("p (m t) -> p m t", m=4)[:, :, qi * 128:(qi + 1) * 128])
                yield

            def interleave(ga, gb, na, nb):
                ia = ib = 0
                da = db = False
                while not (da and db):
                    if not da and (db or ia * nb <= ib * na):
                        try:
                            next(ga)
                            ia += 1
                        except StopIteration:
                            da = True
                    else:
                        try:
                            next(gb)
                            ib += 1
                        except StopIteration:
                            db = True

            for s in range(NS):
                for j in range(2):
                    r0 = s * 256 + j * 128
                    P.dma(xt[:, j, :], x_d[r0:r0 + 128, :], "xin", w=[xt])
                for j in range(2):
                    P.act(lambda e, j=j: e.activation(out=sqj[:, :], in_=xt[:, j, :], func=AF.Square,
                                                      accum_out=st4[:, j:j + 1]), r=[xt], w=[sqj, st4])
                P.act(lambda e: e.activation(out=st4[:, 2:4], in_=st4[:, 0:2], func=AF.Sqrt, scale=1.0 / D,
                                             bias=epsc[:, 0:1]), r=[st4, epsc], w=[st4])
                P.dve(lambda e: e.reciprocal(out=st4[:, 4:6], in_=st4[:, 2:4]), r=[st4], w=[st4])
                for j in range(2):
                    P.dve(lambda e, j=j: e.tensor_scalar(out=dgt[:, j, :], in0=cst[:, C_ID:C_ID + 128],
                                                         scalar1=st4[:, 4 + j:5 + j], scalar2=None, op0=ALU.mult),
                          r=[cst, st4], w=[dgt])
                for kk in range(2):
                    for k4 in range(4):
                        k = kk * 4 + k4
                        for j in range(2):
                            P.pe(lambda e, k=k, k4=k4, j=j: e.matmul(
                                PA[:, k4 * 256 + j * 128:k4 * 256 + (j + 1) * 128],
                                lhsT=xt[:, j, k * 128:(k + 1) * 128], rhs=dgt[:, j, :], start=True, stop=True),
                                r=[xt, dgt], w=[PA])
                    for k4 in range(4):
                        k = kk * 4 + k4
                        P.dve(lambda e, k=k, k4=k4: e.tensor_scalar(
                            out=hT[:, k, :], in0=PA[:, k4 * 256:(k4 + 1) * 256], scalar1=gsh[:, k:k + 1],
                            scalar2=modc[:, k:k + 1], op0=ALU.mult, op1=ALU.add), r=[PA, gsh, modc], w=[hT])
                for c in range(4):
                    for k in range(8):
                        P.pe(lambda e, c=c, k=k: e.matmul(PB[:, c * 256:(c + 1) * 256],
                                                          lhsT=w_att[:, k, c * 128:(c + 1) * 128], rhs=hT[:, k, :],
                                                          start=(k == 0), stop=(k == 7)), r=[w_att, hT], w=[PB])
                P.act(lambda e: e.activation(out=cqraw[:, :, :], in_=PB[:, :].rearrange("p (c t) -> p c t", c=4),
                                             func=AF.Copy), r=[PB], w=[cqraw])
                P.act(lambda e: e.activation(out=sq[:, :, :], in_=PB[:, :].rearrange("p (c t) -> p c t", c=4),
                                             func=AF.Square), r=[PB], w=[sq])
                for j in range(2):
                    for k in range(8):
                        P.pe(lambda e, j=j, k=k: e.matmul(PD[:, j * 8:(j + 1) * 8], lhsT=hT[:, k, j * 128:(j + 1) * 128],
                                                          rhs=w_iw[:, k, :], start=(k == 0), stop=(k == 7)),
                             r=[hT, w_iw], w=[PD])
                P.act(lambda e: e.activation(out=iw[:, :, :], in_=PD[:, 0:16].rearrange("p (j h) -> p j h", j=2),
                                             func=AF.Copy, scale=float(8 ** -0.5 * 64 ** -0.5)), r=[PD], w=[iw])
                for c in range(2):
                    P.pe(lambda e, c=c: e.matmul(PC[:, 0:256], lhsT=cst[:, C_ONE:C_ONE + 128], rhs=sq[:, c, :],
                                                 start=(c == 0), stop=(c == 1)), r=[cst, sq], w=[PC])
                P.pe(lambda e: e.matmul(PC[:, 256:512], lhsT=cst[:, C_ONE:C_ONE + 128], rhs=sq[:, 2, :],
                                        start=True, stop=True), r=[cst, sq], w=[PC])
                P.pe(lambda e: e.matmul(PC[:, 512:768], lhsT=cst[:, C_BLK:C_BLK + 128], rhs=sq[:, 3, :],
                                        start=True, stop=True), r=[cst, sq], w=[PC])
                for i, dv in enumerate((256.0, 128.0, 64.0)):
                    P.act(lambda e, i=i, dv=dv: e.activation(out=rq[:, i, :], in_=PC[:, i * 256:(i + 1) * 256],
                                                             func=AF.Sqrt, scale=1.0 / dv, bias=epsc[:, 0:1]),
                          r=[PC, epsc], w=[rq])
                P.dve(lambda e: e.reciprocal(out=rq[:, :, :], in_=rq[:, :, :]), r=[rq], w=[rq])
                for c in range(2):
                    P.dve(lambda e, c=c: e.scalar_tensor_tensor(out=cqT[:, c, :], in0=cqraw[:, c, :],
                                                                scalar=vec[:, V_QN + c:V_QN + c + 1], in1=rq[:, 0, :],
                                                                op0=ALU.mult, op1=ALU.mult), r=[cqraw, vec, rq], w=[cqT])
                P.act(lambda e: e.activation(out=cqTb[:, :, :], in_=cqT[:, :, :], func=AF.Copy), r=[cqT], w=[cqTb])
                P.dve(lambda e, s=s: e.scalar_tensor_tensor(out=ckvT[:, s * 256:(s + 1) * 256], in0=cqraw[:, 2, :],
                                                            scalar=vec[:, V_KVN:V_KVN + 1], in1=rq[:, 1, :],
                                                            op0=ALU.mult, op1=ALU.mult), r=[cqraw, vec, rq], w=[ckvT])
                P.dve(lambda e: e.scalar_tensor_tensor(out=ikf[:, :], in0=cqraw[:, 3, :],
                                                       scalar=vec[:, V_IKN:V_IKN + 1], in1=rq[:, 2, :],
                                                       op0=ALU.mult, op1=ALU.mult), r=[cqraw, vec, rq], w=[ikf])
                P.act(lambda e, s=s: e.activation(out=ikA[:, s * 256:(s + 1) * 256], in_=ikf[:, :], func=AF.Copy),
                      r=[ikf], w=[ikA])
                P.dve(lambda e, s=s: e.tensor_tensor(out=ikB[0:64, s * 256:(s + 1) * 256], in0=ikf[0:64, :],
                                                     in1=ikA[0:64, s * 256:(s + 1) * 256], op=ALU.subtract),
                      r=[ikf, ikA], w=[ikB])
                for j in range(2):
                    tix = s * 2 + j
                    P.pe(lambda e, j=j, tix=tix: e.transpose(PT[:, j * 128:(j + 1) * 128],
                                                             ckvT[:, tix * 128:(tix + 1) * 128], identb[:, :]),
                         r=[ckvT, identb], w=[PT])
                P.act(lambda e, s=s: e.activation(out=ckvtm[:, 2 * s:2 * s + 2, :],
                                                  in_=PT[:, 0:256].rearrange("p (j c) -> p j c", j=2), func=AF.Copy),
                      r=[PT], w=[ckvtm])
                for hf in range(2):
                    for h4 in range(4):
                        h = hf * 4 + h4
                        for c in range(2):
                            P.pe(lambda e, h=h, h4=h4, c=c: e.matmul(PB[:, h4 * 256:(h4 + 1) * 256],
                                                                     lhsT=w_iq[:, c, h * 128:(h + 1) * 128], rhs=cqT[:, c, :],
                                                                     start=(c == 0), stop=(c == 1)), r=[w_iq, cqT], w=[PB])
                    hsl = slice(hf * 4, hf * 4 + 4)
                    pb3 = lambda rows: PB[rows, :].rearrange("p (m t) -> p m t", m=4)
                    P.act(lambda e, hsl=hsl: e.activation(out=iqT[0:64, hsl, :], in_=pb3(slice(0, 64)), func=AF.Copy),
                          r=[PB], w=[iqT])
                    P.act(lambda e: e.activation(out=iqtmp[64:128, :, :], in_=pb3(slice(64, 128)), func=AF.Copy),
                          r=[PB], w=[iqtmp])
                    P.dve(lambda e, hsl=hsl: e.tensor_tensor(out=iqT[64:128, hsl, :], in0=pb3(slice(64, 128)),
                                                             in1=iqtmp[64:128, :, :], op=ALU.subtract),
                          r=[PB, iqtmp], w=[iqT])
                for m in range(4):
                    for c in range(2):
                        P.pe(lambda e, m=m, c=c: e.matmul(PC[:, m * 256:(m + 1) * 256],
                                                          lhsT=w_uqb[:, c, m * 128:(m + 1) * 128], rhs=cqTb[:, c, :],
                                                          start=(c == 0), stop=(c == 1)), r=[w_uqb, cqTb], w=[PC])
                P.dve(lambda e: e.tensor_copy(out=qTb[:, :, :], in_=PC[:, :].rearrange("p (m t) -> p m t", m=4)),
                      r=[PC], w=[qTb])
                for hf in range(2):
                    for h4 in range(4):
                        h = hf * 4 + h4
                        pr = slice((h % 2) * 64, (h % 2) * 64 + 64)
                        P.pe(lambda e, h=h, h4=h4, pr=pr: e.matmul(PB[:, h4 * 256:(h4 + 1) * 256],
                                                                   lhsT=w_ukb[:, h, :], rhs=qTb[:, h // 2, :],
                                                                   start=True, stop=True), r=[w_ukb, qTb], w=[PB])
                    P.act(lambda e, hf=hf, s=s: e.activation(out=qaT2[s % 2][:, hf * 4:(hf + 1) * 4, :],
                                                             in_=PB[:, :].rearrange("p (h t) -> p h t", h=4), func=AF.Copy,
                                                             scale=0.125), r=[PB], w=[qaT2[s % 2]])
                for j in range(2):
                    qi = s * 2 + j
                    emit_S(qi, j)
                    gB = gen_B(qi)
                    if pending[0] is not None:
                        interleave(gB, pending[0], NB_IT + 1, pending[1])
                    else:
                        for _ in gB:
                            pass
                    pending[0] = gen_P(qi, j, s % 2)
                    pending[1] = 2 * (qi + 1) + 4
            for _ in pending[0]:
                pass
            if dbg:
                dump("thr", thrs[:, :], thrs)

        print("ops after A1", P.nops)
        P.fence()
        if upto < 2:
            P.enabled = False
        x1t = Tl(P, None, "x1_hbm")
        with ExitStack() as es2:
            P.es_cur = es2
            w_rwb = P.tile([128, 8, 1824], BF16, "w_rwb")
            w_lorab = P.tile([128, 1024], BF16, "w_lorab")
            w_gateb = P.tile([128, 2, 512], BF16, "w_gateb")
            w_outb = P.tile([128, 8, D], BF16, "w_outb")
            mucol = P.tile([128, 32], F32, "mucol")
            lnrow = P.tile([128, 2, 512], F32, "lnrow")
            wst = P.tile([128, 2048], F32, "wst2")
            P.dve(lambda e: e.tensor_copy(out=mucol[:, 0:15], in_=vec[:, 108:123]), r=[vec], w=[mucol])
            P.dve(lambda e: e.tensor_scalar(out=mucol[:, 16:31], in0=vec[:, 108:123], scalar1=-1.0, scalar2=1.0,
                                            op0=ALU.mult, op1=ALU.add), r=[vec, mucol], w=[mucol])
            for k in range(8):
                P.dma(wst[:, 0:1824], wrw_d[k * 128:(k + 1) * 128, :], "wst", w=[wst])
                P.act(lambda e, k=k: e.activation(out=w_rwb[:, k, :], in_=wst[:, 0:1824], func=AF.Copy), r=[wst], w=[w_rwb])
            for k in range(8):
                P.dma(wst[:, 0:D], wout_d[k * 128:(k + 1) * 128, :], "wst", w=[wst])
                P.act(lambda e, k=k: e.activation(out=w_outb[:, k, :], in_=wst[:, 0:D], func=AF.Copy), r=[wst], w=[w_outb])
            P.dma(wst[:, 0:1024], wlora_d[:, :], "wst", w=[wst])
            P.act(lambda e: e.activation(out=w_lorab[:, :], in_=wst[:, 0:1024], func=AF.Copy), r=[wst], w=[w_lorab])
            P.dve(lambda e: e.memset(w_gateb[:, :, :], 0.0), w=[w_gateb])
            P.dma(wst[:, 0:512], wgate_d[0:128, :], "wst", w=[wst])
            P.act(lambda e: e.activation(out=w_gateb[:, 0, :], in_=wst[:, 0:512], func=AF.Copy), r=[wst], w=[w_gateb])
            P.dma(wst[96:128, 0:512], wgate_d[128:160, :], "wst", w=[wst])
            P.act(lambda e: e.activation(out=w_gateb[96:128, 1, :], in_=wst[96:128, 0:512], func=AF.Copy), r=[wst], w=[w_gateb])
            for i in range(2):
                P.dma(lnrow[:, i, :], lnrow_d[i, :].partition_broadcast(128), "cw", w=[lnrow])

            xt = P.tile([128, 2, D], F32, "xt2")
            sqj = P.tile([128, D], BF16, "sqj2")
            st4 = P.tile([128, 8], F32, "st42")
            dgt = P.tile([128, 2, 128], F32, "dgt2")
            hTb = P.tile([128, 8, 256], BF16, "hTb")
            lastc = TlG(P, es2.enter_context(nc.sbuf_tensor("sb_lastc", [128, 16], F32)), "lastc", 16)
            ptmp = [P.tile([128, 256], F32, "ptmp%d" % i) for i in range(3)]
            pch = [P.tile([128, 256], F32, "pch%d" % i) for i in range(3)]
            rT = P.tile([128, 4, 256], F32, "rT")
            kT = P.tile([128, 4, 256], F32, "kT")
            vTb = P.tile([128, 4, 256], BF16, "vTb")
            twd = P.tile([128, 256], BF16, "twd")
            sgd = P.tile([128, 2, 256], BF16, "sgd")
            ART = P.tile([128, 4, 2, 256], BF16, "ART")
            btT = P.tile([128, 8, 256], BF16, "btT")
            ktT = P.tile([128, 8, 256], BF16, "ktT")
            Hbz = P.tile([128, 8, 64], BF16, "Hbz")
            P.dve(lambda e: e.memset(btT[:, :, :], 0.0), w=[btT])
            P.dve(lambda e: e.memset(ktT[:, :, :], 0.0), w=[ktT])
            P.dve(lambda e: e.memset(Hbz[:, :, :], 0.0), w=[Hbz])
            bhT = P.tile([128, 4, 256], BF16, "bhT")
            khT = P.tile([128, 4, 256], BF16, "khT")
            rkT = P.tile([128, 4, 256], BF16, "rkT")
            WC = P.tile([128, 4, 2], F32, "WC")
            fsets = [[P.tile([128, 256], F32, "f%d_%d" % (q, i)) for i in range(9)] for q in range(2)]
            kk4 = P.tile([128, 4, 256], F32, "kk4")
            sq4 = P.tile([128, 4, 256], F32, "sq4")
            gtmp = P.tile([128, 256], F32, "gtmp")
            hw = P.tile([128, 8], F32, "hw")
            P.dve(lambda e: e.tensor_scalar(out=hw[:, :], in0=vec[:, V_W0:V_W0 + 8], scalar1=0.5, scalar2=None, op0=ALU.mult),
                  r=[vec], w=[hw])
            Vtm = P.tile([128, 512], BF16, "Vtm")
            Bhtm = P.tile([128, 512], BF16, "Bhtm")
            Khtm = P.tile([128, 512], BF16, "Khtm")
            ATab = P.tile([128, 8, 256], BF16, "ATab")
            ATak = P.tile([128, 8, 256], BF16, "ATak")
            def tlg2(name):
                return TlG(P, es2.enter_context(nc.sbuf_tensor("sb_" + name, [128, 8, 128], BF16)), name, 2)
            Nk = [tlg2("Nk%d" % i) for i in range(2)]
            Mk = [tlg2("Mk%d" % i) for i in range(2)]
            Pk = [tlg2("Pk%d" % i) for i in range(2)]
            Hs = P.tile([128, 4, 64], F32, "Hs")
            Hb = P.tile([128, 4, 64], BF16, "Hb")
            Hd = P.tile([128, 4, 64], F32, "Hd")
            WCf = P.tile([128, 4, 64], F32, "WCf")
            Gb = P.tile([128, 512], BF16, "Gb")
            Ub = P.tile([128, 512], BF16, "Ub")
            Ysb = P.tile([128, 512], F32, "Ysb")
            Ysq = P.tile([128, 512], F32, "Ysq")
            gst = P.tile([128, 48], F32, "gst")
            vtok = P.tile([128, 512], F32, "vtok")
            yrw = P.tile([128, 512], F32, "yrw")
            yrwb = P.tile([128, 512], BF16, "yrwb")
            ycat = P.tile([128, 4, 128], BF16, "ycat")
            mix = P.tile([128, D], F32, "mix")
            x1s = [P.tile([128, D], F32, "x1s%d" % i) for i in range(1)]
            ya = P.tile([128, 4, 128], BF16, "ya")
            yaf = P.tile([128, 512], F32, "yaf")
            oka = P.tile([128, 4], F32, "oka")
            hindb = P.tile([128, 2], BF16, "hindb")
            P.dve(lambda e: e.tensor_scalar(out=oka[:, :], in0=vec[:, V_KA:V_KA + 4], scalar1=-1.0, scalar2=1.0,
                                            op0=ALU.mult, op1=ALU.add), r=[vec], w=[oka])
            P.dve(lambda e: e.tensor_copy(out=hindb[:, :], in_=cst[:, C_HIND:C_HIND + 2]), r=[cst], w=[hindb])
            P.dve(lambda e: e.memset(lastc[:, :], 0.0), w=[lastc])
            print("SBUFREM A2", nc.sbuf_bytes_remaining)
            P.dve(lambda e: e.memset(Hs[:, :, :], 0.0), w=[Hs])
            P.dve(lambda e: e.memset(sgd[:, :, :], 0.0), w=[sgd])

            iprot = [Bank(PD, PD.h, 0), Bank(PC.p[0], PC.h, 0), Bank(PC.p[1], PC.h, 512), Bank(PB.p[1], PB.h, 512)]

            def inproj(n, width=128):
                c0 = n * 128 if n < 14 else 1824 - 128
                bk = iprot[n % 4]
                for k in range(8):
                    P.pe(lambda e, k=k: e.matmul(bk.ap(0, 256), lhsT=w_rwb[:, k, c0:c0 + 128], rhs=hTb[:, k, :],
                                                 start=(k == 0), stop=(k == 7)), r=[w_rwb, hTb], w=[bk.tl])
                pt = ptmp[n % 3]
                pc = pch[n % 3]
                lc = lastc.p[n]
                P.act(lambda e: e.activation(out=pt[:, :], in_=bk.ap(0, 256), func=AF.Copy,
                                             scale=mucol[:, 16 + n:17 + n]), r=[bk.tl, mucol], w=[pt])
                P.dve(lambda e: e.scalar_tensor_tensor(out=pc[:, 1:256], in0=bk.ap(0, 255),
                                                       scalar=mucol[:, n:n + 1], in1=pt[:, 1:256],
                                                       op0=ALU.mult, op1=ALU.add), r=[bk.tl, mucol, pt], w=[pc])
                P.dve(lambda e: e.scalar_tensor_tensor(out=pc[:, 0:1], in0=lastc[:, n:n + 1],
                                                       scalar=mucol[:, n:n + 1], in1=pt[:, 0:1],
                                                       op0=ALU.mult, op1=ALU.add), r=[lc, mucol, pt, pc], w=[pc])
                P.dve(lambda e: e.tensor_copy(out=lastc[:, n:n + 1], in_=bk.ap(255, 256)), r=[bk.tl, lc], w=[lc])
                return pc

            for s in range(NS):
                for j in range(2):
                    r0 = s * 256 + j * 128
                    P.dma(xt[:, j, :], x_d[r0:r0 + 128, :], "xin", w=[xt])
                for j in range(2):
                    P.act(lambda e, j=j: e.activation(out=sqj[:, :], in_=xt[:, j, :], func=AF.Square,
                                                      accum_out=st4[:, j:j + 1]), r=[xt], w=[sqj, st4])
                P.act(lambda e: e.activation(out=st4[:, 2:4], in_=st4[:, 0:2], func=AF.Sqrt, scale=1.0 / D,
                                             bias=epsc[:, 0:1]), r=[st4, epsc], w=[st4])
                P.dve(lambda e: e.reciprocal(out=st4[:, 4:6], in_=st4[:, 2:4]), r=[st4], w=[st4])
                for j in range(2):
                    P.dve(lambda e, j=j: e.tensor_scalar(out=dgt[:, j, :], in0=cst[:, C_ID:C_ID + 128],
                                                         scalar1=st4[:, 4 + j:5 + j], scalar2=None, op0=ALU.mult),
                          r=[cst, st4], w=[dgt])
                for kk in range(2):
                    for k4 in range(4):
                        k = kk * 4 + k4
                        for j in range(2):
                            P.pe(lambda e, k=k, k4=k4, j=j: e.matmul(
                                PA[:, k4 * 256 + j * 128:k4 * 256 + (j + 1) * 128],
                                lhsT=xt[:, j, k * 128:(k + 1) * 128], rhs=dgt[:, j, :], start=True, stop=True),
                                r=[xt, dgt], w=[PA])
                    for k4 in range(4):
                        k = kk * 4 + k4
                        P.dve(lambda e, k=k, k4=k4: e.tensor_scalar(
                            out=hTb[:, k, :], in0=PA[:, k4 * 256:(k4 + 1) * 256], scalar1=gsh[:, k:k + 1],
                            scalar2=modc[:, k:k + 1], op0=ALU.mult, op1=ALU.add), r=[PA, gsh, modc], w=[hTb])
                for m in range(4):
                    pc = inproj(m)
                    P.act(lambda e, m=m, pc=pc: e.activation(out=rT[:, m, :], in_=pc[:, :], func=AF.Copy), r=[pc], w=[rT])
                for m in range(4):
                    pc = inproj(4 + m)
                    P.act(lambda e, m=m, pc=pc: e.activation(out=kT[:, m, :], in_=pc[:, :], func=AF.Copy), r=[pc], w=[kT])
                for m in range(4):
                    pc = inproj(8 + m)
                    P.act(lambda e, m=m, pc=pc: e.activation(out=vTb[:, m, :], in_=pc[:, :], func=AF.Copy), r=[pc], w=[vTb])
                pc = inproj(12)
                P.act(lambda e, pc=pc: e.activation(out=twd[0:64, :], in_=pc[0:64, :], func=AF.Tanh), r=[pc], w=[twd])
                P.act(lambda e, pc=pc: e.activation(out=twd[64:128, :], in_=pc[64:128, :], func=AF.Copy), r=[pc], w=[twd])
                pc = inproj(13)
                P.act(lambda e, pc=pc: e.activation(out=gtmp[:, :], in_=pc[:, :], func=AF.Tanh, scale=0.5), r=[pc], w=[gtmp])
                P.dve(lambda e: e.tensor_scalar(out=sgd[:, 0, :], in0=gtmp[:, :], scalar1=0.5, scalar2=0.5, op0=ALU.mult,
                                                op1=ALU.add), r=[gtmp], w=[sgd])
                pc = inproj(14)
                P.act(lambda e, pc=pc: e.activation(out=gtmp[:, :], in_=pc[:, :], func=AF.Tanh, scale=0.5), r=[pc], w=[gtmp])
                P.dve(lambda e: e.tensor_scalar(out=sgd[:, 1, :], in0=gtmp[:, :], scalar1=0.5, scalar2=0.5, op0=ALU.mult,
                                                op1=ALU.add), r=[gtmp], w=[sgd])
                for m in range(4):
                    P.dve(lambda e, m=m: e.tensor_scalar(out=kk4[:, m, :], in0=kT[:, m, :], scalar1=vec[:, V_KK + m:V_KK + m + 1],
                                                         scalar2=None, op0=ALU.mult), r=[kT, vec], w=[kk4])
                P.act(lambda e: e.activation(out=sq4[:, :, :], in_=kk4[:, :, :], func=AF.Square), r=[kk4], w=[sq4])
                for m in range(4):
                    P.pe(lambda e, m=m: e.matmul(PA[:, m * 256:(m + 1) * 256], lhsT=cst[:, C_BLK:C_BLK + 128], rhs=sq4[:, m, :],
                                                 start=True, stop=True), r=[cst, sq4], w=[PA])
                sq4f = sq4[:, :, :].rearrange("p m t -> p (m t)")
                P.act(lambda e: e.activation(out=sq4f, in_=PA[:, :], func=AF.Sqrt), r=[PA], w=[sq4])
                P.dve(lambda e: e.tensor_scalar(out=sq4f, in0=sq4f, scalar1=1e-12, scalar2=None, op0=ALU.max), r=[sq4], w=[sq4])
                P.dve(lambda e: e.reciprocal(out=sq4f, in_=sq4f), r=[sq4], w=[sq4])
                P.dve(lambda e: e.tensor_tensor(out=kk4[:, :, :], in0=kk4[:, :, :], in1=sq4[:, :, :], op=ALU.mult),
                      r=[kk4, sq4], w=[kk4])
                for m in range(4):
                    ms = slice(m * 128, (m + 1) * 128)
                    pg = PB if m % 2 == 0 else PC
                    P.pe(lambda e, m=m: e.matmul(pg[:, 0:256], lhsT=w_lorab[:, m * 128:(m + 1) * 128], rhs=twd[:, :],
                                                 start=True, stop=True), r=[w_lorab, twd], w=[pg.p[0]])
                    P.pe(lambda e, m=m: e.matmul(pg[:, 256:512], lhsT=w_lorab[:, 512 + m * 128:512 + (m + 1) * 128], rhs=twd[:, :],
                                                 start=True, stop=True), r=[w_lorab, twd], w=[pg.p[0]])
                    sgw, cs, wt, winv, wprev, asig, kp, t1, t2 = fsets[m % 2]
                    P.act(lambda e, m=m: e.activation(out=t1[:, :], in_=pg[:, 0:256], func=AF.Tanh, scale=0.5,
                                                      bias=hw[:, m:m + 1]), r=[pg.p[0], hw], w=[t1])
                    P.dve(lambda e: e.tensor_scalar(out=sgw[:, :], in0=t1[:, :], scalar1=0.5, scalar2=0.5, op0=ALU.mult,
                                                    op1=ALU.add), r=[t1], w=[sgw])
                    P.act(lambda e, m=m: e.activation(out=t2[:, :], in_=pg[:, 256:512], func=AF.Tanh, scale=0.5,
                                                      bias=hw[:, 4 + m:5 + m]), r=[pg.p[0], hw], w=[t2])
                    P.dve(lambda e: e.tensor_scalar(out=asig[:, :], in0=t2[:, :], scalar1=0.5, scalar2=0.5, op0=ALU.mult,
                                                    op1=ALU.add), r=[t2], w=[asig])
                    P.dve(lambda e: e.tensor_tensor_scan(out=cs[:, :], data0=cst[:, C_SCAN:C_SCAN + 256], data1=sgw[:, :],
                                                         initial=0.0, op0=ALU.mult, op1=ALU.add), r=[cst, sgw], w=[cs])
                    P.act(lambda e: e.activation(out=wt[:, :], in_=cs[:, :], func=AF.Exp, scale=-LWC), r=[cs], w=[wt])
                    P.act(lambda e: e.activation(out=winv[:, :], in_=cs[:, :], func=AF.Exp, scale=LWC), r=[cs], w=[winv])
                    P.dve(lambda e: e.tensor_tensor(out=t1[:, :], in0=cs[:, :], in1=sgw[:, :], op=ALU.subtract),
                          r=[cs, sgw], w=[t1])
                    P.act(lambda e: e.activation(out=wprev[:, :], in_=t1[:, :], func=AF.Exp, scale=-LWC), r=[t1], w=[wprev])
                    for j in range(2):
                        cj = slice(j * 128, (j + 1) * 128)
                        P.dve(lambda e, m=m, j=j: e.tensor_scalar(out=WC[:, m, j:j + 1], in0=cs[:, j * 128 + 127:j * 128 + 128],
                                                                  scalar1=-LWC, scalar2=None, op0=ALU.mult),
                              r=[cs], w=[WC])
                        P.act(lambda e, m=m, j=j, cj=cj: e.activation(out=t2[:, cj], in_=cs[:, cj], func=AF.Exp, scale=LWC,
                                                                      bias=WC[:, m, j:j + 1]), r=[cs, WC], w=[t2])
                    P.act(lambda e, m=m: e.activation(out=WC[:, m, :], in_=WC[:, m, :], func=AF.Exp), r=[WC], w=[WC])
                    P.dve(lambda e, m=m: e.tensor_scalar(out=t1[:, :], in0=asig[:, :], scalar1=vec[:, V_KA + m:V_KA + m + 1],
                                                         scalar2=oka[:, m:m + 1], op0=ALU.mult, op1=ALU.add),
                          r=[asig, vec, oka], w=[t1])
                    P.dve(lambda e, m=m: e.tensor_tensor(out=kp[:, :], in0=kT[:, m, :], in1=t1[:, :], op=ALU.mult),
                          r=[kT, t1], w=[kp])
                    P.dve(lambda e, m=m: e.scalar_tensor_tensor(out=rkT[:, m, :], in0=rT[:, m, :],
                                                                scalar=vec[:, V_RK + m:V_RK + m + 1], in1=kp[:, :],
                                                                op0=ALU.mult, op1=ALU.mult), r=[rT, vec, kp], w=[rkT])
                    P.pool(lambda e, m=m: e.tensor_tensor(out=ART[:, m, 1, :], in0=rT[:, m, :], in1=wt[:, :], op=ALU.mult),
                           r=[rT, wt], w=[ART])
                    P.dve(lambda e, m=m: e.scalar_tensor_tensor(out=ART[:, m, 0, :], in0=kk4[:, m, :], scalar=-1.0,
                                                                in1=wprev[:, :], op0=ALU.mult, op1=ALU.mult),
                          r=[kk4, wprev], w=[ART])
                    P.dve(lambda e, m=m: e.tensor_tensor(out=t1[:, :], in0=kk4[:, m, :], in1=asig[:, :], op=ALU.mult),
                          r=[kk4, asig], w=[t1])
                    for hh in range(2):
                        prr = slice(hh * 64, hh * 64 + 64)
                        P.pool(lambda e, m=m, hh=hh, prr=prr: e.tensor_tensor(out=btT[prr, 2 * m + hh, :], in0=t1[prr, :],
                                                                              in1=winv[prr, :], op=ALU.mult),
                               r=[t1, winv], w=[btT])
                    P.pool(lambda e, m=m: e.tensor_tensor(out=bhT[:, m, :], in0=t1[:, :], in1=t2[:, :], op=ALU.mult),
                           r=[t1, t2], w=[bhT])
                    for hh in range(2):
                        prr = slice(hh * 64, hh * 64 + 64)
                        P.dve(lambda e, m=m, hh=hh, prr=prr: e.tensor_tensor(out=ktT[prr, 2 * m + hh, :], in0=kp[prr, :],
                                                                             in1=winv[prr, :], op=ALU.mult),
                              r=[kp, winv], w=[ktT])
                    P.pool(lambda e, m=m: e.tensor_tensor(out=khT[:, m, :], in0=kp[:, :], in1=t2[:, :], op=ALU.mult),
                           r=[kp, t2], w=[khT])
                for j in range(2):
                    ti = s * 2 + j
                    tb = slice(j * 128, (j + 1) * 128)
                    for (src, dst) in ((vTb, Vtm), (bhT, Bhtm), (khT, Khtm)):
                        for m in range(4):
                            P.pe(lambda e, src=src, m=m: e.transpose(PT[:, m * 128:(m + 1) * 128], src[:, m, tb], identb[:, :]),
                                 r=[src, identb], w=[PT])
                        P.act(lambda e, dst=dst: e.activation(out=dst[:, :], in_=PT[:, 0:512], func=AF.Copy), r=[PT], w=[dst])
                    P.dve(lambda e: e.tensor_copy(out=vtok[:, :], in_=Vtm[:, :]), r=[Vtm], w=[vtok])
                    for hf in range(2):
                        for h4 in range(4):
                            h = hf * 4 + h4
                            m = h // 2
                            pr = slice((h % 2) * 64, (h % 2) * 64 + 64)
                            P.pe(lambda e, h4=h4, m=m, h=h: e.matmul(PA[:, h4 * 256:(h4 + 1) * 256], lhsT=btT[:, h, tb],
                                                                     rhs=ART[:, m, :, tb], start=True, stop=True),
                                 r=[btT, ART], w=[PA])
                            P.pe(lambda e, h4=h4, m=m, h=h: e.matmul(PB[:, h4 * 256:(h4 + 1) * 256], lhsT=ktT[:, h, tb],
                                                                     rhs=ART[:, m, :, tb], start=True, stop=True),
                                 r=[ktT, ART], w=[PB])
                        mur = cst[:, C_MSU:C_MSU + 256].unsqueeze(1).to_broadcast([128, 4, 256])
                        msl = cst[:, C_MSL:C_MSL + 128].unsqueeze(1).to_broadcast([128, 4, 128])
                        msu = cst[:, C_MSU:C_MSU + 128].unsqueeze(1).to_broadcast([128, 4, 128])
                        hs4 = slice(hf * 4, hf * 4 + 4)
                        P.dve(lambda e, hs4=hs4, mur=mur: e.tensor_tensor(
                            out=ATab[:, hs4, :], in0=PA[:, :].rearrange("p (h t) -> p h t", h=4), in1=mur, op=ALU.mult),
                            r=[PA, cst], w=[ATab])
                        P.dve(lambda e, hs4=hs4, mur=mur: e.tensor_tensor(
                            out=ATak[:, hs4, :], in0=PB[:, :].rearrange("p (h t) -> p h t", h=4), in1=mur, op=ALU.mult),
                            r=[PB, cst], w=[ATak])
                    P.act(lambda e: e.activation(out=Mk[0][:, :, :], in_=ATab[:, :, 0:128], func=AF.Copy), r=[ATab], w=[Mk[0]])
                    for h in range(8):
                        P.pe(lambda e, h=h: e.transpose(PT[:, h * 128:(h + 1) * 128], Mk[0][:, h, :], identb[:, :]),
                             r=[Mk[0], identb], w=[PT])
                    P.act(lambda e: e.activation(out=Nk[0][:, :, :].rearrange("p h t -> p (h t)"), in_=PT[:, :], func=AF.Copy),
                          r=[PT], w=[Nk[0]])
                    P.dve(lambda e: e.tensor_tensor(out=Pk[0][:, :, :], in0=ATab[:, :, 0:128],
                                                    in1=identb[:, :].unsqueeze(1).to_broadcast([128, 8, 128]), op=ALU.add),
                          r=[ATab, identb], w=[Pk[0]])
                    for lv in range(6):
                        a, b = lv % 2, (lv + 1) % 2
                        for g in range(2):
                            for h in range(4 * g, 4 * g + 4):
                                P.pe(lambda e: e.matmul(PA[:, h * 128:(h + 1) * 128], lhsT=Mk[a][:, h, :], rhs=Nk[a][:, h, :],
                                                        start=True, stop=True), r=[Mk[a].p[g], Nk[a].p[g]], w=[PA.p[g]])
                        for g in range(2):
                            P.act(lambda e: e.activation(out=Nk[b][:, 4 * g:4 * g + 4, :].rearrange("p h t -> p (h t)"),
                                                         in_=PA[:, g * 512:(g + 1) * 512], func=AF.Copy),
                                  r=[PA.p[g]], w=[Nk[b].p[g]])
                        if lv < 5:
                            for g in range(2):
                                for h in range(4 * g, 4 * g + 4):
                                    P.pe(lambda e: e.matmul(PB[:, h * 128:(h + 1) * 128], lhsT=Nk[a][:, h, :], rhs=Mk[a][:, h, :],
                                                            start=True, stop=True), r=[Mk[a].p[g], Nk[a].p[g]], w=[PB.p[g]])
                            P.act(lambda e: e.activation(out=Mk[b][:, 0:4, :].rearrange("p h t -> p (h t)"),
                                                         in_=PB[:, 0:512], func=AF.Copy), r=[PB.p[0]], w=[Mk[b].p[0]])
                            P.dve(lambda e: e.tensor_copy(out=Mk[b][:, 4:8, :].rearrange("p h t -> p (h t)"), in_=PB[:, 512:1024]),
                                  r=[PB.p[1]], w=[Mk[b].p[1]])
                        for g in range(2):
                            for h in range(4 * g, 4 * g + 4):
                                P.pe(lambda e: e.matmul(PC[:, h * 128:(h + 1) * 128], lhsT=Nk[b][:, h, :], rhs=Pk[a][:, h, :],
                                                        start=True, stop=True), r=[Nk[b].p[g], Pk[a].p[g]], w=[PC.p[g]])
                        for g in range(2):
                            P.dve(lambda e: e.tensor_tensor(out=Pk[b][:, 4 * g:4 * g + 4, :].rearrange("p h t -> p (h t)"),
                                                            in0=PC[:, g * 512:(g + 1) * 512],
                                                            in1=Pk[a][:, 4 * g:4 * g + 4, :].rearrange("p h t -> p (h t)"),
                                                            op=ALU.add), r=[PC.p[g], Pk[a].p[g]], w=[Pk[b].p[g]])
                    XT = Pk[0]
                    for m in range(4):
                        P.dve(lambda e, m=m: e.tensor_scalar(out=WCf[:, m, :], in0=cst[:, C_ONE:C_ONE + 64],
                                                             scalar1=WC[:, m, j:j + 1], scalar2=None, op0=ALU.mult),
                              r=[cst, WC], w=[WCf])
                    P.dve(lambda e: e.tensor_tensor(out=Hd[:, :, :], in0=Hs[:, :, :], in1=WCf[:, :, :], op=ALU.mult),
                          r=[Hs, WCf], w=[Hd])
                    for h in range(8):
                        m = h // 2
                        pr = slice((h % 2) * 64, (h % 2) * 64 + 64)
                        hc = slice(h * 64, (h + 1) * 64)
                        P.pe(lambda e, m=m, h=h, hc=hc: e.matmul(PD[:, hc], lhsT=ART[:, m, 0, tb], rhs=Hbz[:, h, :],
                                                                 start=True, stop=False), r=[ART, Hbz], w=[PD])
                        P.pe(lambda e, h=h, hc=hc: e.matmul(PD[:, hc], lhsT=ATak[:, h, 0:128], rhs=Vtm[:, hc],
                                                            start=False, stop=True), r=[ATak, Vtm], w=[PD])
                    P.act(lambda e: e.activation(out=Gb[:, :], in_=PD[:, :], func=AF.Copy), r=[PD], w=[Gb])
                    for h in range(8):
                        hc = slice(h * 64, (h + 1) * 64)
                        P.pe(lambda e, h=h, hc=hc: e.matmul(PA[:, hc], lhsT=XT[:, h, :], rhs=Gb[:, hc], start=True, stop=True),
                             r=[XT, Gb], w=[PA])
                    P.act(lambda e: e.activation(out=Ub[:, :], in_=PA[:, 0:512], func=AF.Copy), r=[PA], w=[Ub])
                    for h in range(8):
                        m = h // 2
                        pr = slice((h % 2) * 64, (h % 2) * 64 + 64)
                        hc = slice(h * 64, (h + 1) * 64)
                        P.pe(lambda e, m=m, h=h, hc=hc: e.matmul(PB[:, hc], lhsT=ART[:, m, 1, tb], rhs=Hbz[:, h, :],
                                                                 start=True, stop=False), r=[ART, Hbz], w=[PB])
                        P.pe(lambda e, h=h, hc=hc: e.matmul(PB[:, hc], lhsT=ATab[:, h, 128:256], rhs=Ub[:, hc],
                                                            start=False, stop=False), r=[ATab, Ub], w=[PB])
                        P.pe(lambda e, h=h, hc=hc: e.matmul(PB[:, hc], lhsT=ATak[:, h, 128:256], rhs=Vtm[:, hc],
                                                            start=False, stop=True), r=[ATak, Vtm], w=[PB])
                    for m in range(4):
                        ms = slice(m * 128, (m + 1) * 128)
                        P.pe(lambda e, ms=ms: e.matmul(PC[:, ms], lhsT=Bhtm[:, ms], rhs=Ub[:, ms], start=True, stop=False),
                             r=[Bhtm, Ub], w=[PC])
                        P.pe(lambda e, ms=ms: e.matmul(PC[:, ms], lhsT=Khtm[:, ms], rhs=Vtm[:, ms], start=False, stop=True),
                             r=[Khtm, Vtm], w=[PC])
                    for hh in range(2):
                        pr = slice(hh * 64, hh * 64 + 64)
                        src = PC[pr, 0:512].rearrange("p (m c) -> p m c", m=4)[:, :, hh * 64:hh * 64 + 64]
                        P.dve(lambda e, pr=pr, src=src: e.tensor_tensor(out=Hs[pr, :, :], in0=Hd[pr, :, :], in1=src, op=ALU.add),
                              r=[Hd, PC], w=[Hs])
                    for hh in range(2):
                        prr = slice(hh * 64, hh * 64 + 64)
                        P.act(lambda e, hh=hh, prr=prr: e.activation(out=Hbz[prr, hh:8:2, :], in_=Hs[prr, :, :], func=AF.Copy),
                              r=[Hs], w=[Hbz])
                    P.act(lambda e: e.activation(out=Ysb[:, :], in_=PB[:, 0:512], func=AF.Copy), r=[PB], w=[Ysb])
                    P.act(lambda e: e.activation(out=Ysq[:, :], in_=PB[:, 0:512], func=AF.Square), r=[PB], w=[Ysq])
                    P.dve(lambda e: e.tensor_reduce(out=gst[:, 0:8], in_=Ysb[:, :].rearrange("p (h i) -> p h i", h=8),
                                                    axis=AX.X, op=ALU.add), r=[Ysb], w=[gst])
                    P.dve(lambda e: e.tensor_reduce(out=gst[:, 8:16], in_=Ysq[:, :].rearrange("p (h i) -> p h i", h=8),
                                                    axis=AX.X, op=ALU.add), r=[Ysq, gst], w=[gst])
                    P.dve(lambda e: e.tensor_scalar(out=gst[:, 16:24], in0=gst[:, 0:8], scalar1=1.0 / 64, scalar2=None,
                                                    op0=ALU.mult), r=[gst], w=[gst])
                    P.dve(lambda e: e.tensor_tensor(out=gst[:, 24:32], in0=gst[:, 16:24], in1=gst[:, 16:24], op=ALU.mult),
                          r=[gst], w=[gst])
                    P.dve(lambda e: e.scalar_tensor_tensor(out=gst[:, 32:40], in0=gst[:, 8:16], scalar=1.0 / 64,
                                                           in1=gst[:, 24:32], op0=ALU.mult, op1=ALU.subtract),
                          r=[gst], w=[gst])
                    P.act(lambda e: e.activation(out=gst[:, 40:48], in_=gst[:, 32:40], func=AF.Sqrt, bias=epsc[:, 1:2]),
                          r=[gst, epsc], w=[gst])
                    P.dve(lambda e: e.reciprocal(out=gst[:, 40:48], in_=gst[:, 40:48]), r=[gst], w=[gst])
                    y3 = lambda t: t[:, :].rearrange("p (h i) -> p h i", h=8)
                    P.dve(lambda e: e.tensor_tensor(out=y3(Ysb), in0=y3(Ysb),
                                                    in1=gst[:, 16:24].unsqueeze(2).to_broadcast([128, 8, 64]), op=ALU.subtract),
                          r=[Ysb, gst], w=[Ysb])
                    P.dve(lambda e: e.tensor_tensor(out=y3(Ysb), in0=y3(Ysb),
                                                    in1=gst[:, 40:48].unsqueeze(2).to_broadcast([128, 8, 64]), op=ALU.mult),
                          r=[Ysb, gst], w=[Ysb])
                    P.dve(lambda e: e.tensor_tensor(out=Ysb[:, :], in0=Ysb[:, :], in1=lnrow[:, 0, :], op=ALU.mult),
                          r=[Ysb, lnrow], w=[Ysb])
                    P.dve(lambda e: e.tensor_tensor(out=Ysb[:, :], in0=Ysb[:, :], in1=lnrow[:, 1, :], op=ALU.add),
                          r=[Ysb, lnrow], w=[Ysb])
                    for m in range(4):
                        P.pe(lambda e, m=m: e.matmul(PD[:, 2 * m:2 * m + 2], lhsT=rkT[:, m, tb], rhs=hindb[:, :],
                                                     start=True, stop=True), r=[rkT, hindb], w=[PD])
                    P.act(lambda e: e.activation(out=gst[:, 0:8], in_=PD[:, 0:8], func=AF.Copy), r=[PD, gst], w=[gst])
                    P.dve(lambda e: e.tensor_tensor(out=y3(Ysq), in0=y3(vtok),
                                                    in1=gst[:, 0:8].unsqueeze(2).to_broadcast([128, 8, 64]), op=ALU.mult),
                          r=[vtok, gst], w=[Ysq])
                    P.dve(lambda e: e.tensor_tensor(out=Ysb[:, :], in0=Ysb[:, :], in1=Ysq[:, :], op=ALU.add),
                          r=[Ysb, Ysq], w=[Ysb])
                    P.pe(lambda e: e.matmul(PA[:, 512:1024], lhsT=sgd[:, 0, tb], rhs=w_gateb[:, 0, :], start=True, stop=False),
                         r=[sgd, w_gateb], w=[PA])
                    P.pe(lambda e: e.matmul(PA[:, 512:1024], lhsT=sgd[:, 1, tb], rhs=w_gateb[:, 1, :], start=False, stop=True),
                         r=[sgd, w_gateb], w=[PA])
                    P.dve(lambda e: e.tensor_tensor(out=yrw[:, :], in0=Ysb[:, :], in1=PA[:, 512:1024], op=ALU.mult),
                          r=[Ysb, PA], w=[yrw])
                    dump("yrw", yrw[:, :], yrw, dbg_d["yrw"][ti * 128:(ti + 1) * 128, :] if dbg else None)
                    P.act(lambda e: e.activation(out=yrwb[:, :], in_=yrw[:, :], func=AF.Copy), r=[yrw], w=[yrwb])
                    for m in range(4):
                        P.pe(lambda e, m=m: e.transpose(PT[:, m * 128:(m + 1) * 128], yrwb[:, m * 128:(m + 1) * 128], identb[:, :]),
                             r=[yrwb, identb], w=[PT])
                    P.act(lambda e: e.activation(out=ycat[:, :, :].rearrange("p m t -> p (m t)"), in_=PT[:, 0:512], func=AF.Copy),
                          r=[PT], w=[ycat])
                    P.dma(yaf[:, :], yscr[ti * 128:(ti + 1) * 128, :], "yld", r=[yscr_t], w=[yaf])
                    P.act(lambda e: e.activation(out=ya[:, :, :].rearrange("p m t -> p (m t)"), in_=yaf[:, :], func=AF.Copy),
                          r=[yaf], w=[ya])
                    for c in range(2):
                        cs_ = slice(c * 512, (c + 1) * 512)
                        for k in range(8):
                            lhs = (lambda k=k: ya[:, k, :]) if k < 4 else (lambda k=k: ycat[:, k - 4, :])
                            P.pe(lambda e, k=k, cs_=cs_, lhs=lhs: e.matmul(PC[:, cs_], lhsT=lhs(), rhs=w_outb[:, k, cs_],
                                                                           start=(k == 0), stop=(k == 7)),
                                 r=[ya, ycat, w_outb], w=[PC])
                    P.act(lambda e: e.activation(out=mix[:, :], in_=PC[:, :], func=AF.Copy), r=[PC], w=[mix])
                    P.act(lambda e: e.activation(out=sqj[:, :], in_=PC[:, :], func=AF.Square, accum_out=st4[:, 6:7]),
                          r=[PC], w=[sqj, st4])
                    P.act(lambda e: e.activation(out=st4[:, 7:8], in_=st4[:, 6:7], func=AF.Sqrt, scale=1.0 / D,
                                                 bias=epsc[:, 0:1]), r=[st4, epsc], w=[st4])
                    P.dve(lambda e: e.reciprocal(out=st4[:, 7:8], in_=st4[:, 7:8]), r=[st4], w=[st4])
                    xo = x1s[0]
                    P.dve(lambda e: e.scalar_tensor_tensor(out=mix[:, :], in0=mix[:, :], scalar=st4[:, 7:8], in1=GMrow[:, :],
                                                           op0=ALU.mult, op1=ALU.mult), r=[mix, st4, GMrow], w=[mix])
                    P.dve(lambda e, xo=xo: e.tensor_tensor(out=xo[:, :], in0=mix[:, :], in1=xt[:, j, :], op=ALU.add),
                          r=[mix, xt], w=[xo])
                    P.dma(out_d[ti * 128:(ti + 1) * 128, :], xo[:, :], "x1st", r=[xo], w=[x1t], q="sp")
                    dump("x1", xo[:, :], xo, dbg_d["x1"][ti * 128:(ti + 1) * 128, :] if dbg else None)

        print("ops after A2", P.nops)
        P.fence()
        if upto < 3:
            P.enabled = False
        outt = Tl(P, None, "out_hbm")
        with ExitStack() as es3:
            P.es_cur = es3
            w_fgb = P.tile([128, 8, DFF], BF16, "w_fgb")
            w_fub = P.tile([128, 8, DFF], BF16, "w_fub")
            w_fdb = P.tile([128, 22, D], BF16, "w_fdb")
            wst = [P.tile([128, 1024], F32, "wst3%d" % i) for i in range(2)]
            i = 0
            jobs = []
            for (wd_, wb) in ((wfg_d, w_fgb), (wfu_d, w_fub)):
                for k in range(8):
                    for (c0, c1) in ((0, 1024), (1024, 2048), (2048, DFF)):
                        jobs.append((wd_, wb, k, c0, c1))
            for k in range(22):
                jobs.append((wfd_d, w_fdb, k, 0, D))
            for (wd_, wb, k, c0, c1) in jobs:
                ws = wst[i % 2]
                P.dma(ws[:, 0:c1 - c0], wd_[k * 128:(k + 1) * 128, c0:c1], "wst3%d" % (i % 2), w=[ws])
                if i % 2 == 0:
                    P.act(lambda e, ws=ws, wb=wb, k=k, c0=c0, c1=c1: e.activation(out=wb[:, k, c0:c1], in_=ws[:, 0:c1 - c0],
                                                                                 func=AF.Copy), r=[ws], w=[wb])
                else:
                    P.dve(lambda e, ws=ws, wb=wb, k=k, c0=c0, c1=c1: e.tensor_copy(out=wb[:, k, c0:c1], in_=ws[:, 0:c1 - c0]),
                          r=[ws], w=[wb])
                i += 1
            xt = P.tile([128, 2, D], F32, "xt3")
            sqj = P.tile([128, D], BF16, "sqj3")
            st4 = P.tile([128, 8], F32, "st43")
            dgt = P.tile([128, 2, 128], F32, "dgt3")
            hfT = P.tile([128, 8, 256], BF16, "hfT")
            sg = [P.tile([128, 256], F32, "sg%d" % i) for i in range(2)]
            aT = P.tile([128, 22, 256], BF16, "aT")
            fo = P.tile([128, D], F32, "fo")
            ost = [P.tile([128, D], F32, "ost%d" % i) for i in range(1)]
            print("SBUFREM B", nc.sbuf_bytes_remaining)
            for s in range(NS):
                for j in range(2):
                    r0 = s * 256 + j * 128
                    P.dma(xt[:, j, :], out_d[r0:r0 + 128, :], "xin3", r=[x1t], w=[xt])
                for j in range(2):
                    P.act(lambda e, j=j: e.activation(out=sqj[:, :], in_=xt[:, j, :], func=AF.Square,
                                                      accum_out=st4[:, j:j + 1]), r=[xt], w=[sqj, st4])
                P.act(lambda e: e.activation(out=st4[:, 2:4], in_=st4[:, 0:2], func=AF.Sqrt, scale=1.0 / D,
                                             bias=epsc[:, 0:1]), r=[st4, epsc], w=[st4])
                P.dve(lambda e: e.reciprocal(out=st4[:, 4:6], in_=st4[:, 2:4]), r=[st4], w=[st4])
                for j in range(2):
                    P.dve(lambda e, j=j: e.tensor_scalar(out=dgt[:, j, :], in0=cst[:, C_ID:C_ID + 128],
                                                         scalar1=st4[:, 4 + j:5 + j], scalar2=None, op0=ALU.mult),
                          r=[cst, st4], w=[dgt])
                for kk in range(2):
                    for k4 in range(4):
                        k = kk * 4 + k4
                        for j in range(2):
                            P.pe(lambda e, k=k, k4=k4, j=j: e.matmul(
                                PA[:, k4 * 256 + j * 128:k4 * 256 + (j + 1) * 128],
                                lhsT=xt[:, j, k * 128:(k + 1) * 128], rhs=dgt[:, j, :], start=True, stop=True),
                                r=[xt, dgt], w=[PA])
                    for k4 in range(4):
                        k = kk * 4 + k4
                        P.dve(lambda e, k=k, k4=k4: e.tensor_scalar(
                            out=hfT[:, k, :], in0=PA[:, k4 * 256:(k4 + 1) * 256], scalar1=gsh[:, 8 + k:9 + k],
                            scalar2=modc[:, 24 + k:25 + k], op0=ALU.mult, op1=ALU.add), r=[PA, gsh, modc], w=[hfT])
                for n in range(22):
                    ns = slice(n * 128, (n + 1) * 128)
                    pg = PB if n % 2 == 0 else PC
                    for k in range(8):
                        P.pe(lambda e, k=k, ns=ns, pg=pg: e.matmul(pg[:, 0:256], lhsT=w_fgb[:, k, ns], rhs=hfT[:, k, :],
                                                                   start=(k == 0), stop=(k == 7)), r=[w_fgb, hfT], w=[pg])
                    for k in range(8):
                        P.pe(lambda e, k=k, ns=ns, pg=pg: e.matmul(pg[:, 256:512], lhsT=w_fub[:, k, ns], rhs=hfT[:, k, :],
                                                                   start=(k == 0), stop=(k == 7)), r=[w_fub, hfT], w=[pg])
                    sgt = sg[n % 2]
                    P.act(lambda e, pg=pg, sgt=sgt: e.activation(out=sgt[:, :], in_=pg[:, 0:256], func=AF.Silu), r=[pg], w=[sgt])
                    P.dve(lambda e, pg=pg, sgt=sgt, n=n: e.tensor_tensor(out=aT[:, n, :], in0=sgt[:, :], in1=pg[:, 256:512],
                                                                         op=ALU.mult), r=[sgt, pg], w=[aT])
                for j in range(2):
                    ti = s * 2 + j
                    for c in range(2):
                        cs_ = slice(c * 512, (c + 1) * 512)
                        for n in range(22):
                            P.pe(lambda e, n=n, cs_=cs_, j=j: e.matmul(PA[:, cs_], lhsT=aT[:, n, j * 128:(j + 1) * 128],
                                                                       rhs=w_fdb[:, n, cs_], start=(n == 0), stop=(n == 21)),
                                 r=[aT, w_fdb], w=[PA])
                    P.act(lambda e: e.activation(out=fo[:, :], in_=PA[:, :], func=AF.Copy), r=[PA], w=[fo])
                    P.act(lambda e: e.activation(out=sqj[:, :], in_=PA[:, :], func=AF.Square, accum_out=st4[:, 6:7]),
                          r=[PA], w=[sqj, st4])
                    P.act(lambda e: e.activation(out=st4[:, 7:8], in_=st4[:, 6:7], func=AF.Sqrt, scale=1.0 / D,
                                                 bias=epsc[:, 0:1]), r=[st4, epsc], w=[st4])
                    P.dve(lambda e: e.reciprocal(out=st4[:, 7:8], in_=st4[:, 7:8]), r=[st4], w=[st4])
                    oo = ost[0]
                    P.dve(lambda e: e.scalar_tensor_tensor(out=fo[:, :], in0=fo[:, :], scalar=st4[:, 7:8], in1=GFrow[:, :],
                                                           op0=ALU.mult, op1=ALU.mult), r=[fo, st4, GFrow], w=[fo])
                    P.dve(lambda e, oo=oo, j=j: e.tensor_tensor(out=oo[:, :], in0=fo[:, :], in1=xt[:, j, :], op=ALU.add),
                          r=[fo, xt], w=[oo])
                    P.dma(out_d[ti * 128:(ti + 1) * 128, :], oo[:, :], "ost", r=[oo], w=[outt], q="sp")
            P.enabled = True
            P.wait_all("sp", ["ost", "x1st", "dbg"] + [k for k in P.streams if k not in ("ost", "x1st", "dbg")])
            P.wait_all("pool", ["ost"])

            with nc.Block() as block:
                @block.sync
                def _(e):
                    P.replay("sp", e)

                @block.tensor
                def _(e):
                    P.replay("pe", e)

                @block.scalar
                def _(e):
                    P.replay("act", e)

                @block.vector
                def _(e):
                    P.replay("dve", e)

                @block.gpsimd
                def _(e):
                    P.replay("pool", e)
    return nc, list(dbg_d.keys())


def t5_bucket_np(rel):
    rel = np.asarray(rel)
    max_exact = 16
    nf = np.maximum(rel, 1).astype(np.float32)
    large = max_exact + (np.log(nf / np.float32(max_exact)) / np.float32(math.log(128 / max_exact))
                         * np.float32(32 - max_exact)).astype(np.int32)
    large = np.minimum(large, 31)
    return np.where(rel < max_exact, rel, large)


def make_consts():
    c = np.zeros((128, C_END), np.float32)
    p = np.arange(128)[:, None]
    f = np.arange(128)[None, :]
    c[:, C_ID:C_ID + 128] = (p == f)
    c[:, C_ONE:C_ONE + 128] = 1.0
    c[:, C_BLK:C_BLK + 128] = ((p // 64) == (f // 64))
    c[:, C_MSU:C_MSU + 128] = (f > p)
    c[:, C_MUI:C_MUI + 128] = (f >= p)
    c[:, C_MSL:C_MSL + 128] = (f < p)
    c[:, C_NEG:C_NEG + 128] = np.where(f > p, -1e30, 0.0)
    c[:, C_J:C_J + 128] = (p + f == 127)
    c[31, C_SEL:C_SEL + 128] = 1.0
    bk = t5_bucket_np(np.arange(256))
    c[0:32, C_OH:C_OH + 256] = (np.arange(32)[:, None] == bk[None, :])
    sm = np.ones((128, 256), np.float32)
    sm[:, 0] = 0.0
    sm[:, 128] = 0.0
    c[:, C_SCAN:C_SCAN + 256] = sm
    c[:, C_HIND] = (np.arange(128) < 64)
    c[:, C_HIND + 1] = (np.arange(128) >= 64)
    return c


def col8(v):
    return np.ascontiguousarray(v.reshape(-1, 128).T)


def prep_shared(inp):
    f32 = np.float32
    g = lambda k: np.asarray(inp[k], f32)
    w_in = g("w_in")[0]
    sh = {}
    sh["ada_w"] = np.ascontiguousarray(g("ada_w")[0])
    sh["cst"] = make_consts()
    sh["w_att"] = np.ascontiguousarray(np.concatenate([w_in[:, 0:384], w_in[:, 384:448], w_in[:, 384:448]], axis=1))
    sh["w_iw"] = np.ascontiguousarray(w_in[:, 448:456])
    sh["w_rw"] = np.ascontiguousarray(w_in[:, 456:])
    wiq = g("w_idx_q")[0]
    sh["w_iq"] = np.ascontiguousarray(np.concatenate([wiq, wiq], axis=2).reshape(256, 1024))
    sh["w_uq"] = np.ascontiguousarray(g("w_uq")[0].reshape(256, 512))
    wuk = g("w_uk")[0]
    t = np.zeros((128, 8, 128), f32)
    for h in range(8):
        t[(h % 2) * 64:(h % 2) * 64 + 64, h, :] = wuk[h].T
    sh["w_ukT"] = t.reshape(128, 1024)
    wuv = g("w_uv")[0]
    t = np.zeros((128, 8, 128), f32)
    for h in range(8):
        t[:, h, (h % 2) * 64:(h % 2) * 64 + 64] = wuv[h]
    sh["w_uv"] = t.reshape(128, 1024)
    sh["rel_bias"] = np.ascontiguousarray(g("rel_bias"))
    t = np.zeros((128, 1024), f32)
    t[0:64, 0:512] = g("w_decay_up")[0]
    t[64:128, 512:1024] = g("w_aaa_up")[0]
    sh["w_lora"] = t
    sh["w_gate"] = np.ascontiguousarray(g("w_gate_up")[0])
    sh["lnrow"] = np.ascontiguousarray(np.stack([g("ln_x_gain")[0], g("ln_x_bias")[0]], axis=0))
    sh["w_out"] = np.ascontiguousarray(g("w_out")[0])
    sh["w_fg"] = np.ascontiguousarray(g("w_ffn_gate")[0])
    sh["w_fu"] = np.ascontiguousarray(g("w_ffn_up")[0])
    sh["w_fd"] = np.ascontiguousarray(g("w_ffn_down")[0])
    vec = np.zeros((128, 128), f32)
    vec[:, 0:8] = col8(g("mix_pre_norm")[0])
    vec[:, 8:16] = col8(g("mix_post_norm")[0])
    vec[:, 16:24] = col8(g("ffn_pre_norm")[0])
    vec[:, 24:32] = col8(g("ffn_post_norm")[0])
    vec[:, 32:80] = g("ada_b")[0].reshape(48, 128).T
    vec[:, 80:82] = g("q_norm")[0].reshape(2, 128).T
    vec[:, 82] = g("kv_norm")[0]
    vec[:, 83] = np.concatenate([g("idx_k_norm")[0], g("idx_k_norm")[0]])
    vec[:, 84:88] = g("w0")[0].reshape(4, 128).T
    vec[:, 88:92] = g("a0")[0].reshape(4, 128).T
    vec[:, 92:96] = g("k_k")[0].reshape(4, 128).T
    vec[:, 96:100] = g("k_a")[0].reshape(4, 128).T
    vec[:, 100:104] = g("r_k")[0].reshape(4, 128).T
    mus = g("mu_shift")[0]
    vec[:, 108:122] = mus[:14 * 128].reshape(14, 128).T
    vec[:, 122] = mus[1824 - 128:1824]
    sh["vecs"] = vec
    sh["adab_row"] = np.ascontiguousarray(g("ada_b")[0].reshape(1, 6 * D))
    return sh


def kernel(**inputs):
    x = np.asarray(inputs["x"], np.float32)
    c = np.asarray(inputs["c"], np.float32)
    B, T, _ = x.shape
    sh = prep_shared(inputs)
    nc, _ = build(T)
    in_maps = []
    for b in range(B):
        m = dict(sh)
        m["x"] = np.ascontiguousarray(x[b])
        m["ccol"] = col8(c[b])
        in_maps.append(m)
    res = run_bass_kernel_spmd(nc, in_maps, core_ids=list(range(B)))
    return np.stack([np.asarray(r["out"], np.float32) for r in res.results], axis=0)
```

```python
import math
from contextlib import ExitStack
import numpy as np
import concourse.bass as bass
import concourse.mybir as mybir
from concourse.bass_utils import run_bass_kernel_spmd

F32 = mybir.dt.float32
BF16 = mybir.dt.bfloat16
AF = mybir.ActivationFunctionType
ALU = mybir.AluOpType
AX = mybir.AxisListType

D = 1024
DFF = 2816
NB_IT = 20
BIS_R = 16.0
LWC = math.exp(-0.5)

C_ID, C_ONE, C_BLK, C_MSU, C_MUI, C_MSL, C_NEG, C_J, C_SEL, C_OH, C_SCAN, C_HIND, C_END = (
    0, 128, 256, 384, 512, 640, 768, 896, 1024, 1152, 1408, 1664, 1668)


class Tl:
    def __init__(self, P, h, name):
        self.h = h
        self.name = name
        self.lw = None
        self.rd = dict(P.fence_tokens)

    def __getitem__(self, i):
        return self.h[i]


class TlG:
    def __init__(self, P, h, name, n):
        self.h = h
        self.name = name
        self.p = [Tl(P, h, "%s_%d" % (name, i)) for i in range(n)]

    def __getitem__(self, i):
        return self.h[i]


class Bank:
    def __init__(self, tl, h, lo):
        self.tl = tl
        self.h = h
        self.lo = lo

    def ap(self, a, b):
        return self.h[:, self.lo + a:self.lo + b]


def _flat(ts):
    out = []
    for t in ts:
        if isinstance(t, TlG):
            out.extend(t.p)
        else:
            out.append(t)
    return out


class Rec:
    def __init__(self):
        self.call = None

    def __getattr__(self, name):
        def f(*a, **k):
            self.call = (name, a, k)
            return self
        return f


class Stream:
    def __init__(self, key):
        self.key = key
        self.count = 0
        self.mark = -1


class Prog:
    ENGS = ("pe", "act", "dve", "pool", "sp")

    def __init__(self, nc, es):
        self.nc = nc
        self.es = es
        self.q = {e: [] for e in self.ENGS}
        self.cnt = {e: 0 for e in self.ENGS}
        self.waited = {e: {} for e in self.ENGS}
        self.semh = {}
        self.streams = {}
        self.fence_tokens = {}
        self.enabled = True
        self.nops = 0
        import os as _os
        self.printops = bool(_os.environ.get("PRINTOPS"))
        self.maxops = int(_os.environ.get("MAXOPS", "100000000"))
        for e in self.ENGS:
            self.semh[e] = es.enter_context(nc.semaphore("s_" + e))

    def stream(self, key):
        if key not in self.streams:
            self.streams[key] = Stream(key)
            self.semh[key] = self.es.enter_context(self.nc.semaphore("d_" + key))
        return self.streams[key]

    def tile(self, shape, dt, name):
        h = self.es_cur.enter_context(self.nc.sbuf_tensor("sb_" + name, list(shape), dt))
        return Tl(self, h, name)

    def tileg(self, shape, dt, name, n):
        h = self.es_cur.enter_context(self.nc.sbuf_tensor("sb_" + name, list(shape), dt))
        return TlG(self, h, name, n)

    def conv(self, i, out, in_, r, w):
        e = i % 3
        if e == 0:
            self.act(lambda q: q.activation(out=out, in_=in_, func=AF.Copy), r=r, w=w)
        elif e == 1:
            self.dve(lambda q: q.tensor_copy(out=out, in_=in_), r=r, w=w)
        else:
            self.pool(lambda q: q.tensor_copy(out=out, in_=in_), r=r, w=w)

    def fence(self):
        ft = {}
        for e in self.ENGS:
            if self.cnt[e] > 0:
                ft[e] = (e, self.cnt[e], e, False)
        for k, st in self.streams.items():
            if st.count > 0:
                ft[k] = (k, st.count, None, True)
        self.fence_tokens = ft

    def emit(self, eng, fn, r=(), w=(), stream=None):
        if not self.enabled:
            return None
        self.nops += 1
        if self.nops > self.maxops:
            return None
        r = _flat(r)
        w = _flat(w)
        rec = Rec()
        fn(rec)
        fn = rec.call
        assert fn is not None
        if self.printops:
            print("OP", self.nops, eng, fn[0], [t.name for t in r], "->", [t.name for t in w])
        need = {}

        def add(tok, kind):
            key, val, teng, isdma = tok
            if isdma:
                val = self.streams[key].count
                self.streams[key].mark = val
            elif stream is None and teng == eng:
                if eng == "pe":
                    return
            if need.get(key, 0) < val:
                need[key] = val

        for t in r:
            if t.lw is not None:
                add(t.lw, "raw")
        for t in w:
            if t.lw is not None:
                add(t.lw, "waw")
            for tok in t.rd.values():
                add(tok, "war")
        if stream is not None:
            st0 = self.stream(stream)
            if st0.mark == st0.count and st0.count > 0:
                need[st0.key] = st0.count
        for key, val in need.items():
            if self.waited[eng].get(key, 0) < val:
                self.waited[eng][key] = val
                self.q[eng].append(("w", key, val))
        if stream is None:
            self.cnt[eng] += 1
            tok = (eng, self.cnt[eng], eng, False)
            self.q[eng].append(("op", fn, eng, 1))
        else:
            st = self.stream(stream)
            st.count += 16
            tok = (st.key, st.count, None, True)
            self.q[eng].append(("op", fn, st.key, 16))
        for t in w:
            t.lw = tok
            t.rd = {}
        for t in r:
            if t not in w:
                t.rd[tok[0]] = tok
        return tok

    def wait_all(self, eng, keys):
        for key in keys:
            if key in self.streams:
                val = self.streams[key].count
            elif key in self.cnt:
                val = self.cnt[key]
            else:
                continue
            if val > 0 and self.waited[eng].get(key, 0) < val:
                self.waited[eng][key] = val
                self.q[eng].append(("w", key, val))

    def replay(self, eng, e):
        for ent in self.q[eng]:
            if ent[0] == "w":
                e.wait_ge(self.semh[ent[1]], ent[2])
            else:
                name, a, k = ent[1]
                ins = getattr(e, name)(*a, **k)
                ins.then_inc(self.semh[ent[2]], ent[3])

    def pe(self, fn, r=(), w=()):
        return self.emit("pe", fn, r, w)

    def act(self, fn, r=(), w=()):
        return self.emit("act", fn, r, w)

    def dve(self, fn, r=(), w=()):
        return self.emit("dve", fn, r, w)

    def pool(self, fn, r=(), w=()):
        return self.emit("pool", fn, r, w)

    def dma(self, out, in_, stream, r=(), w=(), q="sp"):
        return self.emit(q, lambda e: e.dma_start(out=out, in_=in_), r, w, stream=stream)


def build(T, dbg=False, upto=3):
    NT = T // 128
    NS = T // 256
    KTOP = min(256, T // 4)
    nc = bass.Bass("TRN2", target_bir_lowering=False)

    def din(name, shape):
        return nc.dram_tensor(name, list(shape), F32, kind="ExternalInput").ap()

    x_d = din("x", [T, D])
    ccol_d = din("ccol", [128, 8])
    adaw_d = din("ada_w", [D, 6 * D])
    vec_d = din("vecs", [128, 128])
    adab_d = din("adab_row", [1, 6 * D])
    cst_d = din("cst", [128, C_END])
    watt_d = din("w_att", [D, 512])
    wiw_d = din("w_iw", [D, 8])
    wrw_d = din("w_rw", [D, 1824])
    wiq_d = din("w_iq", [256, 1024])
    wuq_d = din("w_uq", [256, 512])
    wuk_d = din("w_ukT", [128, 1024])
    wuv_d = din("w_uv", [128, 1024])
    relb_d = din("rel_bias", [32, 8])
    wlora_d = din("w_lora", [128, 1024])
    wgate_d = din("w_gate", [160, 512])
    lnrow_d = din("lnrow", [2, 512])
    wout_d = din("w_out", [D, D])
    wfg_d = din("w_fg", [D, DFF])
    wfu_d = din("w_fu", [D, DFF])
    wfd_d = din("w_fd", [DFF, D])
    out_d = nc.dram_tensor("out", [T, D], F32, kind="ExternalOutput").ap()
    dscr = nc.dram_tensor("dscr", [8, 384], F32, kind="Internal").ap()
    dbg_d = {}

    def ddbg(name, shape):
        if dbg:
            dbg_d[name] = nc.dram_tensor("dbg_" + name, list(shape), F32, kind="ExternalOutput").ap()

    ddbg("modc", [128, 48])
    ddbg("bias", [128, 3 * 1024])
    ddbg("yatt", [128, 4 * T])
    ddbg("thr", [128, NT])
    ddbg("yrw", [T, 512])
    ddbg("x1", [T, D])

    with ExitStack() as es:
        P = Prog(nc, es)
        P.es_cur = es
        def pst(name, shape, dt):
            return Tl(P, es.enter_context(nc.psum_tensor("ps_" + name, list(shape), dt)), name)
        def pstg(name, shape, dt, n):
            return TlG(P, es.enter_context(nc.psum_tensor("ps_" + name, list(shape), dt)), name, n)
        PA = pstg("PA", [128, 1024], F32, 2)
        PB = pstg("PB", [128, 1024], F32, 2)
        PC = pstg("PC", [128, 1024], F32, 2)
        PD = pst("PD", [128, 512], F32)
        PT = pstg("PT", [128, 1024], BF16, 8)

        cst = P.tile([128, C_END], F32, "cst")
        vec = P.tile([128, 128], F32, "vec")
        modc = P.tile([128, 48], F32, "modc")
        gsh = P.tile([128, 48], F32, "gsh")
        GMrow = P.tile([128, D], F32, "GMrow")
        GFrow = P.tile([128, D], F32, "GFrow")
        identb = P.tile([128, 128], BF16, "identb")
        onesb = P.tile([128, 128], BF16, "onesb")
        epsc = P.tile([128, 4], F32, "epsc")
        yscr = nc.dram_tensor("yscr", [NT * 128, 512], F32, kind="ExternalOutput").ap()
        yscr_t = Tl(P, None, "yscr")

        ident = lambda: cst[:, C_ID:C_ID + 128]
        ones32 = lambda: cst[:, C_ONE:C_ONE + 128]

        P.dma(cst[:, :], cst_d[:, :], "cw", w=[cst])
        P.dma(vec[:, :], vec_d[:, :], "cw", w=[vec])
        P.dve(lambda e: e.tensor_copy(out=identb[:, :], in_=cst[:, C_ID:C_ID + 128]), r=[cst], w=[identb])
        P.dve(lambda e: e.tensor_copy(out=onesb[:, :], in_=cst[:, C_ONE:C_ONE + 128]), r=[cst], w=[onesb])
        P.dve(lambda e: e.memset(epsc[:, 0:1], 1e-6), w=[epsc])
        P.dve(lambda e: e.memset(epsc[:, 1:2], 64e-5), w=[epsc])
        P.dve(lambda e: e.memset(epsc[:, 2:3], 0.0), w=[epsc])
        V_MPRE, V_MPOST, V_FPRE, V_FPOST, V_ADAB, V_QN, V_KVN, V_IKN, V_W0, V_A0, V_KK, V_KA, V_RK = (
            0, 8, 16, 24, 32, 80, 82, 83, 84, 88, 92, 96, 100)

        def dump(name, src_ap, tl, dst_ap=None):
            if dbg:
                P.dma(dbg_d[name][:, :] if dst_ap is None else dst_ap, src_ap, "dbg", r=[tl])

        with ExitStack() as es0:
            P.es_cur = es0
            ccol = P.tile([128, 8], F32, "ccol")
            scol = P.tile([128, 8], F32, "scol")
            stg = [P.tile([128, 8, 512], F32, "adastg%d" % i) for i in range(4)]
            dg = P.tile([128, 8, 128], F32, "dg")
            P.dma(ccol[:, :], ccol_d[:, :], "cw", w=[ccol])
            P.act(lambda e: e.activation(out=scol[:, :], in_=ccol[:, :], func=AF.Silu), r=[ccol], w=[scol])
            adabr = P.tile([1, 6 * D], F32, "adabr")
            modrow = P.tile([1, 6 * D], F32, "modrow")
            P.dma(adabr[:, :], adab_d[:, :], "cw", w=[adabr])
            p0b = [Bank(PD, PD.h, 0), Bank(PC.p[0], PC.h, 0)]
            for s in range(12):
                st = stg[s % 4]
                P.dma(st[:, :, :], adaw_d[:, s * 512:(s + 1) * 512].rearrange("(k p) n -> p k n", p=128),
                      "ada%d" % (s % 4), w=[st])
                bk = p0b[s % 2]
                for k in range(8):
                    P.pe(lambda e: e.matmul(bk.h[0:1, bk.lo:bk.lo + 512], lhsT=scol[:, k:k + 1], rhs=st[:, k, :],
                                            start=(k == 0), stop=(k == 7)), r=[st, scol], w=[bk.tl])
                P.dve(lambda e: e.tensor_tensor(out=modrow[0:1, s * 512:(s + 1) * 512], in0=bk.h[0:1, bk.lo:bk.lo + 512],
                                                in1=adabr[0:1, s * 512:(s + 1) * 512], op=ALU.add),
                      r=[bk.tl, adabr], w=[modrow])
            for m in range(48):
                P.pe(lambda e: e.matmul(PB[:, m:m + 1], lhsT=modrow[0:1, m * 128:(m + 1) * 128],
                                        rhs=cst[0:1, C_ONE:C_ONE + 1], start=True, stop=True),
                     r=[modrow, cst], w=[PB.p[0]])
            P.dve(lambda e: e.tensor_copy(out=modc[:, :], in_=PB[:, 0:48]), r=[PB.p[0]], w=[modc])
            dump("modc", modc[:, :], modc)
            P.dve(lambda e: e.tensor_scalar(out=gsh[:, 32:40], in0=modc[:, 8:16], scalar1=1.0, scalar2=None,
                                            op0=ALU.add), r=[modc], w=[gsh])
            P.dve(lambda e: e.tensor_scalar(out=gsh[:, 40:48], in0=modc[:, 32:40], scalar1=1.0, scalar2=None,
                                            op0=ALU.add), r=[modc, gsh], w=[gsh])
            P.dve(lambda e: e.tensor_tensor(out=gsh[:, 0:8], in0=gsh[:, 32:40], in1=vec[:, V_MPRE:V_MPRE + 8],
                                            op=ALU.mult), r=[gsh, vec], w=[gsh])
            P.dve(lambda e: e.tensor_tensor(out=gsh[:, 8:16], in0=gsh[:, 40:48], in1=vec[:, V_FPRE:V_FPRE + 8],
                                            op=ALU.mult), r=[gsh, vec], w=[gsh])
            P.dve(lambda e: e.tensor_tensor(out=gsh[:, 16:24], in0=modc[:, 16:24], in1=vec[:, V_MPOST:V_MPOST + 8],
                                            op=ALU.mult), r=[gsh, modc, vec], w=[gsh])
            P.dve(lambda e: e.tensor_tensor(out=gsh[:, 24:32], in0=modc[:, 40:48], in1=vec[:, V_FPOST:V_FPOST + 8],
                                            op=ALU.mult), r=[gsh, modc, vec], w=[gsh])
            for (c0, row) in ((16, GMrow), (24, GFrow)):
                for k in range(8):
                    P.dve(lambda e, c0=c0, k=k: e.tensor_scalar(
                        out=dg[:, k, :], in0=cst[:, C_ID:C_ID + 128], scalar1=gsh[:, c0 + k:c0 + k + 1],
                        scalar2=None, op0=ALU.mult), r=[cst, gsh], w=[dg])
                for k in range(8):
                    P.pe(lambda e, k=k: e.matmul(PA[:, k * 128:(k + 1) * 128], lhsT=cst[:, C_ONE:C_ONE + 128],
                                                 rhs=dg[:, k, :], start=True, stop=True), r=[cst, dg], w=[PA])
                P.act(lambda e, row=row: e.activation(out=row[:, :], in_=PA[:, :], func=AF.Copy), r=[PA], w=[row])

        print("ops after P0", P.nops)
        P.fence()
        if upto < 1:
            P.enabled = False
        with ExitStack() as es1:
            P.es_cur = es1
            w_att = P.tile([128, 8, 512], F32, "w_att")
            w_iw = P.tile([128, 8, 8], F32, "w_iw")
            w_iq = P.tile([128, 2, 1024], F32, "w_iq")
            w_uqb = P.tile([128, 2, 512], BF16, "w_uqb")
            w_ukb = P.tile([128, 8, 128], BF16, "w_ukb")
            w_uvb = P.tile([128, 8, 128], BF16, "w_uvb")
            relb = P.tile([32, 8], F32, "relb")
            biasT = P.tile([128, 3, 1024], F32, "biasT")
            ckvT = P.tile([128, T], BF16, "ckvT")
            ckvtm = P.tile([128, NT, 128], BF16, "ckvtm")
            ikA = P.tile([128, T], BF16, "ikA")
            ikB = P.tile([128, T], BF16, "ikB")
            P.dve(lambda e: e.memset(ikB[:, :], 0.0), w=[ikB])
            es1b = ExitStack()
            P.es_cur = es1b
            wsts = [P.tile([128, 1024], F32, "wst1%d" % i) for i in range(3)]
            P.dma(w_att[:, :, :], watt_d.rearrange("(k p) n -> p k n", p=128), "cw", w=[w_att])
            P.dma(w_iw[:, :, :], wiw_d.rearrange("(k p) n -> p k n", p=128), "cw", w=[w_iw])
            P.dma(w_iq[:, :, :], wiq_d.rearrange("(k p) n -> p k n", p=128), "cw", w=[w_iq])
            P.dma(relb[:, :], relb_d[:, :], "cwr", w=[relb])
            P.dma(wsts[0][:, :].rearrange("p (k n) -> p k n", k=2), wuq_d.rearrange("(k p) n -> p k n", p=128), "ws1a", w=[wsts[0]])
            P.conv(1, w_uqb[:, :, :], wsts[0][:, :].rearrange("p (k n) -> p k n", k=2), [wsts[0]], [w_uqb])
            P.dma(wsts[1][:, :], wuk_d[:, :], "ws1b", w=[wsts[1]])
            P.conv(0, w_ukb[:, :, :], wsts[1][:, :].rearrange("p (k n) -> p k n", k=8), [wsts[1]], [w_ukb])
            P.dma(wsts[2][:, :], wuv_d[:, :], "ws1c", w=[wsts[2]])
            P.conv(2, w_uvb[:, :, :], wsts[2][:, :].rearrange("p (k n) -> p k n", k=8), [wsts[2]], [w_uvb])
            brel = P.tile([8, 384], F32, "brel")
            relx = P.tile([32, 8, 128], F32, "relx")
            qtl = P.tile([128, 2, 1024], F32, "qtl")
            P.pe(lambda e: e.matmul(PD[0:8, 0:256], lhsT=relb[:, :], rhs=cst[0:32, C_OH:C_OH + 256],
                                    start=True, stop=True), r=[relb, cst], w=[PD])
            P.dve(lambda e: e.memset(brel[:, :], 0.0), w=[brel])
            P.dve(lambda e: e.tensor_copy(out=brel[:, 127:383], in_=PD[0:8, 0:256]), r=[PD, brel], w=[brel])
            dsc = Tl(P, None, "dscr")
            P.dma(dscr[:, :], brel[:, :], "dsw", r=[brel], w=[dsc])
            for dl in range(2):
                src = bass.AP(tensor=dscr.tensor, offset=128 * dl, ap=[[1, 128], [384, 8], [1, 128]])
                P.dma(qtl[:, dl, :].rearrange("p (h t) -> p h t", h=8), src, "dsr", r=[dsc], w=[qtl])
            for dl in range(2):
                for c in range(2):
                    P.pe(lambda e, dl=dl, c=c: e.matmul(PA[:, c * 512:(c + 1) * 512], lhsT=cst[:, C_J:C_J + 128],
                                                        rhs=qtl[:, dl, c * 512:(c + 1) * 512], start=True, stop=True),
                         r=[cst, qtl], w=[PA])
                P.act(lambda e, dl=dl: e.activation(out=biasT[:, dl, :], in_=PA[:, :], func=AF.Copy), r=[PA], w=[biasT])
            for h in range(8):
                P.dve(lambda e, h=h: e.tensor_scalar(out=relx[:, h, :], in0=cst[0:32, C_ONE:C_ONE + 128],
                                                     scalar1=relb[:, h:h + 1], scalar2=None, op0=ALU.mult),
                      r=[cst, relb], w=[relx])
            for c in range(2):
                P.pe(lambda e, c=c: e.matmul(PA[:, c * 512:(c + 1) * 512], lhsT=cst[0:32, C_SEL:C_SEL + 128],
                                             rhs=relx[:, c * 4:(c + 1) * 4, :], start=True, stop=True),
                     r=[cst, relx], w=[PA])
            P.act(lambda e: e.activation(out=biasT[:, 2, :], in_=PA[:, :], func=AF.Copy), r=[PA], w=[biasT])
            dump("bias", biasT[:, :, :].rearrange("p a b -> p (a b)"), biasT)
            es1b.close()
            P.es_cur = es1
            P.fence()

            xt = P.tile([128, 2, D], F32, "xt")
            sqj = P.tile([128, D], BF16, "sqj")
            st4 = P.tile([128, 8], F32, "st4")
            dgt = P.tile([128, 2, 128], F32, "dgt")
            hT = P.tile([128, 8, 256], F32, "hT")
            cqraw = P.tile([128, 4, 256], F32, "cqraw")
            sq = P.tile([128, 4, 256], F32, "sq")
            rq = P.tile([128, 3, 256], F32, "rq")
            cqT = P.tile([128, 2, 256], F32, "cqT")
            cqTb = P.tile([128, 2, 256], BF16, "cqTb")
            iqT = P.tile([128, 8, 256], BF16, "iqP")
            iqtmp = P.tile([128, 4, 256], BF16, "iqtmp")
            ikf = P.tile([128, 256], F32, "ikf")
            iw = P.tile([128, 2, 8], F32, "iw")
            qTb = P.tile([128, 4, 256], BF16, "qTb")
            qaT2 = [P.tile([128, 8, 256], BF16, "qaT%d" % i) for i in range(2)]
            sc = TlG(P, es1.enter_context(nc.sbuf_tensor("sb_sc", [128, T], F32)), "sc", max(T // 512, 1))
            mk = [P.tile([128, T], BF16, "mk%d" % i) for i in range(2)]
            rl = [P.tile([128, 512], F32, "rl%d" % i) for i in range(3)]
            bsn = P.tile([128, 1], F32, "bsn")
            bss = P.tile([128, 1], F32, "bss")
            bsu = P.tile([128, 1], F32, "bsu")
            lg = [P.tile([128, 512], F32, "lg%d" % i) for i in range(3)]
            Ee = [P.tile([128, 512], BF16, "Ee%d" % i) for i in range(3)]
            Em = [P.tile([128, 512], BF16, "Em%d" % i) for i in range(3)]
            rD = P.tile([128, 1024], F32, "rD")
            oTb = P.tile([128, 8, 128], BF16, "oTb")
            thrs = P.tile([128, NT], F32, "thrs")
            ytl = [P.tile([128, 4, 128], F32, "ytl%d" % i) for i in range(2)]
            ydb = P.tile([128, 4, 128], F32, "ydb") if dbg else None
            print("SBUFREM A1", nc.sbuf_bytes_remaining)
            rot = [Bank(PA.p[0], PA.h, 0), Bank(PA.p[1], PA.h, 512), Bank(PD, PD.h, 0)]
            pending = [None, 0]

            def emit_S(qi, j):
                S = (qi + 1) * 128
                ncc = (S + 511) // 512
                idx = 0
                for h in range(8):
                    for cc in range(ncc):
                        wd = min(512, S - cc * 512)
                        bk = rot[idx % 3]
                        rb = rl[idx % 3]
                        idx += 1
                        scp = sc.p[cc]
                        P.pe(lambda e: e.matmul(bk.ap(0, wd), lhsT=iqT[:, h, j * 128:(j + 1) * 128],
                                                rhs=ikA[:, cc * 512:cc * 512 + wd], start=True, stop=False),
                             r=[iqT, ikA], w=[bk.tl])
                        P.pe(lambda e: e.matmul(bk.ap(0, wd), lhsT=iqT[:, h, j * 128:(j + 1) * 128],
                                                rhs=ikB[:, cc * 512:cc * 512 + wd], start=False, stop=True),
                             r=[iqT, ikB], w=[bk.tl])
                        P.act(lambda e: e.activation(out=rb[:, 0:wd], in_=bk.ap(0, wd), func=AF.Relu), r=[bk.tl], w=[rb])
                        if h == 0:
                            P.dve(lambda e: e.tensor_scalar(out=sc[:, cc * 512:cc * 512 + wd], in0=rb[:, 0:wd],
                                                            scalar1=iw[:, j, 0:1], scalar2=None, op0=ALU.mult),
                                  r=[rb, iw], w=[scp])
                        else:
                            P.dve(lambda e: e.scalar_tensor_tensor(out=sc[:, cc * 512:cc * 512 + wd], in0=rb[:, 0:wd],
                                                                   scalar=iw[:, j, h:h + 1],
                                                                   in1=sc[:, cc * 512:cc * 512 + wd], op0=ALU.mult, op1=ALU.add),
                                  r=[rb, iw, scp], w=[scp])
                scd = sc.p[(qi * 128) // 512]
                P.dve(lambda e: e.tensor_tensor(out=sc[:, qi * 128:(qi + 1) * 128], in0=sc[:, qi * 128:(qi + 1) * 128],
                                                in1=cst[:, C_NEG:C_NEG + 128], op=ALU.add), r=[scd, cst], w=[scd])

            def gen_B(qi):
                S = (qi + 1) * 128
                ncc = (S + 511) // 512
                scr = sc.p[0:ncc]
                mkb = mk[qi % 2]
                thr_c = float(2 * KTOP - S) - 0.5
                P.pool(lambda e: e.memset(bsn[:, :], 0.0), w=[bsn])
                for it in range(NB_IT):
                    ck = BIS_R / (2 ** it)
                    cn = ck / 2 if it < NB_IT - 1 else ck
                    P.act(lambda e: e.activation(out=mkb[:, 0:S], in_=sc[:, 0:S], func=AF.Sign, bias=bsn[:, 0:1],
                                                 accum_out=bss[:, 0:1]), r=scr + [bsn], w=[mkb, bss])
                    P.pool(lambda e: e.tensor_scalar(out=bsu[:, :], in0=bss[:, :], scalar1=thr_c, scalar2=-ck,
                                                     op0=ALU.is_ge, op1=ALU.mult), r=[bss], w=[bsu])
                    P.pool(lambda e: e.tensor_scalar(out=bsn[:, :], in0=bsu[:, :], scalar1=bsn[:, 0:1], scalar2=cn,
                                                     op0=ALU.add, op1=ALU.add), r=[bsu, bsn], w=[bsn])
                    yield
                P.act(lambda e: e.activation(out=mkb[:, 0:S], in_=sc[:, 0:S], func=AF.Sign, bias=bsn[:, 0:1]),
                      r=scr + [bsn], w=[mkb])
                if dbg:
                    P.dve(lambda e: e.tensor_scalar(out=thrs[:, qi:qi + 1], in0=bsn[:, 0:1], scalar1=-1.0, scalar2=None,
                                                    op0=ALU.mult), r=[bsn], w=[thrs])
                yield

            def gen_P(qi, j, qb):
                mkb = mk[qi % 2]
                qa = qaT2[qb]
                steps = [(kj, c) for kj in range(qi + 1) for c in range(2)]
                n = len(steps)

                def slot_of(kj):
                    k2 = kj % 2
                    return PT, PT[:, k2 * 512:k2 * 512 + 128]

                for i in range(n + 3):
                    if i < n:
                        kj, c = steps[i]
                        dl = min(qi - kj, 2)
                        bk = rot[i % 3]
                        if c == 0:
                            pts, ptap = slot_of(kj)
                            P.pe(lambda e: e.transpose(ptap, mkb[:, kj * 128:(kj + 1) * 128], identb[:, :]),
                                 r=[mkb, identb], w=[pts])
                        P.pe(lambda e: e.matmul(bk.ap(0, 512), lhsT=ckvT[:, kj * 128:(kj + 1) * 128],
                                                rhs=qa[:, c * 4:(c + 1) * 4, j * 128:(j + 1) * 128], start=True, stop=True),
                             r=[ckvT, qa], w=[bk.tl])
                    if 0 <= i - 3 < n:
                        kj, c = steps[i - 3]
                        emt = Em[(i - 3) % 3]
                        P.pe(lambda e: e.matmul(PB[:, c * 512:(c + 1) * 512], lhsT=ckvtm[:, kj, :], rhs=emt[:, :],
                                                start=(kj == 0), stop=(kj == qi)), r=[ckvtm, emt], w=[PB.p[c]])
                        P.pe(lambda e: e.matmul(PC[:, c * 512:(c + 1) * 512], lhsT=onesb[:, :], rhs=emt[:, :],
                                                start=(kj == 0), stop=(kj == qi)), r=[onesb, emt], w=[PC.p[c]])
                    if 0 <= i - 2 < n:
                        kj, c = steps[i - 2]
                        pts, ptap = slot_of(kj)
                        eet, emt = Ee[(i - 2) % 3], Em[(i - 2) % 3]
                        P.dve(lambda e: e.scalar_tensor_tensor(
                            out=emt[:, :].rearrange("p (h t) -> p h t", h=4),
                            in0=ptap.unsqueeze(1).to_broadcast([128, 4, 128]), scalar=1.0,
                            in1=eet[:, :].rearrange("p (h t) -> p h t", h=4), op0=ALU.add, op1=ALU.mult),
                            r=[eet, pts], w=[emt])
                    if i < n:
                        kj, c = steps[i]
                        dl = min(qi - kj, 2)
                        bk = rot[i % 3]
                        lgt = lg[i % 3]
                        P.dve(lambda e: e.tensor_tensor(out=lgt[:, :], in0=bk.ap(0, 512),
                                                        in1=biasT[:, dl, c * 512:(c + 1) * 512], op=ALU.add),
                              r=[bk.tl, biasT], w=[lgt])
                    if 0 <= i - 1 < n:
                        lgt, eet = lg[(i - 1) % 3], Ee[(i - 1) % 3]
                        P.act(lambda e: e.activation(out=eet[:, :], in_=lgt[:, :], func=AF.Exp), r=[lgt], w=[eet])
                    yield
                hs = n
                P.dve(lambda e: e.reciprocal(out=rD[:, :], in_=PC[:, :]), r=[PC], w=[rD])
                P.dve(lambda e: e.tensor_tensor(out=oTb[:, :, :].rearrange("p h t -> p (h t)"), in0=PB[:, :],
                                                in1=rD[:, :], op=ALU.mult), r=[PB, rD], w=[oTb])
                bk = rot[hs % 3]
                for m in range(4):
                    for hh in range(2):
                        h = 2 * m + hh
                        P.pe(lambda e: e.matmul(bk.ap(m * 128, (m + 1) * 128), lhsT=w_uvb[:, h, :], rhs=oTb[:, h, :],
                                                start=(hh == 0), stop=(hh == 1)), r=[w_uvb, oTb], w=[bk.tl])
                yt_ = ytl[qi % 2]
                P.act(lambda e: e.activation(out=yt_[:, :, :], in_=bk.ap(0, 512).rearrange("p (m t) -> p m t", m=4),
                                             func=AF.Copy), r=[bk.tl], w=[yt_])
                P.dma(yscr[qi * 128:(qi + 1) * 128, :], yt_[:, :, :].rearrange("p m t -> p (m t)"), "yst",
                      r=[yt_], w=[yscr_t], q="sp")
                if dbg:
                    P.dve(lambda e: e.tensor_copy(out=ydb[:, :, :], in_=bk.ap(0, 512).rearrange("p (m t) -> p m t", m=4)),
                          r=[bk.tl], w=[ydb])
                    dump("yatt", ydb[:, :, :], ydb,
                         dbg_d["yatt"][:, :].rearrange("p (m t) -> p m t", m=4)[:, :, qi * 128:(qi + 1) * 128])
                yield

            def interleave(ga, gb, na, nb):
                ia = ib = 0
                da = db = False
                while not (da and db):
                    if not da and (db or ia * nb <= ib * na):
                        try:
                            next(ga)
                            ia += 1
                        except StopIteration:
                            da = True
                    else:
                        try:
                            next(gb)
                            ib += 1
                        except StopIteration:
                            db = True

            for s in range(NS):
                for j in range(2):
                    r0 = s * 256 + j * 128
                    P.dma(xt[:, j, :], x_d[r0:r0 + 128, :], "xin", w=[xt])
                for j in range(2):
                    P.act(lambda e, j=j: e.activation(out=sqj[:, :], in_=xt[:, j, :], func=AF.Square,
                                                      accum_out=st4[:, j:j + 1]), r=[xt], w=[sqj, st4])
                P.act(lambda e: e.activation(out=st4[:, 2:4], in_=st4[:, 0:2], func=AF.Sqrt, scale=1.0 / D,
                                             bias=epsc[:, 0:1]), r=[st4, epsc], w=[st4])
                P.dve(lambda e: e.reciprocal(out=st4[:, 4:6], in_=st4[:, 2:4]), r=[st4], w=[st4])
                for j in range(2):
                    P.dve(lambda e, j=j: e.tensor_scalar(out=dgt[:, j, :], in0=cst[:, C_ID:C_ID + 128],
                                                         scalar1=st4[:, 4 + j:5 + j], scalar2=None, op0=ALU.mult),
                          r=[cst, st4], w=[dgt])
                for kk in range(2):
                    for k4 in range(4):
                        k = kk * 4 + k4
                        for j in range(2):
                            P.pe(lambda e, k=k, k4=k4, j=j: e.matmul(
                                PA[:, k4 * 256 + j * 128:k4 * 256 + (j + 1) * 128],
                                lhsT=xt[:, j, k * 128:(k + 1) * 128], rhs=dgt[:, j, :], start=True, stop=True),
                                r=[xt, dgt], w=[PA])
                    for k4 in range(4):
                        k = kk * 4 + k4
                        P.dve(lambda e, k=k, k4=k4: e.tensor_scalar(
                            out=hT[:, k, :], in0=PA[:, k4 * 256:(k4 + 1) * 256], scalar1=gsh[:, k:k + 1],
                            scalar2=modc[:, k:k + 1], op0=ALU.mult, op1=ALU.add), r=[PA, gsh, modc], w=[hT])
                for c in range(4):
                    for k in range(8):
                        P.pe(lambda e, c=c, k=k: e.matmul(PB[:, c * 256:(c + 1) * 256],
                                                          lhsT=w_att[:, k, c * 128:(c + 1) * 128], rhs=hT[:, k, :],
                                                          start=(k == 0), stop=(k == 7)), r=[w_att, hT], w=[PB])
                P.act(lambda e: e.activation(out=cqraw[:, :, :], in_=PB[:, :].rearrange("p (c t) -> p c t", c=4),
                                             func=AF.Copy), r=[PB], w=[cqraw])
                P.act(lambda e: e.activation(out=sq[:, :, :], in_=PB[:, :].rearrange("p (c t) -> p c t", c=4),
                                             func=AF.Square), r=[PB], w=[sq])
                for j in range(2):
                    for k in range(8):
                        P.pe(lambda e, j=j, k=k: e.matmul(PD[:, j * 8:(j + 1) * 8], lhsT=hT[:, k, j * 128:(j + 1) * 128],
                                                          rhs=w_iw[:, k, :], start=(k == 0), stop=(k == 7)),
                             r=[hT, w_iw], w=[PD])
                P.act(lambda e: e.activation(out=iw[:, :, :], in_=PD[:, 0:16].rearrange("p (j h) -> p j h", j=2),
                                             func=AF.Copy, scale=float(8 ** -0.5 * 64 ** -0.5)), r=[PD], w=[iw])
                for c in range(2):
                    P.pe(lambda e, c=c: e.matmul(PC[:, 0:256], lhsT=cst[:, C_ONE:C_ONE + 128], rhs=sq[:, c, :],
                                                 start=(c == 0), stop=(c == 1)), r=[cst, sq], w=[PC])
                P.pe(lambda e: e.matmul(PC[:, 256:512], lhsT=cst[:, C_ONE:C_ONE + 128], rhs=sq[:, 2, :],
                                        start=True, stop=True), r=[cst, sq], w=[PC])
                P.pe(lambda e: e.matmul(PC[:, 512:768], lhsT=cst[:, C_BLK:C_BLK + 128], rhs=sq[:, 3, :],
                                        start=True, stop=True), r=[cst, sq], w=[PC])
                for i, dv in enumerate((256.0, 128.0, 64.0)):
                    P.act(lambda e, i=i, dv=dv: e.activation(out=rq[:, i, :], in_=PC[:, i * 256:(i + 1) * 256],
                                                             func=AF.Sqrt, scale=1.0 / dv, bias=epsc[:, 0:1]),
                          r=[PC, epsc], w=[rq])
                P.dve(lambda e: e.reciprocal(out=rq[:, :, :], in_=rq[:, :, :]), r=[rq], w=[rq])
                for c in range(2):
                    P.dve(lambda e, c=c: e.scalar_tensor_tensor(out=cqT[:, c, :], in0=cqraw[:, c, :],
                                                                scalar=vec[:, V_QN + c:V_QN + c + 1], in1=rq[:, 0, :],
                                                                op0=ALU.mult, op1=ALU.mult), r=[cqraw, vec, rq], w=[cqT])
                P.act(lambda e: e.activation(out=cqTb[:, :, :], in_=cqT[:, :, :], func=AF.Copy), r=[cqT], w=[cqTb])
                P.dve(lambda e, s=s: e.scalar_tensor_tensor(out=ckvT[:, s * 256:(s + 1) * 256], in0=cqraw[:, 2, :],
                                                            scalar=vec[:, V_KVN:V_KVN + 1], in1=rq[:, 1, :],
                                                            op0=ALU.mult, op1=ALU.mult), r=[cqraw, vec, rq], w=[ckvT])
                P.dve(lambda e: e.scalar_tensor_tensor(out=ikf[:, :], in0=cqraw[:, 3, :],
                                                       scalar=vec[:, V_IKN:V_IKN + 1], in1=rq[:, 2, :],
                                                       op0=ALU.mult, op1=ALU.mult), r=[cqraw, vec, rq], w=[ikf])
                P.act(lambda e, s=s: e.activation(out=ikA[:, s * 256:(s + 1) * 256], in_=ikf[:, :], func=AF.Copy),
                      r=[ikf], w=[ikA])
                P.dve(lambda e, s=s: e.tensor_tensor(out=ikB[0:64, s * 256:(s + 1) * 256], in0=ikf[0:64, :],
                                                     in1=ikA[0:64, s * 256:(s + 1) * 256], op=ALU.subtract),
                      r=[ikf, ikA], w=[ikB])
                for j in range(2):
                    tix = s * 2 + j
                    P.pe(lambda e, j=j, tix=tix: e.transpose(PT[:, j * 128:(j + 1) * 128],
                                                             ckvT[:, tix * 128:(tix + 1) * 128], identb[:, :]),
                         r=[ckvT, identb], w=[PT])
                P.act(lambda e, s=s: e.activation(out=ckvtm[:, 2 * s:2 * s + 2, :],
                                                  in_=PT[:, 0:256].rearrange("p (j c) -> p j c", j=2), func=AF.Copy),
                      r=[PT], w=[ckvtm])
                for hf in range(2):
                    for h4 in range(4):
                        h = hf * 4 + h4
                        for c in range(2):
                            P.pe(lambda e, h=h, h4=h4, c=c: e.matmul(PB[:, h4 * 256:(h4 + 1) * 256],
                                                                     lhsT=w_iq[:, c, h * 128:(h + 1) * 128], rhs=cqT[:, c, :],
                                                                     start=(c == 0), stop=(c == 1)), r=[w_iq, cqT], w=[PB])
                    hsl = slice(hf * 4, hf * 4 + 4)
                    pb3 = lambda rows: PB[rows, :].rearrange("p (m t) -> p m t", m=4)
                    P.act(lambda e, hsl=hsl: e.activation(out=iqT[0:64, hsl, :], in_=pb3(slice(0, 64)), func=AF.Copy),
                          r=[PB], w=[iqT])
                    P.act(lambda e: e.activation(out=iqtmp[64:128, :, :], in_=pb3(slice(64, 128)), func=AF.Copy),
                          r=[PB], w=[iqtmp])
                    P.dve(lambda e, hsl=hsl: e.tensor_tensor(out=iqT[64:128, hsl, :], in0=pb3(slice(64, 128)),
                                                             in1=iqtmp[64:128, :, :], op=ALU.subtract),
                          r=[PB, iqtmp], w=[iqT])
                for m in range(4):
                    for c in range(2):
                        P.pe(lambda e, m=m, c=c: e.matmul(PC[:, m * 256:(m + 1) * 256],
                                                          lhsT=w_uqb[:, c, m * 128:(m + 1) * 128], rhs=cqTb[:, c, :],
                                                          start=(c == 0), stop=(c == 1)), r=[w_uqb, cqTb], w=[PC])
                P.dve(lambda e: e.tensor_copy(out=qTb[:, :, :], in_=PC[:, :].rearrange("p (m t) -> p m t", m=4)),
                      r=[PC], w=[qTb])
                for hf in range(2):
                    for h4 in range(4):
                        h = hf * 4 + h4
                        pr = slice((h % 2) * 64, (h % 2) * 64 + 64)
                        P.pe(lambda e, h=h, h4=h4, pr=pr: e.matmul(PB[:, h4 * 256:(h4 + 1) * 256],
                                                                   lhsT=w_ukb[:, h, :], rhs=qTb[:, h // 2, :],
                                                                   start=True, stop=True), r=[w_ukb, qTb], w=[PB])
                    P.act(lambda e, hf=hf, s=s: e.activation(out=qaT2[s % 2][:, hf * 4:(hf + 1) * 4, :],
                                                             in_=PB[:, :].rearrange("p (h t) -> p h t", h=4), func=AF.Copy,
                                                             scale=0.125), r=[PB], w=[qaT2[s % 2]])
                for j in range(2):
                    qi = s * 2 + j
                    emit_S(qi, j)
                    gB = gen_B(qi)
                    if pending[0] is not None:
                        interleave(gB, pending[0], NB_IT + 1, pending[1])
                    else:
                        for _ in gB:
                            pass
                    pending[0] = gen_P(qi, j, s % 2)
                    pending[1] = 2 * (qi + 1) + 4
            for _ in pending[0]:
                pass
            if dbg:
                dump("thr", thrs[:, :], thrs)

        print("ops after A1", P.nops)
        P.fence()
        if upto < 2:
            P.enabled = False
        x1t = Tl(P, None, "x1_hbm")
        with ExitStack() as es2:
            P.es_cur = es2
            w_rwb = P.tileg([128, 8, 1824], BF16, "w_rwb", 16)
            w_lorab = P.tile([128, 1024], BF16, "w_lorab")
            w_gateb = P.tile([128, 2, 512], BF16, "w_gateb")
            w_outb = P.tileg([128, 8, D], BF16, "w_outb", 8)
            mucol = P.tile([128, 32], F32, "mucol")
            lnrow = P.tile([128, 2, 512], F32, "lnrow")
            mix = P.tile([128, D], F32, "mix")
            x1s = [P.tile([128, D], F32, "x1s%d" % i) for i in range(1)]
            wsa = P.tile([128, 1024], F32, "wst2a")
            wsb = P.tile([128, 1024], F32, "wst2b")
            sbufs = [wsa, wsb, mix, x1s[0]]
            P.dve(lambda e: e.tensor_copy(out=mucol[:, 0:15], in_=vec[:, 108:123]), r=[vec], w=[mucol])
            P.dve(lambda e: e.tensor_scalar(out=mucol[:, 16:31], in0=vec[:, 108:123], scalar1=-1.0, scalar2=1.0,
                                            op0=ALU.mult, op1=ALU.add), r=[vec, mucol], w=[mucol])
            P.dve(lambda e: e.memset(w_gateb[:, :, :], 0.0), w=[w_gateb])
            jobs2 = []
            for k in range(8):
                for pi, (c0, c1) in enumerate(((0, 1024), (1024, 1824))):
                    jobs2.append((wrw_d[k * 128:(k + 1) * 128, c0:c1], w_rwb[:, k, c0:c1], w_rwb.p[2 * k + pi], c1 - c0, slice(0, 128)))
            for k in range(8):
                jobs2.append((wout_d[k * 128:(k + 1) * 128, :], w_outb[:, k, :], w_outb.p[k], D, slice(0, 128)))
            jobs2.append((wlora_d[:, :], w_lorab[:, :], w_lorab, 1024, slice(0, 128)))
            jobs2.append((wgate_d[0:128, :], w_gateb[:, 0, :], w_gateb, 512, slice(0, 128)))
            jobs2.append((wgate_d[128:160, :], w_gateb[96:128, 1, :], w_gateb, 512, slice(96, 128)))
            for ji, (src, dst, dtl, wd_, rows) in enumerate(jobs2):
                sb_ = sbufs[ji % 4]
                P.dma(sb_[rows, 0:wd_], src, "ws2%d" % (ji % 4), w=[sb_])
                P.conv(ji, dst, sb_[rows, 0:wd_], [sb_], [dtl])
            for i in range(2):
                P.dma(lnrow[:, i, :], lnrow_d[i, :].partition_broadcast(128), "cw", w=[lnrow])

            xt = P.tile([128, 2, D], F32, "xt2")
            sqj = P.tile([128, D], BF16, "sqj2")
            st4 = P.tile([128, 8], F32, "st42")
            dgt = P.tile([128, 2, 128], F32, "dgt2")
            hTb = P.tile([128, 8, 256], BF16, "hTb")
            lastc = TlG(P, es2.enter_context(nc.sbuf_tensor("sb_lastc", [128, 16], F32)), "lastc", 16)
            ptmp = [P.tile([128, 256], F32, "ptmp%d" % i) for i in range(3)]
            pch = [P.tile([128, 256], F32, "pch%d" % i) for i in range(3)]
            rT = P.tile([128, 4, 256], F32, "rT")
            kT = P.tile([128, 4, 256], F32, "kT")
            vTb = P.tile([128, 4, 256], BF16, "vTb")
            twd = P.tile([128, 256], BF16, "twd")
            sgd = P.tile([128, 2, 256], BF16, "sgd")
            ART = P.tile([128, 4, 2, 256], BF16, "ART")
            btT = P.tile([128, 8, 256], BF16, "btT")
            ktT = P.tile([128, 8, 256], BF16, "ktT")
            Hbz = P.tile([128, 8, 64], BF16, "Hbz")
            P.dve(lambda e: e.memset(btT[:, :, :], 0.0), w=[btT])
            P.dve(lambda e: e.memset(ktT[:, :, :], 0.0), w=[ktT])
            P.dve(lambda e: e.memset(Hbz[:, :, :], 0.0), w=[Hbz])
            bhT = P.tile([128, 4, 256], BF16, "bhT")
            khT = P.tile([128, 4, 256], BF16, "khT")
            rkT = P.tile([128, 4, 256], BF16, "rkT")
            WC = P.tile([128, 4, 2], F32, "WC")
            fsets = [[P.tile([128, 256], F32, "f%d_%d" % (q, i)) for i in range(9)] for q in range(2)]
            kk4 = P.tile([128, 4, 256], F32, "kk4")
            sq4 = P.tile([128, 4, 256], F32, "sq4")
            gtmp = P.tile([128, 256], F32, "gtmp")
            hw = P.tile([128, 8], F32, "hw")
            P.dve(lambda e: e.tensor_scalar(out=hw[:, :], in0=vec[:, V_W0:V_W0 + 8], scalar1=0.5, scalar2=None, op0=ALU.mult),
                  r=[vec], w=[hw])
            Vtm = P.tile([128, 512], BF16, "Vtm")
            Bhtm = P.tile([128, 512], BF16, "Bhtm")
            Khtm = P.tile([128, 512], BF16, "Khtm")
            ATab = P.tile([128, 8, 256], BF16, "ATab")
            ATak = P.tile([128, 8, 256], BF16, "ATak")
            def tlg2(name):
                return TlG(P, es2.enter_context(nc.sbuf_tensor("sb_" + name, [128, 8, 128], BF16)), name, 2)
            Nk = [tlg2("Nk%d" % i) for i in range(2)]
            Mk = [tlg2("Mk%d" % i) for i in range(2)]
            Pk = [tlg2("Pk%d" % i) for i in range(2)]
            Hs = P.tile([128, 4, 64], F32, "Hs")
            Hb = P.tile([128, 4, 64], BF16, "Hb")
            Hd = P.tile([128, 4, 64], F32, "Hd")
            WCf = P.tile([128, 4, 64], F32, "WCf")
            Gb = P.tile([128, 512], BF16, "Gb")
            Ub = P.tile([128, 512], BF16, "Ub")
            Ysb = P.tile([128, 512], F32, "Ysb")
            Ysq = P.tile([128, 512], F32, "Ysq")
            gst = P.tile([128, 48], F32, "gst")
            vtok = P.tile([128, 512], F32, "vtok")
            yrw = P.tile([128, 512], F32, "yrw")
            yrwb = P.tile([128, 512], BF16, "yrwb")
            ycat = P.tile([128, 4, 128], BF16, "ycat")
            ya = P.tile([128, 4, 128], BF16, "ya")
            yaf = P.tile([128, 512], F32, "yaf")
            oka = P.tile([128, 4], F32, "oka")
            hindb = P.tile([128, 2], BF16, "hindb")
            P.dve(lambda e: e.tensor_scalar(out=oka[:, :], in0=vec[:, V_KA:V_KA + 4], scalar1=-1.0, scalar2=1.0,
                                            op0=ALU.mult, op1=ALU.add), r=[vec], w=[oka])
            P.dve(lambda e: e.tensor_copy(out=hindb[:, :], in_=cst[:, C_HIND:C_HIND + 2]), r=[cst], w=[hindb])
            P.dve(lambda e: e.memset(lastc[:, :], 0.0), w=[lastc])
            print("SBUFREM A2", nc.sbuf_bytes_remaining)
            P.dve(lambda e: e.memset(Hs[:, :, :], 0.0), w=[Hs])
            P.dve(lambda e: e.memset(sgd[:, :, :], 0.0), w=[sgd])

            iprot = [Bank(PD, PD.h, 0), Bank(PC.p[0], PC.h, 0), Bank(PC.p[1], PC.h, 512), Bank(PB.p[1], PB.h, 512)]

            def inproj(n, width=128):
                c0 = n * 128 if n < 14 else 1824 - 128
                bk = iprot[n % 4]
                for k in range(8):
                    P.pe(lambda e, k=k: e.matmul(bk.ap(0, 256), lhsT=w_rwb[:, k, c0:c0 + 128], rhs=hTb[:, k, :],
                                                 start=(k == 0), stop=(k == 7)), r=[w_rwb, hTb], w=[bk.tl])
                pt = ptmp[n % 3]
                pc = pch[n % 3]
                lc = lastc.p[n]
                P.act(lambda e: e.activation(out=pt[:, :], in_=bk.ap(0, 256), func=AF.Copy,
                                             scale=mucol[:, 16 + n:17 + n]), r=[bk.tl, mucol], w=[pt])
                P.dve(lambda e: e.scalar_tensor_tensor(out=pc[:, 1:256], in0=bk.ap(0, 255),
                                                       scalar=mucol[:, n:n + 1], in1=pt[:, 1:256],
                                                       op0=ALU.mult, op1=ALU.add), r=[bk.tl, mucol, pt], w=[pc])
                P.dve(lambda e: e.scalar_tensor_tensor(out=pc[:, 0:1], in0=lastc[:, n:n + 1],
                                                       scalar=mucol[:, n:n + 1], in1=pt[:, 0:1],
                                                       op0=ALU.mult, op1=ALU.add), r=[lc, mucol, pt, pc], w=[pc])
                P.dve(lambda e: e.tensor_copy(out=lastc[:, n:n + 1], in_=bk.ap(255, 256)), r=[bk.tl, lc], w=[lc])
                return pc

            for s in range(NS):
                for j in range(2):
                    r0 = s * 256 + j * 128
                    P.dma(xt[:, j, :], x_d[r0:r0 + 128, :], "xin", w=[xt])
                for j in range(2):
                    P.act(lambda e, j=j: e.activation(out=sqj[:, :], in_=xt[:, j, :], func=AF.Square,
                                                      accum_out=st4[:, j:j + 1]), r=[xt], w=[sqj, st4])
                P.act(lambda e: e.activation(out=st4[:, 2:4], in_=st4[:, 0:2], func=AF.Sqrt, scale=1.0 / D,
                                             bias=epsc[:, 0:1]), r=[st4, epsc], w=[st4])
                P.dve(lambda e: e.reciprocal(out=st4[:, 4:6], in_=st4[:, 2:4]), r=[st4], w=[st4])
                for j in range(2):
                    P.dve(lambda e, j=j: e.tensor_scalar(out=dgt[:, j, :], in0=cst[:, C_ID:C_ID + 128],
                                                         scalar1=st4[:, 4 + j:5 + j], scalar2=None, op0=ALU.mult),
                          r=[cst, st4], w=[dgt])
                for kk in range(2):
                    for k4 in range(4):
                        k = kk * 4 + k4
                        for j in range(2):
                            P.pe(lambda e, k=k, k4=k4, j=j: e.matmul(
                                PA[:, k4 * 256 + j * 128:k4 * 256 + (j + 1) * 128],
                                lhsT=xt[:, j, k * 128:(k + 1) * 128], rhs=dgt[:, j, :], start=True, stop=True),
                                r=[xt, dgt], w=[PA])
                    for k4 in range(4):
                        k = kk * 4 + k4
                        P.dve(lambda e, k=k, k4=k4: e.tensor_scalar(
                            out=hTb[:, k, :], in0=PA[:, k4 * 256:(k4 + 1) * 256], scalar1=gsh[:, k:k + 1],
                            scalar2=modc[:, k:k + 1], op0=ALU.mult, op1=ALU.add), r=[PA, gsh, modc], w=[hTb])
                for m in range(4):
                    pc = inproj(m)
                    P.act(lambda e, m=m, pc=pc: e.activation(out=rT[:, m, :], in_=pc[:, :], func=AF.Copy), r=[pc], w=[rT])
                for m in range(4):
                    pc = inproj(4 + m)
                    P.act(lambda e, m=m, pc=pc: e.activation(out=kT[:, m, :], in_=pc[:, :], func=AF.Copy), r=[pc], w=[kT])
                for m in range(4):
                    pc = inproj(8 + m)
                    P.act(lambda e, m=m, pc=pc: e.activation(out=vTb[:, m, :], in_=pc[:, :], func=AF.Copy), r=[pc], w=[vTb])
                pc = inproj(12)
                P.act(lambda e, pc=pc: e.activation(out=twd[0:64, :], in_=pc[0:64, :], func=AF.Tanh), r=[pc], w=[twd])
                P.act(lambda e, pc=pc: e.activation(out=twd[64:128, :], in_=pc[64:128, :], func=AF.Copy), r=[pc], w=[twd])
                pc = inproj(13)
                P.act(lambda e, pc=pc: e.activation(out=gtmp[:, :], in_=pc[:, :], func=AF.Tanh, scale=0.5), r=[pc], w=[gtmp])
                P.dve(lambda e: e.tensor_scalar(out=sgd[:, 0, :], in0=gtmp[:, :], scalar1=0.5, scalar2=0.5, op0=ALU.mult,
                                                op1=ALU.add), r=[gtmp], w=[sgd])
                pc = inproj(14)
                P.act(lambda e, pc=pc: e.activation(out=gtmp[:, :], in_=pc[:, :], func=AF.Tanh, scale=0.5), r=[pc], w=[gtmp])
                P.dve(lambda e: e.tensor_scalar(out=sgd[:, 1, :], in0=gtmp[:, :], scalar1=0.5, scalar2=0.5, op0=ALU.mult,
                                                op1=ALU.add), r=[gtmp], w=[sgd])
                for m in range(4):
                    P.dve(lambda e, m=m: e.tensor_scalar(out=kk4[:, m, :], in0=kT[:, m, :], scalar1=vec[:, V_KK + m:V_KK + m + 1],
                                                         scalar2=None, op0=ALU.mult), r=[kT, vec], w=[kk4])
                P.act(lambda e: e.activation(out=sq4[:, :, :], in_=kk4[:, :, :], func=AF.Square), r=[kk4], w=[sq4])
                for m in range(4):
                    P.pe(lambda e, m=m: e.matmul(PA[:, m * 256:(m + 1) * 256], lhsT=cst[:, C_BLK:C_BLK + 128], rhs=sq4[:, m, :],
                                                 start=True, stop=True), r=[cst, sq4], w=[PA])
                sq4f = sq4[:, :, :].rearrange("p m t -> p (m t)")
                P.act(lambda e: e.activation(out=sq4f, in_=PA[:, :], func=AF.Sqrt), r=[PA], w=[sq4])
                P.dve(lambda e: e.tensor_scalar(out=sq4f, in0=sq4f, scalar1=1e-12, scalar2=None, op0=ALU.max), r=[sq4], w=[sq4])
                P.dve(lambda e: e.reciprocal(out=sq4f, in_=sq4f), r=[sq4], w=[sq4])
                P.dve(lambda e: e.tensor_tensor(out=kk4[:, :, :], in0=kk4[:, :, :], in1=sq4[:, :, :], op=ALU.mult),
                      r=[kk4, sq4], w=[kk4])
                for m in range(4):
                    ms = slice(m * 128, (m + 1) * 128)
                    pg = PB if m % 2 == 0 else PC
                    P.pe(lambda e, m=m: e.matmul(pg[:, 0:256], lhsT=w_lorab[:, m * 128:(m + 1) * 128], rhs=twd[:, :],
                                                 start=True, stop=True), r=[w_lorab, twd], w=[pg.p[0]])
                    P.pe(lambda e, m=m: e.matmul(pg[:, 256:512], lhsT=w_lorab[:, 512 + m * 128:512 + (m + 1) * 128], rhs=twd[:, :],
                                                 start=True, stop=True), r=[w_lorab, twd], w=[pg.p[0]])
                    sgw, cs, wt, winv, wprev, asig, kp, t1, t2 = fsets[m % 2]
                    P.act(lambda e, m=m: e.activation(out=t1[:, :], in_=pg[:, 0:256], func=AF.Tanh, scale=0.5,
                                                      bias=hw[:, m:m + 1]), r=[pg.p[0], hw], w=[t1])
                    P.dve(lambda e: e.tensor_scalar(out=sgw[:, :], in0=t1[:, :], scalar1=0.5, scalar2=0.5, op0=ALU.mult,
                                                    op1=ALU.add), r=[t1], w=[sgw])
                    P.act(lambda e, m=m: e.activation(out=t2[:, :], in_=pg[:, 256:512], func=AF.Tanh, scale=0.5,
                                                      bias=hw[:, 4 + m:5 + m]), r=[pg.p[0], hw], w=[t2])
                    P.dve(lambda e: e.tensor_scalar(out=asig[:, :], in0=t2[:, :], scalar1=0.5, scalar2=0.5, op0=ALU.mult,
                                                    op1=ALU.add), r=[t2], w=[asig])
                    P.dve(lambda e: e.tensor_tensor_scan(out=cs[:, :], data0=cst[:, C_SCAN:C_SCAN + 256], data1=sgw[:, :],
                                                         initial=0.0, op0=ALU.mult, op1=ALU.add), r=[cst, sgw], w=[cs])
                    P.act(lambda e: e.activation(out=wt[:, :], in_=cs[:, :], func=AF.Exp, scale=-LWC), r=[cs], w=[wt])
                    P.act(lambda e: e.activation(out=winv[:, :], in_=cs[:, :], func=AF.Exp, scale=LWC), r=[cs], w=[winv])
                    P.dve(lambda e: e.tensor_tensor(out=t1[:, :], in0=cs[:, :], in1=sgw[:, :], op=ALU.subtract),
                          r=[cs, sgw], w=[t1])
                    P.act(lambda e: e.activation(out=wprev[:, :], in_=t1[:, :], func=AF.Exp, scale=-LWC), r=[t1], w=[wprev])
                    for j in range(2):
                        cj = slice(j * 128, (j + 1) * 128)
                        P.dve(lambda e, m=m, j=j: e.tensor_scalar(out=WC[:, m, j:j + 1], in0=cs[:, j * 128 + 127:j * 128 + 128],
                                                                  scalar1=-LWC, scalar2=None, op0=ALU.mult),
                              r=[cs], w=[WC])
                        P.act(lambda e, m=m, j=j, cj=cj: e.activation(out=t2[:, cj], in_=cs[:, cj], func=AF.Exp, scale=LWC,
                                                                      bias=WC[:, m, j:j + 1]), r=[cs, WC], w=[t2])
                    P.act(lambda e, m=m: e.activation(out=WC[:, m, :], in_=WC[:, m, :], func=AF.Exp), r=[WC], w=[WC])
                    P.dve(lambda e, m=m: e.tensor_scalar(out=t1[:, :], in0=asig[:, :], scalar1=vec[:, V_KA + m:V_KA + m + 1],
                                                         scalar2=oka[:, m:m + 1], op0=ALU.mult, op1=ALU.add),
                          r=[asig, vec, oka], w=[t1])
                    P.dve(lambda e, m=m: e.tensor_tensor(out=kp[:, :], in0=kT[:, m, :], in1=t1[:, :], op=ALU.mult),
                          r=[kT, t1], w=[kp])
                    P.dve(lambda e, m=m: e.scalar_tensor_tensor(out=rkT[:, m, :], in0=rT[:, m, :],
                                                                scalar=vec[:, V_RK + m:V_RK + m + 1], in1=kp[:, :],
                                                                op0=ALU.mult, op1=ALU.mult), r=[rT, vec, kp], w=[rkT])
                    P.pool(lambda e, m=m: e.tensor_tensor(out=ART[:, m, 1, :], in0=rT[:, m, :], in1=wt[:, :], op=ALU.mult),
                           r=[rT, wt], w=[ART])
                    P.dve(lambda e, m=m: e.scalar_tensor_tensor(out=ART[:, m, 0, :], in0=kk4[:, m, :], scalar=-1.0,
                                                                in1=wprev[:, :], op0=ALU.mult, op1=ALU.mult),
                          r=[kk4, wprev], w=[ART])
                    P.dve(lambda e, m=m: e.tensor_tensor(out=t1[:, :], in0=kk4[:, m, :], in1=asig[:, :], op=ALU.mult),
                          r=[kk4, asig], w=[t1])
                    for hh in range(2):
                        prr = slice(hh * 64, hh * 64 + 64)
                        P.pool(lambda e, m=m, hh=hh, prr=prr: e.tensor_tensor(out=btT[prr, 2 * m + hh, :], in0=t1[prr, :],
                                                                              in1=winv[prr, :], op=ALU.mult),
                               r=[t1, winv], w=[btT])
                    P.pool(lambda e, m=m: e.tensor_tensor(out=bhT[:, m, :], in0=t1[:, :], in1=t2[:, :], op=ALU.mult),
                           r=[t1, t2], w=[bhT])
                    for hh in range(2):
                        prr = slice(hh * 64, hh * 64 + 64)
                        P.dve(lambda e, m=m, hh=hh, prr=prr: e.tensor_tensor(out=ktT[prr, 2 * m + hh, :], in0=kp[prr, :],
                                                                             in1=winv[prr, :], op=ALU.mult),
                              r=[kp, winv], w=[ktT])
                    P.pool(lambda e, m=m: e.tensor_tensor(out=khT[:, m, :], in0=kp[:, :], in1=t2[:, :], op=ALU.mult),
                           r=[kp, t2], w=[khT])
                for j in range(2):
                    ti = s * 2 + j
                    tb = slice(j * 128, (j + 1) * 128)
                    for (src, dst) in ((vTb, Vtm), (bhT, Bhtm), (khT, Khtm)):
                        for m in range(4):
                            P.pe(lambda e, src=src, m=m: e.transpose(PT[:, m * 128:(m + 1) * 128], src[:, m, tb], identb[:, :]),
                                 r=[src, identb], w=[PT])
                        P.act(lambda e, dst=dst: e.activation(out=dst[:, :], in_=PT[:, 0:512], func=AF.Copy), r=[PT], w=[dst])
                    P.dve(lambda e: e.tensor_copy(out=vtok[:, :], in_=Vtm[:, :]), r=[Vtm], w=[vtok])
                    for hf in range(2):
                        for h4 in range(4):
                            h = hf * 4 + h4
                            m = h // 2
                            pr = slice((h % 2) * 64, (h % 2) * 64 + 64)
                            P.pe(lambda e, h4=h4, m=m, h=h: e.matmul(PA[:, h4 * 256:(h4 + 1) * 256], lhsT=btT[:, h, tb],
                                                                     rhs=ART[:, m, :, tb], start=True, stop=True),
                                 r=[btT, ART], w=[PA])
                            P.pe(lambda e, h4=h4, m=m, h=h: e.matmul(PB[:, h4 * 256:(h4 + 1) * 256], lhsT=ktT[:, h, tb],
                                                                     rhs=ART[:, m, :, tb], start=True, stop=True),
                                 r=[ktT, ART], w=[PB])
                        mur = cst[:, C_MSU:C_MSU + 256].unsqueeze(1).to_broadcast([128, 4, 256])
                        msl = cst[:, C_MSL:C_MSL + 128].unsqueeze(1).to_broadcast([128, 4, 128])
                        msu = cst[:, C_MSU:C_MSU + 128].unsqueeze(1).to_broadcast([128, 4, 128])
                        hs4 = slice(hf * 4, hf * 4 + 4)
                        P.dve(lambda e, hs4=hs4, mur=mur: e.tensor_tensor(
                            out=ATab[:, hs4, :], in0=PA[:, :].rearrange("p (h t) -> p h t", h=4), in1=mur, op=ALU.mult),
                            r=[PA, cst], w=[ATab])
                        P.dve(lambda e, hs4=hs4, mur=mur: e.tensor_tensor(
                            out=ATak[:, hs4, :], in0=PB[:, :].rearrange("p (h t) -> p h t", h=4), in1=mur, op=ALU.mult),
                            r=[PB, cst], w=[ATak])
                    P.act(lambda e: e.activation(out=Mk[0][:, :, :], in_=ATab[:, :, 0:128], func=AF.Copy), r=[ATab], w=[Mk[0]])
                    for h in range(8):
                        P.pe(lambda e, h=h: e.transpose(PT[:, h * 128:(h + 1) * 128], Mk[0][:, h, :], identb[:, :]),
                             r=[Mk[0], identb], w=[PT])
                    P.act(lambda e: e.activation(out=Nk[0][:, :, :].rearrange("p h t -> p (h t)"), in_=PT[:, :], func=AF.Copy),
                          r=[PT], w=[Nk[0]])
                    P.dve(lambda e: e.tensor_tensor(out=Pk[0][:, :, :], in0=ATab[:, :, 0:128],
                                                    in1=identb[:, :].unsqueeze(1).to_broadcast([128, 8, 128]), op=ALU.add),
                          r=[ATab, identb], w=[Pk[0]])
                    for lv in range(6):
                        a, b = lv % 2, (lv + 1) % 2
                        for g in range(2):
                            for h in range(4 * g, 4 * g + 4):
                                P.pe(lambda e: e.matmul(PA[:, h * 128:(h + 1) * 128], lhsT=Mk[a][:, h, :], rhs=Nk[a][:, h, :],
                                                        start=True, stop=True), r=[Mk[a].p[g], Nk[a].p[g]], w=[PA.p[g]])
                        for g in range(2):
                            P.act(lambda e: e.activation(out=Nk[b][:, 4 * g:4 * g + 4, :].rearrange("p h t -> p (h t)"),
                                                         in_=PA[:, g * 512:(g + 1) * 512], func=AF.Copy),
                                  r=[PA.p[g]], w=[Nk[b].p[g]])
                        if lv < 5:
                            for g in range(2):
                                for h in range(4 * g, 4 * g + 4):
                                    P.pe(lambda e: e.matmul(PB[:, h * 128:(h + 1) * 128], lhsT=Nk[a][:, h, :], rhs=Mk[a][:, h, :],
                                                            start=True, stop=True), r=[Mk[a].p[g], Nk[a].p[g]], w=[PB.p[g]])
                            P.act(lambda e: e.activation(out=Mk[b][:, 0:4, :].rearrange("p h t -> p (h t)"),
                                                         in_=PB[:, 0:512], func=AF.Copy), r=[PB.p[0]], w=[Mk[b].p[0]])
                            P.dve(lambda e: e.tensor_copy(out=Mk[b][:, 4:8, :].rearrange("p h t -> p (h t)"), in_=PB[:, 512:1024]),
                                  r=[PB.p[1]], w=[Mk[b].p[1]])
                        for g in range(2):
                            for h in range(4 * g, 4 * g + 4):
                                P.pe(lambda e: e.matmul(PC[:, h * 128:(h + 1) * 128], lhsT=Nk[b][:, h, :], rhs=Pk[a][:, h, :],
                                                        start=True, stop=True), r=[Nk[b].p[g], Pk[a].p[g]], w=[PC.p[g]])
                        for g in range(2):
                            P.dve(lambda e: e.tensor_tensor(out=Pk[b][:, 4 * g:4 * g + 4, :].rearrange("p h t -> p (h t)"),
                                                            in0=PC[:, g * 512:(g + 1) * 512],
                                                            in1=Pk[a][:, 4 * g:4 * g + 4, :].rearrange("p h t -> p (h t)"),
                                                            op=ALU.add), r=[PC.p[g], Pk[a].p[g]], w=[Pk[b].p[g]])
                    XT = Pk[0]
                    for m in range(4):
                        P.dve(lambda e, m=m: e.tensor_scalar(out=WCf[:, m, :], in0=cst[:, C_ONE:C_ONE + 64],
                                                             scalar1=WC[:, m, j:j + 1], scalar2=None, op0=ALU.mult),
                              r=[cst, WC], w=[WCf])
                    P.dve(lambda e: e.tensor_tensor(out=Hd[:, :, :], in0=Hs[:, :, :], in1=WCf[:, :, :], op=ALU.mult),
                          r=[Hs, WCf], w=[Hd])
                    for h in range(8):
                        m = h // 2
                        pr = slice((h % 2) * 64, (h % 2) * 64 + 64)
                        hc = slice(h * 64, (h + 1) * 64)
                        P.pe(lambda e, m=m, h=h, hc=hc: e.matmul(PD[:, hc], lhsT=ART[:, m, 0, tb], rhs=Hbz[:, h, :],
                                                                 start=True, stop=False), r=[ART, Hbz], w=[PD])
                        P.pe(lambda e, h=h, hc=hc: e.matmul(PD[:, hc], lhsT=ATak[:, h, 0:128], rhs=Vtm[:, hc],
                                                            start=False, stop=True), r=[ATak, Vtm], w=[PD])
                    P.act(lambda e: e.activation(out=Gb[:, :], in_=PD[:, :], func=AF.Copy), r=[PD], w=[Gb])
                    for h in range(8):
                        hc = slice(h * 64, (h + 1) * 64)
                        P.pe(lambda e, h=h, hc=hc: e.matmul(PA[:, hc], lhsT=XT[:, h, :], rhs=Gb[:, hc], start=True, stop=True),
                             r=[XT, Gb], w=[PA])
                    P.act(lambda e: e.activation(out=Ub[:, :], in_=PA[:, 0:512], func=AF.Copy), r=[PA], w=[Ub])
                    for h in range(8):
                        m = h // 2
                        pr = slice((h % 2) * 64, (h % 2) * 64 + 64)
                        hc = slice(h * 64, (h + 1) * 64)
                        P.pe(lambda e, m=m, h=h, hc=hc: e.matmul(PB[:, hc], lhsT=ART[:, m, 1, tb], rhs=Hbz[:, h, :],
                                                                 start=True, stop=False), r=[ART, Hbz], w=[PB])
                        P.pe(lambda e, h=h, hc=hc: e.matmul(PB[:, hc], lhsT=ATab[:, h, 128:256], rhs=Ub[:, hc],
                                                            start=False, stop=False), r=[ATab, Ub], w=[PB])
                        P.pe(lambda e, h=h, hc=hc: e.matmul(PB[:, hc], lhsT=ATak[:, h, 128:256], rhs=Vtm[:, hc],
                                                            start=False, stop=True), r=[ATak, Vtm], w=[PB])
                    for m in range(4):
                        ms = slice(m * 128, (m + 1) * 128)
                        P.pe(lambda e, ms=ms: e.matmul(PC[:, ms], lhsT=Bhtm[:, ms], rhs=Ub[:, ms], start=True, stop=False),
                             r=[Bhtm, Ub], w=[PC])
                        P.pe(lambda e, ms=ms: e.matmul(PC[:, ms], lhsT=Khtm[:, ms], rhs=Vtm[:, ms], start=False, stop=True),
                             r=[Khtm, Vtm], w=[PC])
                    for hh in range(2):
                        pr = slice(hh * 64, hh * 64 + 64)
                        src = PC[pr, 0:512].rearrange("p (m c) -> p m c", m=4)[:, :, hh * 64:hh * 64 + 64]
                        P.dve(lambda e, pr=pr, src=src: e.tensor_tensor(out=Hs[pr, :, :], in0=Hd[pr, :, :], in1=src, op=ALU.add),
                              r=[Hd, PC], w=[Hs])
                    for hh in range(2):
                        prr = slice(hh * 64, hh * 64 + 64)
                        P.act(lambda e, hh=hh, prr=prr: e.activation(out=Hbz[prr, hh:8:2, :], in_=Hs[prr, :, :], func=AF.Copy),
                              r=[Hs], w=[Hbz])
                    P.act(lambda e: e.activation(out=Ysb[:, :], in_=PB[:, 0:512], func=AF.Copy), r=[PB], w=[Ysb])
                    P.act(lambda e: e.activation(out=Ysq[:, :], in_=PB[:, 0:512], func=AF.Square), r=[PB], w=[Ysq])
                    P.dve(lambda e: e.tensor_reduce(out=gst[:, 0:8], in_=Ysb[:, :].rearrange("p (h i) -> p h i", h=8),
                                                    axis=AX.X, op=ALU.add), r=[Ysb], w=[gst])
                    P.dve(lambda e: e.tensor_reduce(out=gst[:, 8:16], in_=Ysq[:, :].rearrange("p (h i) -> p h i", h=8),
                                                    axis=AX.X, op=ALU.add), r=[Ysq, gst], w=[gst])
                    P.dve(lambda e: e.tensor_scalar(out=gst[:, 16:24], in0=gst[:, 0:8], scalar1=1.0 / 64, scalar2=None,
                                                    op0=ALU.mult), r=[gst], w=[gst])
                    P.dve(lambda e: e.tensor_tensor(out=gst[:, 24:32], in0=gst[:, 16:24], in1=gst[:, 16:24], op=ALU.mult),
                          r=[gst], w=[gst])
                    P.dve(lambda e: e.scalar_tensor_tensor(out=gst[:, 32:40], in0=gst[:, 8:16], scalar=1.0 / 64,
                                                           in1=gst[:, 24:32], op0=ALU.mult, op1=ALU.subtract),
                          r=[gst], w=[gst])
                    P.act(lambda e: e.activation(out=gst[:, 40:48], in_=gst[:, 32:40], func=AF.Sqrt, bias=epsc[:, 1:2]),
                          r=[gst, epsc], w=[gst])
                    P.dve(lambda e: e.reciprocal(out=gst[:, 40:48], in_=gst[:, 40:48]), r=[gst], w=[gst])
                    y3 = lambda t: t[:, :].rearrange("p (h i) -> p h i", h=8)
                    P.dve(lambda e: e.tensor_tensor(out=y3(Ysb), in0=y3(Ysb),
                                                    in1=gst[:, 16:24].unsqueeze(2).to_broadcast([128, 8, 64]), op=ALU.subtract),
                          r=[Ysb, gst], w=[Ysb])
                    P.dve(lambda e: e.tensor_tensor(out=y3(Ysb), in0=y3(Ysb),
                                                    in1=gst[:, 40:48].unsqueeze(2).to_broadcast([128, 8, 64]), op=ALU.mult),
                          r=[Ysb, gst], w=[Ysb])
                    P.dve(lambda e: e.tensor_tensor(out=Ysb[:, :], in0=Ysb[:, :], in1=lnrow[:, 0, :], op=ALU.mult),
                          r=[Ysb, lnrow], w=[Ysb])
                    P.dve(lambda e: e.tensor_tensor(out=Ysb[:, :], in0=Ysb[:, :], in1=lnrow[:, 1, :], op=ALU.add),
                          r=[Ysb, lnrow], w=[Ysb])
                    for m in range(4):
                        P.pe(lambda e, m=m: e.matmul(PD[:, 2 * m:2 * m + 2], lhsT=rkT[:, m, tb], rhs=hindb[:, :],
                                                     start=True, stop=True), r=[rkT, hindb], w=[PD])
                    P.act(lambda e: e.activation(out=gst[:, 0:8], in_=PD[:, 0:8], func=AF.Copy), r=[PD, gst], w=[gst])
                    P.dve(lambda e: e.tensor_tensor(out=y3(Ysq), in0=y3(vtok),
                                                    in1=gst[:, 0:8].unsqueeze(2).to_broadcast([128, 8, 64]), op=ALU.mult),
                          r=[vtok, gst], w=[Ysq])
                    P.dve(lambda e: e.tensor_tensor(out=Ysb[:, :], in0=Ysb[:, :], in1=Ysq[:, :], op=ALU.add),
                          r=[Ysb, Ysq], w=[Ysb])
                    P.pe(lambda e: e.matmul(PA[:, 512:1024], lhsT=sgd[:, 0, tb], rhs=w_gateb[:, 0, :], start=True, stop=False),
                         r=[sgd, w_gateb], w=[PA])
                    P.pe(lambda e: e.matmul(PA[:, 512:1024], lhsT=sgd[:, 1, tb], rhs=w_gateb[:, 1, :], start=False, stop=True),
                         r=[sgd, w_gateb], w=[PA])
                    P.dve(lambda e: e.tensor_tensor(out=yrw[:, :], in0=Ysb[:, :], in1=PA[:, 512:1024], op=ALU.mult),
                          r=[Ysb, PA], w=[yrw])
                    dump("yrw", yrw[:, :], yrw, dbg_d["yrw"][ti * 128:(ti + 1) * 128, :] if dbg else None)
                    P.act(lambda e: e.activation(out=yrwb[:, :], in_=yrw[:, :], func=AF.Copy), r=[yrw], w=[yrwb])
                    for m in range(4):
                        P.pe(lambda e, m=m: e.transpose(PT[:, m * 128:(m + 1) * 128], yrwb[:, m * 128:(m + 1) * 128], identb[:, :]),
                             r=[yrwb, identb], w=[PT])
                    P.act(lambda e: e.activation(out=ycat[:, :, :].rearrange("p m t -> p (m t)"), in_=PT[:, 0:512], func=AF.Copy),
                          r=[PT], w=[ycat])
                    P.dma(yaf[:, :], yscr[ti * 128:(ti + 1) * 128, :], "yld", r=[yscr_t], w=[yaf])
                    P.act(lambda e: e.activation(out=ya[:, :, :].rearrange("p m t -> p (m t)"), in_=yaf[:, :], func=AF.Copy),
                          r=[yaf], w=[ya])
                    for c in range(2):
                        cs_ = slice(c * 512, (c + 1) * 512)
                        for k in range(8):
                            lhs = (lambda k=k: ya[:, k, :]) if k < 4 else (lambda k=k: ycat[:, k - 4, :])
                            P.pe(lambda e, k=k, cs_=cs_, lhs=lhs: e.matmul(PC[:, cs_], lhsT=lhs(), rhs=w_outb[:, k, cs_],
                                                                           start=(k == 0), stop=(k == 7)),
                                 r=[ya, ycat, w_outb], w=[PC])
                    P.act(lambda e: e.activation(out=mix[:, :], in_=PC[:, :], func=AF.Copy), r=[PC], w=[mix])
                    P.act(lambda e: e.activation(out=sqj[:, :], in_=PC[:, :], func=AF.Square, accum_out=st4[:, 6:7]),
                          r=[PC], w=[sqj, st4])
                    P.act(lambda e: e.activation(out=st4[:, 7:8], in_=st4[:, 6:7], func=AF.Sqrt, scale=1.0 / D,
                                                 bias=epsc[:, 0:1]), r=[st4, epsc], w=[st4])
                    P.dve(lambda e: e.reciprocal(out=st4[:, 7:8], in_=st4[:, 7:8]), r=[st4], w=[st4])
                    xo = x1s[0]
                    P.dve(lambda e: e.scalar_tensor_tensor(out=mix[:, :], in0=mix[:, :], scalar=st4[:, 7:8], in1=GMrow[:, :],
                                                           op0=ALU.mult, op1=ALU.mult), r=[mix, st4, GMrow], w=[mix])
                    P.dve(lambda e, xo=xo: e.tensor_tensor(out=xo[:, :], in0=mix[:, :], in1=xt[:, j, :], op=ALU.add),
                          r=[mix, xt], w=[xo])
                    P.dma(out_d[ti * 128:(ti + 1) * 128, :], xo[:, :], "x1st", r=[xo], w=[x1t], q="sp")
                    dump("x1", xo[:, :], xo, dbg_d["x1"][ti * 128:(ti + 1) * 128, :] if dbg else None)

        print("ops after A2", P.nops)
        P.fence()
        if upto < 3:
            P.enabled = False
        outt = Tl(P, None, "out_hbm")
        with ExitStack() as es3:
            P.es_cur = es3
            w_fgb = P.tileg([128, 8, DFF], BF16, "w_fgb", 24)
            w_fub = P.tileg([128, 8, DFF], BF16, "w_fub", 24)
            w_fdb = P.tileg([128, 22, D], BF16, "w_fdb", 22)
            wst = [P.tile([128, 1024], F32, "wst3%d" % i) for i in range(4)]
            jobs = []
            for (wd_, wb) in ((wfg_d, w_fgb), (wfu_d, w_fub)):
                for k in range(8):
                    for pi, (c0, c1) in enumerate(((0, 1024), (1024, 2048), (2048, DFF))):
                        jobs.append((wd_[k * 128:(k + 1) * 128, c0:c1], wb[:, k, c0:c1], wb.p[3 * k + pi], c1 - c0))
            for k in range(22):
                jobs.append((wfd_d[k * 128:(k + 1) * 128, :], w_fdb[:, k, :], w_fdb.p[k], D))
            for ji, (src, dst, dtl, wd_) in enumerate(jobs):
                ws = wst[ji % 4]
                P.dma(ws[:, 0:wd_], src, "wst3%d" % (ji % 4), w=[ws])
                P.conv(ji, dst, ws[:, 0:wd_], [ws], [dtl])
            xt = P.tile([128, 2, D], F32, "xt3")
            sqj = P.tile([128, D], BF16, "sqj3")
            st4 = P.tile([128, 8], F32, "st43")
            dgt = P.tile([128, 2, 128], F32, "dgt3")
            hfT = P.tile([128, 8, 256], BF16, "hfT")
            sg = [P.tile([128, 256], F32, "sg%d" % i) for i in range(2)]
            aT = P.tile([128, 22, 256], BF16, "aT")
            fo = P.tile([128, D], F32, "fo")
            ost = [P.tile([128, D], F32, "ost%d" % i) for i in range(1)]
            print("SBUFREM B", nc.sbuf_bytes_remaining)
            for s in range(NS):
                for j in range(2):
                    r0 = s * 256 + j * 128
                    P.dma(xt[:, j, :], out_d[r0:r0 + 128, :], "xin3", r=[x1t], w=[xt])
                for j in range(2):
                    P.act(lambda e, j=j: e.activation(out=sqj[:, :], in_=xt[:, j, :], func=AF.Square,
                                                      accum_out=st4[:, j:j + 1]), r=[xt], w=[sqj, st4])
                P.act(lambda e: e.activation(out=st4[:, 2:4], in_=st4[:, 0:2], func=AF.Sqrt, scale=1.0 / D,
                                             bias=epsc[:, 0:1]), r=[st4, epsc], w=[st4])
                P.dve(lambda e: e.reciprocal(out=st4[:, 4:6], in_=st4[:, 2:4]), r=[st4], w=[st4])
                for j in range(2):
                    P.dve(lambda e, j=j: e.tensor_scalar(out=dgt[:, j, :], in0=cst[:, C_ID:C_ID + 128],
                                                         scalar1=st4[:, 4 + j:5 + j], scalar2=None, op0=ALU.mult),
                          r=[cst, st4], w=[dgt])
                for kk in range(2):
                    for k4 in range(4):
                        k = kk * 4 + k4
                        for j in range(2):
                            P.pe(lambda e, k=k, k4=k4, j=j: e.matmul(
                                PA[:, k4 * 256 + j * 128:k4 * 256 + (j + 1) * 128],
                                lhsT=xt[:, j, k * 128:(k + 1) * 128], rhs=dgt[:, j, :], start=True, stop=True),
                                r=[xt, dgt], w=[PA])
                    for k4 in range(4):
                        k = kk * 4 + k4
                        P.dve(lambda e, k=k, k4=k4: e.tensor_scalar(
                            out=hfT[:, k, :], in0=PA[:, k4 * 256:(k4 + 1) * 256], scalar1=gsh[:, 8 + k:9 + k],
                            scalar2=modc[:, 24 + k:25 + k], op0=ALU.mult, op1=ALU.add), r=[PA, gsh, modc], w=[hfT])
                for n in range(22):
                    ns = slice(n * 128, (n + 1) * 128)
                    pg = PB if n % 2 == 0 else PC
                    for k in range(8):
                        P.pe(lambda e, k=k, ns=ns, pg=pg: e.matmul(pg[:, 0:256], lhsT=w_fgb[:, k, ns], rhs=hfT[:, k, :],
                                                                   start=(k == 0), stop=(k == 7)), r=[w_fgb, hfT], w=[pg])
                    for k in range(8):
                        P.pe(lambda e, k=k, ns=ns, pg=pg: e.matmul(pg[:, 256:512], lhsT=w_fub[:, k, ns], rhs=hfT[:, k, :],
                                                                   start=(k == 0), stop=(k == 7)), r=[w_fub, hfT], w=[pg])
                    sgt = sg[n % 2]
                    P.act(lambda e, pg=pg, sgt=sgt: e.activation(out=sgt[:, :], in_=pg[:, 0:256], func=AF.Silu), r=[pg], w=[sgt])
                    P.dve(lambda e, pg=pg, sgt=sgt, n=n: e.tensor_tensor(out=aT[:, n, :], in0=sgt[:, :], in1=pg[:, 256:512],
                                                                         op=ALU.mult), r=[sgt, pg], w=[aT])
                for j in range(2):
                    ti = s * 2 + j
                    for c in range(2):
                        cs_ = slice(c * 512, (c + 1) * 512)
                        for n in range(22):
                            P.pe(lambda e, n=n, cs_=cs_, j=j: e.matmul(PA[:, cs_], lhsT=aT[:, n, j * 128:(j + 1) * 128],
                                                                       rhs=w_fdb[:, n, cs_], start=(n == 0), stop=(n == 21)),
                                 r=[aT, w_fdb], w=[PA])
                    P.act(lambda e: e.activation(out=fo[:, :], in_=PA[:, :], func=AF.Copy), r=[PA], w=[fo])
                    P.act(lambda e: e.activation(out=sqj[:, :], in_=PA[:, :], func=AF.Square, accum_out=st4[:, 6:7]),
                          r=[PA], w=[sqj, st4])
                    P.act(lambda e: e.activation(out=st4[:, 7:8], in_=st4[:, 6:7], func=AF.Sqrt, scale=1.0 / D,
                                                 bias=epsc[:, 0:1]), r=[st4, epsc], w=[st4])
                    P.dve(lambda e: e.reciprocal(out=st4[:, 7:8], in_=st4[:, 7:8]), r=[st4], w=[st4])
                    oo = ost[0]
                    P.dve(lambda e: e.scalar_tensor_tensor(out=fo[:, :], in0=fo[:, :], scalar=st4[:, 7:8], in1=GFrow[:, :],
                                                           op0=ALU.mult, op1=ALU.mult), r=[fo, st4, GFrow], w=[fo])
                    P.dve(lambda e, oo=oo, j=j: e.tensor_tensor(out=oo[:, :], in0=fo[:, :], in1=xt[:, j, :], op=ALU.add),
                          r=[fo, xt], w=[oo])
                    P.dma(out_d[ti * 128:(ti + 1) * 128, :], oo[:, :], "ost", r=[oo], w=[outt], q="sp")
            P.enabled = True
            P.wait_all("sp", ["ost", "x1st", "dbg"] + [k for k in P.streams if k not in ("ost", "x1st", "dbg")])
            P.wait_all("pool", ["ost"])

            with nc.Block() as block:
                @block.sync
                def _(e):
                    P.replay("sp", e)

                @block.tensor
                def _(e):
                    P.replay("pe", e)

                @block.scalar
                def _(e):
                    P.replay("act", e)

                @block.vector
                def _(e):
                    P.replay("dve", e)

                @block.gpsimd
                def _(e):
                    P.replay("pool", e)
    return nc, list(dbg_d.keys())


def t5_bucket_np(rel):
    rel = np.asarray(rel)
    max_exact = 16
    nf = np.maximum(rel, 1).astype(np.float32)
    large = max_exact + (np.log(nf / np.float32(max_exact)) / np.float32(math.log(128 / max_exact))
                         * np.float32(32 - max_exact)).astype(np.int32)
    large = np.minimum(large, 31)
    return np.where(rel < max_exact, rel, large)


def make_consts():
    c = np.zeros((128, C_END), np.float32)
    p = np.arange(128)[:, None]
    f = np.arange(128)[None, :]
    c[:, C_ID:C_ID + 128] = (p == f)
    c[:, C_ONE:C_ONE + 128] = 1.0
    c[:, C_BLK:C_BLK + 128] = ((p // 64) == (f // 64))
    c[:, C_MSU:C_MSU + 128] = (f > p)
    c[:, C_MUI:C_MUI + 128] = (f >= p)
    c[:, C_MSL:C_MSL + 128] = (f < p)
    c[:, C_NEG:C_NEG + 128] = np.where(f > p, -1e30, 0.0)
    c[:, C_J:C_J + 128] = (p + f == 127)
    c[31, C_SEL:C_SEL + 128] = 1.0
    bk = t5_bucket_np(np.arange(256))
    c[0:32, C_OH:C_OH + 256] = (np.arange(32)[:, None] == bk[None, :])
    sm = np.ones((128, 256), np.float32)
    sm[:, 0] = 0.0
    sm[:, 128] = 0.0
    c[:, C_SCAN:C_SCAN + 256] = sm
    c[:, C_HIND] = (np.arange(128) < 64)
    c[:, C_HIND + 1] = (np.arange(128) >= 64)
    return c


def col8(v):
    return np.ascontiguousarray(v.reshape(-1, 128).T)


def prep_shared(inp):
    f32 = np.float32
    g = lambda k: np.asarray(inp[k], f32)
    w_in = g("w_in")[0]
    sh = {}
    sh["ada_w"] = np.ascontiguousarray(g("ada_w")[0])
    sh["cst"] = make_consts()
    sh["w_att"] = np.ascontiguousarray(np.concatenate([w_in[:, 0:384], w_in[:, 384:448], w_in[:, 384:448]], axis=1))
    sh["w_iw"] = np.ascontiguousarray(w_in[:, 448:456])
    sh["w_rw"] = np.ascontiguousarray(w_in[:, 456:])
    wiq = g("w_idx_q")[0]
    sh["w_iq"] = np.ascontiguousarray(np.concatenate([wiq, wiq], axis=2).reshape(256, 1024))
    sh["w_uq"] = np.ascontiguousarray(g("w_uq")[0].reshape(256, 512))
    wuk = g("w_uk")[0]
    t = np.zeros((128, 8, 128), f32)
    for h in range(8):
        t[(h % 2) * 64:(h % 2) * 64 + 64, h, :] = wuk[h].T
    sh["w_ukT"] = t.reshape(128, 1024)
    wuv = g("w_uv")[0]
    t = np.zeros((128, 8, 128), f32)
    for h in range(8):
        t[:, h, (h % 2) * 64:(h % 2) * 64 + 64] = wuv[h]
    sh["w_uv"] = t.reshape(128, 1024)
    sh["rel_bias"] = np.ascontiguousarray(g("rel_bias"))
    t = np.zeros((128, 1024), f32)
    t[0:64, 0:512] = g("w_decay_up")[0]
    t[64:128, 512:1024] = g("w_aaa_up")[0]
    sh["w_lora"] = t
    sh["w_gate"] = np.ascontiguousarray(g("w_gate_up")[0])
    sh["lnrow"] = np.ascontiguousarray(np.stack([g("ln_x_gain")[0], g("ln_x_bias")[0]], axis=0))
    sh["w_out"] = np.ascontiguousarray(g("w_out")[0])
    sh["w_fg"] = np.ascontiguousarray(g("w_ffn_gate")[0])
    sh["w_fu"] = np.ascontiguousarray(g("w_ffn_up")[0])
    sh["w_fd"] = np.ascontiguousarray(g("w_ffn_down")[0])
    vec = np.zeros((128, 128), f32)
    vec[:, 0:8] = col8(g("mix_pre_norm")[0])
    vec[:, 8:16] = col8(g("mix_post_norm")[0])
    vec[:, 16:24] = col8(g("ffn_pre_norm")[0])
    vec[:, 24:32] = col8(g("ffn_post_norm")[0])
    vec[:, 32:80] = g("ada_b")[0].reshape(48, 128).T
    vec[:, 80:82] = g("q_norm")[0].reshape(2, 128).T
    vec[:, 82] = g("kv_norm")[0]
    vec[:, 83] = np.concatenate([g("idx_k_norm")[0], g("idx_k_norm")[0]])
    vec[:, 84:88] = g("w0")[0].reshape(4, 128).T
    vec[:, 88:92] = g("a0")[0].reshape(4, 128).T
    vec[:, 92:96] = g("k_k")[0].reshape(4, 128).T
    vec[:, 96:100] = g("k_a")[0].reshape(4, 128).T
    vec[:, 100:104] = g("r_k")[0].reshape(4, 128).T
    mus = g("mu_shift")[0]
    vec[:, 108:122] = mus[:14 * 128].reshape(14, 128).T
    vec[:, 122] = mus[1824 - 128:1824]
    sh["vecs"] = vec
    sh["adab_row"] = np.ascontiguousarray(g("ada_b")[0].reshape(1, 6 * D))
    return sh


def kernel(**inputs):
    x = np.asarray(inputs["x"], np.float32)
    c = np.asarray(inputs["c"], np.float32)
    B, T, _ = x.shape
    sh = prep_shared(inputs)
    nc, _ = build(T)
    in_maps = []
    for b in range(B):
        m = dict(sh)
        m["x"] = np.ascontiguousarray(x[b])
        m["ccol"] = col8(c[b])
        in_maps.append(m)
    res = run_bass_kernel_spmd(nc, in_maps, core_ids=list(range(B)))
    return np.stack([np.asarray(r["out"], np.float32) for r in res.results], axis=0)
```

```python
import math
from contextlib import ExitStack
import numpy as np
import concourse.bass as bass
import concourse.mybir as mybir
from concourse.bass_utils import run_bass_kernel_spmd

F32 = mybir.dt.float32
BF16 = mybir.dt.bfloat16
AF = mybir.ActivationFunctionType
ALU = mybir.AluOpType
AX = mybir.AxisListType

D = 1024
DFF = 2816
NB_IT = 20
BIS_R = 16.0
LWC = math.exp(-0.5)

C_ID, C_ONE, C_BLK, C_MSU, C_MUI, C_MSL, C_NEG, C_J, C_SEL, C_OH, C_SCAN, C_HIND, C_END = (
    0, 128, 256, 384, 512, 640, 768, 896, 1024, 1152, 1408, 1664, 1668)


class Tl:
    def __init__(self, P, h, name):
        self.h = h
        self.name = name
        self.lw = None
        self.rd = dict(P.fence_tokens)

    def __getitem__(self, i):
        return self.h[i]


class TlG:
    def __init__(self, P, h, name, n):
        self.h = h
        self.name = name
        self.p = [Tl(P, h, "%s_%d" % (name, i)) for i in range(n)]

    def __getitem__(self, i):
        return self.h[i]


class Bank:
    def __init__(self, tl, h, lo):
        self.tl = tl
        self.h = h
        self.lo = lo

    def ap(self, a, b):
        return self.h[:, self.lo + a:self.lo + b]


def _flat(ts):
    out = []
    for t in ts:
        if isinstance(t, TlG):
            out.extend(t.p)
        else:
            out.append(t)
    return out


class Rec:
    def __init__(self):
        self.call = None

    def __getattr__(self, name):
        def f(*a, **k):
            self.call = (name, a, k)
            return self
        return f


class Stream:
    def __init__(self, key):
        self.key = key
        self.count = 0
        self.mark = -1


class Prog:
    ENGS = ("pe", "act", "dve", "pool", "sp")

    def __init__(self, nc, es):
        self.nc = nc
        self.es = es
        self.q = {e: [] for e in self.ENGS}
        self.cnt = {e: 0 for e in self.ENGS}
        self.waited = {e: {} for e in self.ENGS}
        self.semh = {}
        self.streams = {}
        self.fence_tokens = {}
        self.enabled = True
        self.nops = 0
        import os as _os
        self.printops = bool(_os.environ.get("PRINTOPS"))
        self.maxops = int(_os.environ.get("MAXOPS", "100000000"))
        for e in self.ENGS:
            self.semh[e] = es.enter_context(nc.semaphore("s_" + e))

    def stream(self, key):
        if key not in self.streams:
            self.streams[key] = Stream(key)
            self.semh[key] = self.es.enter_context(self.nc.semaphore("d_" + key))
        return self.streams[key]

    def tile(self, shape, dt, name):
        h = self.es_cur.enter_context(self.nc.sbuf_tensor("sb_" + name, list(shape), dt))
        return Tl(self, h, name)

    def tileg(self, shape, dt, name, n):
        h = self.es_cur.enter_context(self.nc.sbuf_tensor("sb_" + name, list(shape), dt))
        return TlG(self, h, name, n)

    def conv(self, i, out, in_, r, w):
        e = i % 3
        if e == 0:
            self.act(lambda q: q.activation(out=out, in_=in_, func=AF.Copy), r=r, w=w)
        elif e == 1:
            self.dve(lambda q: q.tensor_copy(out=out, in_=in_), r=r, w=w)
        else:
            self.pool(lambda q: q.tensor_copy(out=out, in_=in_), r=r, w=w)

    def fence(self):
        ft = {}
        for e in self.ENGS:
            if self.cnt[e] > 0:
                ft[e] = (e, self.cnt[e], e, False)
        for k, st in self.streams.items():
            if st.count > 0:
                ft[k] = (k, st.count, None, True)
        self.fence_tokens = ft

    def emit(self, eng, fn, r=(), w=(), stream=None):
        if not self.enabled:
            return None
        self.nops += 1
        if self.nops > self.maxops:
            return None
        r = _flat(r)
        w = _flat(w)
        rec = Rec()
        fn(rec)
        fn = rec.call
        assert fn is not None
        if self.printops:
            print("OP", self.nops, eng, fn[0], [t.name for t in r], "->", [t.name for t in w])
        need = {}

        def add(tok, kind):
            key, val, teng, isdma = tok
            if isdma:
                val = self.streams[key].count
                self.streams[key].mark = val
            elif stream is None and teng == eng:
                if eng == "pe":
                    return
            if need.get(key, 0) < val:
                need[key] = val

        for t in r:
            if t.lw is not None:
                add(t.lw, "raw")
        for t in w:
            if t.lw is not None:
                add(t.lw, "waw")
            for tok in t.rd.values():
                add(tok, "war")
        if stream is not None:
            st0 = self.stream(stream)
            if st0.mark == st0.count and st0.count > 0:
                need[st0.key] = st0.count
        for key, val in need.items():
            if self.waited[eng].get(key, 0) < val:
                self.waited[eng][key] = val
                self.q[eng].append(("w", key, val))
        if stream is None:
            self.cnt[eng] += 1
            tok = (eng, self.cnt[eng], eng, False)
            self.q[eng].append(("op", fn, eng, 1))
        else:
            st = self.stream(stream)
            st.count += 16
            tok = (st.key, st.count, None, True)
            self.q[eng].append(("op", fn, st.key, 16))
        for t in w:
            t.lw = tok
            t.rd = {}
        for t in r:
            if t not in w:
                t.rd[tok[0]] = tok
        return tok

    def wait_all(self, eng, keys):
        for key in keys:
            if key in self.streams:
                val = self.streams[key].count
            elif key in self.cnt:
                val = self.cnt[key]
            else:
                continue
            if val > 0 and self.waited[eng].get(key, 0) < val:
                self.waited[eng][key] = val
                self.q[eng].append(("w", key, val))

    def replay(self, eng, e):
        for ent in self.q[eng]:
            if ent[0] == "w":
                e.wait_ge(self.semh[ent[1]], ent[2])
            else:
                name, a, k = ent[1]
                ins = getattr(e, name)(*a, **k)
                ins.then_inc(self.semh[ent[2]], ent[3])

    def pe(self, fn, r=(), w=()):
        return self.emit("pe", fn, r, w)

    def act(self, fn, r=(), w=()):
        return self.emit("act", fn, r, w)

    def dve(self, fn, r=(), w=()):
        return self.emit("dve", fn, r, w)

    def pool(self, fn, r=(), w=()):
        return self.emit("pool", fn, r, w)

    def dma(self, out, in_, stream, r=(), w=(), q="sp"):
        return self.emit(q, lambda e: e.dma_start(out=out, in_=in_), r, w, stream=stream)


def build(T, dbg=False, upto=3):
    NT = T // 128
    NS = T // 256
    KTOP = min(256, T // 4)
    nc = bass.Bass("TRN2", target_bir_lowering=False)

    def din(name, shape):
        return nc.dram_tensor(name, list(shape), F32, kind="ExternalInput").ap()

    x_d = din("x", [T, D])
    ccol_d = din("ccol", [128, 8])
    adaw_d = din("ada_w", [D, 6 * D])
    vec_d = din("vecs", [128, 128])
    adab_d = din("adab_row", [1, 6 * D])
    cst_d = din("cst", [128, C_END])
    watt_d = din("w_att", [D, 512])
    wiw_d = din("w_iw", [D, 8])
    wrw_d = din("w_rw", [D, 1824])
    wiq_d = din("w_iq", [256, 1024])
    wuq_d = din("w_uq", [256, 512])
    wuk_d = din("w_ukT", [128, 1024])
    wuv_d = din("w_uv", [128, 1024])
    relb_d = din("rel_bias", [32, 8])
    wlora_d = din("w_lora", [128, 1024])
    wgate_d = din("w_gate", [160, 512])
    lnrow_d = din("lnrow", [2, 512])
    wout_d = din("w_out", [D, D])
    wfg_d = din("w_fg", [D, DFF])
    wfu_d = din("w_fu", [D, DFF])
    wfd_d = din("w_fd", [DFF, D])
    out_d = nc.dram_tensor("out", [T, D], F32, kind="ExternalOutput").ap()
    dscr = nc.dram_tensor("dscr", [8, 384], F32, kind="Internal").ap()
    dbg_d = {}

    def ddbg(name, shape):
        if dbg:
            dbg_d[name] = nc.dram_tensor("dbg_" + name, list(shape), F32, kind="ExternalOutput").ap()

    ddbg("modc", [128, 48])
    ddbg("bias", [128, 3 * 1024])
    ddbg("yatt", [128, 4 * T])
    ddbg("thr", [128, NT])
    ddbg("yrw", [T, 512])
    ddbg("x1", [T, D])

    with ExitStack() as es:
        P = Prog(nc, es)
        P.es_cur = es
        def pst(name, shape, dt):
            return Tl(P, es.enter_context(nc.psum_tensor("ps_" + name, list(shape), dt)), name)
        def pstg(name, shape, dt, n):
            return TlG(P, es.enter_context(nc.psum_tensor("ps_" + name, list(shape), dt)), name, n)
        PA = pstg("PA", [128, 1024], F32, 2)
        PB = pstg("PB", [128, 1024], F32, 2)
        PC = pstg("PC", [128, 1024], F32, 2)
        PD = pst("PD", [128, 512], F32)
        PT = pstg("PT", [128, 1024], BF16, 8)

        cst = P.tile([128, C_END], F32, "cst")
        vec = P.tile([128, 128], F32, "vec")
        modc = P.tile([128, 48], F32, "modc")
        gsh = P.tile([128, 48], F32, "gsh")
        GMrow = P.tile([128, D], F32, "GMrow")
        GFrow = P.tile([128, D], F32, "GFrow")
        identb = P.tile([128, 128], BF16, "identb")
        onesb = P.tile([128, 128], BF16, "onesb")
        epsc = P.tile([128, 4], F32, "epsc")
        yscr = nc.dram_tensor("yscr", [NT * 128, 512], F32, kind="ExternalOutput").ap()
        yscr_t = Tl(P, None, "yscr")

        ident = lambda: cst[:, C_ID:C_ID + 128]
        ones32 = lambda: cst[:, C_ONE:C_ONE + 128]

        P.dma(cst[:, :], cst_d[:, :], "cw", w=[cst])
        P.dma(vec[:, :], vec_d[:, :], "cw", w=[vec])
        P.dve(lambda e: e.tensor_copy(out=identb[:, :], in_=cst[:, C_ID:C_ID + 128]), r=[cst], w=[identb])
        P.dve(lambda e: e.tensor_copy(out=onesb[:, :], in_=cst[:, C_ONE:C_ONE + 128]), r=[cst], w=[onesb])
        P.dve(lambda e: e.memset(epsc[:, 0:1], 1e-6), w=[epsc])
        P.dve(lambda e: e.memset(epsc[:, 1:2], 64e-5), w=[epsc])
        P.dve(lambda e: e.memset(epsc[:, 2:3], 0.0), w=[epsc])
        V_MPRE, V_MPOST, V_FPRE, V_FPOST, V_ADAB, V_QN, V_KVN, V_IKN, V_W0, V_A0, V_KK, V_KA, V_RK = (
            0, 8, 16, 24, 32, 80, 82, 83, 84, 88, 92, 96, 100)

        def dump(name, src_ap, tl, dst_ap=None):
            if dbg:
                P.dma(dbg_d[name][:, :] if dst_ap is None else dst_ap, src_ap, "dbg", r=[tl])

        with ExitStack() as es0:
            P.es_cur = es0
            ccol = P.tile([128, 8], F32, "ccol")
            scol = P.tile([128, 8], F32, "scol")
            stg = [P.tile([128, 8, 512], F32, "adastg%d" % i) for i in range(4)]
            dg = P.tile([128, 8, 128], F32, "dg")
            P.dma(ccol[:, :], ccol_d[:, :], "cw", w=[ccol])
            P.act(lambda e: e.activation(out=scol[:, :], in_=ccol[:, :], func=AF.Silu), r=[ccol], w=[scol])
            adabr = P.tile([1, 6 * D], F32, "adabr")
            modrow = P.tile([1, 6 * D], F32, "modrow")
            P.dma(adabr[:, :], adab_d[:, :], "cw", w=[adabr])
            p0b = [Bank(PD, PD.h, 0), Bank(PC.p[0], PC.h, 0)]
            for s in range(12):
                st = stg[s % 4]
                P.dma(st[:, :, :], adaw_d[:, s * 512:(s + 1) * 512].rearrange("(k p) n -> p k n", p=128),
                      "ada%d" % (s % 4), w=[st])
                bk = p0b[s % 2]
                for k in range(8):
                    P.pe(lambda e: e.matmul(bk.h[0:1, bk.lo:bk.lo + 512], lhsT=scol[:, k:k + 1], rhs=st[:, k, :],
                                            start=(k == 0), stop=(k == 7)), r=[st, scol], w=[bk.tl])
                P.dve(lambda e: e.tensor_tensor(out=modrow[0:1, s * 512:(s + 1) * 512], in0=bk.h[0:1, bk.lo:bk.lo + 512],
                                                in1=adabr[0:1, s * 512:(s + 1) * 512], op=ALU.add),
                      r=[bk.tl, adabr], w=[modrow])
            for m in range(48):
                P.pe(lambda e: e.matmul(PB[:, m:m + 1], lhsT=modrow[0:1, m * 128:(m + 1) * 128],
                                        rhs=cst[0:1, C_ONE:C_ONE + 1], start=True, stop=True),
                     r=[modrow, cst], w=[PB.p[0]])
            P.dve(lambda e: e.tensor_copy(out=modc[:, :], in_=PB[:, 0:48]), r=[PB.p[0]], w=[modc])
            dump("modc", modc[:, :], modc)
            P.dve(lambda e: e.tensor_scalar(out=gsh[:, 32:40], in0=modc[:, 8:16], scalar1=1.0, scalar2=None,
                                            op0=ALU.add), r=[modc], w=[gsh])
            P.dve(lambda e: e.tensor_scalar(out=gsh[:, 40:48], in0=modc[:, 32:40], scalar1=1.0, scalar2=None,
                                            op0=ALU.add), r=[modc, gsh], w=[gsh])
            P.dve(lambda e: e.tensor_tensor(out=gsh[:, 0:8], in0=gsh[:, 32:40], in1=vec[:, V_MPRE:V_MPRE + 8],
                                            op=ALU.mult), r=[gsh, vec], w=[gsh])
            P.dve(lambda e: e.tensor_tensor(out=gsh[:, 8:16], in0=gsh[:, 40:48], in1=vec[:, V_FPRE:V_FPRE + 8],
                                            op=ALU.mult), r=[gsh, vec], w=[gsh])
            P.dve(lambda e: e.tensor_tensor(out=gsh[:, 16:24], in0=modc[:, 16:24], in1=vec[:, V_MPOST:V_MPOST + 8],
                                            op=ALU.mult), r=[gsh, modc, vec], w=[gsh])
            P.dve(lambda e: e.tensor_tensor(out=gsh[:, 24:32], in0=modc[:, 40:48], in1=vec[:, V_FPOST:V_FPOST + 8],
                                            op=ALU.mult), r=[gsh, modc, vec], w=[gsh])
            for (c0, row) in ((16, GMrow), (24, GFrow)):
                for k in range(8):
                    P.dve(lambda e, c0=c0, k=k: e.tensor_scalar(
                        out=dg[:, k, :], in0=cst[:, C_ID:C_ID + 128], scalar1=gsh[:, c0 + k:c0 + k + 1],
                        scalar2=None, op0=ALU.mult), r=[cst, gsh], w=[dg])
                for k in range(8):
                    P.pe(lambda e, k=k: e.matmul(PA[:, k * 128:(k + 1) * 128], lhsT=cst[:, C_ONE:C_ONE + 128],
                                                 rhs=dg[:, k, :], start=True, stop=True), r=[cst, dg], w=[PA])
                P.act(lambda e, row=row: e.activation(out=row[:, :], in_=PA[:, :], func=AF.Copy), r=[PA], w=[row])

        print("ops after P0", P.nops)
        P.fence()
        if upto < 1:
            P.enabled = False
        with ExitStack() as es1:
            P.es_cur = es1
            w_att = P.tile([128, 8, 512], F32, "w_att")
            w_iw = P.tile([128, 8, 8], F32, "w_iw")
            w_iq = P.tile([128, 2, 1024], F32, "w_iq")
            w_uqb = P.tile([128, 2, 512], BF16, "w_uqb")
            w_ukb = P.tile([128, 8, 128], BF16, "w_ukb")
            w_uvb = P.tile([128, 8, 128], BF16, "w_uvb")
            relb = P.tile([32, 8], F32, "relb")
            biasT = P.tile([128, 3, 1024], BF16, "biasT")
            ckvT = P.tile([128, T], BF16, "ckvT")
            ckvtm = P.tile([128, NT, 128], BF16, "ckvtm")
            ikA = P.tile([128, T], BF16, "ikA")
            ikB = P.tile([128, T], BF16, "ikB")
            P.dve(lambda e: e.memset(ikB[:, :], 0.0), w=[ikB])
            es1b = ExitStack()
            P.es_cur = es1b
            wsts = [P.tile([128, 1024], F32, "wst1%d" % i) for i in range(3)]
            P.dma(w_att[:, :, :], watt_d.rearrange("(k p) n -> p k n", p=128), "cw", w=[w_att])
            P.dma(w_iw[:, :, :], wiw_d.rearrange("(k p) n -> p k n", p=128), "cw", w=[w_iw])
            P.dma(w_iq[:, :, :], wiq_d.rearrange("(k p) n -> p k n", p=128), "cw", w=[w_iq])
            P.dma(relb[:, :], relb_d[:, :], "cwr", w=[relb])
            P.dma(wsts[0][:, :].rearrange("p (k n) -> p k n", k=2), wuq_d.rearrange("(k p) n -> p k n", p=128), "ws1a", w=[wsts[0]])
            P.conv(1, w_uqb[:, :, :], wsts[0][:, :].rearrange("p (k n) -> p k n", k=2), [wsts[0]], [w_uqb])
            P.dma(wsts[1][:, :], wuk_d[:, :], "ws1b", w=[wsts[1]])
            P.conv(0, w_ukb[:, :, :], wsts[1][:, :].rearrange("p (k n) -> p k n", k=8), [wsts[1]], [w_ukb])
            P.dma(wsts[2][:, :], wuv_d[:, :], "ws1c", w=[wsts[2]])
            P.conv(2, w_uvb[:, :, :], wsts[2][:, :].rearrange("p (k n) -> p k n", k=8), [wsts[2]], [w_uvb])
            brel = P.tile([8, 384], F32, "brel")
            relx = P.tile([32, 8, 128], F32, "relx")
            qtl = P.tile([128, 2, 1024], F32, "qtl")
            P.pe(lambda e: e.matmul(PD[0:8, 0:256], lhsT=relb[:, :], rhs=cst[0:32, C_OH:C_OH + 256],
                                    start=True, stop=True), r=[relb, cst], w=[PD])
            P.dve(lambda e: e.memset(brel[:, :], 0.0), w=[brel])
            P.dve(lambda e: e.tensor_copy(out=brel[:, 127:383], in_=PD[0:8, 0:256]), r=[PD, brel], w=[brel])
            dsc = Tl(P, None, "dscr")
            P.dma(dscr[:, :], brel[:, :], "dsw", r=[brel], w=[dsc])
            for dl in range(2):
                src = bass.AP(tensor=dscr.tensor, offset=128 * dl, ap=[[1, 128], [384, 8], [1, 128]])
                P.dma(qtl[:, dl, :].rearrange("p (h t) -> p h t", h=8), src, "dsr", r=[dsc], w=[qtl])
            for dl in range(2):
                for c in range(2):
                    P.pe(lambda e, dl=dl, c=c: e.matmul(PA[:, c * 512:(c + 1) * 512], lhsT=cst[:, C_J:C_J + 128],
                                                        rhs=qtl[:, dl, c * 512:(c + 1) * 512], start=True, stop=True),
                         r=[cst, qtl], w=[PA])
                P.act(lambda e, dl=dl: e.activation(out=biasT[:, dl, :], in_=PA[:, :], func=AF.Copy), r=[PA], w=[biasT])
            for h in range(8):
                P.dve(lambda e, h=h: e.tensor_scalar(out=relx[:, h, :], in0=cst[0:32, C_ONE:C_ONE + 128],
                                                     scalar1=relb[:, h:h + 1], scalar2=None, op0=ALU.mult),
                      r=[cst, relb], w=[relx])
            for c in range(2):
                P.pe(lambda e, c=c: e.matmul(PA[:, c * 512:(c + 1) * 512], lhsT=cst[0:32, C_SEL:C_SEL + 128],
                                             rhs=relx[:, c * 4:(c + 1) * 4, :], start=True, stop=True),
                     r=[cst, relx], w=[PA])
            P.act(lambda e: e.activation(out=biasT[:, 2, :], in_=PA[:, :], func=AF.Copy), r=[PA], w=[biasT])
            es1b.close()
            P.es_cur = es1
            P.fence()

            xt = P.tile([128, 2, D], F32, "xt")
            sqj = P.tile([128, D], BF16, "sqj")
            st4 = P.tile([128, 8], F32, "st4")
            dgt = P.tile([128, 2, 128], F32, "dgt")
            hT = P.tile([128, 8, 256], F32, "hT")
            cqraw = P.tile([128, 4, 256], F32, "cqraw")
            sq = P.tile([128, 4, 256], F32, "sq")
            rq = P.tile([128, 3, 256], F32, "rq")
            cqT = P.tile([128, 2, 256], F32, "cqT")
            cqTb = P.tile([128, 2, 256], BF16, "cqTb")
            iqT = P.tile([128, 8, 256], BF16, "iqP")
            iqtmp = P.tile([128, 4, 256], BF16, "iqtmp")
            ikf = P.tile([128, 256], F32, "ikf")
            iw = P.tile([128, 2, 8], F32, "iw")
            qTb = P.tile([128, 4, 256], BF16, "qTb")
            qaT2 = [P.tile([128, 8, 256], BF16, "qaT%d" % i) for i in range(2)]
            sc2 = [TlG(P, es1.enter_context(nc.sbuf_tensor("sb_sc%d" % i, [128, T], F32)), "sc%d" % i, max(T // 512, 1))
                   for i in range(2)]
            mk = [P.tile([128, T], BF16, "mk%d" % i) for i in range(2)]
            rl = [P.tile([128, 512], F32, "rl%d" % i) for i in range(3)]
            bsn = P.tile([128, 1], F32, "bsn")
            bss = P.tile([128, 1], F32, "bss")
            bsu = P.tile([128, 1], F32, "bsu")
            Ee = [P.tile([128, 512], BF16, "Ee%d" % i) for i in range(3)]
            Em = [P.tile([128, 512], BF16, "Em%d" % i) for i in range(3)]
            rD = P.tile([128, 512], F32, "rD")
            oTb = P.tile([128, 8, 128], BF16, "oTb")
            thrs = P.tile([128, NT], F32, "thrs")
            ytl = [P.tile([128, 4, 128], F32, "ytl%d" % i) for i in range(1)]
            ydb = P.tile([128, 4, 128], F32, "ydb") if dbg else None
            print("SBUFREM A1", nc.sbuf_bytes_remaining)

            rotP = [Bank(PA.p[0], PA.h, 0), Bank(PA.p[1], PA.h, 512)]
            bkS = Bank(PD, PD.h, 0)

            def gen_S(qi):
                j = qi % 2
                sc = sc2[qi % 2]
                S = (qi + 1) * 128
                ncc = (S + 511) // 512
                idx = 0
                for h in range(8):
                    for cc in range(ncc):
                        wd = min(512, S - cc * 512)
                        bk = bkS
                        rb = rl[idx % 3]
                        idx += 1
                        scp = sc.p[cc]
                        P.pe(lambda e: e.matmul(bk.ap(0, wd), lhsT=iqT[:, h, j * 128:(j + 1) * 128],
                                                rhs=ikA[:, cc * 512:cc * 512 + wd], start=True, stop=False),
                             r=[iqT, ikA], w=[bk.tl])
                        P.pe(lambda e: e.matmul(bk.ap(0, wd), lhsT=iqT[:, h, j * 128:(j + 1) * 128],
                                                rhs=ikB[:, cc * 512:cc * 512 + wd], start=False, stop=True),
                             r=[iqT, ikB], w=[bk.tl])
                        if h == 0:
                            P.dve(lambda e: e.tensor_scalar(out=sc[:, cc * 512:cc * 512 + wd], in0=bk.ap(0, wd),
                                                            scalar1=0.0, scalar2=iw[:, j, 0:1], op0=ALU.max, op1=ALU.mult),
                                  r=[bk.tl, iw], w=[scp])
                        else:
                            P.dve(lambda e: e.tensor_scalar(out=rb[:, 0:wd], in0=bk.ap(0, wd), scalar1=0.0,
                                                            scalar2=iw[:, j, h:h + 1], op0=ALU.max, op1=ALU.mult),
                                  r=[bk.tl, iw], w=[rb])
                            P.dve(lambda e: e.tensor_tensor(out=sc[:, cc * 512:cc * 512 + wd], in0=sc[:, cc * 512:cc * 512 + wd],
                                                            in1=rb[:, 0:wd], op=ALU.add), r=[rb, scp], w=[scp])
                        yield
                scd = sc.p[(qi * 128) // 512]
                P.dve(lambda e: e.tensor_tensor(out=sc[:, qi * 128:(qi + 1) * 128], in0=sc[:, qi * 128:(qi + 1) * 128],
                                                in1=cst[:, C_NEG:C_NEG + 128], op=ALU.add), r=[scd, cst], w=[scd])
                yield

            def gen_B(qi):
                S = (qi + 1) * 128
                ncc = (S + 511) // 512
                sc = sc2[qi % 2]
                scr = sc.p[0:ncc]
                mkb = mk[qi % 2]
                thr_c = float(2 * KTOP - S) - 0.5
                P.pool(lambda e: e.memset(bsn[:, :], 0.0), w=[bsn])
                for it in range(NB_IT):
                    ck = BIS_R / (2 ** it)
                    cn = ck / 2 if it < NB_IT - 1 else ck
                    P.act(lambda e: e.activation(out=mkb[:, 0:S], in_=sc[:, 0:S], func=AF.Sign, bias=bsn[:, 0:1],
                                                 accum_out=bss[:, 0:1]), r=scr + [bsn], w=[mkb, bss])
                    P.pool(lambda e: e.tensor_scalar(out=bsu[:, :], in0=bss[:, :], scalar1=thr_c, scalar2=-ck,
                                                     op0=ALU.is_ge, op1=ALU.mult), r=[bss], w=[bsu])
                    P.pool(lambda e: e.tensor_scalar(out=bsn[:, :], in0=bsu[:, :], scalar1=bsn[:, 0:1], scalar2=cn,
                                                     op0=ALU.add, op1=ALU.add), r=[bsu, bsn], w=[bsn])
                    yield
                P.act(lambda e: e.activation(out=mkb[:, 0:S], in_=sc[:, 0:S], func=AF.Sign, bias=bsn[:, 0:1]),
                      r=scr + [bsn], w=[mkb])
                if dbg:
                    P.dve(lambda e: e.tensor_scalar(out=thrs[:, qi:qi + 1], in0=bsn[:, 0:1], scalar1=-1.0, scalar2=None,
                                                    op0=ALU.mult), r=[bsn], w=[thrs])
                yield

            def gen_P(qi, j, qb):
                mkb = mk[qi % 2]
                qa = qaT2[qb]
                steps = [(kj, c) for kj in range(qi + 1) for c in range(2)]
                n = len(steps)

                def slot_of(kj):
                    k2 = kj % 2
                    return PT, PT[:, k2 * 512:k2 * 512 + 128]

                for i in range(n + 3):
                    if i < n:
                        kj, c = steps[i]
                        dl = min(qi - kj, 2)
                        bk = rotP[i % 2]
                        if c == 0:
                            pts, ptap = slot_of(kj)
                            P.pe(lambda e: e.transpose(ptap, mkb[:, kj * 128:(kj + 1) * 128], identb[:, :]),
                                 r=[mkb, identb], w=[pts])
                        P.pe(lambda e: e.matmul(bk.ap(0, 512), lhsT=ckvT[:, kj * 128:(kj + 1) * 128],
                                                rhs=qa[:, c * 4:(c + 1) * 4, j * 128:(j + 1) * 128], start=True, stop=False),
                             r=[ckvT, qa], w=[bk.tl])
                        P.pe(lambda e: e.matmul(bk.ap(0, 512), lhsT=identb[:, :], rhs=biasT[:, dl, c * 512:(c + 1) * 512],
                                                start=False, stop=True), r=[identb, biasT], w=[bk.tl])
                    if 0 <= i - 3 < n:
                        kj, c = steps[i - 3]
                        emt = Em[(i - 3) % 3]
                        P.pe(lambda e: e.matmul(PB[:, c * 512:(c + 1) * 512], lhsT=ckvtm[:, kj, :], rhs=emt[:, :],
                                                start=(kj == 0), stop=(kj == qi)), r=[ckvtm, emt], w=[PB.p[c]])
                        P.pe(lambda e: e.matmul(PC[:, c * 512:(c + 1) * 512], lhsT=onesb[:, :], rhs=emt[:, :],
                                                start=(kj == 0), stop=(kj == qi)), r=[onesb, emt], w=[PC.p[c]])
                    if 0 <= i - 2 < n:
                        kj, c = steps[i - 2]
                        pts, ptap = slot_of(kj)
                        eet, emt = Ee[(i - 2) % 3], Em[(i - 2) % 3]
                        P.dve(lambda e: e.scalar_tensor_tensor(
                            out=emt[:, :].rearrange("p (h t) -> p h t", h=4),
                            in0=ptap.unsqueeze(1).to_broadcast([128, 4, 128]), scalar=1.0,
                            in1=eet[:, :].rearrange("p (h t) -> p h t", h=4), op0=ALU.add, op1=ALU.mult),
                            r=[eet, pts], w=[emt])
                    if 0 <= i - 1 < n:
                        bkp, eet = rotP[(i - 1) % 2], Ee[(i - 1) % 3]
                        P.act(lambda e: e.activation(out=eet[:, :], in_=bkp.ap(0, 512), func=AF.Exp), r=[bkp.tl], w=[eet])
                    yield
                hs = n
                for c in range(2):
                    P.dve(lambda e: e.reciprocal(out=rD[:, :], in_=PC[:, c * 512:(c + 1) * 512]), r=[PC.p[c]], w=[rD])
                    P.dve(lambda e: e.tensor_tensor(out=oTb[:, 4 * c:4 * c + 4, :].rearrange("p h t -> p (h t)"),
                                                    in0=PB[:, c * 512:(c + 1) * 512], in1=rD[:, :], op=ALU.mult),
                          r=[PB.p[c], rD], w=[oTb])
                bk = rotP[hs % 2]
                for m in range(4):
                    for hh in range(2):
                        h = 2 * m + hh
                        P.pe(lambda e: e.matmul(bk.ap(m * 128, (m + 1) * 128), lhsT=w_uvb[:, h, :], rhs=oTb[:, h, :],
                                                start=(hh == 0), stop=(hh == 1)), r=[w_uvb, oTb], w=[bk.tl])
                yt_ = ytl[0]
                P.act(lambda e: e.activation(out=yt_[:, :, :], in_=bk.ap(0, 512).rearrange("p (m t) -> p m t", m=4),
                                             func=AF.Copy), r=[bk.tl], w=[yt_])
                P.dma(yscr[qi * 128:(qi + 1) * 128, :], yt_[:, :, :].rearrange("p m t -> p (m t)"), "yst",
                      r=[yt_], w=[yscr_t], q="sp")
                if dbg:
                    P.dve(lambda e: e.tensor_copy(out=ydb[:, :, :], in_=bk.ap(0, 512).rearrange("p (m t) -> p m t", m=4)),
                          r=[bk.tl], w=[ydb])
                    dump("yatt", ydb[:, :, :], ydb,
                         dbg_d["yatt"][:, :].rearrange("p (m t) -> p m t", m=4)[:, :, qi * 128:(qi + 1) * 128])
                yield

            def interleave_n(gens, cnts):
                n = len(gens)
                done = [False] * n
                prog = [0] * n
                while not all(done):
                    best = min((i for i in range(n) if not done[i]), key=lambda i: prog[i] / float(cnts[i]))
                    try:
                        next(gens[best])
                        prog[best] += 1
                    except StopIteration:
                        done[best] = True

            def block(q):
                gens, cnts = [], []
                if 0 <= q < NT:
                    gens.append(gen_B(q))
                    cnts.append(NB_IT + 1)
                if q + 1 < NT:
                    gens.append(gen_S(q + 1))
                    cnts.append(8 * ((q + 2) * 128 + 511) // 512 + 1)
                if 0 <= q - 1 < NT:
                    gens.append(gen_P(q - 1, (q - 1) % 2, ((q - 1) // 2) % 2))
                    cnts.append(2 * q + 3)
                interleave_n(gens, cnts)

            for s in range(NS):
                for j in range(2):
                    r0 = s * 256 + j * 128
                    P.dma(xt[:, j, :], x_d[r0:r0 + 128, :], "xin", w=[xt])
                for j in range(2):
                    P.act(lambda e, j=j: e.activation(out=sqj[:, :], in_=xt[:, j, :], func=AF.Square,
                                                      accum_out=st4[:, j:j + 1]), r=[xt], w=[sqj, st4])
                P.act(lambda e: e.activation(out=st4[:, 2:4], in_=st4[:, 0:2], func=AF.Sqrt, scale=1.0 / D,
                                             bias=epsc[:, 0:1]), r=[st4, epsc], w=[st4])
                P.dve(lambda e: e.reciprocal(out=st4[:, 4:6], in_=st4[:, 2:4]), r=[st4], w=[st4])
                for j in range(2):
                    P.dve(lambda e, j=j: e.tensor_scalar(out=dgt[:, j, :], in0=cst[:, C_ID:C_ID + 128],
                                                         scalar1=st4[:, 4 + j:5 + j], scalar2=None, op0=ALU.mult),
                          r=[cst, st4], w=[dgt])
                for kk in range(2):
                    for k4 in range(4):
                        k = kk * 4 + k4
                        for j in range(2):
                            P.pe(lambda e, k=k, k4=k4, j=j: e.matmul(
                                PA[:, k4 * 256 + j * 128:k4 * 256 + (j + 1) * 128],
                                lhsT=xt[:, j, k * 128:(k + 1) * 128], rhs=dgt[:, j, :], start=True, stop=True),
                                r=[xt, dgt], w=[PA])
                    for k4 in range(4):
                        k = kk * 4 + k4
                        P.dve(lambda e, k=k, k4=k4: e.tensor_scalar(
                            out=hT[:, k, :], in0=PA[:, k4 * 256:(k4 + 1) * 256], scalar1=gsh[:, k:k + 1],
                            scalar2=modc[:, k:k + 1], op0=ALU.mult, op1=ALU.add), r=[PA, gsh, modc], w=[hT])
                for c in range(4):
                    for k in range(8):
                        P.pe(lambda e, c=c, k=k: e.matmul(PB[:, c * 256:(c + 1) * 256],
                                                          lhsT=w_att[:, k, c * 128:(c + 1) * 128], rhs=hT[:, k, :],
                                                          start=(k == 0), stop=(k == 7)), r=[w_att, hT], w=[PB])
                P.act(lambda e: e.activation(out=cqraw[:, :, :], in_=PB[:, :].rearrange("p (c t) -> p c t", c=4),
                                             func=AF.Copy), r=[PB], w=[cqraw])
                P.act(lambda e: e.activation(out=sq[:, :, :], in_=PB[:, :].rearrange("p (c t) -> p c t", c=4),
                                             func=AF.Square), r=[PB], w=[sq])
                for j in range(2):
                    for k in range(8):
                        P.pe(lambda e, j=j, k=k: e.matmul(PD[:, j * 8:(j + 1) * 8], lhsT=hT[:, k, j * 128:(j + 1) * 128],
                                                          rhs=w_iw[:, k, :], start=(k == 0), stop=(k == 7)),
                             r=[hT, w_iw], w=[PD])
                P.act(lambda e: e.activation(out=iw[:, :, :], in_=PD[:, 0:16].rearrange("p (j h) -> p j h", j=2),
                                             func=AF.Copy, scale=float(8 ** -0.5 * 64 ** -0.5)), r=[PD], w=[iw])
                for c in range(2):
                    P.pe(lambda e, c=c: e.matmul(PC[:, 0:256], lhsT=cst[:, C_ONE:C_ONE + 128], rhs=sq[:, c, :],
                                                 start=(c == 0), stop=(c == 1)), r=[cst, sq], w=[PC])
                P.pe(lambda e: e.matmul(PC[:, 256:512], lhsT=cst[:, C_ONE:C_ONE + 128], rhs=sq[:, 2, :],
                                        start=True, stop=True), r=[cst, sq], w=[PC])
                P.pe(lambda e: e.matmul(PC[:, 512:768], lhsT=cst[:, C_BLK:C_BLK + 128], rhs=sq[:, 3, :],
                                        start=True, stop=True), r=[cst, sq], w=[PC])
                for i, dv in enumerate((256.0, 128.0, 64.0)):
                    P.act(lambda e, i=i, dv=dv: e.activation(out=rq[:, i, :], in_=PC[:, i * 256:(i + 1) * 256],
                                                             func=AF.Sqrt, scale=1.0 / dv, bias=epsc[:, 0:1]),
                          r=[PC, epsc], w=[rq])
                P.dve(lambda e: e.reciprocal(out=rq[:, :, :], in_=rq[:, :, :]), r=[rq], w=[rq])
                for c in range(2):
                    P.dve(lambda e, c=c: e.scalar_tensor_tensor(out=cqT[:, c, :], in0=cqraw[:, c, :],
                                                                scalar=vec[:, V_QN + c:V_QN + c + 1], in1=rq[:, 0, :],
                                                                op0=ALU.mult, op1=ALU.mult), r=[cqraw, vec, rq], w=[cqT])
                P.act(lambda e: e.activation(out=cqTb[:, :, :], in_=cqT[:, :, :], func=AF.Copy), r=[cqT], w=[cqTb])
                P.dve(lambda e, s=s: e.scalar_tensor_tensor(out=ckvT[:, s * 256:(s + 1) * 256], in0=cqraw[:, 2, :],
                                                            scalar=vec[:, V_KVN:V_KVN + 1], in1=rq[:, 1, :],
                                                            op0=ALU.mult, op1=ALU.mult), r=[cqraw, vec, rq], w=[ckvT])
                P.dve(lambda e: e.scalar_tensor_tensor(out=ikf[:, :], in0=cqraw[:, 3, :],
                                                       scalar=vec[:, V_IKN:V_IKN + 1], in1=rq[:, 2, :],
                                                       op0=ALU.mult, op1=ALU.mult), r=[cqraw, vec, rq], w=[ikf])
                P.act(lambda e, s=s: e.activation(out=ikA[:, s * 256:(s + 1) * 256], in_=ikf[:, :], func=AF.Copy),
                      r=[ikf], w=[ikA])
                P.dve(lambda e, s=s: e.tensor_tensor(out=ikB[0:64, s * 256:(s + 1) * 256], in0=ikf[0:64, :],
                                                     in1=ikA[0:64, s * 256:(s + 1) * 256], op=ALU.subtract),
                      r=[ikf, ikA], w=[ikB])
                for j in range(2):
                    tix = s * 2 + j
                    P.pe(lambda e, j=j, tix=tix: e.transpose(PT[:, j * 128:(j + 1) * 128],
                                                             ckvT[:, tix * 128:(tix + 1) * 128], identb[:, :]),
                         r=[ckvT, identb], w=[PT])
                P.act(lambda e, s=s: e.activation(out=ckvtm[:, 2 * s:2 * s + 2, :],
                                                  in_=PT[:, 0:256].rearrange("p (j c) -> p j c", j=2), func=AF.Copy),
                      r=[PT], w=[ckvtm])
                for hf in range(2):
                    for h4 in range(4):
                        h = hf * 4 + h4
                        for c in range(2):
                            P.pe(lambda e, h=h, h4=h4, c=c: e.matmul(PB[:, h4 * 256:(h4 + 1) * 256],
                                                                     lhsT=w_iq[:, c, h * 128:(h + 1) * 128], rhs=cqT[:, c, :],
                                                                     start=(c == 0), stop=(c == 1)), r=[w_iq, cqT], w=[PB])
                    hsl = slice(hf * 4, hf * 4 + 4)
                    pb3 = lambda rows: PB[rows, :].rearrange("p (m t) -> p m t", m=4)
                    P.act(lambda e, hsl=hsl: e.activation(out=iqT[0:64, hsl, :], in_=pb3(slice(0, 64)), func=AF.Copy),
                          r=[PB], w=[iqT])
                    P.act(lambda e: e.activation(out=iqtmp[64:128, :, :], in_=pb3(slice(64, 128)), func=AF.Copy),
                          r=[PB], w=[iqtmp])
                    P.dve(lambda e, hsl=hsl: e.tensor_tensor(out=iqT[64:128, hsl, :], in0=pb3(slice(64, 128)),
                                                             in1=iqtmp[64:128, :, :], op=ALU.subtract),
                          r=[PB, iqtmp], w=[iqT])
                for m in range(4):
                    for c in range(2):
                        P.pe(lambda e, m=m, c=c: e.matmul(PC[:, m * 256:(m + 1) * 256],
                                                          lhsT=w_uqb[:, c, m * 128:(m + 1) * 128], rhs=cqTb[:, c, :],
                                                          start=(c == 0), stop=(c == 1)), r=[w_uqb, cqTb], w=[PC])
                P.dve(lambda e: e.tensor_copy(out=qTb[:, :, :], in_=PC[:, :].rearrange("p (m t) -> p m t", m=4)),
                      r=[PC], w=[qTb])
                for hf in range(2):
                    for h4 in range(4):
                        h = hf * 4 + h4
                        pr = slice((h % 2) * 64, (h % 2) * 64 + 64)
                        P.pe(lambda e, h=h, h4=h4, pr=pr: e.matmul(PB[:, h4 * 256:(h4 + 1) * 256],
                                                                   lhsT=w_ukb[:, h, :], rhs=qTb[:, h // 2, :],
                                                                   start=True, stop=True), r=[w_ukb, qTb], w=[PB])
                    P.act(lambda e, hf=hf, s=s: e.activation(out=qaT2[s % 2][:, hf * 4:(hf + 1) * 4, :],
                                                             in_=PB[:, :].rearrange("p (h t) -> p h t", h=4), func=AF.Copy,
                                                             scale=0.125), r=[PB], w=[qaT2[s % 2]])
                block(2 * s - 1)
                block(2 * s)
            block(NT - 1)
            block(NT)
            if dbg:
                dump("thr", thrs[:, :], thrs)

        print("ops after A1", P.nops)
        P.fence()
        if upto < 2:
            P.enabled = False
        x1t = Tl(P, None, "x1_hbm")
        with ExitStack() as es2:
            P.es_cur = es2
            w_rwb = P.tileg([128, 8, 1824], BF16, "w_rwb", 16)
            w_lorab = P.tile([128, 1024], BF16, "w_lorab")
            w_gateb = P.tile([128, 2, 512], BF16, "w_gateb")
            w_outb = P.tileg([128, 8, D], BF16, "w_outb", 8)
            mucol = P.tile([128, 32], F32, "mucol")
            lnrow = P.tile([128, 2, 512], F32, "lnrow")
            mix = P.tile([128, D], F32, "mix")
            x1s = [P.tile([128, D], F32, "x1s%d" % i) for i in range(1)]
            wsa = P.tile([128, 1024], F32, "wst2a")
            wsb = P.tile([128, 1024], F32, "wst2b")
            sbufs = [wsa, wsb, mix, x1s[0]]
            P.dve(lambda e: e.tensor_copy(out=mucol[:, 0:15], in_=vec[:, 108:123]), r=[vec], w=[mucol])
            P.dve(lambda e: e.tensor_scalar(out=mucol[:, 16:31], in0=vec[:, 108:123], scalar1=-1.0, scalar2=1.0,
                                            op0=ALU.mult, op1=ALU.add), r=[vec, mucol], w=[mucol])
            P.dve(lambda e: e.memset(w_gateb[:, :, :], 0.0), w=[w_gateb])
            jobs2 = []
            for k in range(8):
                for pi, (c0, c1) in enumerate(((0, 1024), (1024, 1824))):
                    jobs2.append((wrw_d[k * 128:(k + 1) * 128, c0:c1], w_rwb[:, k, c0:c1], w_rwb.p[2 * k + pi], c1 - c0, slice(0, 128)))
            for k in range(8):
                jobs2.append((wout_d[k * 128:(k + 1) * 128, :], w_outb[:, k, :], w_outb.p[k], D, slice(0, 128)))
            jobs2.append((wlora_d[:, :], w_lorab[:, :], w_lorab, 1024, slice(0, 128)))
            jobs2.append((wgate_d[0:128, :], w_gateb[:, 0, :], w_gateb, 512, slice(0, 128)))
            jobs2.append((wgate_d[128:160, :], w_gateb[96:128, 1, :], w_gateb, 512, slice(96, 128)))
            for ji, (src, dst, dtl, wd_, rows) in enumerate(jobs2):
                sb_ = sbufs[ji % 4]
                P.dma(sb_[rows, 0:wd_], src, "ws2%d" % (ji % 4), w=[sb_])
                P.conv(ji, dst, sb_[rows, 0:wd_], [sb_], [dtl])
            for i in range(2):
                P.dma(lnrow[:, i, :], lnrow_d[i, :].partition_broadcast(128), "cw", w=[lnrow])

            xt = P.tile([128, 2, D], F32, "xt2")
            sqj = P.tile([128, D], BF16, "sqj2")
            st4 = P.tile([128, 8], F32, "st42")
            dgt = P.tile([128, 2, 128], F32, "dgt2")
            hTb = P.tile([128, 8, 256], BF16, "hTb")
            lastc = TlG(P, es2.enter_context(nc.sbuf_tensor("sb_lastc", [128, 16], F32)), "lastc", 16)
            ptmp = [P.tile([128, 256], F32, "ptmp%d" % i) for i in range(3)]
            pch = [P.tile([128, 256], F32, "pch%d" % i) for i in range(3)]
            rT = P.tile([128, 4, 256], F32, "rT")
            kT = P.tile([128, 4, 256], F32, "kT")
            vTb = P.tile([128, 4, 256], BF16, "vTb")
            twd = P.tile([128, 256], BF16, "twd")
            sgd = P.tile([128, 2, 256], BF16, "sgd")
            ART = P.tile([128, 4, 2, 256], BF16, "ART")
            btT = P.tile([128, 8, 256], BF16, "btT")
            ktT = P.tile([128, 8, 256], BF16, "ktT")
            Hbz = P.tile([128, 8, 64], BF16, "Hbz")
            P.dve(lambda e: e.memset(btT[:, :, :], 0.0), w=[btT])
            P.dve(lambda e: e.memset(ktT[:, :, :], 0.0), w=[ktT])
            P.dve(lambda e: e.memset(Hbz[:, :, :], 0.0), w=[Hbz])
            bhT = P.tile([128, 4, 256], BF16, "bhT")
            khT = P.tile([128, 4, 256], BF16, "khT")
            rkT = P.tile([128, 4, 256], BF16, "rkT")
            WC = P.tile([128, 4, 2], F32, "WC")
            fsets = [[P.tile([128, 256], F32, "f%d_%d" % (q, i)) for i in range(9)] for q in range(2)]
            kk4 = P.tile([128, 4, 256], F32, "kk4")
            sq4 = P.tile([128, 4, 256], F32, "sq4")
            gtmp = P.tile([128, 256], F32, "gtmp")
            hw = P.tile([128, 8], F32, "hw")
            P.dve(lambda e: e.tensor_scalar(out=hw[:, :], in0=vec[:, V_W0:V_W0 + 8], scalar1=0.5, scalar2=None, op0=ALU.mult),
                  r=[vec], w=[hw])
            Vtm = P.tile([128, 512], BF16, "Vtm")
            Bhtm = P.tile([128, 512], BF16, "Bhtm")
            Khtm = P.tile([128, 512], BF16, "Khtm")
            ATab = P.tile([128, 8, 256], BF16, "ATab")
            ATak = P.tile([128, 8, 256], BF16, "ATak")
            def tlg2(name):
                return TlG(P, es2.enter_context(nc.sbuf_tensor("sb_" + name, [128, 8, 128], BF16)), name, 2)
            Nk = [tlg2("Nk%d" % i) for i in range(2)]
            Mk = [tlg2("Mk%d" % i) for i in range(2)]
            Pk = [tlg2("Pk%d" % i) for i in range(2)]
            Hs = P.tile([128, 4, 64], F32, "Hs")
            Hb = P.tile([128, 4, 64], BF16, "Hb")
            Hd = P.tile([128, 4, 64], F32, "Hd")
            WCf = P.tile([128, 4, 64], F32, "WCf")
            Gb = P.tile([128, 512], BF16, "Gb")
            Ub = P.tile([128, 512], BF16, "Ub")
            Ysb = P.tile([128, 512], F32, "Ysb")
            Ysq = P.tile([128, 512], F32, "Ysq")
            gst = P.tile([128, 48], F32, "gst")
            vtok = P.tile([128, 512], F32, "vtok")
            yrw = P.tile([128, 512], F32, "yrw")
            yrwb = P.tile([128, 512], BF16, "yrwb")
            ycat = P.tile([128, 4, 128], BF16, "ycat")
            ya = P.tile([128, 4, 128], BF16, "ya")
            yaf = P.tile([128, 512], F32, "yaf")
            oka = P.tile([128, 4], F32, "oka")
            hindb = P.tile([128, 2], BF16, "hindb")
            P.dve(lambda e: e.tensor_scalar(out=oka[:, :], in0=vec[:, V_KA:V_KA + 4], scalar1=-1.0, scalar2=1.0,
                                            op0=ALU.mult, op1=ALU.add), r=[vec], w=[oka])
            P.dve(lambda e: e.tensor_copy(out=hindb[:, :], in_=cst[:, C_HIND:C_HIND + 2]), r=[cst], w=[hindb])
            P.dve(lambda e: e.memset(lastc[:, :], 0.0), w=[lastc])
            print("SBUFREM A2", nc.sbuf_bytes_remaining)
            P.dve(lambda e: e.memset(Hs[:, :, :], 0.0), w=[Hs])
            P.dve(lambda e: e.memset(sgd[:, :, :], 0.0), w=[sgd])

            iprot = [Bank(PD, PD.h, 0), Bank(PC.p[0], PC.h, 0), Bank(PC.p[1], PC.h, 512), Bank(PB.p[1], PB.h, 512)]

            def inproj(n, width=128):
                c0 = n * 128 if n < 14 else 1824 - 128
                bk = iprot[n % 4]
                for k in range(8):
                    P.pe(lambda e, k=k: e.matmul(bk.ap(0, 256), lhsT=w_rwb[:, k, c0:c0 + 128], rhs=hTb[:, k, :],
                                                 start=(k == 0), stop=(k == 7)), r=[w_rwb, hTb], w=[bk.tl])
                pt = ptmp[n % 3]
                pc = pch[n % 3]
                lc = lastc.p[n]
                P.act(lambda e: e.activation(out=pt[:, :], in_=bk.ap(0, 256), func=AF.Copy,
                                             scale=mucol[:, 16 + n:17 + n]), r=[bk.tl, mucol], w=[pt])
                P.dve(lambda e: e.scalar_tensor_tensor(out=pc[:, 1:256], in0=bk.ap(0, 255),
                                                       scalar=mucol[:, n:n + 1], in1=pt[:, 1:256],
                                                       op0=ALU.mult, op1=ALU.add), r=[bk.tl, mucol, pt], w=[pc])
                P.dve(lambda e: e.scalar_tensor_tensor(out=pc[:, 0:1], in0=lastc[:, n:n + 1],
                                                       scalar=mucol[:, n:n + 1], in1=pt[:, 0:1],
                                                       op0=ALU.mult, op1=ALU.add), r=[lc, mucol, pt, pc], w=[pc])
                P.dve(lambda e: e.tensor_copy(out=lastc[:, n:n + 1], in_=bk.ap(255, 256)), r=[bk.tl, lc], w=[lc])
                return pc

            for s in range(NS):
                for j in range(2):
                    r0 = s * 256 + j * 128
                    P.dma(xt[:, j, :], x_d[r0:r0 + 128, :], "xin", w=[xt])
                for j in range(2):
                    P.act(lambda e, j=j: e.activation(out=sqj[:, :], in_=xt[:, j, :], func=AF.Square,
                                                      accum_out=st4[:, j:j + 1]), r=[xt], w=[sqj, st4])
                P.act(lambda e: e.activation(out=st4[:, 2:4], in_=st4[:, 0:2], func=AF.Sqrt, scale=1.0 / D,
                                             bias=epsc[:, 0:1]), r=[st4, epsc], w=[st4])
                P.dve(lambda e: e.reciprocal(out=st4[:, 4:6], in_=st4[:, 2:4]), r=[st4], w=[st4])
                for j in range(2):
                    P.dve(lambda e, j=j: e.tensor_scalar(out=dgt[:, j, :], in0=cst[:, C_ID:C_ID + 128],
                                                         scalar1=st4[:, 4 + j:5 + j], scalar2=None, op0=ALU.mult),
                          r=[cst, st4], w=[dgt])
                for kk in range(2):
                    for k4 in range(4):
                        k = kk * 4 + k4
                        for j in range(2):
                            P.pe(lambda e, k=k, k4=k4, j=j: e.matmul(
                                PA[:, k4 * 256 + j * 128:k4 * 256 + (j + 1) * 128],
                                lhsT=xt[:, j, k * 128:(k + 1) * 128], rhs=dgt[:, j, :], start=True, stop=True),
                                r=[xt, dgt], w=[PA])
                    for k4 in range(4):
                        k = kk * 4 + k4
                        P.dve(lambda e, k=k, k4=k4: e.tensor_scalar(
                            out=hTb[:, k, :], in0=PA[:, k4 * 256:(k4 + 1) * 256], scalar1=gsh[:, k:k + 1],
                            scalar2=modc[:, k:k + 1], op0=ALU.mult, op1=ALU.add), r=[PA, gsh, modc], w=[hTb])
                for m in range(4):
                    pc = inproj(m)
                    P.act(lambda e, m=m, pc=pc: e.activation(out=rT[:, m, :], in_=pc[:, :], func=AF.Copy), r=[pc], w=[rT])
                for m in range(4):
                    pc = inproj(4 + m)
                    P.act(lambda e, m=m, pc=pc: e.activation(out=kT[:, m, :], in_=pc[:, :], func=AF.Copy), r=[pc], w=[kT])
                for m in range(4):
                    pc = inproj(8 + m)
                    P.act(lambda e, m=m, pc=pc: e.activation(out=vTb[:, m, :], in_=pc[:, :], func=AF.Copy), r=[pc], w=[vTb])
                pc = inproj(12)
                P.act(lambda e, pc=pc: e.activation(out=twd[0:64, :], in_=pc[0:64, :], func=AF.Tanh), r=[pc], w=[twd])
                P.act(lambda e, pc=pc: e.activation(out=twd[64:128, :], in_=pc[64:128, :], func=AF.Copy), r=[pc], w=[twd])
                pc = inproj(13)
                P.act(lambda e, pc=pc: e.activation(out=gtmp[:, :], in_=pc[:, :], func=AF.Tanh, scale=0.5), r=[pc], w=[gtmp])
                P.dve(lambda e: e.tensor_scalar(out=sgd[:, 0, :], in0=gtmp[:, :], scalar1=0.5, scalar2=0.5, op0=ALU.mult,
                                                op1=ALU.add), r=[gtmp], w=[sgd])
                pc = inproj(14)
                P.act(lambda e, pc=pc: e.activation(out=gtmp[:, :], in_=pc[:, :], func=AF.Tanh, scale=0.5), r=[pc], w=[gtmp])
                P.dve(lambda e: e.tensor_scalar(out=sgd[:, 1, :], in0=gtmp[:, :], scalar1=0.5, scalar2=0.5, op0=ALU.mult,
                                                op1=ALU.add), r=[gtmp], w=[sgd])
                for m in range(4):
                    P.dve(lambda e, m=m: e.tensor_scalar(out=kk4[:, m, :], in0=kT[:, m, :], scalar1=vec[:, V_KK + m:V_KK + m + 1],
                                                         scalar2=None, op0=ALU.mult), r=[kT, vec], w=[kk4])
                P.act(lambda e: e.activation(out=sq4[:, :, :], in_=kk4[:, :, :], func=AF.Square), r=[kk4], w=[sq4])
                for m in range(4):
                    P.pe(lambda e, m=m: e.matmul(PA[:, m * 256:(m + 1) * 256], lhsT=cst[:, C_BLK:C_BLK + 128], rhs=sq4[:, m, :],
                                                 start=True, stop=True), r=[cst, sq4], w=[PA])
                sq4f = sq4[:, :, :].rearrange("p m t -> p (m t)")
                P.act(lambda e: e.activation(out=sq4f, in_=PA[:, :], func=AF.Sqrt), r=[PA], w=[sq4])
                P.dve(lambda e: e.tensor_scalar(out=sq4f, in0=sq4f, scalar1=1e-12, scalar2=None, op0=ALU.max), r=[sq4], w=[sq4])
                P.dve(lambda e: e.reciprocal(out=sq4f, in_=sq4f), r=[sq4], w=[sq4])
                P.dve(lambda e: e.tensor_tensor(out=kk4[:, :, :], in0=kk4[:, :, :], in1=sq4[:, :, :], op=ALU.mult),
                      r=[kk4, sq4], w=[kk4])
                for m in range(4):
                    ms = slice(m * 128, (m + 1) * 128)
                    pg = PB if m % 2 == 0 else PC
                    P.pe(lambda e, m=m: e.matmul(pg[:, 0:256], lhsT=w_lorab[:, m * 128:(m + 1) * 128], rhs=twd[:, :],
                                                 start=True, stop=True), r=[w_lorab, twd], w=[pg.p[0]])
                    P.pe(lambda e, m=m: e.matmul(pg[:, 256:512], lhsT=w_lorab[:, 512 + m * 128:512 + (m + 1) * 128], rhs=twd[:, :],
                                                 start=True, stop=True), r=[w_lorab, twd], w=[pg.p[0]])
                    sgw, cs, wt, winv, wprev, asig, kp, t1, t2 = fsets[m % 2]
                    P.act(lambda e, m=m: e.activation(out=t1[:, :], in_=pg[:, 0:256], func=AF.Tanh, scale=0.5,
                                                      bias=hw[:, m:m + 1]), r=[pg.p[0], hw], w=[t1])
                    P.dve(lambda e: e.tensor_scalar(out=sgw[:, :], in0=t1[:, :], scalar1=0.5, scalar2=0.5, op0=ALU.mult,
                                                    op1=ALU.add), r=[t1], w=[sgw])
                    P.act(lambda e, m=m: e.activation(out=t2[:, :], in_=pg[:, 256:512], func=AF.Tanh, scale=0.5,
                                                      bias=hw[:, 4 + m:5 + m]), r=[pg.p[0], hw], w=[t2])
                    P.dve(lambda e: e.tensor_scalar(out=asig[:, :], in0=t2[:, :], scalar1=0.5, scalar2=0.5, op0=ALU.mult,
                                                    op1=ALU.add), r=[t2], w=[asig])
                    P.dve(lambda e: e.tensor_tensor_scan(out=cs[:, :], data0=cst[:, C_SCAN:C_SCAN + 256], data1=sgw[:, :],
                                                         initial=0.0, op0=ALU.mult, op1=ALU.add), r=[cst, sgw], w=[cs])
                    P.act(lambda e: e.activation(out=wt[:, :], in_=cs[:, :], func=AF.Exp, scale=-LWC), r=[cs], w=[wt])
                    P.act(lambda e: e.activation(out=winv[:, :], in_=cs[:, :], func=AF.Exp, scale=LWC), r=[cs], w=[winv])
                    P.dve(lambda e: e.tensor_tensor(out=t1[:, :], in0=cs[:, :], in1=sgw[:, :], op=ALU.subtract),
                          r=[cs, sgw], w=[t1])
                    P.act(lambda e: e.activation(out=wprev[:, :], in_=t1[:, :], func=AF.Exp, scale=-LWC), r=[t1], w=[wprev])
                    for j in range(2):
                        cj = slice(j * 128, (j + 1) * 128)
                        P.dve(lambda e, m=m, j=j: e.tensor_scalar(out=WC[:, m, j:j + 1], in0=cs[:, j * 128 + 127:j * 128 + 128],
                                                                  scalar1=-LWC, scalar2=None, op0=ALU.mult),
                              r=[cs], w=[WC])
                        P.act(lambda e, m=m, j=j, cj=cj: e.activation(out=t2[:, cj], in_=cs[:, cj], func=AF.Exp, scale=LWC,
                                                                      bias=WC[:, m, j:j + 1]), r=[cs, WC], w=[t2])
                    P.act(lambda e, m=m: e.activation(out=WC[:, m, :], in_=WC[:, m, :], func=AF.Exp), r=[WC], w=[WC])
                    P.dve(lambda e, m=m: e.tensor_scalar(out=t1[:, :], in0=asig[:, :], scalar1=vec[:, V_KA + m:V_KA + m + 1],
                                                         scalar2=oka[:, m:m + 1], op0=ALU.mult, op1=ALU.add),
                          r=[asig, vec, oka], w=[t1])
                    P.dve(lambda e, m=m: e.tensor_tensor(out=kp[:, :], in0=kT[:, m, :], in1=t1[:, :], op=ALU.mult),
                          r=[kT, t1], w=[kp])
                    P.dve(lambda e, m=m: e.scalar_tensor_tensor(out=rkT[:, m, :], in0=rT[:, m, :],
                                                                scalar=vec[:, V_RK + m:V_RK + m + 1], in1=kp[:, :],
                                                                op0=ALU.mult, op1=ALU.mult), r=[rT, vec, kp], w=[rkT])
                    P.pool(lambda e, m=m: e.tensor_tensor(out=ART[:, m, 1, :], in0=rT[:, m, :], in1=wt[:, :], op=ALU.mult),
                           r=[rT, wt], w=[ART])
                    P.dve(lambda e, m=m: e.scalar_tensor_tensor(out=ART[:, m, 0, :], in0=kk4[:, m, :], scalar=-1.0,
                                                                in1=wprev[:, :], op0=ALU.mult, op1=ALU.mult),
                          r=[kk4, wprev], w=[ART])
                    P.dve(lambda e, m=m: e.tensor_tensor(out=t1[:, :], in0=kk4[:, m, :], in1=asig[:, :], op=ALU.mult),
                          r=[kk4, asig], w=[t1])
                    for hh in range(2):
                        prr = slice(hh * 64, hh * 64 + 64)
                        P.pool(lambda e, m=m, hh=hh, prr=prr: e.tensor_tensor(out=btT[prr, 2 * m + hh, :], in0=t1[prr, :],
                                                                              in1=winv[prr, :], op=ALU.mult),
                               r=[t1, winv], w=[btT])
                    P.pool(lambda e, m=m: e.tensor_tensor(out=bhT[:, m, :], in0=t1[:, :], in1=t2[:, :], op=ALU.mult),
                           r=[t1, t2], w=[bhT])
                    for hh in range(2):
                        prr = slice(hh * 64, hh * 64 + 64)
                        P.dve(lambda e, m=m, hh=hh, prr=prr: e.tensor_tensor(out=ktT[prr, 2 * m + hh, :], in0=kp[prr, :],
                                                                             in1=winv[prr, :], op=ALU.mult),
                              r=[kp, winv], w=[ktT])
                    P.pool(lambda e, m=m: e.tensor_tensor(out=khT[:, m, :], in0=kp[:, :], in1=t2[:, :], op=ALU.mult),
                           r=[kp, t2], w=[khT])
                for j in range(2):
                    ti = s * 2 + j
                    tb = slice(j * 128, (j + 1) * 128)
                    for (src, dst) in ((vTb, Vtm), (bhT, Bhtm), (khT, Khtm)):
                        for m in range(4):
                            P.pe(lambda e, src=src, m=m: e.transpose(PT[:, m * 128:(m + 1) * 128], src[:, m, tb], identb[:, :]),
                                 r=[src, identb], w=[PT])
                        P.act(lambda e, dst=dst: e.activation(out=dst[:, :], in_=PT[:, 0:512], func=AF.Copy), r=[PT], w=[dst])
                    P.dve(lambda e: e.tensor_copy(out=vtok[:, :], in_=Vtm[:, :]), r=[Vtm], w=[vtok])
                    for hf in range(2):
                        for h4 in range(4):
                            h = hf * 4 + h4
                            m = h // 2
                            pr = slice((h % 2) * 64, (h % 2) * 64 + 64)
                            P.pe(lambda e, h4=h4, m=m, h=h: e.matmul(PA[:, h4 * 256:(h4 + 1) * 256], lhsT=btT[:, h, tb],
                                                                     rhs=ART[:, m, :, tb], start=True, stop=True),
                                 r=[btT, ART], w=[PA])
                            P.pe(lambda e, h4=h4, m=m, h=h: e.matmul(PB[:, h4 * 256:(h4 + 1) * 256], lhsT=ktT[:, h, tb],
                                                                     rhs=ART[:, m, :, tb], start=True, stop=True),
                                 r=[ktT, ART], w=[PB])
                        mur = cst[:, C_MSU:C_MSU + 256].unsqueeze(1).to_broadcast([128, 4, 256])
                        msl = cst[:, C_MSL:C_MSL + 128].unsqueeze(1).to_broadcast([128, 4, 128])
                        msu = cst[:, C_MSU:C_MSU + 128].unsqueeze(1).to_broadcast([128, 4, 128])
                        hs4 = slice(hf * 4, hf * 4 + 4)
                        P.dve(lambda e, hs4=hs4, mur=mur: e.tensor_tensor(
                            out=ATab[:, hs4, :], in0=PA[:, :].rearrange("p (h t) -> p h t", h=4), in1=mur, op=ALU.mult),
                            r=[PA, cst], w=[ATab])
                        P.dve(lambda e, hs4=hs4, mur=mur: e.tensor_tensor(
                            out=ATak[:, hs4, :], in0=PB[:, :].rearrange("p (h t) -> p h t", h=4), in1=mur, op=ALU.mult),
                            r=[PB, cst], w=[ATak])
                    P.act(lambda e: e.activation(out=Mk[0][:, :, :], in_=ATab[:, :, 0:128], func=AF.Copy), r=[ATab], w=[Mk[0]])
                    for h in range(8):
                        P.pe(lambda e, h=h: e.transpose(PT[:, h * 128:(h + 1) * 128], Mk[0][:, h, :], identb[:, :]),
                             r=[Mk[0], identb], w=[PT])
                    P.act(lambda e: e.activation(out=Nk[0][:, :, :].rearrange("p h t -> p (h t)"), in_=PT[:, :], func=AF.Copy),
                          r=[PT], w=[Nk[0]])
                    P.dve(lambda e: e.tensor_tensor(out=Pk[0][:, :, :], in0=ATab[:, :, 0:128],
                                                    in1=identb[:, :].unsqueeze(1).to_broadcast([128, 8, 128]), op=ALU.add),
                          r=[ATab, identb], w=[Pk[0]])
                    for lv in range(6):
                        a, b = lv % 2, (lv + 1) % 2
                        for g in range(2):
                            for h in range(4 * g, 4 * g + 4):
                                P.pe(lambda e: e.matmul(PA[:, h * 128:(h + 1) * 128], lhsT=Mk[a][:, h, :], rhs=Nk[a][:, h, :],
                                                        start=True, stop=True), r=[Mk[a].p[g], Nk[a].p[g]], w=[PA.p[g]])
                        for g in range(2):
                            P.act(lambda e: e.activation(out=Nk[b][:, 4 * g:4 * g + 4, :].rearrange("p h t -> p (h t)"),
                                                         in_=PA[:, g * 512:(g + 1) * 512], func=AF.Copy),
                                  r=[PA.p[g]], w=[Nk[b].p[g]])
                        if lv < 5:
                            for g in range(2):
                                for h in range(4 * g, 4 * g + 4):
                                    P.pe(lambda e: e.matmul(PB[:, h * 128:(h + 1) * 128], lhsT=Nk[a][:, h, :], rhs=Mk[a][:, h, :],
                                                            start=True, stop=True), r=[Mk[a].p[g], Nk[a].p[g]], w=[PB.p[g]])
                            P.act(lambda e: e.activation(out=Mk[b][:, 0:4, :].rearrange("p h t -> p (h t)"),
                                                         in_=PB[:, 0:512], func=AF.Copy), r=[PB.p[0]], w=[Mk[b].p[0]])
                            P.dve(lambda e: e.tensor_copy(out=Mk[b][:, 4:8, :].rearrange("p h t -> p (h t)"), in_=PB[:, 512:1024]),
                                  r=[PB.p[1]], w=[Mk[b].p[1]])
                        for g in range(2):
                            for h in range(4 * g, 4 * g + 4):
                                P.pe(lambda e: e.matmul(PC[:, h * 128:(h + 1) * 128], lhsT=Nk[b][:, h, :], rhs=Pk[a][:, h, :],
                                                        start=True, stop=True), r=[Nk[b].p[g], Pk[a].p[g]], w=[PC.p[g]])
                        for g in range(2):
                            P.dve(lambda e: e.tensor_tensor(out=Pk[b][:, 4 * g:4 * g + 4, :].rearrange("p h t -> p (h t)"),
                                                            in0=PC[:, g * 512:(g + 1) * 512],
                                                            in1=Pk[a][:, 4 * g:4 * g + 4, :].rearrange("p h t -> p (h t)"),
                                                            op=ALU.add), r=[PC.p[g], Pk[a].p[g]], w=[Pk[b].p[g]])
                    XT = Pk[0]
                    for m in range(4):
                        P.dve(lambda e, m=m: e.tensor_scalar(out=WCf[:, m, :], in0=cst[:, C_ONE:C_ONE + 64],
                                                             scalar1=WC[:, m, j:j + 1], scalar2=None, op0=ALU.mult),
                              r=[cst, WC], w=[WCf])
                    P.dve(lambda e: e.tensor_tensor(out=Hd[:, :, :], in0=Hs[:, :, :], in1=WCf[:, :, :], op=ALU.mult),
                          r=[Hs, WCf], w=[Hd])
                    for h in range(8):
                        m = h // 2
                        pr = slice((h % 2) * 64, (h % 2) * 64 + 64)
                        hc = slice(h * 64, (h + 1) * 64)
                        P.pe(lambda e, m=m, h=h, hc=hc: e.matmul(PD[:, hc], lhsT=ART[:, m, 0, tb], rhs=Hbz[:, h, :],
                                                                 start=True, stop=False), r=[ART, Hbz], w=[PD])
                        P.pe(lambda e, h=h, hc=hc: e.matmul(PD[:, hc], lhsT=ATak[:, h, 0:128], rhs=Vtm[:, hc],
                                                            start=False, stop=True), r=[ATak, Vtm], w=[PD])
                    P.act(lambda e: e.activation(out=Gb[:, :], in_=PD[:, :], func=AF.Copy), r=[PD], w=[Gb])
                    for h in range(8):
                        hc = slice(h * 64, (h + 1) * 64)
                        P.pe(lambda e, h=h, hc=hc: e.matmul(PA[:, hc], lhsT=XT[:, h, :], rhs=Gb[:, hc], start=True, stop=True),
                             r=[XT, Gb], w=[PA])
                    P.act(lambda e: e.activation(out=Ub[:, :], in_=PA[:, 0:512], func=AF.Copy), r=[PA], w=[Ub])
                    for h in range(8):
                        m = h // 2
                        pr = slice((h % 2) * 64, (h % 2) * 64 + 64)
                        hc = slice(h * 64, (h + 1) * 64)
                        P.pe(lambda e, m=m, h=h, hc=hc: e.matmul(PB[:, hc], lhsT=ART[:, m, 1, tb], rhs=Hbz[:, h, :],
                                                                 start=True, stop=False), r=[ART, Hbz], w=[PB])
                        P.pe(lambda e, h=h, hc=hc: e.matmul(PB[:, hc], lhsT=ATab[:, h, 128:256], rhs=Ub[:, hc],
                                                            start=False, stop=False), r=[ATab, Ub], w=[PB])
                        P.pe(lambda e, h=h, hc=hc: e.matmul(PB[:, hc], lhsT=ATak[:, h, 128:256], rhs=Vtm[:, hc],
                                                            start=False, stop=True), r=[ATak, Vtm], w=[PB])
                    for m in range(4):
                        ms = slice(m * 128, (m + 1) * 128)
                        P.pe(lambda e, ms=ms: e.matmul(PC[:, ms], lhsT=Bhtm[:, ms], rhs=Ub[:, ms], start=True, stop=False),
                             r=[Bhtm, Ub], w=[PC])
                        P.pe(lambda e, ms=ms: e.matmul(PC[:, ms], lhsT=Khtm[:, ms], rhs=Vtm[:, ms], start=False, stop=True),
                             r=[Khtm, Vtm], w=[PC])
                    for hh in range(2):
                        pr = slice(hh * 64, hh * 64 + 64)
                        src = PC[pr, 0:512].rearrange("p (m c) -> p m c", m=4)[:, :, hh * 64:hh * 64 + 64]
                        P.dve(lambda e, pr=pr, src=src: e.tensor_tensor(out=Hs[pr, :, :], in0=Hd[pr, :, :], in1=src, op=ALU.add),
                              r=[Hd, PC], w=[Hs])
                    for hh in range(2):
                        prr = slice(hh * 64, hh * 64 + 64)
                        P.act(lambda e, hh=hh, prr=prr: e.activation(out=Hbz[prr, hh:8:2, :], in_=Hs[prr, :, :], func=AF.Copy),
                              r=[Hs], w=[Hbz])
                    P.act(lambda e: e.activation(out=Ysb[:, :], in_=PB[:, 0:512], func=AF.Copy), r=[PB], w=[Ysb])
                    P.act(lambda e: e.activation(out=Ysq[:, :], in_=PB[:, 0:512], func=AF.Square), r=[PB], w=[Ysq])
                    P.dve(lambda e: e.tensor_reduce(out=gst[:, 0:8], in_=Ysb[:, :].rearrange("p (h i) -> p h i", h=8),
                                                    axis=AX.X, op=ALU.add), r=[Ysb], w=[gst])
                    P.dve(lambda e: e.tensor_reduce(out=gst[:, 8:16], in_=Ysq[:, :].rearrange("p (h i) -> p h i", h=8),
                                                    axis=AX.X, op=ALU.add), r=[Ysq, gst], w=[gst])
                    P.dve(lambda e: e.tensor_scalar(out=gst[:, 16:24], in0=gst[:, 0:8], scalar1=1.0 / 64, scalar2=None,
                                                    op0=ALU.mult), r=[gst], w=[gst])
                    P.dve(lambda e: e.tensor_tensor(out=gst[:, 24:32], in0=gst[:, 16:24], in1=gst[:, 16:24], op=ALU.mult),
                          r=[gst], w=[gst])
                    P.dve(lambda e: e.scalar_tensor_tensor(out=gst[:, 32:40], in0=gst[:, 8:16], scalar=1.0 / 64,
                                                           in1=gst[:, 24:32], op0=ALU.mult, op1=ALU.subtract),
                          r=[gst], w=[gst])
                    P.act(lambda e: e.activation(out=gst[:, 40:48], in_=gst[:, 32:40], func=AF.Sqrt, bias=epsc[:, 1:2]),
                          r=[gst, epsc], w=[gst])
                    P.dve(lambda e: e.reciprocal(out=gst[:, 40:48], in_=gst[:, 40:48]), r=[gst], w=[gst])
                    y3 = lambda t: t[:, :].rearrange("p (h i) -> p h i", h=8)
                    P.dve(lambda e: e.tensor_tensor(out=y3(Ysb), in0=y3(Ysb),
                                                    in1=gst[:, 16:24].unsqueeze(2).to_broadcast([128, 8, 64]), op=ALU.subtract),
                          r=[Ysb, gst], w=[Ysb])
                    P.dve(lambda e: e.tensor_tensor(out=y3(Ysb), in0=y3(Ysb),
                                                    in1=gst[:, 40:48].unsqueeze(2).to_broadcast([128, 8, 64]), op=ALU.mult),
                          r=[Ysb, gst], w=[Ysb])
                    P.dve(lambda e: e.tensor_tensor(out=Ysb[:, :], in0=Ysb[:, :], in1=lnrow[:, 0, :], op=ALU.mult),
                          r=[Ysb, lnrow], w=[Ysb])
                    P.dve(lambda e: e.tensor_tensor(out=Ysb[:, :], in0=Ysb[:, :], in1=lnrow[:, 1, :], op=ALU.add),
                          r=[Ysb, lnrow], w=[Ysb])
                    for m in range(4):
                        P.pe(lambda e, m=m: e.matmul(PD[:, 2 * m:2 * m + 2], lhsT=rkT[:, m, tb], rhs=hindb[:, :],
                                                     start=True, stop=True), r=[rkT, hindb], w=[PD])
                    P.act(lambda e: e.activation(out=gst[:, 0:8], in_=PD[:, 0:8], func=AF.Copy), r=[PD, gst], w=[gst])
                    P.dve(lambda e: e.tensor_tensor(out=y3(Ysq), in0=y3(vtok),
                                                    in1=gst[:, 0:8].unsqueeze(2).to_broadcast([128, 8, 64]), op=ALU.mult),
                          r=[vtok, gst], w=[Ysq])
                    P.dve(lambda e: e.tensor_tensor(out=Ysb[:, :], in0=Ysb[:, :], in1=Ysq[:, :], op=ALU.add),
                          r=[Ysb, Ysq], w=[Ysb])
                    P.pe(lambda e: e.matmul(PA[:, 512:1024], lhsT=sgd[:, 0, tb], rhs=w_gateb[:, 0, :], start=True, stop=False),
                         r=[sgd, w_gateb], w=[PA])
                    P.pe(lambda e: e.matmul(PA[:, 512:1024], lhsT=sgd[:, 1, tb], rhs=w_gateb[:, 1, :], start=False, stop=True),
                         r=[sgd, w_gateb], w=[PA])
                    P.dve(lambda e: e.tensor_tensor(out=yrw[:, :], in0=Ysb[:, :], in1=PA[:, 512:1024], op=ALU.mult),
                          r=[Ysb, PA], w=[yrw])
                    dump("yrw", yrw[:, :], yrw, dbg_d["yrw"][ti * 128:(ti + 1) * 128, :] if dbg else None)
                    P.act(lambda e: e.activation(out=yrwb[:, :], in_=yrw[:, :], func=AF.Copy), r=[yrw], w=[yrwb])
                    for m in range(4):
                        P.pe(lambda e, m=m: e.transpose(PT[:, m * 128:(m + 1) * 128], yrwb[:, m * 128:(m + 1) * 128], identb[:, :]),
                             r=[yrwb, identb], w=[PT])
                    P.act(lambda e: e.activation(out=ycat[:, :, :].rearrange("p m t -> p (m t)"), in_=PT[:, 0:512], func=AF.Copy),
                          r=[PT], w=[ycat])
                    P.dma(yaf[:, :], yscr[ti * 128:(ti + 1) * 128, :], "yld", r=[yscr_t], w=[yaf])
                    P.act(lambda e: e.activation(out=ya[:, :, :].rearrange("p m t -> p (m t)"), in_=yaf[:, :], func=AF.Copy),
                          r=[yaf], w=[ya])
                    for c in range(2):
                        cs_ = slice(c * 512, (c + 1) * 512)
                        for k in range(8):
                            lhs = (lambda k=k: ya[:, k, :]) if k < 4 else (lambda k=k: ycat[:, k - 4, :])
                            P.pe(lambda e, k=k, cs_=cs_, lhs=lhs: e.matmul(PC[:, cs_], lhsT=lhs(), rhs=w_outb[:, k, cs_],
                                                                           start=(k == 0), stop=(k == 7)),
                                 r=[ya, ycat, w_outb], w=[PC])
                    P.act(lambda e: e.activation(out=mix[:, :], in_=PC[:, :], func=AF.Copy), r=[PC], w=[mix])
                    P.act(lambda e: e.activation(out=sqj[:, :], in_=PC[:, :], func=AF.Square, accum_out=st4[:, 6:7]),
                          r=[PC], w=[sqj, st4])
                    P.act(lambda e: e.activation(out=st4[:, 7:8], in_=st4[:, 6:7], func=AF.Sqrt, scale=1.0 / D,
                                                 bias=epsc[:, 0:1]), r=[st4, epsc], w=[st4])
                    P.dve(lambda e: e.reciprocal(out=st4[:, 7:8], in_=st4[:, 7:8]), r=[st4], w=[st4])
                    xo = x1s[0]
                    P.dve(lambda e: e.scalar_tensor_tensor(out=mix[:, :], in0=mix[:, :], scalar=st4[:, 7:8], in1=GMrow[:, :],
                                                           op0=ALU.mult, op1=ALU.mult), r=[mix, st4, GMrow], w=[mix])
                    P.dve(lambda e, xo=xo: e.tensor_tensor(out=xo[:, :], in0=mix[:, :], in1=xt[:, j, :], op=ALU.add),
                          r=[mix, xt], w=[xo])
                    P.dma(out_d[ti * 128:(ti + 1) * 128, :], xo[:, :], "x1st", r=[xo], w=[x1t], q="sp")
                    dump("x1", xo[:, :], xo, dbg_d["x1"][ti * 128:(ti + 1) * 128, :] if dbg else None)

        print("ops after A2", P.nops)
        P.fence()
        if upto < 3:
            P.enabled = False
        outt = Tl(P, None, "out_hbm")
        with ExitStack() as es3:
            P.es_cur = es3
            w_fgb = P.tileg([128, 8, DFF], BF16, "w_fgb", 24)
            w_fub = P.tileg([128, 8, DFF], BF16, "w_fub", 24)
            w_fdb = P.tileg([128, 22, D], BF16, "w_fdb", 22)
            wst = [P.tile([128, 1024], F32, "wst3%d" % i) for i in range(4)]
            jobs = []
            for (wd_, wb) in ((wfg_d, w_fgb), (wfu_d, w_fub)):
                for k in range(8):
                    for pi, (c0, c1) in enumerate(((0, 1024), (1024, 2048), (2048, DFF))):
                        jobs.append((wd_[k * 128:(k + 1) * 128, c0:c1], wb[:, k, c0:c1], wb.p[3 * k + pi], c1 - c0))
            for k in range(22):
                jobs.append((wfd_d[k * 128:(k + 1) * 128, :], w_fdb[:, k, :], w_fdb.p[k], D))
            for ji, (src, dst, dtl, wd_) in enumerate(jobs):
                ws = wst[ji % 4]
                P.dma(ws[:, 0:wd_], src, "wst3%d" % (ji % 4), w=[ws])
                P.conv(ji, dst, ws[:, 0:wd_], [ws], [dtl])
            xt = P.tile([128, 2, D], F32, "xt3")
            sqj = P.tile([128, D], BF16, "sqj3")
            st4 = P.tile([128, 8], F32, "st43")
            dgt = P.tile([128, 2, 128], F32, "dgt3")
            hfT = P.tile([128, 8, 256], BF16, "hfT")
            sg = [P.tile([128, 256], F32, "sg%d" % i) for i in range(2)]
            aT = P.tile([128, 22, 256], BF16, "aT")
            fo = P.tile([128, D], F32, "fo")
            ost = [P.tile([128, D], F32, "ost%d" % i) for i in range(1)]
            print("SBUFREM B", nc.sbuf_bytes_remaining)
            for s in range(NS):
                for j in range(2):
                    r0 = s * 256 + j * 128
                    P.dma(xt[:, j, :], out_d[r0:r0 + 128, :], "xin3", r=[x1t], w=[xt])
                for j in range(2):
                    P.act(lambda e, j=j: e.activation(out=sqj[:, :], in_=xt[:, j, :], func=AF.Square,
                                                      accum_out=st4[:, j:j + 1]), r=[xt], w=[sqj, st4])
                P.act(lambda e: e.activation(out=st4[:, 2:4], in_=st4[:, 0:2], func=AF.Sqrt, scale=1.0 / D,
                                             bias=epsc[:, 0:1]), r=[st4, epsc], w=[st4])
                P.dve(lambda e: e.reciprocal(out=st4[:, 4:6], in_=st4[:, 2:4]), r=[st4], w=[st4])
                for j in range(2):
                    P.dve(lambda e, j=j: e.tensor_scalar(out=dgt[:, j, :], in0=cst[:, C_ID:C_ID + 128],
                                                         scalar1=st4[:, 4 + j:5 + j], scalar2=None, op0=ALU.mult),
                          r=[cst, st4], w=[dgt])
                for kk in range(2):
                    for k4 in range(4):
                        k = kk * 4 + k4
                        for j in range(2):
                            P.pe(lambda e, k=k, k4=k4, j=j: e.matmul(
                                PA[:, k4 * 256 + j * 128:k4 * 256 + (j + 1) * 128],
                                lhsT=xt[:, j, k * 128:(k + 1) * 128], rhs=dgt[:, j, :], start=True, stop=True),
                                r=[xt, dgt], w=[PA])
                    for k4 in range(4):
                        k = kk * 4 + k4
                        P.dve(lambda e, k=k, k4=k4: e.tensor_scalar(
                            out=hfT[:, k, :], in0=PA[:, k4 * 256:(k4 + 1) * 256], scalar1=gsh[:, 8 + k:9 + k],
                            scalar2=modc[:, 24 + k:25 + k], op0=ALU.mult, op1=ALU.add), r=[PA, gsh, modc], w=[hfT])
                for n in range(22):
                    ns = slice(n * 128, (n + 1) * 128)
                    pg = PB if n % 2 == 0 else PC
                    for k in range(8):
                        P.pe(lambda e, k=k, ns=ns, pg=pg: e.matmul(pg[:, 0:256], lhsT=w_fgb[:, k, ns], rhs=hfT[:, k, :],
                                                                   start=(k == 0), stop=(k == 7)), r=[w_fgb, hfT], w=[pg])
                    for k in range(8):
                        P.pe(lambda e, k=k, ns=ns, pg=pg: e.matmul(pg[:, 256:512], lhsT=w_fub[:, k, ns], rhs=hfT[:, k, :],
                                                                   start=(k == 0), stop=(k == 7)), r=[w_fub, hfT], w=[pg])
                    sgt = sg[n % 2]
                    P.act(lambda e, pg=pg, sgt=sgt: e.activation(out=sgt[:, :], in_=pg[:, 0:256], func=AF.Silu), r=[pg], w=[sgt])
                    P.dve(lambda e, pg=pg, sgt=sgt, n=n: e.tensor_tensor(out=aT[:, n, :], in0=sgt[:, :], in1=pg[:, 256:512],
                                                                         op=ALU.mult), r=[sgt, pg], w=[aT])
                for j in range(2):
                    ti = s * 2 + j
                    for c in range(2):
                        cs_ = slice(c * 512, (c + 1) * 512)
                        for n in range(22):
                            P.pe(lambda e, n=n, cs_=cs_, j=j: e.matmul(PA[:, cs_], lhsT=aT[:, n, j * 128:(j + 1) * 128],
                                                                       rhs=w_fdb[:, n, cs_], start=(n == 0), stop=(n == 21)),
                                 r=[aT, w_fdb], w=[PA])
                    P.act(lambda e: e.activation(out=fo[:, :], in_=PA[:, :], func=AF.Copy), r=[PA], w=[fo])
                    P.act(lambda e: e.activation(out=sqj[:, :], in_=PA[:, :], func=AF.Square, accum_out=st4[:, 6:7]),
                          r=[PA], w=[sqj, st4])
                    P.act(lambda e: e.activation(out=st4[:, 7:8], in_=st4[:, 6:7], func=AF.Sqrt, scale=1.0 / D,
                                                 bias=epsc[:, 0:1]), r=[st4, epsc], w=[st4])
                    P.dve(lambda e: e.reciprocal(out=st4[:, 7:8], in_=st4[:, 7:8]), r=[st4], w=[st4])
                    oo = ost[0]
                    P.dve(lambda e: e.scalar_tensor_tensor(out=fo[:, :], in0=fo[:, :], scalar=st4[:, 7:8], in1=GFrow[:, :],
                                                           op0=ALU.mult, op1=ALU.mult), r=[fo, st4, GFrow], w=[fo])
                    P.dve(lambda e, oo=oo, j=j: e.tensor_tensor(out=oo[:, :], in0=fo[:, :], in1=xt[:, j, :], op=ALU.add),
                          r=[fo, xt], w=[oo])
                    P.dma(out_d[ti * 128:(ti + 1) * 128, :], oo[:, :], "ost", r=[oo], w=[outt], q="sp")
            P.enabled = True
            P.wait_all("sp", ["ost", "x1st", "dbg"] + [k for k in P.streams if k not in ("ost", "x1st", "dbg")])
            P.wait_all("pool", ["ost"])

            with nc.Block() as block:
                @block.sync
                def _(e):
                    P.replay("sp", e)

                @block.tensor
                def _(e):
                    P.replay("pe", e)

                @block.scalar
                def _(e):
                    P.replay("act", e)

                @block.vector
                def _(e):
                    P.replay("dve", e)

                @block.gpsimd
                def _(e):
                    P.replay("pool", e)
    return nc, list(dbg_d.keys())


def t5_bucket_np(rel):
    rel = np.asarray(rel)
    max_exact = 16
    nf = np.maximum(rel, 1).astype(np.float32)
    large = max_exact + (np.log(nf / np.float32(max_exact)) / np.float32(math.log(128 / max_exact))
                         * np.float32(32 - max_exact)).astype(np.int32)
    large = np.minimum(large, 31)
    return np.where(rel < max_exact, rel, large)


def make_consts():
    c = np.zeros((128, C_END), np.float32)
    p = np.arange(128)[:, None]
    f = np.arange(128)[None, :]
    c[:, C_ID:C_ID + 128] = (p == f)
    c[:, C_ONE:C_ONE + 128] = 1.0
    c[:, C_BLK:C_BLK + 128] = ((p // 64) == (f // 64))
    c[:, C_MSU:C_MSU + 128] = (f > p)
    c[:, C_MUI:C_MUI + 128] = (f >= p)
    c[:, C_MSL:C_MSL + 128] = (f < p)
    c[:, C_NEG:C_NEG + 128] = np.where(f > p, -1e30, 0.0)
    c[:, C_J:C_J + 128] = (p + f == 127)
    c[31, C_SEL:C_SEL + 128] = 1.0
    bk = t5_bucket_np(np.arange(256))
    c[0:32, C_OH:C_OH + 256] = (np.arange(32)[:, None] == bk[None, :])
    sm = np.ones((128, 256), np.float32)
    sm[:, 0] = 0.0
    sm[:, 128] = 0.0
    c[:, C_SCAN:C_SCAN + 256] = sm
    c[:, C_HIND] = (np.arange(128) < 64)
    c[:, C_HIND + 1] = (np.arange(128) >= 64)
    return c


def col8(v):
    return np.ascontiguousarray(v.reshape(-1, 128).T)


def prep_shared(inp):
    f32 = np.float32
    g = lambda k: np.asarray(inp[k], f32)
    w_in = g("w_in")[0]
    sh = {}
    sh["ada_w"] = np.ascontiguousarray(g("ada_w")[0])
    sh["cst"] = make_consts()
    sh["w_att"] = np.ascontiguousarray(np.concatenate([w_in[:, 0:384], w_in[:, 384:448], w_in[:, 384:448]], axis=1))
    sh["w_iw"] = np.ascontiguousarray(w_in[:, 448:456])
    sh["w_rw"] = np.ascontiguousarray(w_in[:, 456:])
    wiq = g("w_idx_q")[0]
    sh["w_iq"] = np.ascontiguousarray(np.concatenate([wiq, wiq], axis=2).reshape(256, 1024))
    sh["w_uq"] = np.ascontiguousarray(g("w_uq")[0].reshape(256, 512))
    wuk = g("w_uk")[0]
    t = np.zeros((128, 8, 128), f32)
    for h in range(8):
        t[(h % 2) * 64:(h % 2) * 64 + 64, h, :] = wuk[h].T
    sh["w_ukT"] = t.reshape(128, 1024)
    wuv = g("w_uv")[0]
    t = np.zeros((128, 8, 128), f32)
    for h in range(8):
        t[:, h, (h % 2) * 64:(h % 2) * 64 + 64] = wuv[h]
    sh["w_uv"] = t.reshape(128, 1024)
    sh["rel_bias"] = np.ascontiguousarray(g("rel_bias"))
    t = np.zeros((128, 1024), f32)
    t[0:64, 0:512] = g("w_decay_up")[0]
    t[64:128, 512:1024] = g("w_aaa_up")[0]
    sh["w_lora"] = t
    sh["w_gate"] = np.ascontiguousarray(g("w_gate_up")[0])
    sh["lnrow"] = np.ascontiguousarray(np.stack([g("ln_x_gain")[0], g("ln_x_bias")[0]], axis=0))
    sh["w_out"] = np.ascontiguousarray(g("w_out")[0])
    sh["w_fg"] = np.ascontiguousarray(g("w_ffn_gate")[0])
    sh["w_fu"] = np.ascontiguousarray(g("w_ffn_up")[0])
    sh["w_fd"] = np.ascontiguousarray(g("w_ffn_down")[0])
    vec = np.zeros((128, 128), f32)
    vec[:, 0:8] = col8(g("mix_pre_norm")[0])
    vec[:, 8:16] = col8(g("mix_post_norm")[0])
    vec[:, 16:24] = col8(g("ffn_pre_norm")[0])
    vec[:, 24:32] = col8(g("ffn_post_norm")[0])
    vec[:, 32:80] = g("ada_b")[0].reshape(48, 128).T
    vec[:, 80:82] = g("q_norm")[0].reshape(2, 128).T
    vec[:, 82] = g("kv_norm")[0]
    vec[:, 83] = np.concatenate([g("idx_k_norm")[0], g("idx_k_norm")[0]])
    vec[:, 84:88] = g("w0")[0].reshape(4, 128).T
    vec[:, 88:92] = g("a0")[0].reshape(4, 128).T
    vec[:, 92:96] = g("k_k")[0].reshape(4, 128).T
    vec[:, 96:100] = g("k_a")[0].reshape(4, 128).T
    vec[:, 100:104] = g("r_k")[0].reshape(4, 128).T
    mus = g("mu_shift")[0]
    vec[:, 108:122] = mus[:14 * 128].reshape(14, 128).T
    vec[:, 122] = mus[1824 - 128:1824]
    sh["vecs"] = vec
    sh["adab_row"] = np.ascontiguousarray(g("ada_b")[0].reshape(1, 6 * D))
    return sh


def kernel(**inputs):
    x = np.asarray(inputs["x"], np.float32)
    c = np.asarray(inputs["c"], np.float32)
    B, T, _ = x.shape
    sh = prep_shared(inputs)
    nc, _ = build(T)
    in_maps = []
    for b in range(B):
        m = dict(sh)
        m["x"] = np.ascontiguousarray(x[b])
        m["ccol"] = col8(c[b])
        in_maps.append(m)
    res = run_bass_kernel_spmd(nc, in_maps, core_ids=list(range(B)))
    return np.stack([np.asarray(r["out"], np.float32) for r in res.results], axis=0)
```
